# Optimizing a Trainium2 kernel written in Bass

```python
import math
import jax, jax.numpy as jnp
from jax import lax
import numpy as np

D_MODEL = 1024
BATCH = 4
SEQ = 8192
DEPTH = 2

MEM_LEN = 256
D_FF = 2816
CONV_CH = 256
CONV_W = 31
SC_CH = 256
SC_W = 3
N_HEADS = 8
HEAD_DIM = 64
ATT_W = N_HEADS * HEAD_DIM
IDX_HEADS = 4
IDX_DIM = 64
TOPK_MAX = 256
Q_BLOCK = 128
ROPE_THETA = 10000.0
XA_HEADS = 4
XA_DIM = D_MODEL // XA_HEADS
MIX_W = CONV_CH + SC_CH + ATT_W
SPLIT_SIZES = (CONV_CH, CONV_CH,
               SC_CH, SC_CH, SC_CH,
               ATT_W, ATT_W, ATT_W,
               IDX_HEADS * IDX_DIM, IDX_DIM, IDX_HEADS)
N_IN = sum(SPLIT_SIZES)
SPLIT_POINTS = tuple(int(v) for v in np.cumsum(SPLIT_SIZES)[:-1])
IDX_SCALE = (IDX_HEADS ** -0.5) * (IDX_DIM ** -0.5)

kernel_name = "hymba_style_conformer_shortconv_dsa_hybrid"


def rms_norm(x, g, eps=1e-6):
    xf = x.astype(jnp.float32)
    y = xf * lax.rsqrt(jnp.mean(xf * xf, axis=-1, keepdims=True) + eps)
    return (y * g.astype(jnp.float32)).astype(x.dtype)


def layer_norm(x, g, b, eps=1e-5):
    xf = x.astype(jnp.float32)
    mu = jnp.mean(xf, axis=-1, keepdims=True)
    var = jnp.mean(jnp.square(xf - mu), axis=-1, keepdims=True)
    y = (xf - mu) * lax.rsqrt(var + eps)
    return (y * g.astype(jnp.float32) + b.astype(jnp.float32)).astype(x.dtype)


def swiglu(h, w_gate, w_up, w_down):
    return (jax.nn.silu(h @ w_gate) * (h @ w_up)) @ w_down


def causal_dwconv(x, w):
    width, ch = w.shape
    return lax.conv_general_dilated(
        x, w[:, None, :].astype(x.dtype), window_strides=(1,), padding=[(width - 1, 0)],
        dimension_numbers=('NWC', 'WIO', 'NWC'), feature_group_count=ch)


def rope_tables(positions, dim):
    inv_freq = ROPE_THETA ** (-jnp.arange(0, dim, 2, dtype=jnp.float32) / dim)
    ang = positions.astype(jnp.float32)[..., None] * inv_freq
    return jnp.cos(ang), jnp.sin(ang)


def apply_rope(t, cos, sin):
    tf = t.astype(jnp.float32)
    half = tf.shape[-1] // 2
    t1, t2 = tf[..., :half], tf[..., half:]
    c, s = cos[:, :, None, :], sin[:, :, None, :]
    return jnp.concatenate([t1 * c - t2 * s, t2 * c + t1 * s], axis=-1).astype(t.dtype)


def conformer_conv(a_val, a_gate, dw, dw_b, ln_g, ln_b):
    h = a_val * jax.nn.sigmoid(a_gate)
    h = causal_dwconv(h, dw) + dw_b
    return jax.nn.silu(layer_norm(h, ln_g, ln_b))


def short_gated_conv(b_gate, c_gate, h, w):
    return b_gate * causal_dwconv(c_gate * h, w)


def dsa_attention(q, k, v, q_idx, k_idx, w_idx):
    bsz, seq, nh, dh = q.shape
    topk = min(TOPK_MAX, seq // 4)
    nb = seq // Q_BLOCK
    key_pos = jnp.arange(seq)
    b_ar = jnp.arange(bsz)[:, None, None]

    def to_blocks(t):
        return jnp.moveaxis(t.reshape(bsz, nb, Q_BLOCK, *t.shape[2:]), 1, 0)

    q_pos = jnp.arange(seq).reshape(nb, Q_BLOCK)

    def one_block(args):
        qb, qib, wb, tb = args
        logits = jnp.einsum('bqhd,bsd->bqhs', qib, k_idx).astype(jnp.float32)
        score = jnp.einsum('bqh,bqhs->bqs', wb.astype(jnp.float32), jax.nn.relu(logits)) * IDX_SCALE
        causal = key_pos[None, :] <= tb[:, None]
        score = jnp.where(causal[None], score, -jnp.inf)
        top_val, top_idx = lax.top_k(score, topk)
        k_sel = k[b_ar, top_idx]
        v_sel = v[b_ar, top_idx]
        s = jnp.einsum('bqhd,bqkhd->bqhk', qb, k_sel).astype(jnp.float32) * (dh ** -0.5)
        s = jnp.where(jnp.isfinite(top_val)[:, :, None, :], s, -jnp.inf)
        p = jax.nn.softmax(s, axis=-1).astype(v.dtype)
        return jnp.einsum('bqhk,bqkhd->bqhd', p, v_sel)

    out = lax.map(one_block, (to_blocks(q), to_blocks(q_idx), to_blocks(w_idx), q_pos))
    return jnp.moveaxis(out, 0, 1).reshape(bsz, seq, nh * dh)


def memory_cross_attention(h, mem_n, wq, wkv, wo):
    bsz, seq, _ = h.shape
    m = mem_n.shape[1]
    q = (h @ wq).reshape(bsz, seq, XA_HEADS, XA_DIM)
    k, v = jnp.split(mem_n @ wkv, 2, axis=-1)
    k = k.reshape(bsz, m, XA_HEADS, XA_DIM)
    v = v.reshape(bsz, m, XA_HEADS, XA_DIM)
    s = jnp.einsum('bshd,bmhd->bhsm', q, k).astype(jnp.float32) * (XA_DIM ** -0.5)
    p = jax.nn.softmax(s, axis=-1).astype(v.dtype)
    o = jnp.einsum('bhsm,bmhd->bshd', p, v).reshape(bsz, seq, D_MODEL)
    return o @ wo


def setup_inputs(seed: int = 0) -> dict:
    key = jax.random.key(seed)
    ks = iter(jax.random.split(key, 32))
    L, D, F = DEPTH, D_MODEL, D_FF

    def w(shape, fan_in):
        return jax.random.normal(next(ks), shape, jnp.float32) * (fan_in ** -0.5)

    def gain(shape):
        return 1.0 + 0.01 * jax.random.normal(next(ks), shape, jnp.float32)

    def bias(shape):
        return 0.01 * jax.random.normal(next(ks), shape, jnp.float32)

    x = jax.random.normal(next(ks), (BATCH, SEQ, D), jnp.float32)
    mem = jax.random.normal(next(ks), (BATCH, MEM_LEN, D), jnp.float32)
    start = jax.random.randint(next(ks), (BATCH, 1), 0, 1024, dtype=jnp.int32)
    positions = (start + jnp.arange(SEQ, dtype=jnp.int32)[None, :]).astype(jnp.int32)
    return {
        "x": x, "mem": mem, "positions": positions,
        "ffn1_norm": gain((L, D)), "ffn1_w_gate": w((L, D, F), D), "ffn1_w_up": w((L, D, F), D),
        "ffn1_w_down": w((L, F, D), F),
        "mix_norm": gain((L, D)), "w_in": w((L, D, N_IN), D),
        "conf_dw": w((L, CONV_W, CONV_CH), CONV_W), "conf_dw_b": bias((L, CONV_CH)),
        "conf_ln_g": gain((L, CONV_CH)), "conf_ln_b": bias((L, CONV_CH)),
        "sc_dw": w((L, SC_W, SC_CH), SC_W),
        "w_out": w((L, MIX_W, D), MIX_W),
        "xa_norm": gain((L, D)), "mem_norm": gain((L, D)),
        "xa_wq": w((L, D, D), D), "xa_wkv": w((L, D, 2 * D), D), "xa_wo": w((L, D, D), D),
        "ffn2_norm": gain((L, D)), "ffn2_w_gate": w((L, D, F), D), "ffn2_w_up": w((L, D, F), D),
        "ffn2_w_down": w((L, F, D), F),
        "final_norm": gain((D,)),
    }


def reference(x, mem, positions, ffn1_norm, ffn1_w_gate, ffn1_w_up, ffn1_w_down, mix_norm, w_in,
              conf_dw, conf_dw_b, conf_ln_g, conf_ln_b, sc_dw, w_out, xa_norm, mem_norm,
              xa_wq, xa_wkv, xa_wo, ffn2_norm, ffn2_w_gate, ffn2_w_up, ffn2_w_down, final_norm):
    bsz, seq, _ = x.shape
    cos, sin = rope_tables(positions, HEAD_DIM)
    for l in range(DEPTH):
        x = x + 0.5 * swiglu(rms_norm(x, ffn1_norm[l]), ffn1_w_gate[l], ffn1_w_up[l], ffn1_w_down[l])
        h = rms_norm(x, mix_norm[l])
        (a_val, a_gate, b_gate, c_gate, b_h, q, k, v,
         q_idx, k_idx, w_idx) = jnp.split(h @ w_in[l], SPLIT_POINTS, axis=-1)
        y_a = conformer_conv(a_val, a_gate, conf_dw[l], conf_dw_b[l], conf_ln_g[l], conf_ln_b[l])
        y_b = short_gated_conv(b_gate, c_gate, b_h, sc_dw[l])
        q = apply_rope(q.reshape(bsz, seq, N_HEADS, HEAD_DIM), cos, sin)
        k = apply_rope(k.reshape(bsz, seq, N_HEADS, HEAD_DIM), cos, sin)
        v = v.reshape(bsz, seq, N_HEADS, HEAD_DIM)
        q_idx = apply_rope(q_idx.reshape(bsz, seq, IDX_HEADS, IDX_DIM), cos, sin)
        k_idx = apply_rope(k_idx[:, :, None, :], cos, sin)[:, :, 0, :]
        y_c = dsa_attention(q, k, v, q_idx, k_idx, w_idx)
        x = x + jnp.concatenate([y_a, y_b, y_c], axis=-1) @ w_out[l]
        x = x + memory_cross_attention(rms_norm(x, xa_norm[l]), rms_norm(mem, mem_norm[l]),
                                       xa_wq[l], xa_wkv[l], xa_wo[l])
        x = x + 0.5 * swiglu(rms_norm(x, ffn2_norm[l]), ffn2_w_gate[l], ffn2_w_up[l], ffn2_w_down[l])
    return rms_norm(x, final_norm)
```

```python
import contextlib
import numpy as np
import concourse.bass as bass
import concourse.mybir as mybir
from concourse.bass_utils import run_bass_kernel_spmd

ALU = mybir.AluOpType
AF = mybir.ActivationFunctionType
AX = mybir.AxisListType
F32 = mybir.dt.float32
BF16 = mybir.dt.bfloat16
I32 = mybir.dt.int32

D = 1024
DFF = 2816
T = 512
NT = 8
TOK = T * NT
NITER = 16
IDX_SCALE = 0.5 * 0.125
NEG = -1.0e30
NDSEM = 24
LP = 114
PV_FIN = 228
PV_INVF = 236
PV_SGN = 237
PV_SEL = 238
PV_EPS6 = 240
PV_EPS5 = 241
NPV = 242
SLOTW = 4608
NSLOT = 4
LA = 3
TWO_PI = 6.283185307179586
TR = 1192

BLK = [("gu1", 11, 4096), ("dn1", 8, 3072), ("win", 8, 4096), ("wv", 1, 4608), ("wout", 2, 4096),
       ("wq", 2, 4096), ("wkv", 4, 4096), ("wo", 2, 4096), ("gu2", 11, 4096), ("dn2", 8, 3072)]
BLK_OFF = {}
_r = 0
for _n, _c, _w in BLK:
    BLK_OFF[_n] = (_r, _w)
    _r += _c * (_w // 4)
BLOB_ROWS = _r


class Res:
    __slots__ = ("name", "lw", "rd", "rd_dma")

    def __init__(self, name=""):
        self.name = name
        self.lw = None
        self.rd = {}
        self.rd_dma = []


class Node:
    __slots__ = ("eng", "fn", "deps", "sig", "sigidx", "dma", "dsem", "dcnt", "inc")

    def __init__(self, eng, fn, dma):
        self.eng = eng
        self.fn = fn
        self.dma = dma
        self.deps = []
        self.sig = False
        self.sigidx = 0
        self.dsem = None
        self.dcnt = 0
        self.inc = 16


class Prog:
    def __init__(self, nc):
        self.nc = nc
        self.q = {k: [] for k in ("pe", "act", "dve", "pool", "sp")}
        self.dq = {k: {"n": 0, "last": [None] * NDSEM, "cnt": [0] * NDSEM} for k in ("sp", "pool", "act")}
        self.final = []

    def op(self, eng, fn, reads=(), writes=(), dma=False, inc=16):
        n = Node(eng, fn, dma)
        n.inc = inc
        deps = []
        for r in reads:
            if r.lw is not None:
                deps.append(r.lw)
        for w in writes:
            if w.lw is not None and (dma or w.lw.dma or w.lw.eng != eng):
                deps.append(w.lw)
            for e, rn in w.rd.items():
                if dma or e != eng:
                    deps.append(rn)
            deps.extend(w.rd_dma)
        if dma:
            d = self.dq[eng]
            k = d["n"] % NDSEM
            d["n"] += 1
            if d["last"][k] is not None:
                deps.append(d["last"][k])
            d["cnt"][k] += inc
            n.dsem = (eng, k)
            n.dcnt = d["cnt"][k]
            d["last"][k] = n
        seen = set()
        for x in deps:
            if id(x) not in seen and x is not n:
                seen.add(id(x))
                n.deps.append(x)
                if not x.dma:
                    x.sig = True
        for r in reads:
            if dma:
                r.rd_dma.append(n)
            else:
                r.rd[eng] = n
        for w in writes:
            w.lw = n
            w.rd = {}
            w.rd_dma = []
        self.q[eng].append(n)
        return n

    def emit(self):
        nc = self.nc
        engobj = {"pe": nc.tensor, "act": nc.scalar, "dve": nc.vector, "pool": nc.gpsimd, "sp": nc.sync}
        fin = Node("sp", None, False)
        fin.deps = list(self.final)
        for x in fin.deps:
            if not x.dma:
                x.sig = True
        self.q["sp"].append(fin)
        for e in self.q:
            c = 0
            for n in self.q[e]:
                if n.dma:
                    continue
                if n.sig:
                    c += 1
                    n.sigidx = c
        with contextlib.ExitStack() as st:
            esem = {e: st.enter_context(nc.semaphore("S_" + e)) for e in self.q}
            dsem = {}
            for qn in self.dq:
                for k in range(NDSEM):
                    dsem[(qn, k)] = st.enter_context(nc.semaphore("D_%s%d" % (qn, k)))
            block = st.enter_context(nc.Block())

            def run(e):
                eo = engobj[e]
                waited = {}
                for n in self.q[e]:
                    for d in n.deps:
                        if d.dma:
                            key = ("d",) + d.dsem
                            sem = dsem[d.dsem]
                            val = d.dcnt
                        else:
                            key = ("e", d.eng)
                            sem = esem[d.eng]
                            val = d.sigidx
                        if waited.get(key, 0) < val:
                            eo.wait_ge(sem, val)
                            waited[key] = val
                    if n.fn is None:
                        continue
                    ins = n.fn(eo)
                    if n.dma:
                        ins.then_inc(dsem[n.dsem], n.inc)
                    elif n.sig:
                        ins.then_inc(esem[e], 1)

            @block.tensor
            def _(t):
                run("pe")

            @block.scalar
            def _(t):
                run("act")

            @block.vector
            def _(t):
                run("dve")

            @block.gpsimd
            def _(t):
                run("pool")

            @block.sync
            def _(t):
                run("sp")


class KB:
    def __init__(self, debug=False, stop=None):
        self.debug = debug
        self.stop = stop
        self.nc = bass.Bass("TRN2", target_bir_lowering=False)
        self.P = Prog(self.nc)
        self.st = contextlib.ExitStack()
        self.dbg_outs = []

    def dram(self, name, shape, dt, kind="Internal"):
        if self.debug and kind == "Internal" and name.startswith("dbg_"):
            kind = "ExternalOutput"
            self.dbg_outs.append(name)
        return self.nc.dram_tensor(name, shape, dt, kind=kind).ap()

    def sb(self, name, shape, dt):
        t = self.st.enter_context(self.nc.sbuf_tensor(name, shape, dt))
        return t, Res(name)

    def op(self, *a, **k):
        return self.P.op(*a, **k)

    def dma(self, out, in_, reads=(), writes=(), q="sp"):
        n = self.P.op(q, lambda e: e.dma_start(out=out, in_=in_), reads=reads, writes=writes, dma=True)
        if self.debug:
            self.P.final.append(n)
        return n

    def bank(self):
        i = self.bank_i % 6
        self.bank_i += 1
        return self.banks[i][:], self.bank_res[i]

    def wpush(self, l, name, i):
        r0, w = BLK_OFF[name]
        rows = w // 4
        a = r0 + i * rows
        ap = self.blob[l][a:a + rows, :].rearrange("(p m) c -> p (m c)", p=128)
        self.wfifo.append((ap, w, self.blk_res[(l, name, i)]))

    def wpop(self):
        while self.w_issued < min(len(self.wfifo), self.w_popped + LA):
            ap, w, bres = self.wfifo[self.w_issued]
            s = self.w_issued % NSLOT
            slot = self.wslots[s]
            self.dma(slot[:, 0:w], ap, reads=[bres], writes=[self.wslot_res[s]])
            self.w_issued += 1
        s = self.w_popped % NSLOT
        self.w_popped += 1
        return self.wslots[s], self.wslot_res[s]

    def build(self):
        nc = self.nc
        P = self.P
        self.xin = self.dram("xin", [NT, 8, 128, T], F32, kind="ExternalInput")
        self.posd = self.dram("pos", [TOK], I32, kind="ExternalInput")
        self.memT = self.dram("memT", [8, 128, 256], F32, kind="ExternalInput")
        self.cmaskd = self.dram("cmask", [128, 4, 1024], F32, kind="ExternalInput")
        self.pvecd = self.dram("pvec", [128, NPV], F32, kind="ExternalInput")
        self.identd = self.dram("ident", [128, 128], F32, kind="ExternalInput")
        self.wsl = [self.dram("wsl%d" % l, [BLOB_ROWS, 512], F32, kind="ExternalInput") for l in range(2)]
        self.outd = self.dram("out", [NT, 8, 128, T], F32, kind="ExternalOutput")
        self.blob = [self.dram("blob%d" % l, [BLOB_ROWS, 512], BF16) for l in range(2)]
        self.blk_res = {}
        self.xTd = self.dram("dbg_xT", [NT, 8, 128, T], F32)
        self.xT_res = [Res("xT%d" % j) for j in range(NT)]
        self.hAd = self.dram("dbg_hA", [2, 128, TOK], BF16)
        self.cghd = self.dram("dbg_cgh", [2, 128, TOK], BF16)
        self.bgd = self.dram("dbg_bg", [2, 128, TOK], BF16)
        self.qd = self.dram("dbg_q", [4, 128, TOK], BF16)
        self.qid = self.dram("dbg_qi", [2, 128, TOK], BF16)
        self.widxd = self.dram("dbg_widx", [TOK, 4], F32)
        self.loc_res = [Res("loc%d" % j) for j in range(NT)]
        self.send = [[self.dram("send%d_%d" % (l, j), [TR, 512], BF16) for j in range(NT)] for l in range(2)]
        self.send_res = [[Res("send") for j in range(NT)] for l in range(2)]
        self.ag = [[self.dram("agbuf%d_%d" % (l, j), [2 * TR, 512], BF16) for j in range(NT)] for l in range(2)]
        self.ag_res = [[Res("ag") for j in range(NT)] for l in range(2)]

        if self.debug:
            self.dbg_y = self.dram("dbg_y", [NT, 8, 128, T], BF16)
            self.dbg_x2 = self.dram("dbg_x2", [NT, 8, 128, T], F32)
            self.dbg_x3 = self.dram("dbg_x3", [NT, 8, 128, T], F32)
            self.dbg_x4 = self.dram("dbg_x4", [NT, 8, 128, T], F32)
            self.dbg_sc = self.dram("dbg_sc", [4, 128, 1024], F32)
            self.dbg_sel = self.dram("dbg_sel", [4, 128, 1024], BF16)
            self.dbg_bis = self.dram("dbg_bis", [4, 128, 64], F32)
            self.dbg_ycs = self.dram("dbg_ycs", [4, 128, 512], BF16)
        sb = self.sb
        self.wslots = []
        self.wslot_res = []
        for s in range(NSLOT):
            t, r = sb("wslot%d" % s, [128, SLOTW], BF16)
            self.wslots.append(t)
            self.wslot_res.append(r)
        self.wfifo = []
        self.w_issued = 0
        self.w_popped = 0
        self.xt, self.xt_r = sb("xt", [128, 8, T], F32)
        self.xn, self.xn_r = sb("xn", [128, 8, T], BF16)
        self.big, self.big_r = sb("big", [128, 8192], F32)
        self.sel, self.sel_r = sb("sel", [128, 8192], BF16)
        self.kbuf, self.kbuf_r = sb("kbuf", [128, 8192], BF16)
        self.kvb, self.kvb_r = sb("kvb", [128, 8320], BF16)
        self.cmask, self.cmask_r = sb("cmaskb", [128, 4, 1024], BF16)
        self.pvec, self.pvec_r = sb("pvecs", [128, NPV], F32)
        self.identf, self.identf_r = sb("identf", [128, 128], F32)
        self.ident, self.ident_r = sb("identb", [128, 128], BF16)
        self.onesf, self.onesf_r = sb("onesf", [128, 128], F32)
        self.cosT, self.cos_r = sb("cosT", [128, T], F32)
        self.sinT, self.sin_r = sb("sinT", [128, T], F32)
        self.tmpA = [sb("tmpA%d" % i, [128, T], F32) for i in range(4)]
        self.tmpB = [sb("tmpB%d" % i, [128, T], BF16) for i in range(4)]
        self.rtmp, self.rtmp_r = sb("rtmp", [128, 4, T], F32)
        self.yT, self.yT_r = sb("yT", [128, 8, T], BF16)
        self.qT, self.qT_r = sb("qT", [128, 4, T], BF16)
        self.qiT, self.qiT_r = sb("qiT", [128, 2, T], BF16)
        self.vt, self.vt_r = sb("vt", [128, 4, 520], BF16)
        self.wsc, self.wsc_r = sb("wsc", [128, 4, 4], F32)
        self.small, self.small_r = sb("small", [128, 64], F32)
        self.bis, self.bis_r = sb("bis", [128, 64], F32)
        self.rstd, self.rstd_r = sb("rstd", [128, T], F32)
        self.pT, self.pT_r = sb("pT", [128, 4, 128], BF16)
        self.ycs, self.ycs_r = sb("ycs", [128, 512], BF16)
        self.kmT, self.kmT_r = sb("kmT", [128, 8, 256], BF16)
        self.vm, self.vm_r = sb("vm", [128, 2, 1024], BF16)
        self.memx, self.memx_r = self.rtmp[:].rearrange("p a (b n) -> p (a b) n", b=2), self.rtmp_r
        self.memn, self.memn_r = self.qT[:].rearrange("p a (b n) -> p (a b) n", b=2), self.qT_r
        self.halo, self.halo_r = sb("halo", [128, 2, 2, 32], BF16)
        self.cin, self.cin_r = sb("cin", [128, 2, 32 + T], BF16)
        self.cacc, self.cacc_r = self.xn[:].rearrange("p a n -> p (a n)").bitcast(F32)[:, 0:2 * T].rearrange("p (c n) -> p c n", c=2), self.xn_r
        self.act = self.big[:].bitcast(BF16)
        self.banks = []
        self.bank_res = []
        for i in range(8):
            t = self.st.enter_context(nc.psum_tensor("bank%d" % i, [128, 512], F32))
            self.banks.append(t)
            self.bank_res.append(Res("bank%d" % i))
        self.bank_i = 0
        self.tmp_i = 0

        self.dma(self.pvec[:], self.pvecd, writes=[self.pvec_r])
        self.dma(self.identf[:], self.identd, writes=[self.identf_r])
        self.op("dve", lambda e: e.tensor_copy(out=self.ident[:], in_=self.identf[:]), reads=[self.identf_r], writes=[self.ident_r])
        self.op("dve", lambda e: e.memset(self.onesf[:], 1.0), writes=[self.onesf_r])
        self.op("dve", lambda e: e.memset(self.vt[:], 1.0), writes=[self.vt_r])
        self.dma(self.cmask[:], self.cmaskd, writes=[self.cmask_r], q="pool")
        for l in range(2):
            for name, cnt, w in BLK:
                r0, _ = BLK_OFF[name]
                rows = w // 4
                for i in range(cnt):
                    r = Res("blk")
                    self.blk_res[(l, name, i)] = r
                    a = r0 + i * rows
                    self.dma(self.blob[l][a:a + rows, :], self.wsl[l][a:a + rows, :], writes=[r], q="pool")
        if self.stop == "T0":
            self.load_x(self.xin, 0, None)
            self.phaseA(0, 0)
            self.exchange(0, 0)
            self.mem_kv(0)
            self.load_x(self.xTd, 0, self.xT_res[0])
            self.phaseB(0, 0)
            return self.finish()
        for j in range(NT):
            self.load_x(self.xin, j, None)
            self.phaseA(0, j)
            self.exchange(0, j)
        if self.stop == "A0":
            return self.finish()
        for l in range(2):
            self.mem_kv(l)
            for j in range(NT):
                self.load_x(self.xTd, j, self.xT_res[j])
                self.phaseB(l, j)
                if l == 0:
                    self.phaseA(1, j)
                    self.exchange(1, j)
                else:
                    self.final_out(j)
            if l == 0 and self.stop == "B0":
                return self.finish()
        return self.finish()

    def finish(self):
        self.P.emit()
        self.st.close()
        return self.nc

    def pcol(self, c):
        return self.pvec[:, c:c + 1]

    def load_x(self, src, j, res):
        self.dma(self.xt[:], src[j].rearrange("c p n -> p c n"), reads=[res] if res else [], writes=[self.xt_r])

    def tmpa(self):
        i = self.tmp_i % 4
        self.tmp_i += 1
        return self.tmpA[i][0], self.tmpA[i][1]

    def tmpb(self):
        i = self.tmp_i % 4
        self.tmp_i += 1
        return self.tmpB[i][0], self.tmpB[i][1]

    def rmsnorm(self, x, x_r, gc0, out, out_r, nch, N, eps_col=PV_EPS6):
        pb, pbr = self.bank()
        for c in range(nch):
            sq, sqr = self.tmpa()
            self.op("act", lambda e, c=c, sq=sq: e.activation(out=sq[:, 0:N], in_=x[:, c, :], func=AF.Square), reads=[x_r], writes=[sqr])
            self.op("pe", lambda e, c=c, sq=sq: e.matmul(pb[:, 0:N], lhsT=self.onesf[:], rhs=sq[:, 0:N], start=(c == 0), stop=(c == nch - 1)),
                    reads=[sqr, self.onesf_r], writes=[pbr])
        sd, sdr = self.tmpa()
        self.op("act", lambda e: e.activation(out=sd[:, 0:N], in_=pb[:, 0:N], func=AF.Sqrt, bias=self.pcol(eps_col), scale=1.0 / (nch * 128)),
                reads=[pbr, self.pvec_r], writes=[sdr])
        self.op("dve", lambda e: e.reciprocal(out=self.rstd[:, 0:N], in_=sd[:, 0:N]), reads=[sdr], writes=[self.rstd_r])
        for c in range(nch):
            self.op("dve", lambda e, c=c: e.scalar_tensor_tensor(out=out[:, c, :], in0=x[:, c, :], scalar=self.pcol(gc0 + c), in1=self.rstd[:, 0:N],
                                                                 op0=ALU.mult, op1=ALU.mult),
                    reads=[x_r, self.rstd_r, self.pvec_r], writes=[out_r])

    def ffn(self, l, which):
        gu = "gu%d" % which
        dn = "dn%d" % which
        for i in range(11):
            self.wpush(l, gu, i)
        for i in range(8):
            self.wpush(l, dn, i)
        self.rmsnorm(self.xt, self.xt_r, l * LP + (0 if which == 1 else 32), self.xn, self.xn_r, 8, T)
        act = self.act
        for i in range(11):
            slot, sres = self.wpop()
            sv = slot[:, 0:4096].rearrange("p (k n) -> p k n", k=8)
            for cc in range(2):
                pg, pgr = self.bank()
                pu, pur = self.bank()
                for k in range(8):
                    self.op("pe", lambda e, k=k, cc=cc, sv=sv, pg=pg: e.matmul(pg, lhsT=sv[:, k, cc * 128:(cc + 1) * 128], rhs=self.xn[:, k, :],
                                                                          start=(k == 0), stop=(k == 7)),
                            reads=[sres, self.xn_r], writes=[pgr])
                for k in range(8):
                    self.op("pe", lambda e, k=k, cc=cc, sv=sv, pu=pu: e.matmul(pu, lhsT=sv[:, k, 256 + cc * 128:256 + (cc + 1) * 128], rhs=self.xn[:, k, :],
                                                                          start=(k == 0), stop=(k == 7)),
                            reads=[sres, self.xn_r], writes=[pur])
                sg, sgr = self.tmpa()
                self.op("act", lambda e, sg=sg, pg=pg: e.activation(out=sg[:], in_=pg, func=AF.Silu), reads=[pgr], writes=[sgr])
                ch = 2 * i + cc
                self.op("dve", lambda e, sg=sg, pu=pu, ch=ch: e.tensor_tensor(out=act[:, ch * T:(ch + 1) * T], in0=sg[:], in1=pu, op=ALU.mult),
                        reads=[sgr, pur], writes=[self.big_r])
        for oc in range(8):
            slot, sres = self.wpop()
            sv = slot[:, 0:2816].rearrange("p (k n) -> p k n", k=22)
            po, por = self.bank()
            for k in range(22):
                self.op("pe", lambda e, k=k, sv=sv, po=po: e.matmul(po, lhsT=sv[:, k, :], rhs=act[:, k * T:(k + 1) * T], start=(k == 0), stop=(k == 21)),
                        reads=[sres, self.big_r], writes=[por])
            self.op("dve", lambda e, oc=oc, po=po: e.scalar_tensor_tensor(out=self.xt[:, oc, :], in0=po, scalar=0.5, in1=self.xt[:, oc, :],
                                                                         op0=ALU.mult, op1=ALU.add),
                    reads=[por, self.xt_r], writes=[self.xt_r])

    def rope_tables(self, j):
        e_ = self
        pi_, pi_r = self.tmpa()
        posi = pi_[:].bitcast(I32)
        self.dma(posi, self.posd[j * T:(j + 1) * T].partition_broadcast(128), writes=[pi_r])
        a, ar = self.tmpa()
        k, kr = self.tmpa()
        ki, kir = self.tmpa()
        kiv = ki[:].bitcast(I32)
        op = self.op
        op("dve", lambda e: e.tensor_copy(out=a[:], in_=posi), reads=[pi_r], writes=[ar])
        op("dve", lambda e: e.tensor_scalar(out=a[:], in0=a[:], scalar1=self.pcol(PV_INVF), scalar2=None, op0=ALU.mult), reads=[ar, self.pvec_r], writes=[ar])
        op("dve", lambda e: e.tensor_scalar(out=k[:], in0=a[:], scalar1=1.0 / TWO_PI, scalar2=None, op0=ALU.mult), reads=[ar], writes=[kr])
        op("dve", lambda e: e.tensor_copy(out=kiv, in_=k[:]), reads=[kr], writes=[kir])
        op("dve", lambda e: e.tensor_copy(out=k[:], in_=kiv), reads=[kir], writes=[kr])
        op("dve", lambda e: e.scalar_tensor_tensor(out=a[:], in0=k[:], scalar=-TWO_PI, in1=a[:], op0=ALU.mult, op1=ALU.add), reads=[kr, ar], writes=[ar])

        def fold(t, tr):
            op("dve", lambda e: e.tensor_scalar(out=k[:], in0=t[:], scalar1=np.pi, scalar2=-TWO_PI, op0=ALU.is_gt, op1=ALU.mult), reads=[tr], writes=[kr])
            op("dve", lambda e: e.tensor_tensor(out=t[:], in0=t[:], in1=k[:], op=ALU.add), reads=[tr, kr], writes=[tr])
            op("dve", lambda e: e.tensor_scalar(out=k[:], in0=t[:], scalar1=-np.pi, scalar2=TWO_PI, op0=ALU.is_lt, op1=ALU.mult), reads=[tr], writes=[kr])
            op("dve", lambda e: e.tensor_tensor(out=t[:], in0=t[:], in1=k[:], op=ALU.add), reads=[tr, kr], writes=[tr])
        fold(a, ar)
        op("act", lambda e: e.activation(out=self.sinT[:], in_=a[:], func=AF.Sin), reads=[ar], writes=[self.sin_r])
        op("dve", lambda e: e.tensor_scalar(out=self.sinT[:], in0=self.sinT[:], scalar1=self.pcol(PV_SGN), scalar2=None, op0=ALU.mult),
           reads=[self.sin_r, self.pvec_r], writes=[self.sin_r])
        op("dve", lambda e: e.tensor_scalar(out=a[:], in0=a[:], scalar1=0.5 * np.pi, scalar2=None, op0=ALU.add), reads=[ar], writes=[ar])
        fold(a, ar)
        op("act", lambda e: e.activation(out=self.cosT[:], in_=a[:], func=AF.Sin), reads=[ar], writes=[self.cos_r])

    def phaseA(self, l, j):
        op = self.op
        self.ffn(l, 1)
        self.dma(self.xTd[j].rearrange("c p n -> p c n"), self.xt[:], reads=[self.xt_r], writes=[self.xT_res[j]])
        for i in range(8):
            self.wpush(l, "win", i)
        self.wpush(l, "wv", 0)
        self.rmsnorm(self.xt, self.xt_r, l * LP + 8, self.xn, self.xn_r, 8, T)
        self.rope_tables(j)
        hn = self.xn
        yT = self.yT
        loc = self.loc_res[j]
        cols = slice(j * T, (j + 1) * T)
        held = {}
        sendv = self.send[l][j]
        sres_ = self.send_res[l][j]
        kview = sendv[0:512, :].rearrange("(c p) n -> p c n", c=4)
        kiview = sendv[512:640, :]
        hview = [sendv[1160 + 16 * i:1160 + 16 * (i + 1), :].rearrange("(c q) (h t) -> (q h) c t", c=2, q=8, h=16, t=32) for i in range(2)]
        for blk in range(8):
            slot, sres = self.wpop()
            sv = slot[:, 0:4096].rearrange("p (k n) -> p k n", k=8)
            for cc in range(4):
                ch = blk * 4 + cc
                pb, pbr = self.bank()
                for k in range(8):
                    op("pe", lambda e, k=k, cc=cc, sv=sv, pb=pb: e.matmul(pb, lhsT=sv[:, k, cc * 128:(cc + 1) * 128], rhs=hn[:, k, :], start=(k == 0), stop=(k == 7)),
                       reads=[sres, self.xn_r], writes=[pbr])
                if ch in (0, 1, 6, 7):
                    held[ch] = (pb, pbr)
                elif ch in (2, 3):
                    c = ch - 2
                    sg, sgr = self.tmpa()
                    op("act", lambda e, sg=sg, pb=pb: e.activation(out=sg[:], in_=pb, func=AF.Sigmoid), reads=[pbr], writes=[sgr])
                    pv, pvr = held[c]
                    op("dve", lambda e, sg=sg, pv=pv, c=c: e.tensor_tensor(out=yT[:, c, :], in0=sg[:], in1=pv, op=ALU.mult), reads=[sgr, pvr], writes=[self.yT_r])
                    if c == 1:
                        self.dma(self.hAd[:, :, cols].rearrange("c p n -> p c n"), yT[:, 0:2, :], reads=[self.yT_r], writes=[loc])
                        self.dma(hview[0], yT[:, 0:2, T - 32:T], reads=[self.yT_r], writes=[sres_])
                elif ch in (4, 5):
                    c = ch - 4
                    op("act", lambda e, pb=pb, c=c: e.copy(out=yT[:, 2 + c, :], in_=pb), reads=[pbr], writes=[self.yT_r])
                    if c == 1:
                        self.dma(self.bgd[:, :, cols].rearrange("c p n -> p c n"), yT[:, 2:4, :], reads=[self.yT_r], writes=[loc])
                elif ch in (8, 9):
                    c = ch - 8
                    sg, sgr = self.tmpa()
                    pv, pvr = held[6 + c]
                    op("act", lambda e, sg=sg, pv=pv: e.copy(out=sg[:], in_=pv), reads=[pvr], writes=[sgr])
                    op("dve", lambda e, sg=sg, pb=pb, c=c: e.tensor_tensor(out=yT[:, 4 + c, :], in0=sg[:], in1=pb, op=ALU.mult), reads=[sgr, pbr], writes=[self.yT_r])
                    if c == 1:
                        self.dma(self.cghd[:, :, cols].rearrange("c p n -> p c n"), yT[:, 4:6, :], reads=[self.yT_r], writes=[loc])
                        self.dma(hview[1], yT[:, 4:6, T - 32:T], reads=[self.yT_r], writes=[sres_])
                else:
                    for (c0, n, name) in ((10, 4, "q"), (18, 4, "k"), (26, 2, "qi"), (30, 1, "ki")):
                        if c0 <= ch < c0 + n:
                            c = ch - c0
                            op("dve", lambda e, pb=pb, c=c: e.tensor_tensor(out=self.rtmp[:, c, :], in0=pb, in1=self.cosT[:], op=ALU.mult),
                               reads=[pbr, self.cos_r], writes=[self.rtmp_r])
                        elif c0 + n <= ch < c0 + 2 * n:
                            c = ch - c0 - n
                            t2, t2r = self.tmpa()
                            op("dve", lambda e, pb=pb, t2=t2: e.tensor_tensor(out=t2[:], in0=pb, in1=self.sinT[:], op=ALU.mult),
                               reads=[pbr, self.sin_r], writes=[t2r])
                            op("pool", lambda e, t2=t2, c=c: e.tensor_tensor(out=yT[:, c, :], in0=t2[:], in1=self.rtmp[:, c, :], op=ALU.add),
                               reads=[t2r, self.rtmp_r], writes=[self.yT_r])
                            if c == n - 1:
                                if name == "q":
                                    self.dma(self.qd[:, :, cols].rearrange("c p n -> p c n"), yT[:, 0:4, :], reads=[self.yT_r], writes=[loc])
                                elif name == "k":
                                    self.dma(kview, yT[:, 0:4, :], reads=[self.yT_r], writes=[sres_])
                                elif name == "qi":
                                    self.dma(self.qid[:, :, cols].rearrange("c p n -> p c n"), yT[:, 0:2, :], reads=[self.yT_r], writes=[loc])
                                else:
                                    self.dma(kiview, yT[:, 0, :], reads=[self.yT_r], writes=[sres_])
        slot, sres = self.wpop()
        sv = slot[:, 0:4608].rearrange("p (k n) -> p k n", k=8)
        vview = sendv[640:1160, :].rearrange("r c -> (r c)").rearrange("(t f) -> t f", f=520)
        for tb in range(4):
            pv, pvr = self.bank()
            pw, pwr = self.bank()
            for k in range(8):
                op("pe", lambda e, k=k, tb=tb, sv=sv, pv=pv: e.matmul(pv, lhsT=hn[:, k, tb * 128:(tb + 1) * 128], rhs=sv[:, k, 0:512], start=(k == 0), stop=(k == 7)),
                   reads=[sres, self.xn_r], writes=[pvr])
            for k in range(8):
                op("pe", lambda e, k=k, tb=tb, sv=sv, pw=pw: e.matmul(pw[:, 0:4], lhsT=hn[:, k, tb * 128:(tb + 1) * 128], rhs=sv[:, k, 512:516], start=(k == 0), stop=(k == 7)),
                   reads=[sres, self.xn_r], writes=[pwr])
            op("act", lambda e, tb=tb, pv=pv: e.copy(out=self.vt[:, tb, :].rearrange("p (h f) -> p h f", f=65)[:, :, 0:64], in_=pv.rearrange("p (h f) -> p h f", f=64)),
               reads=[pvr], writes=[self.vt_r])
            op("dve", lambda e, tb=tb, pw=pw: e.tensor_copy(out=self.wsc[:, tb, :], in_=pw[:, 0:4]), reads=[pwr], writes=[self.wsc_r])
        self.dma(vview.rearrange("(n p) f -> p n f", p=128), self.vt[:], reads=[self.vt_r], writes=[sres_])
        self.dma(self.widxd[j * T:(j + 1) * T, :].rearrange("(n p) f -> p n f", p=128), self.wsc[:], reads=[self.wsc_r], writes=[loc])

    def exchange(self, l, j):
        self.op("pool", lambda e: e.collective_compute("AllGather", ALU.bypass, replica_groups=[[0, 1], [2, 3], [4, 5], [6, 7]],
                                                       ins=[self.send[l][j]], outs=[self.ag[l][j]]),
                reads=[self.send_res[l][j]], writes=[self.ag_res[l][j]], dma=True, inc=1)

    def mem_kv(self, l):
        op = self.op
        for i in range(4):
            self.wpush(l, "wkv", i)
        self.dma(self.memx, self.memT.rearrange("c p n -> p c n"), writes=[self.memx_r])
        self.rmsnorm(self.memx, self.memx_r, l * LP + 24, self.memn, self.memn_r, 8, 256)
        for blk in range(2):
            slot, sres = self.wpop()
            sv = slot[:, 0:4096].rearrange("p (k n) -> p k n", k=8)
            for cc in range(4):
                pb, pbr = self.bank()
                for k in range(8):
                    op("pe", lambda e, k=k, cc=cc, sv=sv, pb=pb: e.matmul(pb[:, 0:256], lhsT=sv[:, k, cc * 128:(cc + 1) * 128], rhs=self.memn[:, k, :], start=(k == 0), stop=(k == 7)),
                       reads=[sres, self.memn_r], writes=[pbr])
                op("act", lambda e, pb=pb, ch=blk * 4 + cc: e.copy(out=self.kmT[:, ch, :], in_=pb[:, 0:256]), reads=[pbr], writes=[self.kmT_r])
        for blk in range(2):
            slot, sres = self.wpop()
            sv = slot[:, 0:4096].rearrange("p (k n) -> p k n", k=8)
            for mb in range(2):
                pb, pbr = self.bank()
                for k in range(8):
                    op("pe", lambda e, k=k, mb=mb, sv=sv, pb=pb: e.matmul(pb, lhsT=self.memn[:, k, mb * 128:(mb + 1) * 128], rhs=sv[:, k, :], start=(k == 0), stop=(k == 7)),
                       reads=[sres, self.memn_r], writes=[pbr])
                op("act", lambda e, pb=pb, mb=mb, blk=blk: e.copy(out=self.vm[:, mb, blk * 512:(blk + 1) * 512], in_=pb), reads=[pbr], writes=[self.vm_r])

    def linear_res(self, l, name, inp, inp_r, nk):
        op = self.op
        for blk in range(2):
            slot, sres = self.wpop()
            sv = slot[:, 0:4096].rearrange("p (k n) -> p k n", k=8)
            for cc in range(4):
                oc = blk * 4 + cc
                pb, pbr = self.bank()
                for k in range(nk):
                    op("pe", lambda e, k=k, cc=cc, sv=sv, pb=pb: e.matmul(pb, lhsT=sv[:, k, cc * 128:(cc + 1) * 128], rhs=inp[:, k, :], start=(k == 0), stop=(k == nk - 1)),
                       reads=[sres, inp_r], writes=[pbr])
                op("dve", lambda e, oc=oc, pb=pb: e.tensor_tensor(out=self.xt[:, oc, :], in0=pb, in1=self.xt[:, oc, :], op=ALU.add),
                   reads=[pbr, self.xt_r], writes=[self.xt_r])

    def conv_branches(self, l, j):
        op = self.op
        base = l * LP
        loc = self.loc_res[j]
        cols = slice(j * T, (j + 1) * T)
        for br in range(2):
            src = self.hAd if br == 0 else self.cghd
            def hv(jj, rr):
                a = rr * TR + 1160 + 16 * br
                return self.ag[l][jj][a:a + 16, :].rearrange("(c q) (h t) -> (q h) c t", c=2, q=8, h=16, t=32)
            self.dma(self.halo[:, 0, :, :], hv(j, 0), reads=[self.ag_res[l][j]], writes=[self.halo_r])
            if j > 0:
                self.dma(self.halo[:, 1, :, :], hv(j - 1, 1), reads=[self.ag_res[l][j - 1]], writes=[self.halo_r])
            else:
                op("dve", lambda e: e.memset(self.halo[:, 1, :, :], 0.0), writes=[self.halo_r])
            self.dma(self.cin[:, :, 32:32 + T], src[:, :, cols].rearrange("c p n -> p c n"), reads=[loc], writes=[self.cin_r])
            op("dve", lambda e: e.tensor_scalar(out=self.cin[:, :, 0:32], in0=self.halo[:, 0, :, :], scalar1=self.pcol(PV_SEL), scalar2=None, op0=ALU.mult),
               reads=[self.halo_r, self.pvec_r], writes=[self.cin_r])
            op("dve", lambda e: e.scalar_tensor_tensor(out=self.cin[:, :, 0:32], in0=self.halo[:, 1, :, :], scalar=self.pcol(PV_SEL + 1), in1=self.cin[:, :, 0:32],
                                                       op0=ALU.mult, op1=ALU.add),
               reads=[self.halo_r, self.pvec_r, self.cin_r], writes=[self.cin_r])
            W = 31 if br == 0 else 3
            wc0 = base + (40 if br == 0 else 108)
            for c in range(2):
                for tap in range(W):
                    sh = 32 - (W - 1) + tap
                    colw = self.pcol(wc0 + c * W + tap)
                    if tap == 0:
                        op("pool", lambda e, c=c, sh=sh, colw=colw: e.tensor_scalar(out=self.cacc[:, c, :], in0=self.cin[:, c, sh:sh + T], scalar1=colw, scalar2=None, op0=ALU.mult),
                           reads=[self.cin_r, self.pvec_r], writes=[self.cacc_r])
                    else:
                        op("pool", lambda e, c=c, sh=sh, colw=colw: e.tensor_scalar(out=self.rstd[:], in0=self.cin[:, c, sh:sh + T], scalar1=colw, scalar2=None, op0=ALU.mult),
                           reads=[self.cin_r, self.pvec_r], writes=[self.rstd_r])
                        op("pool", lambda e, c=c: e.tensor_tensor(out=self.cacc[:, c, :], in0=self.cacc[:, c, :], in1=self.rstd[:], op=ALU.add),
                           reads=[self.rstd_r, self.cacc_r], writes=[self.cacc_r])
            if br == 0:
                for c in range(2):
                    op("dve", lambda e, c=c: e.tensor_scalar(out=self.cacc[:, c, :], in0=self.cacc[:, c, :], scalar1=self.pcol(base + 102 + c), scalar2=None, op0=ALU.add),
                       reads=[self.cacc_r, self.pvec_r], writes=[self.cacc_r])
                pm, pmr = self.bank()
                for c in range(2):
                    op("pe", lambda e, c=c, pm=pm: e.matmul(pm, lhsT=self.onesf[:], rhs=self.cacc[:, c, :], start=(c == 0), stop=(c == 1)),
                       reads=[self.cacc_r, self.onesf_r], writes=[pmr])
                mean, meanr = self.tmpa()
                op("act", lambda e, pm=pm, mean=mean: e.activation(out=mean[:], in_=pm, func=AF.Copy, scale=1.0 / 256), reads=[pmr], writes=[meanr])
                for c in range(2):
                    op("dve", lambda e, c=c, mean=mean: e.tensor_tensor(out=self.cacc[:, c, :], in0=self.cacc[:, c, :], in1=mean[:], op=ALU.subtract),
                       reads=[self.cacc_r, meanr], writes=[self.cacc_r])
                pq, pqr = self.bank()
                for c in range(2):
                    sq, sqr = self.tmpa()
                    op("act", lambda e, c=c, sq=sq: e.activation(out=sq[:], in_=self.cacc[:, c, :], func=AF.Square), reads=[self.cacc_r], writes=[sqr])
                    op("pe", lambda e, c=c, sq=sq, pq=pq: e.matmul(pq, lhsT=self.onesf[:], rhs=sq[:], start=(c == 0), stop=(c == 1)), reads=[sqr, self.onesf_r], writes=[pqr])
                sd, sdr = self.tmpa()
                op("act", lambda e, sd=sd, pq=pq: e.activation(out=sd[:], in_=pq, func=AF.Sqrt, bias=self.pcol(PV_EPS5), scale=1.0 / 256), reads=[pqr, self.pvec_r], writes=[sdr])
                op("dve", lambda e, sd=sd: e.reciprocal(out=self.rstd[:], in_=sd[:]), reads=[sdr], writes=[self.rstd_r])
                for c in range(2):
                    op("dve", lambda e, c=c: e.scalar_tensor_tensor(out=self.cacc[:, c, :], in0=self.cacc[:, c, :], scalar=self.pcol(base + 104 + c), in1=self.rstd[:],
                                                                    op0=ALU.mult, op1=ALU.mult), reads=[self.cacc_r, self.rstd_r, self.pvec_r], writes=[self.cacc_r])
                    op("act", lambda e, c=c: e.activation(out=self.yT[:, c, :], in_=self.cacc[:, c, :], func=AF.Silu, bias=self.pcol(base + 106 + c), scale=1.0),
                       reads=[self.cacc_r, self.pvec_r], writes=[self.yT_r])
            else:
                self.dma(self.cin[:, :, 32:32 + T], self.bgd[:, :, cols].rearrange("c p n -> p c n"), reads=[loc], writes=[self.cin_r])
                for c in range(2):
                    op("dve", lambda e, c=c: e.tensor_tensor(out=self.yT[:, 2 + c, :], in0=self.cacc[:, c, :], in1=self.cin[:, c, 32:32 + T], op=ALU.mult),
                       reads=[self.cacc_r, self.cin_r], writes=[self.yT_r])

    def dsa(self, l, j):
        op = self.op
        loc = self.loc_res[j]
        cols = slice(j * T, (j + 1) * T)
        S = 1024 * (j + 1)
        nch = S // 512
        sc = self.big
        self.dma(self.qT[:], self.qd[:, :, cols].rearrange("c p n -> p c n"), reads=[loc], writes=[self.qT_r])
        self.dma(self.qiT[:], self.qid[:, :, cols].rearrange("c p n -> p c n"), reads=[loc], writes=[self.qiT_r])
        self.dma(self.wsc[:], self.widxd[j * T:(j + 1) * T, :].rearrange("(n p) f -> p n f", p=128), reads=[loc], writes=[self.wsc_r])
        op("dve", lambda e: e.tensor_scalar(out=self.wsc[:], in0=self.wsc[:], scalar1=IDX_SCALE, scalar2=None, op0=ALU.mult), reads=[self.wsc_r], writes=[self.wsc_r])
        vb_v = self.kvb[:, 0:8320].rearrange("p (k f) -> p k f", f=130)
        nkt = 2 * (j + 1)
        def agt(kt):
            return self.ag[l][kt // 2], (kt % 2) * TR, self.ag_res[l][kt // 2]
        bis = self.bis

        def _blk(b):
            qs = slice(b * 128, (b + 1) * 128)
            for kt in range(nkt):
                a_, o_, r_ = agt(kt)
                self.dma(self.kvb[:, kt * 512:(kt + 1) * 512], a_[o_ + 512:o_ + 640, :], reads=[r_], writes=[self.kvb_r])
            for ch in range(nch):
                cs = slice(ch * 512, (ch + 1) * 512)
                for h in range(4):
                    ps_ = slice((h % 2) * 64, (h % 2) * 64 + 64)
                    pb, pbr = self.bank()
                    op("pe", lambda e, pb=pb, ps_=ps_, h=h, cs=cs: e.matmul(pb, lhsT=self.qiT[ps_, h // 2, qs], rhs=self.kvb[ps_, cs], start=True, stop=True),
                       reads=[self.qiT_r, self.kvb_r], writes=[pbr])
                    rl, rlr = self.tmpa()
                    op("act", lambda e, pb=pb, rl=rl: e.activation(out=rl[:], in_=pb, func=AF.Relu), reads=[pbr], writes=[rlr])
                    if h == 0:
                        op("dve", lambda e, rl=rl, cs=cs: e.tensor_scalar(out=sc[:, cs], in0=rl[:], scalar1=self.wsc[:, b, 0:1], scalar2=None, op0=ALU.mult),
                           reads=[rlr, self.wsc_r], writes=[self.big_r])
                    else:
                        op("dve", lambda e, rl=rl, cs=cs, h=h: e.scalar_tensor_tensor(out=sc[:, cs], in0=rl[:], scalar=self.wsc[:, b, h:h + 1], in1=sc[:, cs],
                                                                                     op0=ALU.mult, op1=ALU.add),
                           reads=[rlr, self.wsc_r, self.big_r], writes=[self.big_r])
            last = slice(S - 1024, S)
            for hh in range(2):
                t1, t1r = self.tmpa()
                ls = slice(S - 1024 + hh * 512, S - 1024 + (hh + 1) * 512)
                op("dve", lambda e, t1=t1, ls=ls, hh=hh: e.tensor_tensor(out=t1[:], in0=sc[:, ls], in1=self.cmask[:, b, hh * 512:(hh + 1) * 512], op=ALU.subtract),
                   reads=[self.big_r, self.cmask_r], writes=[t1r])
                op("dve", lambda e, t1=t1, hh=hh: e.tensor_reduce(out=bis[:, 40 + hh:41 + hh], in_=t1[:], axis=AX.X, op=ALU.min), reads=[t1r], writes=[self.bis_r])
            op("dve", lambda e: e.tensor_tensor(out=sc[:, last], in0=sc[:, last], in1=self.cmask[:, b, :], op=ALU.add), reads=[self.big_r, self.cmask_r], writes=[self.big_r])
            if S > 1024:
                op("dve", lambda e: e.tensor_reduce(out=bis[:, 42:43], in_=sc[:, 0:S - 1024], axis=AX.X, op=ALU.min), reads=[self.big_r], writes=[self.bis_r])
                nm = 3
            else:
                nm = 2
            op("dve", lambda e: e.tensor_reduce(out=bis[:, 0:1], in_=bis[:, 40:40 + nm], axis=AX.X, op=ALU.min), reads=[self.bis_r], writes=[self.bis_r])
            op("dve", lambda e: e.tensor_reduce(out=bis[:, 1:2], in_=sc[:, 0:S], axis=AX.X, op=ALU.max), reads=[self.big_r], writes=[self.bis_r])
            op("dve", lambda e: e.tensor_tensor(out=bis[:, 2:3], in0=bis[:, 1:2], in1=bis[:, 0:1], op=ALU.subtract), reads=[self.bis_r], writes=[self.bis_r])
            op("dve", lambda e: e.memset(bis[:, 20:20 + NITER], 0.0), writes=[self.bis_r])
            for it in range(NITER):
                w = 0.5 ** (it + 1)
                op("dve", lambda e, w=w: e.scalar_tensor_tensor(out=bis[:, 3:4], in0=bis[:, 2:3], scalar=w, in1=bis[:, 0:1], op0=ALU.mult, op1=ALU.add),
                   reads=[self.bis_r], writes=[self.bis_r])
                op("dve", lambda e, it=it: e.tensor_scalar(out=self.sel[:, 0:S], in0=sc[:, 0:S], scalar1=bis[:, 3:4], scalar2=0.0, op0=ALU.is_ge, op1=ALU.add,
                                                          accum_out=bis[:, 20 + it:21 + it]),
                   reads=[self.big_r, self.bis_r], writes=[self.sel_r, self.bis_r])
                op("dve", lambda e, it=it: e.tensor_scalar(out=bis[:, 4:5], in0=bis[:, 20 + it:21 + it], scalar1=255.5, scalar2=None, op0=ALU.is_ge),
                   reads=[self.bis_r], writes=[self.bis_r])
                op("dve", lambda e, w=w: e.scalar_tensor_tensor(out=bis[:, 5:6], in0=bis[:, 2:3], scalar=w, in1=bis[:, 4:5], op0=ALU.mult, op1=ALU.mult),
                   reads=[self.bis_r], writes=[self.bis_r])
                op("dve", lambda e: e.tensor_tensor(out=bis[:, 0:1], in0=bis[:, 0:1], in1=bis[:, 5:6], op=ALU.add), reads=[self.bis_r], writes=[self.bis_r])
            op("dve", lambda e: e.tensor_scalar(out=self.sel[:, 0:S], in0=sc[:, 0:S], scalar1=bis[:, 0:1], scalar2=None, op0=ALU.is_ge),
               reads=[self.big_r, self.bis_r], writes=[self.sel_r])
            if self.debug and j == 0:
                self.dma(self.dbg_sc[b], sc[:, 0:1024], reads=[self.big_r])
                self.dma(self.dbg_sel[b], self.sel[:, 0:1024], reads=[self.sel_r])
                self.dma(self.dbg_bis[b], bis[:], reads=[self.bis_r])
            ycp = [self.banks[6][:], self.banks[7][:]]
            ycr = [self.bank_res[6], self.bank_res[7]]
            for c in range(4):
                for kt in range(nkt):
                    a_, o_, r_ = agt(kt)
                    self.dma(self.kbuf[:, kt * 512:(kt + 1) * 512], a_[o_ + c * 128:o_ + (c + 1) * 128, :], reads=[r_], writes=[self.kbuf_r])
                    vv_ = a_[o_ + 640:o_ + 1160, :].rearrange("r c -> (r c)").rearrange("(t f) -> t f", f=520)
                    self.dma(vb_v[:, kt * 4:(kt + 1) * 4, :], vv_[:, c * 130:(c + 1) * 130].rearrange("(n p) f -> p n f", p=128), reads=[r_], writes=[self.kvb_r])
                for half in range(2):
                    h = 2 * c + half
                    ps_ = slice(half * 64, half * 64 + 64)
                    for ch in range(nch):
                        cs = slice(ch * 512, (ch + 1) * 512)
                        pb, pbr = self.bank()
                        op("pe", lambda e, pb=pb, ps_=ps_, cs=cs, c=c: e.matmul(pb, lhsT=self.qT[ps_, c, qs], rhs=self.kbuf[ps_, cs], start=True, stop=True),
                           reads=[self.qT_r, self.kbuf_r], writes=[pbr])
                        op("dve", lambda e, pb=pb, ch=ch: e.tensor_reduce(out=self.small[:, ch:ch + 1], in_=pb, axis=AX.X, op=ALU.max), reads=[pbr], writes=[self.small_r])
                    op("dve", lambda e: e.tensor_reduce(out=self.small[:, 32:33], in_=self.small[:, 0:nch], axis=AX.X, op=ALU.max), reads=[self.small_r], writes=[self.small_r])
                    op("dve", lambda e: e.tensor_scalar(out=self.small[:, 33:34], in0=self.small[:, 32:33], scalar1=-0.125, scalar2=None, op0=ALU.mult),
                       reads=[self.small_r], writes=[self.small_r])
                    yb = ycp[h // 4]
                    ybr = ycr[h // 4]
                    oc0 = (h % 4) * 65
                    for ch in range(nch):
                        cs = slice(ch * 512, (ch + 1) * 512)
                        pb, pbr = self.bank()
                        op("pe", lambda e, pb=pb, ps_=ps_, cs=cs, c=c: e.matmul(pb, lhsT=self.qT[ps_, c, qs], rhs=self.kbuf[ps_, cs], start=True, stop=True),
                           reads=[self.qT_r, self.kbuf_r], writes=[pbr])
                        ex, exr = self.tmpb()
                        op("act", lambda e, pb=pb, ex=ex: e.activation(out=ex[:], in_=pb, func=AF.Exp, bias=self.small[:, 33:34], scale=0.125),
                           reads=[pbr, self.small_r], writes=[exr])
                        pm, pmr = self.tmpb()
                        op("dve", lambda e, ex=ex, pm=pm, cs=cs: e.tensor_tensor(out=pm[:], in0=ex[:], in1=self.sel[:, cs], op=ALU.mult),
                           reads=[exr, self.sel_r], writes=[pmr])
                        tb_, tbr = self.bank()
                        tbv = tb_.bitcast(BF16)
                        for t in range(4):
                            op("pe", lambda e, t=t, pm=pm, tbv=tbv: e.transpose(tbv[:, t * 128:(t + 1) * 128], pm[:, t * 128:(t + 1) * 128], self.ident[:]),
                               reads=[pmr, self.ident_r], writes=[tbr])
                        op("act", lambda e, tbv=tbv: e.copy(out=self.pT[:].rearrange("p a b -> p (a b)"), in_=tbv[:, 0:512]), reads=[tbr], writes=[self.pT_r])
                        for t in range(4):
                            kc = ch * 4 + t
                            first = (ch == 0 and t == 0)
                            lastm = (ch == nch - 1 and t == 3)
                            op("pe", lambda e, t=t, kc=kc, first=first, lastm=lastm, yb=yb, oc0=oc0, half=half:
                               e.matmul(yb[:, oc0:oc0 + 65], lhsT=self.pT[:, t, :], rhs=self.kvb[:, kc * 130 + half * 65: kc * 130 + half * 65 + 65], start=first, stop=lastm),
                               reads=[self.pT_r, self.kvb_r], writes=[ybr])
            for h in range(8):
                yb = ycp[h // 4]
                ybr = ycr[h // 4]
                oc0 = (h % 4) * 65
                op("dve", lambda e, yb=yb, oc0=oc0, h=h: e.reciprocal(out=self.small[:, 40 + h:41 + h], in_=yb[:, oc0 + 64:oc0 + 65]), reads=[ybr], writes=[self.small_r])
                op("dve", lambda e, yb=yb, oc0=oc0, h=h: e.tensor_scalar(out=self.ycs[:, h * 64:(h + 1) * 64], in0=yb[:, oc0:oc0 + 64], scalar1=self.small[:, 40 + h:41 + h],
                                                                        scalar2=None, op0=ALU.mult),
                   reads=[ybr, self.small_r], writes=[self.ycs_r])
            if self.debug and j == 0:
                self.dma(self.dbg_ycs[b], self.ycs[:], reads=[self.ycs_r])
            tb_, tbr = self.bank()
            tbv = tb_.bitcast(BF16)
            for c in range(4):
                op("pe", lambda e, c=c, tbv=tbv: e.transpose(tbv[:, c * 128:(c + 1) * 128], self.ycs[:, c * 128:(c + 1) * 128], self.ident[:]),
                   reads=[self.ycs_r, self.ident_r], writes=[tbr])
            op("act", lambda e, tbv=tbv: e.copy(out=self.yT[:, 4:8, qs], in_=tbv[:, 0:512].rearrange("p (c q) -> p c q", c=4)), reads=[tbr], writes=[self.yT_r])

        for b in range(4):
            _blk(b)

    def xattn(self, l):
        op = self.op
        for i in range(2):
            self.wpush(l, "wq", i)
        for i in range(2):
            self.wpush(l, "wo", i)
        self.rmsnorm(self.xt, self.xt_r, l * LP + 16, self.xn, self.xn_r, 8, T)
        for blk in range(2):
            slot, sres = self.wpop()
            sv = slot[:, 0:4096].rearrange("p (k n) -> p k n", k=8)
            for cc in range(4):
                oc = blk * 4 + cc
                pb, pbr = self.bank()
                for k in range(8):
                    op("pe", lambda e, k=k, cc=cc, sv=sv, pb=pb: e.matmul(pb, lhsT=sv[:, k, cc * 128:(cc + 1) * 128], rhs=self.xn[:, k, :], start=(k == 0), stop=(k == 7)),
                       reads=[sres, self.xn_r], writes=[pbr])
                op("act", lambda e, pb=pb, oc=oc: e.copy(out=self.yT[:, oc, :], in_=pb), reads=[pbr], writes=[self.yT_r])
        o_tok, o_tok_r = self.ycs, self.ycs_r

        def _blk(b):
            qs = slice(b * 128, (b + 1) * 128)
            for half2 in range(2):
                for hh in range(2):
                    h = half2 * 2 + hh
                    pb, pbr = self.bank()
                    for k in range(2):
                        op("pe", lambda e, k=k, h=h, pb=pb: e.matmul(pb[:, 0:256], lhsT=self.yT[:, 2 * h + k, qs], rhs=self.kmT[:, 2 * h + k, :], start=(k == 0), stop=(k == 1)),
                           reads=[self.yT_r, self.kmT_r], writes=[pbr])
                    op("dve", lambda e, pb=pb: e.tensor_reduce(out=self.small[:, 50:51], in_=pb[:, 0:256], axis=AX.X, op=ALU.max), reads=[pbr], writes=[self.small_r])
                    op("dve", lambda e: e.tensor_scalar(out=self.small[:, 51:52], in0=self.small[:, 50:51], scalar1=-1.0 / 16, scalar2=None, op0=ALU.mult),
                       reads=[self.small_r], writes=[self.small_r])
                    op("dve", lambda e: e.memset(self.small[:, 52:53], 0.0), writes=[self.small_r])
                    ex, exr = self.tmpb()
                    op("act", lambda e, pb=pb, ex=ex: e.activation(out=ex[:, 0:256], in_=pb[:, 0:256], func=AF.Exp, bias=self.small[:, 51:52], scale=1.0 / 16,
                                                                  accum_out=self.small[:, 52:53]),
                       reads=[pbr, self.small_r], writes=[exr, self.small_r])
                    tb_, tbr = self.bank()
                    tbv = tb_.bitcast(BF16)
                    for t in range(2):
                        op("pe", lambda e, t=t, ex=ex, tbv=tbv: e.transpose(tbv[:, t * 128:(t + 1) * 128], ex[:, t * 128:(t + 1) * 128], self.ident[:]),
                           reads=[exr, self.ident_r], writes=[tbr])
                    op("act", lambda e, tbv=tbv: e.copy(out=self.pT[:, 0:2, :].rearrange("p a b -> p (a b)"), in_=tbv[:, 0:256]), reads=[tbr], writes=[self.pT_r])
                    po, por = self.bank()
                    for t in range(2):
                        op("pe", lambda e, t=t, h=h, po=po: e.matmul(po[:, 0:256], lhsT=self.pT[:, t, :], rhs=self.vm[:, t, h * 256:(h + 1) * 256], start=(t == 0), stop=(t == 1)),
                           reads=[self.pT_r, self.vm_r], writes=[por])
                    op("dve", lambda e: e.reciprocal(out=self.small[:, 53:54], in_=self.small[:, 52:53]), reads=[self.small_r], writes=[self.small_r])
                    op("dve", lambda e, po=po, hh=hh: e.tensor_scalar(out=o_tok[:, hh * 256:(hh + 1) * 256], in0=po[:, 0:256], scalar1=self.small[:, 53:54], scalar2=None, op0=ALU.mult),
                       reads=[por, self.small_r], writes=[o_tok_r])
                tb_, tbr = self.bank()
                tbv = tb_.bitcast(BF16)
                for c in range(4):
                    op("pe", lambda e, c=c, tbv=tbv: e.transpose(tbv[:, c * 128:(c + 1) * 128], o_tok[:, c * 128:(c + 1) * 128], self.ident[:]),
                       reads=[o_tok_r, self.ident_r], writes=[tbr])
                op("act", lambda e, tbv=tbv, half2=half2: e.copy(out=self.xn[:, half2 * 4:half2 * 4 + 4, qs], in_=tbv[:, 0:512].rearrange("p (c q) -> p c q", c=4)),
                   reads=[tbr], writes=[self.xn_r])
        for b in range(4):
            _blk(b)
        self.linear_res(l, "wo", self.xn, self.xn_r, 8)

    def phaseB(self, l, j):
        for i in range(2):
            self.wpush(l, "wout", i)
        self.conv_branches(l, j)
        self.dsa(l, j)
        if self.debug and l == 0:
            self.dma(self.dbg_y[j].rearrange("c p n -> p c n"), self.yT[:], reads=[self.yT_r])
        self.linear_res(l, "wout", self.yT, self.yT_r, 8)
        if self.debug and l == 0:
            self.dma(self.dbg_x2[j].rearrange("c p n -> p c n"), self.xt[:], reads=[self.xt_r])
        self.xattn(l)
        if self.debug and l == 0:
            self.dma(self.dbg_x3[j].rearrange("c p n -> p c n"), self.xt[:], reads=[self.xt_r])
        self.ffn(l, 2)
        if self.debug and l == 0:
            self.dma(self.dbg_x4[j].rearrange("c p n -> p c n"), self.xt[:], reads=[self.xt_r])

    def final_out(self, j):
        op = self.op
        pb, pbr = self.bank()
        x = self.xt
        for c in range(8):
            sq, sqr = self.tmpa()
            op("act", lambda e, c=c, sq=sq: e.activation(out=sq[:], in_=x[:, c, :], func=AF.Square), reads=[self.xt_r], writes=[sqr])
            op("pe", lambda e, c=c, sq=sq: e.matmul(pb, lhsT=self.onesf[:], rhs=sq[:], start=(c == 0), stop=(c == 7)), reads=[sqr, self.onesf_r], writes=[pbr])
        sd, sdr = self.tmpa()
        op("act", lambda e: e.activation(out=sd[:], in_=pb, func=AF.Sqrt, bias=self.pcol(PV_EPS6), scale=1.0 / 1024), reads=[pbr, self.pvec_r], writes=[sdr])
        op("dve", lambda e: e.reciprocal(out=self.rstd[:], in_=sd[:]), reads=[sdr], writes=[self.rstd_r])
        for c in range(8):
            op("dve", lambda e, c=c: e.scalar_tensor_tensor(out=x[:, c, :], in0=x[:, c, :], scalar=self.pcol(PV_FIN + c), in1=self.rstd[:], op0=ALU.mult, op1=ALU.mult),
               reads=[self.xt_r, self.rstd_r, self.pvec_r], writes=[self.xt_r])
        n = self.dma(self.outd[j].rearrange("c p n -> p c n"), self.xt[:], reads=[self.xt_r])
        self.P.final.append(n)


def _swap(w):
    K, N = w.shape
    w = w.reshape(K, N // 64, 2, 32)
    return np.ascontiguousarray(w[:, :, ::-1, :]).reshape(K, N)


def _blk(w, kc, ncols, width):
    a = w.reshape(kc, 128, ncols).transpose(1, 0, 2).reshape(128, kc * ncols)
    if a.shape[1] < width:
        a = np.concatenate([a, np.zeros((128, width - a.shape[1]), np.float32)], 1)
    return a


def _layer_blob(p, l):
    blocks = []

    def gu(g, u):
        for i in range(11):
            w = np.concatenate([g[:, 256 * i:256 * (i + 1)], u[:, 256 * i:256 * (i + 1)]], 1)
            blocks.append(_blk(w, 8, 512, 4096))

    def dn(dw):
        for oc in range(8):
            blocks.append(_blk(dw[:, oc * 128:(oc + 1) * 128], 22, 128, 3072))

    def sq(w, nb):
        for i in range(nb):
            blocks.append(_blk(w[:, 512 * i:512 * (i + 1)], 8, 512, 4096))

    gu(p["ffn1_w_gate"][l], p["ffn1_w_up"][l])
    dn(p["ffn1_w_down"][l])
    wi = p["w_in"][l]
    a_val, a_gate, b_gate, c_gate, b_h = (wi[:, 256 * i:256 * (i + 1)] for i in range(5))
    q = wi[:, 1280:1792]
    k = wi[:, 1792:2304]
    v = wi[:, 2304:2816]
    qi = wi[:, 2816:3072]
    ki = wi[:, 3072:3136]
    wx = wi[:, 3136:3140]
    ext = np.concatenate([a_val, a_gate, b_gate, c_gate, b_h, q, _swap(q), k, _swap(k), qi, _swap(qi), ki, ki, _swap(ki), _swap(ki)], 1)
    assert ext.shape[1] == 4096
    sq(ext, 8)
    blocks.append(_blk(np.concatenate([v, wx, np.zeros((1024, 60), np.float32)], 1), 8, 576, 4608))
    sq(p["w_out"][l], 2)
    sq(p["xa_wq"][l], 2)
    sq(p["xa_wkv"][l], 4)
    sq(p["xa_wo"][l], 2)
    gu(p["ffn2_w_gate"][l], p["ffn2_w_up"][l])
    dn(p["ffn2_w_down"][l])
    flat = np.concatenate([b.reshape(-1) for b in blocks])
    assert flat.size == BLOB_ROWS * 512, (flat.size, BLOB_ROWS * 512)
    return flat.reshape(BLOB_ROWS, 512)


def _pvec(p, r):
    pv = np.zeros((128, NPV), np.float32)

    def put(c0, vec):
        n = vec.size // 128
        pv[:, c0:c0 + n] = vec.reshape(n, 128).T

    for l in range(2):
        b = l * LP
        put(b + 0, p["ffn1_norm"][l])
        put(b + 8, p["mix_norm"][l])
        put(b + 16, p["xa_norm"][l])
        put(b + 24, p["mem_norm"][l])
        put(b + 32, p["ffn2_norm"][l])
        dw = p["conf_dw"][l]
        for c in range(2):
            pv[:, b + 40 + c * 31: b + 40 + (c + 1) * 31] = dw[:, c * 128:(c + 1) * 128].T
        put(b + 102, p["conf_dw_b"][l])
        put(b + 104, p["conf_ln_g"][l])
        put(b + 106, p["conf_ln_b"][l])
        sw = p["sc_dw"][l]
        for c in range(2):
            pv[:, b + 108 + c * 3: b + 108 + (c + 1) * 3] = sw[:, c * 128:(c + 1) * 128].T
    put(PV_FIN, p["final_norm"])
    pidx = np.arange(128)
    inv_freq = (10000.0 ** (-np.arange(0, 64, 2, dtype=np.float32) / 64)).astype(np.float32)
    pv[:, PV_INVF] = inv_freq[pidx % 32]
    pv[:, PV_SGN] = np.where((pidx % 64) < 32, -1.0, 1.0)
    pv[:, PV_SEL] = 1.0 if r == 1 else 0.0
    pv[:, PV_SEL + 1] = 1.0 if r == 0 else 0.0
    pv[:, PV_EPS6] = 1e-6
    pv[:, PV_EPS5] = 1e-5
    return pv


def _cmask(r):
    i = np.arange(128)[:, None, None]
    b = np.arange(4)[None, :, None]
    c = np.arange(1024)[None, None, :]
    vis = (c - 512 * r) <= (128 * b + i)
    return np.where(vis, 0.0, NEG).astype(np.float32)


_CACHE = {}


def make_inputs(p):
    p = {k: np.asarray(v) for k, v in p.items()}
    blobs = [_layer_blob(p, l) for l in range(2)]
    ident = np.eye(128, dtype=np.float32)
    in_maps = []
    for c in range(8):
        b, r = c // 2, c % 2
        xb = p["x"][b].reshape(16, 512, 8, 128)[r::2]
        xin = np.ascontiguousarray(xb.transpose(0, 2, 3, 1))
        pos = np.ascontiguousarray(p["positions"][b].reshape(16, 512)[r::2].reshape(-1)).astype(np.int32)
        memT = np.ascontiguousarray(p["mem"][b].reshape(256, 8, 128).transpose(1, 2, 0))
        m = {"xin": xin, "pos": pos, "memT": memT, "cmask": _cmask(r), "pvec": _pvec(p, r), "ident": ident,
             "wsl0": blobs[0], "wsl1": blobs[1]}
        in_maps.append(m)
    return in_maps


def assemble(res):
    out = np.zeros((4, 16, 512, 8, 128), np.float32)
    for c in range(8):
        b, r = c // 2, c % 2
        o = res.results[c]["out"]
        out[b, r::2] = o.transpose(0, 3, 1, 2)
    return out.reshape(4, 8192, 1024)


def kernel(**inputs):
    in_maps = make_inputs(inputs)
    if "nc" not in _CACHE:
        _CACHE["nc"] = KB().build()
    res = run_bass_kernel_spmd(_CACHE["nc"], in_maps, core_ids=list(range(8)))
    return assemble(res)
```

```python
import contextlib
import numpy as np
import concourse.bass as bass
import concourse.mybir as mybir
from concourse.bass_utils import run_bass_kernel_spmd

ALU = mybir.AluOpType
AF = mybir.ActivationFunctionType
AX = mybir.AxisListType
F32 = mybir.dt.float32
BF16 = mybir.dt.bfloat16
I32 = mybir.dt.int32

D = 1024
DFF = 2816
T = 512
NT = 8
TOK = T * NT
NITER = 16
IDX_SCALE = 0.5 * 0.125
NEG = -1.0e30
NDSEM = 24
LP = 114
PV_FIN = 228
PV_INVF = 236
PV_SGN = 237
PV_SEL = 238
PV_EPS6 = 240
PV_EPS5 = 241
NPV = 242
SLOTW = 4608
NSLOT = 4
LA = 3
TWO_PI = 6.283185307179586
TR = 1192

BLK = [("gu1", 11, 4096), ("dn1", 8, 3072), ("win", 8, 4096), ("wv", 1, 4608), ("wout", 2, 4096),
       ("wq", 2, 4096), ("wkv", 4, 4096), ("wo", 2, 4096), ("gu2", 11, 4096), ("dn2", 8, 3072)]
BLK_OFF = {}
_r = 0
for _n, _c, _w in BLK:
    BLK_OFF[_n] = (_r, _w)
    _r += _c * (_w // 4)
BLOB_ROWS = _r


class Res:
    __slots__ = ("name", "lw", "rd", "rd_dma")

    def __init__(self, name=""):
        self.name = name
        self.lw = None
        self.rd = {}
        self.rd_dma = []


class Node:
    __slots__ = ("eng", "fn", "deps", "sig", "sigidx", "dma", "dsem", "dcnt", "inc")

    def __init__(self, eng, fn, dma):
        self.eng = eng
        self.fn = fn
        self.dma = dma
        self.deps = []
        self.sig = False
        self.sigidx = 0
        self.dsem = None
        self.dcnt = 0
        self.inc = 16


class Prog:
    def __init__(self, nc):
        self.nc = nc
        self.q = {k: [] for k in ("pe", "act", "dve", "pool", "sp")}
        self.dq = {k: {"n": 0, "last": [None] * NDSEM, "cnt": [0] * NDSEM} for k in ("sp", "pool", "act")}
        self.final = []

    def op(self, eng, fn, reads=(), writes=(), dma=False, inc=16):
        n = Node(eng, fn, dma)
        n.inc = inc
        deps = []
        for r in reads:
            if r.lw is not None:
                deps.append(r.lw)
        for w in writes:
            if w.lw is not None and (dma or w.lw.dma or w.lw.eng != eng or eng != "pe"):
                deps.append(w.lw)
            for e, rn in w.rd.items():
                if dma or e != eng or eng != "pe":
                    deps.append(rn)
            deps.extend(w.rd_dma)
        if dma:
            d = self.dq[eng]
            k = d["n"] % NDSEM
            d["n"] += 1
            if d["last"][k] is not None:
                deps.append(d["last"][k])
            d["cnt"][k] += inc
            n.dsem = (eng, k)
            n.dcnt = d["cnt"][k]
            d["last"][k] = n
        seen = set()
        for x in deps:
            if id(x) not in seen and x is not n:
                seen.add(id(x))
                n.deps.append(x)
                if not x.dma:
                    x.sig = True
        for r in reads:
            if dma:
                r.rd_dma.append(n)
            else:
                r.rd[eng] = n
        for w in writes:
            w.lw = n
            w.rd = {}
            w.rd_dma = []
        self.q[eng].append(n)
        return n

    def emit(self):
        nc = self.nc
        engobj = {"pe": nc.tensor, "act": nc.scalar, "dve": nc.vector, "pool": nc.gpsimd, "sp": nc.sync}
        fin = Node("sp", None, False)
        fin.deps = list(self.final)
        for x in fin.deps:
            if not x.dma:
                x.sig = True
        self.q["sp"].append(fin)
        for e in self.q:
            c = 0
            for n in self.q[e]:
                if n.dma:
                    continue
                if n.sig:
                    c += 1
                    n.sigidx = c
        with contextlib.ExitStack() as st:
            esem = {e: st.enter_context(nc.semaphore("S_" + e)) for e in self.q}
            dsem = {}
            for qn in self.dq:
                for k in range(NDSEM):
                    dsem[(qn, k)] = st.enter_context(nc.semaphore("D_%s%d" % (qn, k)))
            block = st.enter_context(nc.Block())

            def run(e):
                eo = engobj[e]
                waited = {}
                for n in self.q[e]:
                    for d in n.deps:
                        if d.dma:
                            key = ("d",) + d.dsem
                            sem = dsem[d.dsem]
                            val = d.dcnt
                        else:
                            key = ("e", d.eng)
                            sem = esem[d.eng]
                            val = d.sigidx
                        if waited.get(key, 0) < val:
                            eo.wait_ge(sem, val)
                            waited[key] = val
                    if n.fn is None:
                        continue
                    ins = n.fn(eo)
                    if n.dma:
                        ins.then_inc(dsem[n.dsem], n.inc)
                    elif n.sig:
                        ins.then_inc(esem[e], 1)

            @block.tensor
            def _(t):
                run("pe")

            @block.scalar
            def _(t):
                run("act")

            @block.vector
            def _(t):
                run("dve")

            @block.gpsimd
            def _(t):
                run("pool")

            @block.sync
            def _(t):
                run("sp")


class KB:
    def __init__(self, debug=False, stop=None):
        self.debug = debug
        self.stop = stop
        self.nc = bass.Bass("TRN2", target_bir_lowering=False)
        self.P = Prog(self.nc)
        self.st = contextlib.ExitStack()
        self.dbg_outs = []

    def dram(self, name, shape, dt, kind="Internal"):
        if self.debug and kind == "Internal" and name.startswith("dbg_"):
            kind = "ExternalOutput"
            self.dbg_outs.append(name)
        return self.nc.dram_tensor(name, shape, dt, kind=kind).ap()

    def sb(self, name, shape, dt):
        t = self.st.enter_context(self.nc.sbuf_tensor(name, shape, dt))
        return t, Res(name)

    def op(self, *a, **k):
        return self.P.op(*a, **k)

    def dma(self, out, in_, reads=(), writes=(), q="sp"):
        n = self.P.op(q, lambda e: e.dma_start(out=out, in_=in_), reads=reads, writes=writes, dma=True)
        if self.debug:
            self.P.final.append(n)
        return n

    def bank(self):
        i = self.bank_i % 6
        self.bank_i += 1
        return self.banks[i][:], self.bank_res[i]

    def wpush(self, l, name, i):
        r0, w = BLK_OFF[name]
        rows = w // 4
        a = r0 + i * rows
        ap = self.blob[l][a:a + rows, :].rearrange("(p m) c -> p (m c)", p=128)
        self.wfifo.append((ap, w, self.blk_res[(l, name, i)]))

    def wkick(self):
        while self.w_issued < min(len(self.wfifo), self.w_popped + LA):
            ap, w, bres = self.wfifo[self.w_issued]
            s = self.w_issued % NSLOT
            slot = self.wslots[s]
            self.dma(slot[:, 0:w], ap, reads=[bres], writes=[self.wslot_res[s]])
            self.w_issued += 1

    def push_group(self, l, names):
        for name in names:
            cnt = [c for n, c, w in BLK if n == name][0]
            for i in range(cnt):
                self.wpush(l, name, i)
        self.wkick()

    def wpop(self):
        while self.w_issued < min(len(self.wfifo), self.w_popped + LA):
            ap, w, bres = self.wfifo[self.w_issued]
            s = self.w_issued % NSLOT
            slot = self.wslots[s]
            self.dma(slot[:, 0:w], ap, reads=[bres], writes=[self.wslot_res[s]])
            self.w_issued += 1
        s = self.w_popped % NSLOT
        self.w_popped += 1
        return self.wslots[s], self.wslot_res[s]

    def build(self):
        nc = self.nc
        P = self.P
        self.xin = self.dram("xin", [NT, 8, 128, T], F32, kind="ExternalInput")
        self.posd = self.dram("pos", [TOK], I32, kind="ExternalInput")
        self.memT = self.dram("memT", [8, 128, 256], F32, kind="ExternalInput")
        self.cmaskd = self.dram("cmask", [128, 4, 1024], F32, kind="ExternalInput")
        self.pvecd = self.dram("pvec", [128, NPV], F32, kind="ExternalInput")
        self.identd = self.dram("ident", [128, 128], F32, kind="ExternalInput")
        self.wsl = [self.dram("wsl%d" % l, [BLOB_ROWS, 512], F32, kind="ExternalInput") for l in range(2)]
        self.outd = self.dram("out", [NT, 8, 128, T], F32, kind="ExternalOutput")
        self.blob = [self.dram("blob%d" % l, [BLOB_ROWS, 512], BF16) for l in range(2)]
        self.blk_res = {}
        self.xTd = self.dram("dbg_xT", [NT, 8, 128, T], F32)
        self.xT_res = [Res("xT%d" % j) for j in range(NT)]
        self.hAd = self.dram("dbg_hA", [2, 128, TOK], BF16)
        self.cghd = self.dram("dbg_cgh", [2, 128, TOK], BF16)
        self.bgd = self.dram("dbg_bg", [2, 128, TOK], BF16)
        self.qd = self.dram("dbg_q", [4, 128, TOK], BF16)
        self.qid = self.dram("dbg_qi", [2, 128, TOK], BF16)
        self.widxd = self.dram("dbg_widx", [TOK, 4], F32)
        self.loc_res = [Res("loc%d" % j) for j in range(NT)]
        self.send = [[self.dram("send%d_%d" % (l, j), [TR, 512], BF16) for j in range(NT)] for l in range(2)]
        self.send_res = [[Res("send") for j in range(NT)] for l in range(2)]
        self.ag = [[self.dram("agbuf%d_%d" % (l, j), [2 * TR, 512], BF16) for j in range(NT)] for l in range(2)]
        self.ag_res = [[Res("ag") for j in range(NT)] for l in range(2)]

        if self.debug:
            self.dbg_y = self.dram("dbg_y", [NT, 8, 128, T], BF16)
            self.dbg_x2 = self.dram("dbg_x2", [NT, 8, 128, T], F32)
            self.dbg_x3 = self.dram("dbg_x3", [NT, 8, 128, T], F32)
            self.dbg_x4 = self.dram("dbg_x4", [NT, 8, 128, T], F32)
            self.dbg_sc = self.dram("dbg_sc", [4, 128, 1024], F32)
            self.dbg_sel = self.dram("dbg_sel", [4, 128, 1024], BF16)
            self.dbg_bis = self.dram("dbg_bis", [4, 128, 64], F32)
            self.dbg_ycs = self.dram("dbg_ycs", [4, 128, 512], BF16)
        sb = self.sb
        self.wslots = []
        self.wslot_res = []
        for s in range(NSLOT):
            t, r = sb("wslot%d" % s, [128, SLOTW], BF16)
            self.wslots.append(t)
            self.wslot_res.append(r)
        self.wfifo = []
        self.w_issued = 0
        self.w_popped = 0
        self.xt, self.xt_r = sb("xt", [128, 8, T], F32)
        self.xn, self.xn_r = sb("xn", [128, 8, T], BF16)
        self.big, self.big_r = sb("big", [128, 8192], F32)
        self.sel, self.sel_r = sb("sel", [128, 8192], BF16)
        self.kbuf, self.kbuf_r = sb("kbuf", [128, 8192], BF16)
        self.kvb, self.kvb_r = sb("kvb", [128, 8320], BF16)
        self.cmask, self.cmask_r = sb("cmaskb", [128, 4, 1024], BF16)
        self.pvec, self.pvec_r = sb("pvecs", [128, NPV], F32)
        self.identf, self.identf_r = sb("identf", [128, 128], F32)
        self.ident, self.ident_r = sb("identb", [128, 128], BF16)
        self.onesf, self.onesf_r = sb("onesf", [128, 128], F32)
        self.cosT, self.cos_r = sb("cosT", [128, T], F32)
        self.sinT, self.sin_r = sb("sinT", [128, T], F32)
        self.tmpA = [sb("tmpA%d" % i, [128, T], F32) for i in range(4)]
        self.tmpB = [sb("tmpB%d" % i, [128, T], BF16) for i in range(4)]
        cb = self.cosT[:].bitcast(BF16)
        sbv = self.sinT[:].bitcast(BF16)
        self.tmpB += [(cb[:, 0:T], self.cos_r), (sbv[:, 0:T], self.sin_r), (cb[:, T:2 * T], self.cos_r), (sbv[:, T:2 * T], self.sin_r)]
        self.rtmp, self.rtmp_r = sb("rtmp", [128, 4, T], F32)
        rb = self.rtmp[:].rearrange("p a n -> p (a n)").bitcast(BF16)
        self.pTs = [(rb[:, i * 512:(i + 1) * 512].rearrange("p (a b) -> p a b", a=4), Res("pT%d" % i)) for i in range(8)]
        self.pT_i = 0
        self.rtmp_w = [self.rtmp_r] + [r for _, r in self.pTs]
        self.kbuf_rt = [Res("kbuf%d" % i) for i in range(16)]
        self.negm_r = [Res("negm%d" % i) for i in range(8)]
        self.kvb_rt = [Res("kvb%d" % i) for i in range(16)]
        self.yT, self.yT_r = sb("yT", [128, 8, T], BF16)
        self.qT, self.qT_r = sb("qT", [128, 4, T], BF16)
        self.qiT, self.qiT_r = sb("qiT", [128, 2, T], BF16)
        self.vt, self.vt_r = sb("vt", [128, 4, 520], BF16)
        self.wsc, self.wsc_r = sb("wsc", [128, 4, 4], F32)
        self.small, self.small_r = sb("small", [128, 64], F32)
        self.bis, self.bis_r = sb("bis", [128, 64], F32)
        self.rstd, self.rstd_r = sb("rstd", [128, T], F32)
        self.ycs, self.ycs_r = sb("ycs", [128, 512], BF16)
        self.kmT, self.kmT_r = sb("kmT", [128, 8, 256], BF16)
        self.vm, self.vm_r = sb("vm", [128, 2, 1024], BF16)
        self.memx, self.memx_r = self.rtmp[:].rearrange("p a (b n) -> p (a b) n", b=2), self.rtmp_r
        self.memn, self.memn_r = self.qT[:].rearrange("p a (b n) -> p (a b) n", b=2), self.qT_r
        self.halo, self.halo_r = sb("halo", [128, 2, 2, 32], BF16)
        self.cin, self.cin_r = sb("cin", [128, 2, 32 + T], BF16)
        self.cacc, self.cacc_r = self.xn[:].rearrange("p a n -> p (a n)").bitcast(F32)[:, 0:2 * T].rearrange("p (c n) -> p c n", c=2), self.xn_r
        self.cacc2 = self.xn[:].rearrange("p a n -> p (a n)").bitcast(F32)[:, 2 * T:4 * T].rearrange("p (c n) -> p c n", c=2)
        self.cin2 = self.vt[:].rearrange("p a f -> p (a f)")[:, 0:2 * (32 + T)].rearrange("p (c n) -> p c n", c=2)
        self.ptmp, self.ptmp_r = self.tmpA[3]
        self.act = self.big[:].bitcast(BF16)
        self.banks = []
        self.bank_res = []
        for i in range(8):
            t = self.st.enter_context(nc.psum_tensor("bank%d" % i, [128, 512], F32))
            self.banks.append(t)
            self.bank_res.append(Res("bank%d" % i))
        self.bank_i = 0
        self.tmp_i = 0
        self.tmpb_i = 0

        self.dma(self.pvec[:], self.pvecd, writes=[self.pvec_r])
        self.dma(self.identf[:], self.identd, writes=[self.identf_r])
        self.op("dve", lambda e: e.tensor_copy(out=self.ident[:], in_=self.identf[:]), reads=[self.identf_r], writes=[self.ident_r])
        self.op("dve", lambda e: e.memset(self.onesf[:], 1.0), writes=[self.onesf_r])
        self.op("dve", lambda e: e.memset(self.vt[:], 1.0), writes=[self.vt_r])
        self.dma(self.cmask[:], self.cmaskd, writes=[self.cmask_r], q="pool")
        for l in range(2):
            for name, cnt, w in BLK:
                r0, _ = BLK_OFF[name]
                rows = w // 4
                for i in range(cnt):
                    r = Res("blk")
                    self.blk_res[(l, name, i)] = r
                    a = r0 + i * rows
                    self.dma(self.blob[l][a:a + rows, :], self.wsl[l][a:a + rows, :], writes=[r], q="pool")
        if self.stop == "T0":
            self.load_x(self.xin, 0, None)
            self.phaseA(0, 0)
            self.exchange(0, 0)
            self.mem_kv(0)
            self.load_x(self.xTd, 0, self.xT_res[0])
            self.phaseB(0, 0)
            return self.finish()
        for j in range(NT):
            self.load_x(self.xin, j, None)
            self.phaseA(0, j)
            self.exchange(0, j)
        if self.stop == "A0":
            return self.finish()
        for l in range(2):
            self.mem_kv(l)
            for j in range(NT):
                self.load_x(self.xTd, j, self.xT_res[j])
                self.phaseB(l, j)
                if l == 0:
                    self.phaseA(1, j, push=False)
                    self.exchange(1, j)
                else:
                    self.final_out(j)
            if l == 0 and self.stop == "B0":
                return self.finish()
        return self.finish()

    def finish(self):
        self.P.emit()
        self.st.close()
        return self.nc

    def pcol(self, c):
        return self.pvec[:, c:c + 1]

    def load_x(self, src, j, res):
        self.dma(self.xt[:], src[j].rearrange("c p n -> p c n"), reads=[res] if res else [], writes=[self.xt_r])

    def tmpa(self):
        i = self.tmp_i % 3
        self.tmp_i += 1
        return self.tmpA[i][0], self.tmpA[i][1]

    def tmpb(self):
        i = self.tmpb_i % 8
        self.tmpb_i += 1
        return self.tmpB[i][0], self.tmpB[i][1]

    def ptbuf(self):
        i = self.pT_i % 8
        self.pT_i += 1
        return self.pTs[i]

    def rmsnorm(self, x, x_r, gc0, out, out_r, nch, N, eps_col=PV_EPS6):
        pb, pbr = self.bank()
        for c in range(nch):
            sq, sqr = self.tmpa()
            self.op("act", lambda e, c=c, sq=sq: e.activation(out=sq[:, 0:N], in_=x[:, c, :], func=AF.Square), reads=[x_r], writes=[sqr])
            self.op("pe", lambda e, c=c, sq=sq: e.matmul(pb[:, 0:N], lhsT=self.onesf[:], rhs=sq[:, 0:N], start=(c == 0), stop=(c == nch - 1)),
                    reads=[sqr, self.onesf_r], writes=[pbr])
        sd, sdr = self.tmpa()
        self.op("act", lambda e: e.activation(out=sd[:, 0:N], in_=pb[:, 0:N], func=AF.Sqrt, bias=self.pcol(eps_col), scale=1.0 / (nch * 128)),
                reads=[pbr, self.pvec_r], writes=[sdr])
        self.op("dve", lambda e: e.reciprocal(out=self.rstd[:, 0:N], in_=sd[:, 0:N]), reads=[sdr], writes=[self.rstd_r])
        for c in range(nch):
            self.op("dve", lambda e, c=c: e.scalar_tensor_tensor(out=out[:, c, :], in0=x[:, c, :], scalar=self.pcol(gc0 + c), in1=self.rstd[:, 0:N],
                                                                 op0=ALU.mult, op1=ALU.mult),
                    reads=[x_r, self.rstd_r, self.pvec_r], writes=[out_r])

    def ffn(self, l, which):
        self.rmsnorm(self.xt, self.xt_r, l * LP + (0 if which == 1 else 32), self.xn, self.xn_r, 8, T)
        act = self.act
        for i in range(11):
            slot, sres = self.wpop()
            sv = slot[:, 0:4096].rearrange("p (k n) -> p k n", k=8)
            for cc in range(2):
                pg, pgr = self.bank()
                pu, pur = self.bank()
                for k in range(8):
                    self.op("pe", lambda e, k=k, cc=cc, sv=sv, pg=pg: e.matmul(pg, lhsT=sv[:, k, cc * 128:(cc + 1) * 128], rhs=self.xn[:, k, :],
                                                                          start=(k == 0), stop=(k == 7)),
                            reads=[sres, self.xn_r], writes=[pgr])
                for k in range(8):
                    self.op("pe", lambda e, k=k, cc=cc, sv=sv, pu=pu: e.matmul(pu, lhsT=sv[:, k, 256 + cc * 128:256 + (cc + 1) * 128], rhs=self.xn[:, k, :],
                                                                          start=(k == 0), stop=(k == 7)),
                            reads=[sres, self.xn_r], writes=[pur])
                sg, sgr = self.tmpa()
                self.op("act", lambda e, sg=sg, pg=pg: e.activation(out=sg[:], in_=pg, func=AF.Silu), reads=[pgr], writes=[sgr])
                ch = 2 * i + cc
                self.op("dve", lambda e, sg=sg, pu=pu, ch=ch: e.tensor_tensor(out=act[:, ch * T:(ch + 1) * T], in0=sg[:], in1=pu, op=ALU.mult),
                        reads=[sgr, pur], writes=[self.big_r])
        for oc in range(8):
            slot, sres = self.wpop()
            sv = slot[:, 0:2816].rearrange("p (k n) -> p k n", k=22)
            po, por = self.bank()
            for k in range(22):
                self.op("pe", lambda e, k=k, sv=sv, po=po: e.matmul(po, lhsT=sv[:, k, :], rhs=act[:, k * T:(k + 1) * T], start=(k == 0), stop=(k == 21)),
                        reads=[sres, self.big_r], writes=[por])
            self.op("dve", lambda e, oc=oc, po=po: e.scalar_tensor_tensor(out=self.xt[:, oc, :], in0=po, scalar=0.5, in1=self.xt[:, oc, :],
                                                                         op0=ALU.mult, op1=ALU.add),
                    reads=[por, self.xt_r], writes=[self.xt_r])

    def rope_tables(self, j):
        e_ = self
        pi_, pi_r = self.tmpa()
        posi = pi_[:].bitcast(I32)
        self.dma(posi, self.posd[j * T:(j + 1) * T].partition_broadcast(128), writes=[pi_r])
        a, ar = self.tmpa()
        k, kr = self.tmpa()
        ki, kir = self.tmpa()
        kiv = ki[:].bitcast(I32)
        op = self.op
        op("dve", lambda e: e.tensor_copy(out=a[:], in_=posi), reads=[pi_r], writes=[ar])
        op("dve", lambda e: e.tensor_scalar(out=a[:], in0=a[:], scalar1=self.pcol(PV_INVF), scalar2=None, op0=ALU.mult), reads=[ar, self.pvec_r], writes=[ar])
        op("dve", lambda e: e.tensor_scalar(out=k[:], in0=a[:], scalar1=1.0 / TWO_PI, scalar2=None, op0=ALU.mult), reads=[ar], writes=[kr])
        op("dve", lambda e: e.tensor_copy(out=kiv, in_=k[:]), reads=[kr], writes=[kir])
        op("dve", lambda e: e.tensor_copy(out=k[:], in_=kiv), reads=[kir], writes=[kr])
        op("dve", lambda e: e.scalar_tensor_tensor(out=a[:], in0=k[:], scalar=-TWO_PI, in1=a[:], op0=ALU.mult, op1=ALU.add), reads=[kr, ar], writes=[ar])

        def fold(t, tr):
            op("dve", lambda e: e.tensor_scalar(out=k[:], in0=t[:], scalar1=np.pi, scalar2=-TWO_PI, op0=ALU.is_gt, op1=ALU.mult), reads=[tr], writes=[kr])
            op("dve", lambda e: e.tensor_tensor(out=t[:], in0=t[:], in1=k[:], op=ALU.add), reads=[tr, kr], writes=[tr])
            op("dve", lambda e: e.tensor_scalar(out=k[:], in0=t[:], scalar1=-np.pi, scalar2=TWO_PI, op0=ALU.is_lt, op1=ALU.mult), reads=[tr], writes=[kr])
            op("dve", lambda e: e.tensor_tensor(out=t[:], in0=t[:], in1=k[:], op=ALU.add), reads=[tr, kr], writes=[tr])
        fold(a, ar)
        op("act", lambda e: e.activation(out=self.sinT[:], in_=a[:], func=AF.Sin), reads=[ar], writes=[self.sin_r])
        op("dve", lambda e: e.tensor_scalar(out=self.sinT[:], in0=self.sinT[:], scalar1=self.pcol(PV_SGN), scalar2=None, op0=ALU.mult),
           reads=[self.sin_r, self.pvec_r], writes=[self.sin_r])
        op("dve", lambda e: e.tensor_scalar(out=a[:], in0=a[:], scalar1=0.5 * np.pi, scalar2=None, op0=ALU.add), reads=[ar], writes=[ar])
        fold(a, ar)
        op("act", lambda e: e.activation(out=self.cosT[:], in_=a[:], func=AF.Sin), reads=[ar], writes=[self.cos_r])

    def phaseA(self, l, j, push=True):
        op = self.op
        if push:
            self.push_group(l, ["gu1", "dn1", "win", "wv"])
        self.ffn(l, 1)
        self.dma(self.xTd[j].rearrange("c p n -> p c n"), self.xt[:], reads=[self.xt_r], writes=[self.xT_res[j]])
        self.rmsnorm(self.xt, self.xt_r, l * LP + 8, self.xn, self.xn_r, 8, T)
        self.rope_tables(j)
        hn = self.xn
        yT = self.yT
        loc = self.loc_res[j]
        cols = slice(j * T, (j + 1) * T)
        held = {}
        sendv = self.send[l][j]
        sres_ = self.send_res[l][j]
        kview = sendv[0:512, :].rearrange("(c p) n -> p c n", c=4)
        kiview = sendv[512:640, :]
        hview = [sendv[1160 + 16 * i:1160 + 16 * (i + 1), :].rearrange("(c q) (h t) -> (q h) c t", c=2, q=8, h=16, t=32) for i in range(2)]
        for blk in range(8):
            slot, sres = self.wpop()
            sv = slot[:, 0:4096].rearrange("p (k n) -> p k n", k=8)
            for cc in range(4):
                ch = blk * 4 + cc
                pb, pbr = self.bank()
                for k in range(8):
                    op("pe", lambda e, k=k, cc=cc, sv=sv, pb=pb: e.matmul(pb, lhsT=sv[:, k, cc * 128:(cc + 1) * 128], rhs=hn[:, k, :], start=(k == 0), stop=(k == 7)),
                       reads=[sres, self.xn_r], writes=[pbr])
                if ch in (0, 1, 6, 7):
                    held[ch] = (pb, pbr)
                elif ch in (2, 3):
                    c = ch - 2
                    sg, sgr = self.tmpa()
                    op("act", lambda e, sg=sg, pb=pb: e.activation(out=sg[:], in_=pb, func=AF.Sigmoid), reads=[pbr], writes=[sgr])
                    pv, pvr = held[c]
                    op("dve", lambda e, sg=sg, pv=pv, c=c: e.tensor_tensor(out=yT[:, c, :], in0=sg[:], in1=pv, op=ALU.mult), reads=[sgr, pvr], writes=[self.yT_r])
                    if c == 1:
                        self.dma(self.hAd[:, :, cols].rearrange("c p n -> p c n"), yT[:, 0:2, :], reads=[self.yT_r], writes=[loc])
                        self.dma(hview[0], yT[:, 0:2, T - 32:T], reads=[self.yT_r], writes=[sres_])
                elif ch in (4, 5):
                    c = ch - 4
                    op("act", lambda e, pb=pb, c=c: e.copy(out=yT[:, 2 + c, :], in_=pb), reads=[pbr], writes=[self.yT_r])
                    if c == 1:
                        self.dma(self.bgd[:, :, cols].rearrange("c p n -> p c n"), yT[:, 2:4, :], reads=[self.yT_r], writes=[loc])
                elif ch in (8, 9):
                    c = ch - 8
                    sg, sgr = self.tmpa()
                    pv, pvr = held[6 + c]
                    op("act", lambda e, sg=sg, pv=pv: e.copy(out=sg[:], in_=pv), reads=[pvr], writes=[sgr])
                    op("dve", lambda e, sg=sg, pb=pb, c=c: e.tensor_tensor(out=yT[:, 4 + c, :], in0=sg[:], in1=pb, op=ALU.mult), reads=[sgr, pbr], writes=[self.yT_r])
                    if c == 1:
                        self.dma(self.cghd[:, :, cols].rearrange("c p n -> p c n"), yT[:, 4:6, :], reads=[self.yT_r], writes=[loc])
                        self.dma(hview[1], yT[:, 4:6, T - 32:T], reads=[self.yT_r], writes=[sres_])
                else:
                    for (c0, n, name) in ((10, 4, "q"), (18, 4, "k"), (26, 2, "qi"), (30, 1, "ki")):
                        if c0 <= ch < c0 + n:
                            c = ch - c0
                            op("dve", lambda e, pb=pb, c=c: e.tensor_tensor(out=self.rtmp[:, c, :], in0=pb, in1=self.cosT[:], op=ALU.mult),
                               reads=[pbr, self.cos_r], writes=self.rtmp_w)
                        elif c0 + n <= ch < c0 + 2 * n:
                            c = ch - c0 - n
                            t2, t2r = self.tmpa()
                            op("dve", lambda e, pb=pb, t2=t2: e.tensor_tensor(out=t2[:], in0=pb, in1=self.sinT[:], op=ALU.mult),
                               reads=[pbr, self.sin_r], writes=[t2r])
                            op("pool", lambda e, t2=t2, c=c: e.tensor_tensor(out=yT[:, c, :], in0=t2[:], in1=self.rtmp[:, c, :], op=ALU.add),
                               reads=[t2r, self.rtmp_r], writes=[self.yT_r])
                            if c == n - 1:
                                if name == "q":
                                    self.dma(self.qd[:, :, cols].rearrange("c p n -> p c n"), yT[:, 0:4, :], reads=[self.yT_r], writes=[loc])
                                elif name == "k":
                                    self.dma(kview, yT[:, 0:4, :], reads=[self.yT_r], writes=[sres_])
                                elif name == "qi":
                                    self.dma(self.qid[:, :, cols].rearrange("c p n -> p c n"), yT[:, 0:2, :], reads=[self.yT_r], writes=[loc])
                                else:
                                    self.dma(kiview, yT[:, 0, :], reads=[self.yT_r], writes=[sres_])
        op("dve", lambda e: e.memset(self.vt[:].rearrange("p a (h f) -> p a h f", f=65)[:, :, :, 64:65], 1.0), writes=[self.vt_r])
        slot, sres = self.wpop()
        sv = slot[:, 0:4608].rearrange("p (k n) -> p k n", k=8)
        vview = sendv[640:1160, :].rearrange("r c -> (r c)").rearrange("(t f) -> t f", f=520)
        for tb in range(4):
            pv, pvr = self.bank()
            pw, pwr = self.bank()
            for k in range(8):
                op("pe", lambda e, k=k, tb=tb, sv=sv, pv=pv: e.matmul(pv, lhsT=hn[:, k, tb * 128:(tb + 1) * 128], rhs=sv[:, k, 0:512], start=(k == 0), stop=(k == 7)),
                   reads=[sres, self.xn_r], writes=[pvr])
            for k in range(8):
                op("pe", lambda e, k=k, tb=tb, sv=sv, pw=pw: e.matmul(pw[:, 0:4], lhsT=hn[:, k, tb * 128:(tb + 1) * 128], rhs=sv[:, k, 512:516], start=(k == 0), stop=(k == 7)),
                   reads=[sres, self.xn_r], writes=[pwr])
            op("act", lambda e, tb=tb, pv=pv: e.copy(out=self.vt[:, tb, :].rearrange("p (h f) -> p h f", f=65)[:, :, 0:64], in_=pv.rearrange("p (h f) -> p h f", f=64)),
               reads=[pvr], writes=[self.vt_r])
            op("dve", lambda e, tb=tb, pw=pw: e.tensor_copy(out=self.wsc[:, tb, :], in_=pw[:, 0:4]), reads=[pwr], writes=[self.wsc_r])
        self.dma(vview.rearrange("(n p) f -> p n f", p=128), self.vt[:], reads=[self.vt_r], writes=[sres_])
        self.dma(self.widxd[j * T:(j + 1) * T, :].rearrange("(n p) f -> p n f", p=128), self.wsc[:], reads=[self.wsc_r], writes=[loc])

    def exchange(self, l, j):
        self.op("pool", lambda e: e.collective_compute("AllGather", ALU.bypass, replica_groups=[[0, 1], [2, 3], [4, 5], [6, 7]],
                                                       ins=[self.send[l][j]], outs=[self.ag[l][j]]),
                reads=[self.send_res[l][j]], writes=[self.ag_res[l][j]], dma=True, inc=1)

    def mem_kv(self, l):
        op = self.op
        self.push_group(l, ["wkv"])
        self.dma(self.memx, self.memT.rearrange("c p n -> p c n"), writes=self.rtmp_w)
        self.rmsnorm(self.memx, self.memx_r, l * LP + 24, self.memn, self.memn_r, 8, 256)
        for blk in range(2):
            slot, sres = self.wpop()
            sv = slot[:, 0:4096].rearrange("p (k n) -> p k n", k=8)
            for cc in range(4):
                pb, pbr = self.bank()
                for k in range(8):
                    op("pe", lambda e, k=k, cc=cc, sv=sv, pb=pb: e.matmul(pb[:, 0:256], lhsT=sv[:, k, cc * 128:(cc + 1) * 128], rhs=self.memn[:, k, :], start=(k == 0), stop=(k == 7)),
                       reads=[sres, self.memn_r], writes=[pbr])
                op("act", lambda e, pb=pb, ch=blk * 4 + cc: e.copy(out=self.kmT[:, ch, :], in_=pb[:, 0:256]), reads=[pbr], writes=[self.kmT_r])
        for blk in range(2):
            slot, sres = self.wpop()
            sv = slot[:, 0:4096].rearrange("p (k n) -> p k n", k=8)
            for mb in range(2):
                pb, pbr = self.bank()
                for k in range(8):
                    op("pe", lambda e, k=k, mb=mb, sv=sv, pb=pb: e.matmul(pb, lhsT=self.memn[:, k, mb * 128:(mb + 1) * 128], rhs=sv[:, k, :], start=(k == 0), stop=(k == 7)),
                       reads=[sres, self.memn_r], writes=[pbr])
                op("act", lambda e, pb=pb, mb=mb, blk=blk: e.copy(out=self.vm[:, mb, blk * 512:(blk + 1) * 512], in_=pb), reads=[pbr], writes=[self.vm_r])

    def linear_res(self, l, name, inp, inp_r, nk):
        op = self.op
        for blk in range(2):
            slot, sres = self.wpop()
            sv = slot[:, 0:4096].rearrange("p (k n) -> p k n", k=8)
            for cc in range(4):
                oc = blk * 4 + cc
                pb, pbr = self.bank()
                for k in range(nk):
                    op("pe", lambda e, k=k, cc=cc, sv=sv, pb=pb: e.matmul(pb, lhsT=sv[:, k, cc * 128:(cc + 1) * 128], rhs=inp[:, k, :], start=(k == 0), stop=(k == nk - 1)),
                       reads=[sres, inp_r], writes=[pbr])
                op("dve", lambda e, oc=oc, pb=pb: e.tensor_tensor(out=self.xt[:, oc, :], in0=pb, in1=self.xt[:, oc, :], op=ALU.add),
                   reads=[pbr, self.xt_r], writes=[self.xt_r])

    def conv_taps(self, l, j):
        op = self.op
        base = l * LP
        loc = self.loc_res[j]
        cols = slice(j * T, (j + 1) * T)
        for br in range(2):
            src = self.hAd if br == 0 else self.cghd
            cin, cin_r = (self.cin, self.cin_r) if br == 0 else (self.cin2, self.vt_r)
            cacc = self.cacc if br == 0 else self.cacc2
            def hv(jj, rr, br=br):
                a = rr * TR + 1160 + 16 * br
                return self.ag[l][jj][a:a + 16, :].rearrange("(c q) (h t) -> (q h) c t", c=2, q=8, h=16, t=32)
            self.dma(self.halo[:, 0, :, :], hv(j, 0), reads=[self.ag_res[l][j]], writes=[self.halo_r])
            if j > 0:
                self.dma(self.halo[:, 1, :, :], hv(j - 1, 1), reads=[self.ag_res[l][j - 1]], writes=[self.halo_r])
            else:
                op("dve", lambda e: e.memset(self.halo[:, 1, :, :], 0.0), writes=[self.halo_r])
            self.dma(cin[:, :, 32:32 + T], src[:, :, cols].rearrange("c p n -> p c n"), reads=[loc], writes=[cin_r])
            op("dve", lambda e, cin=cin: e.tensor_scalar(out=cin[:, :, 0:32], in0=self.halo[:, 0, :, :], scalar1=self.pcol(PV_SEL), scalar2=None, op0=ALU.mult),
               reads=[self.halo_r, self.pvec_r], writes=[cin_r])
            op("dve", lambda e, cin=cin: e.scalar_tensor_tensor(out=cin[:, :, 0:32], in0=self.halo[:, 1, :, :], scalar=self.pcol(PV_SEL + 1), in1=cin[:, :, 0:32],
                                                                op0=ALU.mult, op1=ALU.add),
               reads=[self.halo_r, self.pvec_r, cin_r], writes=[cin_r])
            W = 31 if br == 0 else 3
            wc0 = base + (40 if br == 0 else 108)
            for c in range(2):
                for tap in range(W):
                    sh = 32 - (W - 1) + tap
                    colw = self.pcol(wc0 + c * W + tap)
                    if tap == 0:
                        op("pool", lambda e, c=c, sh=sh, colw=colw, cin=cin, cacc=cacc: e.tensor_scalar(out=cacc[:, c, :], in0=cin[:, c, sh:sh + T], scalar1=colw, scalar2=None, op0=ALU.mult),
                           reads=[cin_r, self.pvec_r], writes=[self.cacc_r])
                    else:
                        op("pool", lambda e, c=c, sh=sh, colw=colw, cin=cin: e.tensor_scalar(out=self.ptmp[:], in0=cin[:, c, sh:sh + T], scalar1=colw, scalar2=None, op0=ALU.mult),
                           reads=[cin_r, self.pvec_r], writes=[self.ptmp_r])
                        op("pool", lambda e, c=c, cacc=cacc: e.tensor_tensor(out=cacc[:, c, :], in0=cacc[:, c, :], in1=self.ptmp[:], op=ALU.add),
                           reads=[self.ptmp_r, self.cacc_r], writes=[self.cacc_r])

    def conv_finish(self, l, j):
        op = self.op
        base = l * LP
        loc = self.loc_res[j]
        cols = slice(j * T, (j + 1) * T)
        for c in range(2):
            op("dve", lambda e, c=c: e.tensor_scalar(out=self.cacc[:, c, :], in0=self.cacc[:, c, :], scalar1=self.pcol(base + 102 + c), scalar2=None, op0=ALU.add),
               reads=[self.cacc_r, self.pvec_r], writes=[self.cacc_r])
        pm, pmr = self.bank()
        for c in range(2):
            op("pe", lambda e, c=c, pm=pm: e.matmul(pm, lhsT=self.onesf[:], rhs=self.cacc[:, c, :], start=(c == 0), stop=(c == 1)),
               reads=[self.cacc_r, self.onesf_r], writes=[pmr])
        mean, meanr = self.tmpa()
        op("act", lambda e, pm=pm, mean=mean: e.activation(out=mean[:], in_=pm, func=AF.Copy, scale=1.0 / 256), reads=[pmr], writes=[meanr])
        for c in range(2):
            op("dve", lambda e, c=c, mean=mean: e.tensor_tensor(out=self.cacc[:, c, :], in0=self.cacc[:, c, :], in1=mean[:], op=ALU.subtract),
               reads=[self.cacc_r, meanr], writes=[self.cacc_r])
        pq, pqr = self.bank()
        for c in range(2):
            sq, sqr = self.tmpa()
            op("act", lambda e, c=c, sq=sq: e.activation(out=sq[:], in_=self.cacc[:, c, :], func=AF.Square), reads=[self.cacc_r], writes=[sqr])
            op("pe", lambda e, c=c, sq=sq, pq=pq: e.matmul(pq, lhsT=self.onesf[:], rhs=sq[:], start=(c == 0), stop=(c == 1)), reads=[sqr, self.onesf_r], writes=[pqr])
        sd, sdr = self.tmpa()
        op("act", lambda e, sd=sd, pq=pq: e.activation(out=sd[:], in_=pq, func=AF.Sqrt, bias=self.pcol(PV_EPS5), scale=1.0 / 256), reads=[pqr, self.pvec_r], writes=[sdr])
        op("dve", lambda e, sd=sd: e.reciprocal(out=self.rstd[:], in_=sd[:]), reads=[sdr], writes=[self.rstd_r])
        for c in range(2):
            op("dve", lambda e, c=c: e.scalar_tensor_tensor(out=self.cacc[:, c, :], in0=self.cacc[:, c, :], scalar=self.pcol(base + 104 + c), in1=self.rstd[:],
                                                            op0=ALU.mult, op1=ALU.mult), reads=[self.cacc_r, self.rstd_r, self.pvec_r], writes=[self.cacc_r])
            op("act", lambda e, c=c: e.activation(out=self.yT[:, c, :], in_=self.cacc[:, c, :], func=AF.Silu, bias=self.pcol(base + 106 + c), scale=1.0),
               reads=[self.cacc_r, self.pvec_r], writes=[self.yT_r])
        self.dma(self.cin[:, :, 32:32 + T], self.bgd[:, :, cols].rearrange("c p n -> p c n"), reads=[loc], writes=[self.cin_r])
        for c in range(2):
            op("dve", lambda e, c=c: e.tensor_tensor(out=self.yT[:, 2 + c, :], in0=self.cacc2[:, c, :], in1=self.cin[:, c, 32:32 + T], op=ALU.mult),
               reads=[self.cacc_r, self.cin_r], writes=[self.yT_r])

    def dsa(self, l, j):
        op = self.op
        loc = self.loc_res[j]
        cols = slice(j * T, (j + 1) * T)
        S = 1024 * (j + 1)
        nch = S // 512
        sc = self.big
        self.dma(self.qT[:], self.qd[:, :, cols].rearrange("c p n -> p c n"), reads=[loc], writes=[self.qT_r])
        self.dma(self.qiT[:], self.qid[:, :, cols].rearrange("c p n -> p c n"), reads=[loc], writes=[self.qiT_r])
        self.dma(self.wsc[:], self.widxd[j * T:(j + 1) * T, :].rearrange("(n p) f -> p n f", p=128), reads=[loc], writes=[self.wsc_r])
        op("dve", lambda e: e.tensor_scalar(out=self.wsc[:], in0=self.wsc[:], scalar1=IDX_SCALE, scalar2=None, op0=ALU.mult), reads=[self.wsc_r], writes=[self.wsc_r])
        vb_v = self.kvb[:, 0:8320].rearrange("p (k f) -> p k f", f=130)
        nkt = 2 * (j + 1)
        def agt(kt):
            return self.ag[l][kt // 2], (kt % 2) * TR, self.ag_res[l][kt // 2]
        bis = self.bis

        def _blk(b):
            qs = slice(b * 128, (b + 1) * 128)
            for kt in range(nkt):
                a_, o_, r_ = agt(kt)
                self.dma(self.kvb[:, kt * 520:kt * 520 + 512], a_[o_ + 512:o_ + 640, :], reads=[r_], writes=[self.kvb_rt[kt]])
            for ch in range(nch):
                cs = slice(ch * 512, (ch + 1) * 512)
                for h in range(4):
                    ps_ = slice((h % 2) * 64, (h % 2) * 64 + 64)
                    pb, pbr = self.bank()
                    op("pe", lambda e, pb=pb, ps_=ps_, h=h, ch=ch: e.matmul(pb, lhsT=self.qiT[ps_, h // 2, qs], rhs=self.kvb[ps_, ch * 520:ch * 520 + 512], start=True, stop=True),
                       reads=[self.qiT_r, self.kvb_rt[ch]], writes=[pbr])
                    rl, rlr = self.tmpa()
                    op("act", lambda e, pb=pb, rl=rl: e.activation(out=rl[:], in_=pb, func=AF.Relu), reads=[pbr], writes=[rlr])
                    if h == 0:
                        op("dve", lambda e, rl=rl, cs=cs: e.tensor_scalar(out=sc[:, cs], in0=rl[:], scalar1=self.wsc[:, b, 0:1], scalar2=None, op0=ALU.mult),
                           reads=[rlr, self.wsc_r], writes=[self.big_r])
                    else:
                        op("dve", lambda e, rl=rl, cs=cs, h=h: e.scalar_tensor_tensor(out=sc[:, cs], in0=rl[:], scalar=self.wsc[:, b, h:h + 1], in1=sc[:, cs],
                                                                                     op0=ALU.mult, op1=ALU.add),
                           reads=[rlr, self.wsc_r, self.big_r], writes=[self.big_r])
            last = slice(S - 1024, S)
            for hh in range(2):
                t1, t1r = self.tmpa()
                ls = slice(S - 1024 + hh * 512, S - 1024 + (hh + 1) * 512)
                op("dve", lambda e, t1=t1, ls=ls, hh=hh: e.tensor_tensor(out=t1[:], in0=sc[:, ls], in1=self.cmask[:, b, hh * 512:(hh + 1) * 512], op=ALU.subtract),
                   reads=[self.big_r, self.cmask_r], writes=[t1r])
                op("dve", lambda e, t1=t1, hh=hh: e.tensor_reduce(out=bis[:, 40 + hh:41 + hh], in_=t1[:], axis=AX.X, op=ALU.min), reads=[t1r], writes=[self.bis_r])
            op("dve", lambda e: e.tensor_tensor(out=sc[:, last], in0=sc[:, last], in1=self.cmask[:, b, :], op=ALU.add), reads=[self.big_r, self.cmask_r], writes=[self.big_r])
            if S > 1024:
                op("dve", lambda e: e.tensor_reduce(out=bis[:, 42:43], in_=sc[:, 0:S - 1024], axis=AX.X, op=ALU.min), reads=[self.big_r], writes=[self.bis_r])
                nm = 3
            else:
                nm = 2
            op("dve", lambda e: e.tensor_reduce(out=bis[:, 0:1], in_=bis[:, 40:40 + nm], axis=AX.X, op=ALU.min), reads=[self.bis_r], writes=[self.bis_r])
            op("dve", lambda e: e.tensor_reduce(out=bis[:, 1:2], in_=sc[:, 0:S], axis=AX.X, op=ALU.max), reads=[self.big_r], writes=[self.bis_r])
            op("dve", lambda e: e.tensor_tensor(out=bis[:, 2:3], in0=bis[:, 1:2], in1=bis[:, 0:1], op=ALU.subtract), reads=[self.bis_r], writes=[self.bis_r])
            op("dve", lambda e: e.memset(bis[:, 20:20 + NITER], 0.0), writes=[self.bis_r])
            for it in range(NITER):
                w = 0.5 ** (it + 1)
                op("dve", lambda e, w=w: e.scalar_tensor_tensor(out=bis[:, 3:4], in0=bis[:, 2:3], scalar=w, in1=bis[:, 0:1], op0=ALU.mult, op1=ALU.add),
                   reads=[self.bis_r], writes=[self.bis_r])
                op("dve", lambda e, it=it: e.tensor_scalar(out=self.sel[:, 0:S], in0=sc[:, 0:S], scalar1=bis[:, 3:4], scalar2=0.0, op0=ALU.is_ge, op1=ALU.add,
                                                          accum_out=bis[:, 20 + it:21 + it]),
                   reads=[self.big_r, self.bis_r], writes=[self.sel_r, self.bis_r])
                op("dve", lambda e, it=it: e.tensor_scalar(out=bis[:, 4:5], in0=bis[:, 20 + it:21 + it], scalar1=255.5, scalar2=None, op0=ALU.is_ge),
                   reads=[self.bis_r], writes=[self.bis_r])
                op("dve", lambda e, w=w: e.scalar_tensor_tensor(out=bis[:, 5:6], in0=bis[:, 2:3], scalar=w, in1=bis[:, 4:5], op0=ALU.mult, op1=ALU.mult),
                   reads=[self.bis_r], writes=[self.bis_r])
                op("dve", lambda e: e.tensor_tensor(out=bis[:, 0:1], in0=bis[:, 0:1], in1=bis[:, 5:6], op=ALU.add), reads=[self.bis_r], writes=[self.bis_r])
            op("dve", lambda e: e.tensor_scalar(out=self.sel[:, 0:S], in0=sc[:, 0:S], scalar1=bis[:, 0:1], scalar2=None, op0=ALU.is_ge),
               reads=[self.big_r, self.bis_r], writes=[self.sel_r])
            if self.debug and j == 0:
                self.dma(self.dbg_sc[b], sc[:, 0:1024], reads=[self.big_r])
                self.dma(self.dbg_sel[b], self.sel[:, 0:1024], reads=[self.sel_r])
                self.dma(self.dbg_bis[b], bis[:], reads=[self.bis_r])
            ycp = [self.banks[6][:], self.banks[7][:]]
            ycr = [self.bank_res[6], self.bank_res[7]]
            LB, LC = 2, 4
            for c in range(4):
                for kt in range(nkt):
                    a_, o_, r_ = agt(kt)
                    self.dma(self.kbuf[:, kt * 512:(kt + 1) * 512], a_[o_ + c * 128:o_ + (c + 1) * 128, :], reads=[r_], writes=[self.kbuf_rt[kt]])
                    vv_ = a_[o_ + 640:o_ + 1160, :].rearrange("r c -> (r c)").rearrange("(t f) -> t f", f=520)
                    self.dma(vb_v[:, kt * 4:(kt + 1) * 4, :], vv_[:, c * 130:(c + 1) * 130].rearrange("(n p) f -> p n f", p=128), reads=[r_], writes=[self.kvb_rt[kt]])
                for half in range(2):
                    h = 2 * c + half
                    ps_ = slice(half * 64, half * 64 + 64)
                    for ch in range(nch):
                        cs = slice(ch * 512, (ch + 1) * 512)
                        pb, pbr = self.bank()
                        op("pe", lambda e, pb=pb, ps_=ps_, cs=cs, c=c: e.matmul(pb, lhsT=self.qT[ps_, c, qs], rhs=self.kbuf[ps_, cs], start=True, stop=True),
                           reads=[self.qT_r, self.kbuf_rt[ch]], writes=[pbr])
                        op("dve", lambda e, pb=pb, ch=ch: e.tensor_reduce(out=self.small[:, ch:ch + 1], in_=pb, axis=AX.X, op=ALU.max), reads=[pbr], writes=[self.small_r])
                    op("dve", lambda e: e.tensor_reduce(out=self.small[:, 32:33], in_=self.small[:, 0:nch], axis=AX.X, op=ALU.max), reads=[self.small_r], writes=[self.small_r])
                    nb_ = 34 + h
                    op("dve", lambda e, nb_=nb_: e.tensor_scalar(out=self.small[:, nb_:nb_ + 1], in0=self.small[:, 32:33], scalar1=-0.125, scalar2=None, op0=ALU.mult),
                       reads=[self.small_r], writes=[self.negm_r[h]])
                    yb = ycp[h // 4]
                    ybr = ycr[h // 4]
                    oc0 = (h % 4) * 65
                    pms = {}
                    pts = {}

                    def stA(ch, ps_=ps_, c=c, nb_=nb_, h=h):
                        cs = slice(ch * 512, (ch + 1) * 512)
                        pb, pbr = self.bank()
                        op("pe", lambda e: e.matmul(pb, lhsT=self.qT[ps_, c, qs], rhs=self.kbuf[ps_, cs], start=True, stop=True),
                           reads=[self.qT_r, self.kbuf_rt[ch]], writes=[pbr])
                        ex, exr = self.tmpb()
                        op("act", lambda e: e.activation(out=ex[:], in_=pb, func=AF.Exp, bias=self.small[:, nb_:nb_ + 1], scale=0.125),
                           reads=[pbr, self.negm_r[h]], writes=[exr])
                        pm, pmr = self.tmpb()
                        op("dve", lambda e: e.tensor_tensor(out=pm[:], in0=ex[:], in1=self.sel[:, cs], op=ALU.mult),
                           reads=[exr, self.sel_r], writes=[pmr])
                        pms[ch] = (pm, pmr)

                    def stB(ch):
                        pm, pmr = pms.pop(ch)
                        tb_, tbr = self.bank()
                        tbv = tb_.bitcast(BF16)
                        for t in range(4):
                            op("pe", lambda e, t=t: e.transpose(tbv[:, t * 128:(t + 1) * 128], pm[:, t * 128:(t + 1) * 128], self.ident[:]),
                               reads=[pmr, self.ident_r], writes=[tbr])
                        pt, ptr = self.ptbuf()
                        op("act", lambda e: e.copy(out=pt.rearrange("p a b -> p (a b)"), in_=tbv[:, 0:512]), reads=[tbr], writes=[ptr, self.rtmp_r])
                        pts[ch] = (pt, ptr)

                    def stC(ch, yb=yb, ybr=ybr, oc0=oc0, half=half):
                        pt, ptr = pts.pop(ch)
                        for t in range(4):
                            kc = ch * 4 + t
                            first = (ch == 0 and t == 0)
                            lastm = (ch == nch - 1 and t == 3)
                            op("pe", lambda e, t=t, kc=kc, first=first, lastm=lastm:
                               e.matmul(yb[:, oc0:oc0 + 65], lhsT=pt[:, t, :], rhs=self.kvb[:, kc * 130 + half * 65: kc * 130 + half * 65 + 65], start=first, stop=lastm),
                               reads=[ptr, self.kvb_rt[ch]], writes=[ybr])

                    for st_ in range(nch + LC):
                        if st_ < nch:
                            stA(st_)
                        if 0 <= st_ - LB < nch:
                            stB(st_ - LB)
                        if 0 <= st_ - LC < nch:
                            stC(st_ - LC)
            for h in range(8):
                yb = ycp[h // 4]
                ybr = ycr[h // 4]
                oc0 = (h % 4) * 65
                op("dve", lambda e, yb=yb, oc0=oc0, h=h: e.reciprocal(out=self.small[:, 44 + h:45 + h], in_=yb[:, oc0 + 64:oc0 + 65]), reads=[ybr], writes=[self.small_r])
                op("dve", lambda e, yb=yb, oc0=oc0, h=h: e.tensor_scalar(out=self.ycs[:, h * 64:(h + 1) * 64], in0=yb[:, oc0:oc0 + 64], scalar1=self.small[:, 44 + h:45 + h],
                                                                        scalar2=None, op0=ALU.mult),
                   reads=[ybr, self.small_r], writes=[self.ycs_r])
            if self.debug and j == 0:
                self.dma(self.dbg_ycs[b], self.ycs[:], reads=[self.ycs_r])
            tb_, tbr = self.bank()
            tbv = tb_.bitcast(BF16)
            for c in range(4):
                op("pe", lambda e, c=c, tbv=tbv: e.transpose(tbv[:, c * 128:(c + 1) * 128], self.ycs[:, c * 128:(c + 1) * 128], self.ident[:]),
                   reads=[self.ycs_r, self.ident_r], writes=[tbr])
            op("act", lambda e, tbv=tbv: e.copy(out=self.yT[:, 4:8, qs], in_=tbv[:, 0:512].rearrange("p (c q) -> p c q", c=4)), reads=[tbr], writes=[self.yT_r])

        for b in range(4):
            _blk(b)

    def xattn(self, l):
        op = self.op
        self.rmsnorm(self.xt, self.xt_r, l * LP + 16, self.xn, self.xn_r, 8, T)
        for blk in range(2):
            slot, sres = self.wpop()
            sv = slot[:, 0:4096].rearrange("p (k n) -> p k n", k=8)
            for cc in range(4):
                oc = blk * 4 + cc
                pb, pbr = self.bank()
                for k in range(8):
                    op("pe", lambda e, k=k, cc=cc, sv=sv, pb=pb: e.matmul(pb, lhsT=sv[:, k, cc * 128:(cc + 1) * 128], rhs=self.xn[:, k, :], start=(k == 0), stop=(k == 7)),
                       reads=[sres, self.xn_r], writes=[pbr])
                op("act", lambda e, pb=pb, oc=oc: e.copy(out=self.yT[:, oc, :], in_=pb), reads=[pbr], writes=[self.yT_r])
        o_tok, o_tok_r = self.ycs, self.ycs_r

        def _blk(b):
            qs = slice(b * 128, (b + 1) * 128)
            for half2 in range(2):
                for hh in range(2):
                    h = half2 * 2 + hh
                    pb, pbr = self.bank()
                    for k in range(2):
                        op("pe", lambda e, k=k, h=h, pb=pb: e.matmul(pb[:, 0:256], lhsT=self.yT[:, 2 * h + k, qs], rhs=self.kmT[:, 2 * h + k, :], start=(k == 0), stop=(k == 1)),
                           reads=[self.yT_r, self.kmT_r], writes=[pbr])
                    op("dve", lambda e, pb=pb: e.tensor_reduce(out=self.small[:, 50:51], in_=pb[:, 0:256], axis=AX.X, op=ALU.max), reads=[pbr], writes=[self.small_r])
                    op("dve", lambda e: e.tensor_scalar(out=self.small[:, 51:52], in0=self.small[:, 50:51], scalar1=-1.0 / 16, scalar2=None, op0=ALU.mult),
                       reads=[self.small_r], writes=[self.small_r])
                    op("dve", lambda e: e.memset(self.small[:, 52:53], 0.0), writes=[self.small_r])
                    ex, exr = self.tmpb()
                    op("act", lambda e, pb=pb, ex=ex: e.activation(out=ex[:, 0:256], in_=pb[:, 0:256], func=AF.Exp, bias=self.small[:, 51:52], scale=1.0 / 16,
                                                                  accum_out=self.small[:, 52:53]),
                       reads=[pbr, self.small_r], writes=[exr, self.small_r])
                    tb_, tbr = self.bank()
                    tbv = tb_.bitcast(BF16)
                    for t in range(2):
                        op("pe", lambda e, t=t, ex=ex, tbv=tbv: e.transpose(tbv[:, t * 128:(t + 1) * 128], ex[:, t * 128:(t + 1) * 128], self.ident[:]),
                           reads=[exr, self.ident_r], writes=[tbr])
                    pt, ptr = self.ptbuf()
                    op("act", lambda e, tbv=tbv, pt=pt: e.copy(out=pt[:, 0:2, :].rearrange("p a b -> p (a b)"), in_=tbv[:, 0:256]), reads=[tbr], writes=[ptr, self.rtmp_r])
                    po, por = self.bank()
                    for t in range(2):
                        op("pe", lambda e, t=t, h=h, po=po, pt=pt: e.matmul(po[:, 0:256], lhsT=pt[:, t, :], rhs=self.vm[:, t, h * 256:(h + 1) * 256], start=(t == 0), stop=(t == 1)),
                           reads=[ptr, self.vm_r], writes=[por])
                    op("dve", lambda e: e.reciprocal(out=self.small[:, 53:54], in_=self.small[:, 52:53]), reads=[self.small_r], writes=[self.small_r])
                    op("dve", lambda e, po=po, hh=hh: e.tensor_scalar(out=o_tok[:, hh * 256:(hh + 1) * 256], in0=po[:, 0:256], scalar1=self.small[:, 53:54], scalar2=None, op0=ALU.mult),
                       reads=[por, self.small_r], writes=[o_tok_r])
                tb_, tbr = self.bank()
                tbv = tb_.bitcast(BF16)
                for c in range(4):
                    op("pe", lambda e, c=c, tbv=tbv: e.transpose(tbv[:, c * 128:(c + 1) * 128], o_tok[:, c * 128:(c + 1) * 128], self.ident[:]),
                       reads=[o_tok_r, self.ident_r], writes=[tbr])
                op("act", lambda e, tbv=tbv, half2=half2: e.copy(out=self.xn[:, half2 * 4:half2 * 4 + 4, qs], in_=tbv[:, 0:512].rearrange("p (c q) -> p c q", c=4)),
                   reads=[tbr], writes=[self.xn_r])
        for b in range(4):
            _blk(b)
        self.linear_res(l, "wo", self.xn, self.xn_r, 8)

    def phaseB(self, l, j):
        self.push_group(l, ["wout", "wq", "wo", "gu2", "dn2"])
        if l == 0 and self.stop != "T0":
            self.push_group(1, ["gu1", "dn1", "win", "wv"])
        self.conv_taps(l, j)
        self.dsa(l, j)
        self.conv_finish(l, j)
        if self.debug and l == 0:
            self.dma(self.dbg_y[j].rearrange("c p n -> p c n"), self.yT[:], reads=[self.yT_r])
        self.linear_res(l, "wout", self.yT, self.yT_r, 8)
        if self.debug and l == 0:
            self.dma(self.dbg_x2[j].rearrange("c p n -> p c n"), self.xt[:], reads=[self.xt_r])
        self.xattn(l)
        if self.debug and l == 0:
            self.dma(self.dbg_x3[j].rearrange("c p n -> p c n"), self.xt[:], reads=[self.xt_r])
        self.ffn(l, 2)
        if self.debug and l == 0:
            self.dma(self.dbg_x4[j].rearrange("c p n -> p c n"), self.xt[:], reads=[self.xt_r])

    def final_out(self, j):
        op = self.op
        pb, pbr = self.bank()
        x = self.xt
        for c in range(8):
            sq, sqr = self.tmpa()
            op("act", lambda e, c=c, sq=sq: e.activation(out=sq[:], in_=x[:, c, :], func=AF.Square), reads=[self.xt_r], writes=[sqr])
            op("pe", lambda e, c=c, sq=sq: e.matmul(pb, lhsT=self.onesf[:], rhs=sq[:], start=(c == 0), stop=(c == 7)), reads=[sqr, self.onesf_r], writes=[pbr])
        sd, sdr = self.tmpa()
        op("act", lambda e: e.activation(out=sd[:], in_=pb, func=AF.Sqrt, bias=self.pcol(PV_EPS6), scale=1.0 / 1024), reads=[pbr, self.pvec_r], writes=[sdr])
        op("dve", lambda e: e.reciprocal(out=self.rstd[:], in_=sd[:]), reads=[sdr], writes=[self.rstd_r])
        for c in range(8):
            op("dve", lambda e, c=c: e.scalar_tensor_tensor(out=x[:, c, :], in0=x[:, c, :], scalar=self.pcol(PV_FIN + c), in1=self.rstd[:], op0=ALU.mult, op1=ALU.mult),
               reads=[self.xt_r, self.rstd_r, self.pvec_r], writes=[self.xt_r])
        n = self.dma(self.outd[j].rearrange("c p n -> p c n"), self.xt[:], reads=[self.xt_r])
        self.P.final.append(n)


def _swap(w):
    K, N = w.shape
    w = w.reshape(K, N // 64, 2, 32)
    return np.ascontiguousarray(w[:, :, ::-1, :]).reshape(K, N)


def _blk(w, kc, ncols, width):
    a = w.reshape(kc, 128, ncols).transpose(1, 0, 2).reshape(128, kc * ncols)
    if a.shape[1] < width:
        a = np.concatenate([a, np.zeros((128, width - a.shape[1]), np.float32)], 1)
    return a


def _layer_blob(p, l):
    blocks = []

    def gu(g, u):
        for i in range(11):
            w = np.concatenate([g[:, 256 * i:256 * (i + 1)], u[:, 256 * i:256 * (i + 1)]], 1)
            blocks.append(_blk(w, 8, 512, 4096))

    def dn(dw):
        for oc in range(8):
            blocks.append(_blk(dw[:, oc * 128:(oc + 1) * 128], 22, 128, 3072))

    def sq(w, nb):
        for i in range(nb):
            blocks.append(_blk(w[:, 512 * i:512 * (i + 1)], 8, 512, 4096))

    gu(p["ffn1_w_gate"][l], p["ffn1_w_up"][l])
    dn(p["ffn1_w_down"][l])
    wi = p["w_in"][l]
    a_val, a_gate, b_gate, c_gate, b_h = (wi[:, 256 * i:256 * (i + 1)] for i in range(5))
    q = wi[:, 1280:1792]
    k = wi[:, 1792:2304]
    v = wi[:, 2304:2816]
    qi = wi[:, 2816:3072]
    ki = wi[:, 3072:3136]
    wx = wi[:, 3136:3140]
    ext = np.concatenate([a_val, a_gate, b_gate, c_gate, b_h, q, _swap(q), k, _swap(k), qi, _swap(qi), ki, ki, _swap(ki), _swap(ki)], 1)
    assert ext.shape[1] == 4096
    sq(ext, 8)
    blocks.append(_blk(np.concatenate([v, wx, np.zeros((1024, 60), np.float32)], 1), 8, 576, 4608))
    sq(p["w_out"][l], 2)
    sq(p["xa_wq"][l], 2)
    sq(p["xa_wkv"][l], 4)
    sq(p["xa_wo"][l], 2)
    gu(p["ffn2_w_gate"][l], p["ffn2_w_up"][l])
    dn(p["ffn2_w_down"][l])
    flat = np.concatenate([b.reshape(-1) for b in blocks])
    assert flat.size == BLOB_ROWS * 512, (flat.size, BLOB_ROWS * 512)
    return flat.reshape(BLOB_ROWS, 512)


def _pvec(p, r):
    pv = np.zeros((128, NPV), np.float32)

    def put(c0, vec):
        n = vec.size // 128
        pv[:, c0:c0 + n] = vec.reshape(n, 128).T

    for l in range(2):
        b = l * LP
        put(b + 0, p["ffn1_norm"][l])
        put(b + 8, p["mix_norm"][l])
        put(b + 16, p["xa_norm"][l])
        put(b + 24, p["mem_norm"][l])
        put(b + 32, p["ffn2_norm"][l])
        dw = p["conf_dw"][l]
        for c in range(2):
            pv[:, b + 40 + c * 31: b + 40 + (c + 1) * 31] = dw[:, c * 128:(c + 1) * 128].T
        put(b + 102, p["conf_dw_b"][l])
        put(b + 104, p["conf_ln_g"][l])
        put(b + 106, p["conf_ln_b"][l])
        sw = p["sc_dw"][l]
        for c in range(2):
            pv[:, b + 108 + c * 3: b + 108 + (c + 1) * 3] = sw[:, c * 128:(c + 1) * 128].T
    put(PV_FIN, p["final_norm"])
    pidx = np.arange(128)
    inv_freq = (10000.0 ** (-np.arange(0, 64, 2, dtype=np.float32) / 64)).astype(np.float32)
    pv[:, PV_INVF] = inv_freq[pidx % 32]
    pv[:, PV_SGN] = np.where((pidx % 64) < 32, -1.0, 1.0)
    pv[:, PV_SEL] = 1.0 if r == 1 else 0.0
    pv[:, PV_SEL + 1] = 1.0 if r == 0 else 0.0
    pv[:, PV_EPS6] = 1e-6
    pv[:, PV_EPS5] = 1e-5
    return pv


def _cmask(r):
    i = np.arange(128)[:, None, None]
    b = np.arange(4)[None, :, None]
    c = np.arange(1024)[None, None, :]
    vis = (c - 512 * r) <= (128 * b + i)
    return np.where(vis, 0.0, NEG).astype(np.float32)


_CACHE = {}


def make_inputs(p):
    p = {k: np.asarray(v) for k, v in p.items()}
    blobs = [_layer_blob(p, l) for l in range(2)]
    ident = np.eye(128, dtype=np.float32)
    in_maps = []
    for c in range(8):
        b, r = c // 2, c % 2
        xb = p["x"][b].reshape(16, 512, 8, 128)[r::2]
        xin = np.ascontiguousarray(xb.transpose(0, 2, 3, 1))
        pos = np.ascontiguousarray(p["positions"][b].reshape(16, 512)[r::2].reshape(-1)).astype(np.int32)
        memT = np.ascontiguousarray(p["mem"][b].reshape(256, 8, 128).transpose(1, 2, 0))
        m = {"xin": xin, "pos": pos, "memT": memT, "cmask": _cmask(r), "pvec": _pvec(p, r), "ident": ident,
             "wsl0": blobs[0], "wsl1": blobs[1]}
        in_maps.append(m)
    return in_maps


def assemble(res):
    out = np.zeros((4, 16, 512, 8, 128), np.float32)
    for c in range(8):
        b, r = c // 2, c % 2
        o = res.results[c]["out"]
        out[b, r::2] = o.transpose(0, 3, 1, 2)
    return out.reshape(4, 8192, 1024)


def kernel(**inputs):
    in_maps = make_inputs(inputs)
    if "nc" not in _CACHE:
        _CACHE["nc"] = KB().build()
    res = run_bass_kernel_spmd(_CACHE["nc"], in_maps, core_ids=list(range(8)))
    return assemble(res)
```

```python
import contextlib
import numpy as np
import concourse.bass as bass
import concourse.mybir as mybir
from concourse.bass_utils import run_bass_kernel_spmd

ALU = mybir.AluOpType
AF = mybir.ActivationFunctionType
AX = mybir.AxisListType
F32 = mybir.dt.float32
BF16 = mybir.dt.bfloat16
I32 = mybir.dt.int32

D = 1024
DFF = 2816
T = 512
NT = 8
TOK = T * NT
NITER = 13
IDX_SCALE = 0.5 * 0.125
NEG = -1.0e30
NDSEM = 24
LP = 114
PV_FIN = 228
PV_INVF = 236
PV_SGN = 237
PV_SEL = 238
PV_EPS6 = 240
PV_EPS5 = 241
NPV = 242
SLOTW = 4608
NSLOT = 4
LA = 3
TWO_PI = 6.283185307179586
TR = 1192

BLK = [("gu1", 11, 4096), ("dn1", 8, 3072), ("win", 8, 4096), ("wv", 1, 4608), ("wout", 2, 4096),
       ("wq", 2, 4096), ("wkv", 4, 4096), ("wo", 2, 4096), ("gu2", 11, 4096), ("dn2", 8, 3072)]
BLK_OFF = {}
_r = 0
for _n, _c, _w in BLK:
    BLK_OFF[_n] = (_r, _w)
    _r += _c * (_w // 4)
BLOB_ROWS = _r


class Res:
    __slots__ = ("name", "lw", "rd", "rd_dma")

    def __init__(self, name=""):
        self.name = name
        self.lw = None
        self.rd = {}
        self.rd_dma = []


class Node:
    __slots__ = ("eng", "fn", "deps", "sig", "sigidx", "dma", "dsem", "dcnt", "inc")

    def __init__(self, eng, fn, dma):
        self.eng = eng
        self.fn = fn
        self.dma = dma
        self.deps = []
        self.sig = False
        self.sigidx = 0
        self.dsem = None
        self.dcnt = 0
        self.inc = 16


class Prog:
    def __init__(self, nc):
        self.nc = nc
        self.q = {k: [] for k in ("pe", "act", "dve", "pool", "sp")}
        self.dq = {k: {"n": 0, "last": [None] * NDSEM, "cnt": [0] * NDSEM} for k in ("sp", "pool", "act")}
        self.final = []

    def op(self, eng, fn, reads=(), writes=(), dma=False, inc=16):
        n = Node(eng, fn, dma)
        n.inc = inc
        deps = []
        for r in reads:
            if r.lw is not None:
                deps.append(r.lw)
        for w in writes:
            if w.lw is not None and (dma or w.lw.dma or w.lw.eng != eng or eng != "pe"):
                deps.append(w.lw)
            for e, rn in w.rd.items():
                if dma or e != eng or eng != "pe":
                    deps.append(rn)
            deps.extend(w.rd_dma)
        if dma:
            d = self.dq[eng]
            k = d["n"] % NDSEM
            d["n"] += 1
            if d["last"][k] is not None:
                deps.append(d["last"][k])
            d["cnt"][k] += inc
            n.dsem = (eng, k)
            n.dcnt = d["cnt"][k]
            d["last"][k] = n
        seen = set()
        for x in deps:
            if id(x) not in seen and x is not n:
                seen.add(id(x))
                n.deps.append(x)
                if not x.dma:
                    x.sig = True
        for r in reads:
            if dma:
                r.rd_dma.append(n)
            else:
                r.rd[eng] = n
        for w in writes:
            w.lw = n
            w.rd = {}
            w.rd_dma = []
        self.q[eng].append(n)
        return n

    def emit(self):
        nc = self.nc
        engobj = {"pe": nc.tensor, "act": nc.scalar, "dve": nc.vector, "pool": nc.gpsimd, "sp": nc.sync}
        fin = Node("sp", None, False)
        fin.deps = list(self.final)
        for x in fin.deps:
            if not x.dma:
                x.sig = True
        self.q["sp"].append(fin)
        for e in self.q:
            c = 0
            for n in self.q[e]:
                if n.dma:
                    continue
                if n.sig:
                    c += 1
                    n.sigidx = c
        with contextlib.ExitStack() as st:
            esem = {e: st.enter_context(nc.semaphore("S_" + e)) for e in self.q}
            dsem = {}
            for qn in self.dq:
                for k in range(NDSEM):
                    dsem[(qn, k)] = st.enter_context(nc.semaphore("D_%s%d" % (qn, k)))
            block = st.enter_context(nc.Block())

            def run(e):
                eo = engobj[e]
                waited = {}
                for n in self.q[e]:
                    for d in n.deps:
                        if d.dma:
                            key = ("d",) + d.dsem
                            sem = dsem[d.dsem]
                            val = d.dcnt
                        else:
                            key = ("e", d.eng)
                            sem = esem[d.eng]
                            val = d.sigidx
                        if waited.get(key, 0) < val:
                            eo.wait_ge(sem, val)
                            waited[key] = val
                    if n.fn is None:
                        continue
                    ins = n.fn(eo)
                    if n.dma:
                        ins.then_inc(dsem[n.dsem], n.inc)
                    elif n.sig:
                        ins.then_inc(esem[e], 1)

            @block.tensor
            def _(t):
                run("pe")

            @block.scalar
            def _(t):
                run("act")

            @block.vector
            def _(t):
                run("dve")

            @block.gpsimd
            def _(t):
                run("pool")

            @block.sync
            def _(t):
                run("sp")


class KB:
    def __init__(self, debug=False, stop=None):
        self.debug = debug
        self.stop = stop
        self.nc = bass.Bass("TRN2", target_bir_lowering=False)
        self.P = Prog(self.nc)
        self.st = contextlib.ExitStack()
        self.dbg_outs = []

    def dram(self, name, shape, dt, kind="Internal"):
        if self.debug and kind == "Internal" and name.startswith("dbg_"):
            kind = "ExternalOutput"
            self.dbg_outs.append(name)
        return self.nc.dram_tensor(name, shape, dt, kind=kind).ap()

    def sb(self, name, shape, dt):
        t = self.st.enter_context(self.nc.sbuf_tensor(name, shape, dt))
        return t, Res(name)

    def op(self, *a, **k):
        return self.P.op(*a, **k)

    def dma(self, out, in_, reads=(), writes=(), q="sp"):
        n = self.P.op(q, lambda e: e.dma_start(out=out, in_=in_), reads=reads, writes=writes, dma=True)
        if self.debug:
            self.P.final.append(n)
        return n

    def bank(self):
        i = self.bank_i % 6
        self.bank_i += 1
        return self.banks[i][:], self.bank_res[i]

    def wpush(self, l, name, i):
        r0, w = BLK_OFF[name]
        rows = w // 4
        a = r0 + i * rows
        ap = self.blob[l][a:a + rows, :].rearrange("(p m) c -> p (m c)", p=128)
        self.wfifo.append((ap, w, self.blk_res[(l, name, i)]))

    def wkick(self):
        while self.w_issued < min(len(self.wfifo), self.w_popped + LA):
            ap, w, bres = self.wfifo[self.w_issued]
            s = self.w_issued % NSLOT
            slot = self.wslots[s]
            self.dma(slot[:, 0:w], ap, reads=[bres], writes=[self.wslot_res[s]])
            self.w_issued += 1

    def push_group(self, l, names):
        for name in names:
            cnt = [c for n, c, w in BLK if n == name][0]
            for i in range(cnt):
                self.wpush(l, name, i)
        self.wkick()

    def wpop(self):
        while self.w_issued < min(len(self.wfifo), self.w_popped + LA):
            ap, w, bres = self.wfifo[self.w_issued]
            s = self.w_issued % NSLOT
            slot = self.wslots[s]
            self.dma(slot[:, 0:w], ap, reads=[bres], writes=[self.wslot_res[s]])
            self.w_issued += 1
        s = self.w_popped % NSLOT
        self.w_popped += 1
        return self.wslots[s], self.wslot_res[s]

    def build(self):
        nc = self.nc
        P = self.P
        self.xin = self.dram("xin", [NT, 8, 128, T], F32, kind="ExternalInput")
        self.posd = self.dram("pos", [TOK], I32, kind="ExternalInput")
        self.memT = self.dram("memT", [8, 128, 256], F32, kind="ExternalInput")
        self.cmaskd = self.dram("cmask", [128, 4, 1024], F32, kind="ExternalInput")
        self.pvecd = self.dram("pvec", [128, NPV], F32, kind="ExternalInput")
        self.identd = self.dram("ident", [128, 128], F32, kind="ExternalInput")
        self.wsl = [self.dram("wsl%d" % l, [BLOB_ROWS, 512], F32, kind="ExternalInput") for l in range(2)]
        self.outd = self.dram("out", [NT, 8, 128, T], F32, kind="ExternalOutput")
        self.blob = [self.dram("blob%d" % l, [BLOB_ROWS, 512], BF16) for l in range(2)]
        self.blk_res = {}
        self.xTd = self.dram("dbg_xT", [NT, 8, 128, T], F32)
        self.xT_res = [Res("xT%d" % j) for j in range(NT)]
        self.hAd = self.dram("dbg_hA", [2, 128, TOK], BF16)
        self.cghd = self.dram("dbg_cgh", [2, 128, TOK], BF16)
        self.bgd = self.dram("dbg_bg", [2, 128, TOK], BF16)
        self.qd = self.dram("dbg_q", [4, 128, TOK], BF16)
        self.qid = self.dram("dbg_qi", [2, 128, TOK], BF16)
        self.widxd = self.dram("dbg_widx", [TOK, 4], F32)
        self.loc_res = [Res("loc%d" % j) for j in range(NT)]
        self.send = [[self.dram("send%d_%d" % (l, j), [TR, 512], BF16) for j in range(NT)] for l in range(2)]
        self.send_res = [[Res("send") for j in range(NT)] for l in range(2)]
        self.ag = [[self.dram("agbuf%d_%d" % (l, j), [2 * TR, 512], BF16) for j in range(NT)] for l in range(2)]
        self.ag_res = [[Res("ag") for j in range(NT)] for l in range(2)]

        if self.debug:
            self.dbg_y = self.dram("dbg_y", [NT, 8, 128, T], BF16)
            self.dbg_x2 = self.dram("dbg_x2", [NT, 8, 128, T], F32)
            self.dbg_x3 = self.dram("dbg_x3", [NT, 8, 128, T], F32)
            self.dbg_x4 = self.dram("dbg_x4", [NT, 8, 128, T], F32)
            self.dbg_sc = self.dram("dbg_sc", [4, 128, 1024], F32)
            self.dbg_sel = self.dram("dbg_sel", [4, 128, 1024], BF16)
            self.dbg_bis = self.dram("dbg_bis", [4, 128, 64], F32)
            self.dbg_ycs = self.dram("dbg_ycs", [4, 128, 512], BF16)
        sb = self.sb
        self.wslots = []
        self.wslot_res = []
        for s in range(NSLOT):
            t, r = sb("wslot%d" % s, [128, SLOTW], BF16)
            self.wslots.append(t)
            self.wslot_res.append(r)
        self.wfifo = []
        self.w_issued = 0
        self.w_popped = 0
        self.xt, self.xt_r = sb("xt", [128, 8, T], F32)
        self.xn, self.xn_r = sb("xn", [128, 8, T], BF16)
        self.big, self.big_r = sb("big", [128, 8192], F32)
        self.sel, self.sel_r = sb("sel", [128, 8192], BF16)
        self.kbuf, self.kbuf_r = sb("kbuf", [128, 8192], BF16)
        self.kvb, self.kvb_r = sb("kvb", [128, 8320], BF16)
        self.cmask, self.cmask_r = sb("cmaskb", [128, 4, 1024], BF16)
        self.pvec, self.pvec_r = sb("pvecs", [128, NPV], F32)
        self.identf, self.identf_r = sb("identf", [128, 128], F32)
        self.ident, self.ident_r = sb("identb", [128, 128], BF16)
        self.onesf, self.onesf_r = sb("onesf", [128, 128], F32)
        self.cosT, self.cos_r = sb("cosT", [128, T], F32)
        self.sinT, self.sin_r = sb("sinT", [128, T], F32)
        self.tmpA = [sb("tmpA%d" % i, [128, T], F32) for i in range(4)]
        self.tmpB = [sb("tmpB%d" % i, [128, T], BF16) for i in range(4)]
        cb = self.cosT[:].bitcast(BF16)
        sbv = self.sinT[:].bitcast(BF16)
        self.tmpB += [(cb[:, 0:T], self.cos_r), (sbv[:, 0:T], self.sin_r), (cb[:, T:2 * T], self.cos_r), (sbv[:, T:2 * T], self.sin_r)]
        self.rtmp, self.rtmp_r = sb("rtmp", [128, 4, T], F32)
        rb = self.rtmp[:].rearrange("p a n -> p (a n)").bitcast(BF16)
        self.pTs = [(rb[:, i * 512:(i + 1) * 512].rearrange("p (a b) -> p a b", a=4), Res("pT%d" % i)) for i in range(8)]
        self.pT_i = 0
        self.rtmp_w = [self.rtmp_r] + [r for _, r in self.pTs]
        self.kbuf_rt = [Res("kbuf%d" % i) for i in range(16)]
        self.negm_r = [Res("negm%d" % i) for i in range(8)]
        self.kvb_rt = [Res("kvb%d" % i) for i in range(16)]
        self.yT, self.yT_r = sb("yT", [128, 8, T], BF16)
        self.qT, self.qT_r = sb("qT", [128, 4, T], BF16)
        self.qiT, self.qiT_r = sb("qiT", [128, 2, T], BF16)
        self.vt, self.vt_r = sb("vt", [128, 4, 520], BF16)
        self.wsc, self.wsc_r = sb("wsc", [128, 4, 4], F32)
        self.wab, _ = sb("wab", [128, 4, 4], F32)
        self.wsg, _ = sb("wsg", [128, 4, 4], F32)
        self.dg, self.dg_r = sb("dg", [128, 4, 128], BF16)
        self.pw2, self.pw2_r = sb("pw2", [128, 16], F32)
        self.small, self.small_r = sb("small", [128, 64], F32)
        self.bis, self.bis_r = sb("bis", [128, 64], F32)
        self.rstd, self.rstd_r = sb("rstd", [128, T], F32)
        self.ycs, self.ycs_r = sb("ycs", [128, 512], BF16)
        self.kmT, self.kmT_r = sb("kmT", [128, 8, 256], BF16)
        self.vm, self.vm_r = sb("vm", [128, 2, 1024], BF16)
        self.memx, self.memx_r = self.rtmp[:].rearrange("p a (b n) -> p (a b) n", b=2), self.rtmp_r
        self.memn, self.memn_r = self.qT[:].rearrange("p a (b n) -> p (a b) n", b=2), self.qT_r
        self.halo, self.halo_r = sb("halo", [128, 2, 2, 32], BF16)
        self.cin, self.cin_r = sb("cin", [128, 2, 32 + T], BF16)
        self.cacc, self.cacc_r = self.xn[:].rearrange("p a n -> p (a n)").bitcast(F32)[:, 0:2 * T].rearrange("p (c n) -> p c n", c=2), self.xn_r
        self.cacc2 = self.xn[:].rearrange("p a n -> p (a n)").bitcast(F32)[:, 2 * T:4 * T].rearrange("p (c n) -> p c n", c=2)
        self.cin2 = self.vt[:].rearrange("p a f -> p (a f)")[:, 0:2 * (32 + T)].rearrange("p (c n) -> p c n", c=2)
        self.ptmp, self.ptmp_r = self.tmpA[3]
        self.act = self.big[:].bitcast(BF16)
        self.banks = []
        self.bank_res = []
        for i in range(8):
            t = self.st.enter_context(nc.psum_tensor("bank%d" % i, [128, 512], F32))
            self.banks.append(t)
            self.bank_res.append(Res("bank%d" % i))
        self.bank_i = 0
        self.tmp_i = 0
        self.tmpb_i = 0

        self.dma(self.pvec[:], self.pvecd, writes=[self.pvec_r])
        self.dma(self.identf[:], self.identd, writes=[self.identf_r])
        self.op("dve", lambda e: e.tensor_copy(out=self.ident[:], in_=self.identf[:]), reads=[self.identf_r], writes=[self.ident_r])
        self.op("dve", lambda e: e.memset(self.onesf[:], 1.0), writes=[self.onesf_r])
        for i in range(16):
            self.op("dve", lambda e, i=i: e.memset(self.pw2[:, i:i + 1], 0.5 ** (i + 1)), writes=[self.pw2_r])
        self.op("dve", lambda e: e.memset(self.vt[:], 1.0), writes=[self.vt_r])
        self.dma(self.cmask[:], self.cmaskd, writes=[self.cmask_r], q="pool")
        for l in range(2):
            for name, cnt, w in BLK:
                r0, _ = BLK_OFF[name]
                rows = w // 4
                for i in range(cnt):
                    r = Res("blk")
                    self.blk_res[(l, name, i)] = r
                    a = r0 + i * rows
                    self.dma(self.blob[l][a:a + rows, :], self.wsl[l][a:a + rows, :], writes=[r], q="pool")
        if self.stop == "T0":
            self.load_x(self.xin, 0, None)
            self.phaseA(0, 0)
            self.exchange(0, 0)
            self.mem_kv(0)
            self.load_x(self.xTd, 0, self.xT_res[0])
            self.phaseB(0, 0)
            return self.finish()
        for j in range(NT):
            self.load_x(self.xin, j, None)
            self.phaseA(0, j)
            self.exchange(0, j)
        if self.stop == "A0":
            return self.finish()
        for l in range(2):
            self.mem_kv(l)
            for j in range(NT):
                self.load_x(self.xTd, j, self.xT_res[j])
                self.phaseB(l, j)
                if l == 0:
                    self.phaseA(1, j, push=False)
                    self.exchange(1, j)
                else:
                    self.final_out(j)
            if l == 0 and self.stop == "B0":
                return self.finish()
        return self.finish()

    def finish(self):
        self.P.emit()
        self.st.close()
        return self.nc

    def pcol(self, c):
        return self.pvec[:, c:c + 1]

    def load_x(self, src, j, res):
        self.dma(self.xt[:], src[j].rearrange("c p n -> p c n"), reads=[res] if res else [], writes=[self.xt_r])

    def tmpa(self):
        i = self.tmp_i % 3
        self.tmp_i += 1
        return self.tmpA[i][0], self.tmpA[i][1]

    def tmpb(self):
        i = self.tmpb_i % 8
        self.tmpb_i += 1
        return self.tmpB[i][0], self.tmpB[i][1]

    def ptbuf(self):
        i = self.pT_i % 8
        self.pT_i += 1
        return self.pTs[i]

    def rmsnorm(self, x, x_r, gc0, out, out_r, nch, N, eps_col=PV_EPS6):
        pb, pbr = self.bank()
        for c in range(nch):
            sq, sqr = self.tmpa()
            self.op("act", lambda e, c=c, sq=sq: e.activation(out=sq[:, 0:N], in_=x[:, c, :], func=AF.Square), reads=[x_r], writes=[sqr])
            self.op("pe", lambda e, c=c, sq=sq: e.matmul(pb[:, 0:N], lhsT=self.onesf[:], rhs=sq[:, 0:N], start=(c == 0), stop=(c == nch - 1)),
                    reads=[sqr, self.onesf_r], writes=[pbr])
        sd, sdr = self.tmpa()
        self.op("act", lambda e: e.activation(out=sd[:, 0:N], in_=pb[:, 0:N], func=AF.Sqrt, bias=self.pcol(eps_col), scale=1.0 / (nch * 128)),
                reads=[pbr, self.pvec_r], writes=[sdr])
        self.op("dve", lambda e: e.reciprocal(out=self.rstd[:, 0:N], in_=sd[:, 0:N]), reads=[sdr], writes=[self.rstd_r])
        for c in range(nch):
            self.op("dve", lambda e, c=c: e.scalar_tensor_tensor(out=out[:, c, :], in0=x[:, c, :], scalar=self.pcol(gc0 + c), in1=self.rstd[:, 0:N],
                                                                 op0=ALU.mult, op1=ALU.mult),
                    reads=[x_r, self.rstd_r, self.pvec_r], writes=[out_r])

    def ffn(self, l, which):
        self.rmsnorm(self.xt, self.xt_r, l * LP + (0 if which == 1 else 32), self.xn, self.xn_r, 8, T)
        act = self.act
        for i in range(11):
            slot, sres = self.wpop()
            sv = slot[:, 0:4096].rearrange("p (k n) -> p k n", k=8)
            for cc in range(2):
                pg, pgr = self.bank()
                pu, pur = self.bank()
                for k in range(8):
                    self.op("pe", lambda e, k=k, cc=cc, sv=sv, pg=pg: e.matmul(pg, lhsT=sv[:, k, cc * 128:(cc + 1) * 128], rhs=self.xn[:, k, :],
                                                                          start=(k == 0), stop=(k == 7)),
                            reads=[sres, self.xn_r], writes=[pgr])
                for k in range(8):
                    self.op("pe", lambda e, k=k, cc=cc, sv=sv, pu=pu: e.matmul(pu, lhsT=sv[:, k, 256 + cc * 128:256 + (cc + 1) * 128], rhs=self.xn[:, k, :],
                                                                          start=(k == 0), stop=(k == 7)),
                            reads=[sres, self.xn_r], writes=[pur])
                sg, sgr = self.tmpa()
                self.op("act", lambda e, sg=sg, pg=pg: e.activation(out=sg[:], in_=pg, func=AF.Silu), reads=[pgr], writes=[sgr])
                ch = 2 * i + cc
                self.op("dve", lambda e, sg=sg, pu=pu, ch=ch: e.tensor_tensor(out=act[:, ch * T:(ch + 1) * T], in0=sg[:], in1=pu, op=ALU.mult),
                        reads=[sgr, pur], writes=[self.big_r])
        for oc in range(8):
            slot, sres = self.wpop()
            sv = slot[:, 0:2816].rearrange("p (k n) -> p k n", k=22)
            po, por = self.bank()
            for k in range(22):
                self.op("pe", lambda e, k=k, sv=sv, po=po: e.matmul(po, lhsT=sv[:, k, :], rhs=act[:, k * T:(k + 1) * T], start=(k == 0), stop=(k == 21)),
                        reads=[sres, self.big_r], writes=[por])
            self.op("dve", lambda e, oc=oc, po=po: e.scalar_tensor_tensor(out=self.xt[:, oc, :], in0=po, scalar=0.5, in1=self.xt[:, oc, :],
                                                                         op0=ALU.mult, op1=ALU.add),
                    reads=[por, self.xt_r], writes=[self.xt_r])

    def rope_tables(self, j):
        e_ = self
        pi_, pi_r = self.tmpa()
        posi = pi_[:].bitcast(I32)
        self.dma(posi, self.posd[j * T:(j + 1) * T].partition_broadcast(128), writes=[pi_r])
        a, ar = self.tmpa()
        k, kr = self.tmpa()
        ki, kir = self.tmpa()
        kiv = ki[:].bitcast(I32)
        op = self.op
        op("dve", lambda e: e.tensor_copy(out=a[:], in_=posi), reads=[pi_r], writes=[ar])
        op("dve", lambda e: e.tensor_scalar(out=a[:], in0=a[:], scalar1=self.pcol(PV_INVF), scalar2=None, op0=ALU.mult), reads=[ar, self.pvec_r], writes=[ar])
        op("dve", lambda e: e.tensor_scalar(out=k[:], in0=a[:], scalar1=1.0 / TWO_PI, scalar2=None, op0=ALU.mult), reads=[ar], writes=[kr])
        op("dve", lambda e: e.tensor_copy(out=kiv, in_=k[:]), reads=[kr], writes=[kir])
        op("dve", lambda e: e.tensor_copy(out=k[:], in_=kiv), reads=[kir], writes=[kr])
        op("dve", lambda e: e.scalar_tensor_tensor(out=a[:], in0=k[:], scalar=-TWO_PI, in1=a[:], op0=ALU.mult, op1=ALU.add), reads=[kr, ar], writes=[ar])

        def fold(t, tr):
            op("dve", lambda e: e.tensor_scalar(out=k[:], in0=t[:], scalar1=np.pi, scalar2=-TWO_PI, op0=ALU.is_gt, op1=ALU.mult), reads=[tr], writes=[kr])
            op("dve", lambda e: e.tensor_tensor(out=t[:], in0=t[:], in1=k[:], op=ALU.add), reads=[tr, kr], writes=[tr])
            op("dve", lambda e: e.tensor_scalar(out=k[:], in0=t[:], scalar1=-np.pi, scalar2=TWO_PI, op0=ALU.is_lt, op1=ALU.mult), reads=[tr], writes=[kr])
            op("dve", lambda e: e.tensor_tensor(out=t[:], in0=t[:], in1=k[:], op=ALU.add), reads=[tr, kr], writes=[tr])
        fold(a, ar)
        op("act", lambda e: e.activation(out=self.sinT[:], in_=a[:], func=AF.Sin), reads=[ar], writes=[self.sin_r])
        op("dve", lambda e: e.tensor_scalar(out=self.sinT[:], in0=self.sinT[:], scalar1=self.pcol(PV_SGN), scalar2=None, op0=ALU.mult),
           reads=[self.sin_r, self.pvec_r], writes=[self.sin_r])
        op("dve", lambda e: e.tensor_scalar(out=a[:], in0=a[:], scalar1=0.5 * np.pi, scalar2=None, op0=ALU.add), reads=[ar], writes=[ar])
        fold(a, ar)
        op("act", lambda e: e.activation(out=self.cosT[:], in_=a[:], func=AF.Sin), reads=[ar], writes=[self.cos_r])

    def phaseA(self, l, j, push=True):
        op = self.op
        if push:
            self.push_group(l, ["gu1", "dn1", "win", "wv"])
        self.ffn(l, 1)
        self.dma(self.xTd[j].rearrange("c p n -> p c n"), self.xt[:], reads=[self.xt_r], writes=[self.xT_res[j]])
        self.rmsnorm(self.xt, self.xt_r, l * LP + 8, self.xn, self.xn_r, 8, T)
        self.rope_tables(j)
        hn = self.xn
        yT = self.yT
        loc = self.loc_res[j]
        cols = slice(j * T, (j + 1) * T)
        held = {}
        sendv = self.send[l][j]
        sres_ = self.send_res[l][j]
        kview = sendv[0:512, :].rearrange("(c p) n -> p c n", c=4)
        kiview = sendv[512:640, :]
        hview = [sendv[1160 + 16 * i:1160 + 16 * (i + 1), :].rearrange("(c q) (h t) -> (q h) c t", c=2, q=8, h=16, t=32) for i in range(2)]
        for blk in range(8):
            slot, sres = self.wpop()
            sv = slot[:, 0:4096].rearrange("p (k n) -> p k n", k=8)
            for cc in range(4):
                ch = blk * 4 + cc
                pb, pbr = self.bank()
                for k in range(8):
                    op("pe", lambda e, k=k, cc=cc, sv=sv, pb=pb: e.matmul(pb, lhsT=sv[:, k, cc * 128:(cc + 1) * 128], rhs=hn[:, k, :], start=(k == 0), stop=(k == 7)),
                       reads=[sres, self.xn_r], writes=[pbr])
                if ch in (0, 1, 6, 7):
                    held[ch] = (pb, pbr)
                elif ch in (2, 3):
                    c = ch - 2
                    sg, sgr = self.tmpa()
                    op("act", lambda e, sg=sg, pb=pb: e.activation(out=sg[:], in_=pb, func=AF.Sigmoid), reads=[pbr], writes=[sgr])
                    pv, pvr = held[c]
                    op("dve", lambda e, sg=sg, pv=pv, c=c: e.tensor_tensor(out=yT[:, c, :], in0=sg[:], in1=pv, op=ALU.mult), reads=[sgr, pvr], writes=[self.yT_r])
                    if c == 1:
                        self.dma(self.hAd[:, :, cols].rearrange("c p n -> p c n"), yT[:, 0:2, :], reads=[self.yT_r], writes=[loc])
                        self.dma(hview[0], yT[:, 0:2, T - 32:T], reads=[self.yT_r], writes=[sres_])
                elif ch in (4, 5):
                    c = ch - 4
                    op("act", lambda e, pb=pb, c=c: e.copy(out=yT[:, 2 + c, :], in_=pb), reads=[pbr], writes=[self.yT_r])
                    if c == 1:
                        self.dma(self.bgd[:, :, cols].rearrange("c p n -> p c n"), yT[:, 2:4, :], reads=[self.yT_r], writes=[loc])
                elif ch in (8, 9):
                    c = ch - 8
                    sg, sgr = self.tmpa()
                    pv, pvr = held[6 + c]
                    op("act", lambda e, sg=sg, pv=pv: e.copy(out=sg[:], in_=pv), reads=[pvr], writes=[sgr])
                    op("dve", lambda e, sg=sg, pb=pb, c=c: e.tensor_tensor(out=yT[:, 4 + c, :], in0=sg[:], in1=pb, op=ALU.mult), reads=[sgr, pbr], writes=[self.yT_r])
                    if c == 1:
                        self.dma(self.cghd[:, :, cols].rearrange("c p n -> p c n"), yT[:, 4:6, :], reads=[self.yT_r], writes=[loc])
                        self.dma(hview[1], yT[:, 4:6, T - 32:T], reads=[self.yT_r], writes=[sres_])
                else:
                    for (c0, n, name) in ((10, 4, "q"), (18, 4, "k"), (26, 2, "qi"), (30, 1, "ki")):
                        if c0 <= ch < c0 + n:
                            c = ch - c0
                            op("dve", lambda e, pb=pb, c=c: e.tensor_tensor(out=self.rtmp[:, c, :], in0=pb, in1=self.cosT[:], op=ALU.mult),
                               reads=[pbr, self.cos_r], writes=self.rtmp_w)
                        elif c0 + n <= ch < c0 + 2 * n:
                            c = ch - c0 - n
                            t2, t2r = self.tmpa()
                            op("dve", lambda e, pb=pb, t2=t2: e.tensor_tensor(out=t2[:], in0=pb, in1=self.sinT[:], op=ALU.mult),
                               reads=[pbr, self.sin_r], writes=[t2r])
                            op("pool", lambda e, t2=t2, c=c: e.tensor_tensor(out=yT[:, c, :], in0=t2[:], in1=self.rtmp[:, c, :], op=ALU.add),
                               reads=[t2r, self.rtmp_r], writes=[self.yT_r])
                            if c == n - 1:
                                if name == "q":
                                    self.dma(self.qd[:, :, cols].rearrange("c p n -> p c n"), yT[:, 0:4, :], reads=[self.yT_r], writes=[loc])
                                elif name == "k":
                                    self.dma(kview, yT[:, 0:4, :], reads=[self.yT_r], writes=[sres_])
                                elif name == "qi":
                                    self.dma(self.qid[:, :, cols].rearrange("c p n -> p c n"), yT[:, 0:2, :], reads=[self.yT_r], writes=[loc])
                                else:
                                    self.dma(kiview, yT[:, 0, :], reads=[self.yT_r], writes=[sres_])
        op("dve", lambda e: e.memset(self.vt[:].rearrange("p a (h f) -> p a h f", f=65)[:, :, :, 64:65], 1.0), writes=[self.vt_r])
        slot, sres = self.wpop()
        sv = slot[:, 0:4608].rearrange("p (k n) -> p k n", k=8)
        vview = sendv[640:1160, :].rearrange("r c -> (r c)").rearrange("(t f) -> t f", f=520)
        for tb in range(4):
            pv, pvr = self.bank()
            pw, pwr = self.bank()
            for k in range(8):
                op("pe", lambda e, k=k, tb=tb, sv=sv, pv=pv: e.matmul(pv, lhsT=hn[:, k, tb * 128:(tb + 1) * 128], rhs=sv[:, k, 0:512], start=(k == 0), stop=(k == 7)),
                   reads=[sres, self.xn_r], writes=[pvr])
            for k in range(8):
                op("pe", lambda e, k=k, tb=tb, sv=sv, pw=pw: e.matmul(pw[:, 0:4], lhsT=hn[:, k, tb * 128:(tb + 1) * 128], rhs=sv[:, k, 512:516], start=(k == 0), stop=(k == 7)),
                   reads=[sres, self.xn_r], writes=[pwr])
            op("act", lambda e, tb=tb, pv=pv: e.copy(out=self.vt[:, tb, :].rearrange("p (h f) -> p h f", f=65)[:, :, 0:64], in_=pv.rearrange("p (h f) -> p h f", f=64)),
               reads=[pvr], writes=[self.vt_r])
            op("dve", lambda e, tb=tb, pw=pw: e.tensor_copy(out=self.wsc[:, tb, :], in_=pw[:, 0:4]), reads=[pwr], writes=[self.wsc_r])
        self.dma(vview.rearrange("(n p) f -> p n f", p=128), self.vt[:], reads=[self.vt_r], writes=[sres_])
        self.dma(self.widxd[j * T:(j + 1) * T, :].rearrange("(n p) f -> p n f", p=128), self.wsc[:], reads=[self.wsc_r], writes=[loc])

    def exchange(self, l, j):
        self.op("pool", lambda e: e.collective_compute("AllGather", ALU.bypass, replica_groups=[[0, 1], [2, 3], [4, 5], [6, 7]],
                                                       ins=[self.send[l][j]], outs=[self.ag[l][j]]),
                reads=[self.send_res[l][j]], writes=[self.ag_res[l][j]], dma=True, inc=1)

    def mem_kv(self, l):
        op = self.op
        self.push_group(l, ["wkv"])
        self.dma(self.memx, self.memT.rearrange("c p n -> p c n"), writes=self.rtmp_w)
        self.rmsnorm(self.memx, self.memx_r, l * LP + 24, self.memn, self.memn_r, 8, 256)
        for blk in range(2):
            slot, sres = self.wpop()
            sv = slot[:, 0:4096].rearrange("p (k n) -> p k n", k=8)
            for cc in range(4):
                pb, pbr = self.bank()
                for k in range(8):
                    op("pe", lambda e, k=k, cc=cc, sv=sv, pb=pb: e.matmul(pb[:, 0:256], lhsT=sv[:, k, cc * 128:(cc + 1) * 128], rhs=self.memn[:, k, :], start=(k == 0), stop=(k == 7)),
                       reads=[sres, self.memn_r], writes=[pbr])
                op("act", lambda e, pb=pb, ch=blk * 4 + cc: e.copy(out=self.kmT[:, ch, :], in_=pb[:, 0:256]), reads=[pbr], writes=[self.kmT_r])
        for blk in range(2):
            slot, sres = self.wpop()
            sv = slot[:, 0:4096].rearrange("p (k n) -> p k n", k=8)
            for mb in range(2):
                pb, pbr = self.bank()
                for k in range(8):
                    op("pe", lambda e, k=k, mb=mb, sv=sv, pb=pb: e.matmul(pb, lhsT=self.memn[:, k, mb * 128:(mb + 1) * 128], rhs=sv[:, k, :], start=(k == 0), stop=(k == 7)),
                       reads=[sres, self.memn_r], writes=[pbr])
                op("act", lambda e, pb=pb, mb=mb, blk=blk: e.copy(out=self.vm[:, mb, blk * 512:(blk + 1) * 512], in_=pb), reads=[pbr], writes=[self.vm_r])

    def linear_res(self, l, name, inp, inp_r, nk):
        op = self.op
        for blk in range(2):
            slot, sres = self.wpop()
            sv = slot[:, 0:4096].rearrange("p (k n) -> p k n", k=8)
            for cc in range(4):
                oc = blk * 4 + cc
                pb, pbr = self.bank()
                for k in range(nk):
                    op("pe", lambda e, k=k, cc=cc, sv=sv, pb=pb: e.matmul(pb, lhsT=sv[:, k, cc * 128:(cc + 1) * 128], rhs=inp[:, k, :], start=(k == 0), stop=(k == nk - 1)),
                       reads=[sres, inp_r], writes=[pbr])
                op("dve", lambda e, oc=oc, pb=pb: e.tensor_tensor(out=self.xt[:, oc, :], in0=pb, in1=self.xt[:, oc, :], op=ALU.add),
                   reads=[pbr, self.xt_r], writes=[self.xt_r])

    def conv_taps(self, l, j):
        op = self.op
        base = l * LP
        loc = self.loc_res[j]
        cols = slice(j * T, (j + 1) * T)
        for br in range(2):
            src = self.hAd if br == 0 else self.cghd
            cin, cin_r = (self.cin, self.cin_r) if br == 0 else (self.cin2, self.vt_r)
            cacc = self.cacc if br == 0 else self.cacc2
            def hv(jj, rr, br=br):
                a = rr * TR + 1160 + 16 * br
                return self.ag[l][jj][a:a + 16, :].rearrange("(c q) (h t) -> (q h) c t", c=2, q=8, h=16, t=32)
            self.dma(self.halo[:, 0, :, :], hv(j, 0), reads=[self.ag_res[l][j]], writes=[self.halo_r])
            if j > 0:
                self.dma(self.halo[:, 1, :, :], hv(j - 1, 1), reads=[self.ag_res[l][j - 1]], writes=[self.halo_r])
            else:
                op("dve", lambda e: e.memset(self.halo[:, 1, :, :], 0.0), writes=[self.halo_r])
            self.dma(cin[:, :, 32:32 + T], src[:, :, cols].rearrange("c p n -> p c n"), reads=[loc], writes=[cin_r])
            op("dve", lambda e, cin=cin: e.tensor_scalar(out=cin[:, :, 0:32], in0=self.halo[:, 0, :, :], scalar1=self.pcol(PV_SEL), scalar2=None, op0=ALU.mult),
               reads=[self.halo_r, self.pvec_r], writes=[cin_r])
            op("dve", lambda e, cin=cin: e.scalar_tensor_tensor(out=cin[:, :, 0:32], in0=self.halo[:, 1, :, :], scalar=self.pcol(PV_SEL + 1), in1=cin[:, :, 0:32],
                                                                op0=ALU.mult, op1=ALU.add),
               reads=[self.halo_r, self.pvec_r, cin_r], writes=[cin_r])
            W = 31 if br == 0 else 3
            wc0 = base + (40 if br == 0 else 108)
            for c in range(2):
                for tap in range(W):
                    sh = 32 - (W - 1) + tap
                    colw = self.pcol(wc0 + c * W + tap)
                    if tap == 0:
                        op("pool", lambda e, c=c, sh=sh, colw=colw, cin=cin, cacc=cacc: e.tensor_scalar(out=cacc[:, c, :], in0=cin[:, c, sh:sh + T], scalar1=colw, scalar2=None, op0=ALU.mult),
                           reads=[cin_r, self.pvec_r], writes=[self.cacc_r])
                    else:
                        op("pool", lambda e, c=c, sh=sh, colw=colw, cin=cin: e.tensor_scalar(out=self.ptmp[:], in0=cin[:, c, sh:sh + T], scalar1=colw, scalar2=None, op0=ALU.mult),
                           reads=[cin_r, self.pvec_r], writes=[self.ptmp_r])
                        op("pool", lambda e, c=c, cacc=cacc: e.tensor_tensor(out=cacc[:, c, :], in0=cacc[:, c, :], in1=self.ptmp[:], op=ALU.add),
                           reads=[self.ptmp_r, self.cacc_r], writes=[self.cacc_r])

    def conv_finish(self, l, j):
        op = self.op
        base = l * LP
        loc = self.loc_res[j]
        cols = slice(j * T, (j + 1) * T)
        for c in range(2):
            op("dve", lambda e, c=c: e.tensor_scalar(out=self.cacc[:, c, :], in0=self.cacc[:, c, :], scalar1=self.pcol(base + 102 + c), scalar2=None, op0=ALU.add),
               reads=[self.cacc_r, self.pvec_r], writes=[self.cacc_r])
        pm, pmr = self.bank()
        for c in range(2):
            op("pe", lambda e, c=c, pm=pm: e.matmul(pm, lhsT=self.onesf[:], rhs=self.cacc[:, c, :], start=(c == 0), stop=(c == 1)),
               reads=[self.cacc_r, self.onesf_r], writes=[pmr])
        mean, meanr = self.tmpa()
        op("act", lambda e, pm=pm, mean=mean: e.activation(out=mean[:], in_=pm, func=AF.Copy, scale=1.0 / 256), reads=[pmr], writes=[meanr])
        for c in range(2):
            op("dve", lambda e, c=c, mean=mean: e.tensor_tensor(out=self.cacc[:, c, :], in0=self.cacc[:, c, :], in1=mean[:], op=ALU.subtract),
               reads=[self.cacc_r, meanr], writes=[self.cacc_r])
        pq, pqr = self.bank()
        for c in range(2):
            sq, sqr = self.tmpa()
            op("act", lambda e, c=c, sq=sq: e.activation(out=sq[:], in_=self.cacc[:, c, :], func=AF.Square), reads=[self.cacc_r], writes=[sqr])
            op("pe", lambda e, c=c, sq=sq, pq=pq: e.matmul(pq, lhsT=self.onesf[:], rhs=sq[:], start=(c == 0), stop=(c == 1)), reads=[sqr, self.onesf_r], writes=[pqr])
        sd, sdr = self.tmpa()
        op("act", lambda e, sd=sd, pq=pq: e.activation(out=sd[:], in_=pq, func=AF.Sqrt, bias=self.pcol(PV_EPS5), scale=1.0 / 256), reads=[pqr, self.pvec_r], writes=[sdr])
        op("dve", lambda e, sd=sd: e.reciprocal(out=self.rstd[:], in_=sd[:]), reads=[sdr], writes=[self.rstd_r])
        for c in range(2):
            op("dve", lambda e, c=c: e.scalar_tensor_tensor(out=self.cacc[:, c, :], in0=self.cacc[:, c, :], scalar=self.pcol(base + 104 + c), in1=self.rstd[:],
                                                            op0=ALU.mult, op1=ALU.mult), reads=[self.cacc_r, self.rstd_r, self.pvec_r], writes=[self.cacc_r])
            op("act", lambda e, c=c: e.activation(out=self.yT[:, c, :], in_=self.cacc[:, c, :], func=AF.Silu, bias=self.pcol(base + 106 + c), scale=1.0),
               reads=[self.cacc_r, self.pvec_r], writes=[self.yT_r])
        self.dma(self.cin[:, :, 32:32 + T], self.bgd[:, :, cols].rearrange("c p n -> p c n"), reads=[loc], writes=[self.cin_r])
        for c in range(2):
            op("dve", lambda e, c=c: e.tensor_tensor(out=self.yT[:, 2 + c, :], in0=self.cacc2[:, c, :], in1=self.cin[:, c, 32:32 + T], op=ALU.mult),
               reads=[self.cacc_r, self.cin_r], writes=[self.yT_r])

    def dsa(self, l, j):
        op = self.op
        loc = self.loc_res[j]
        cols = slice(j * T, (j + 1) * T)
        S = 1024 * (j + 1)
        nch = S // 512
        sc = self.big
        self.dma(self.qT[:], self.qd[:, :, cols].rearrange("c p n -> p c n"), reads=[loc], writes=[self.qT_r])
        self.dma(self.qiT[:], self.qid[:, :, cols].rearrange("c p n -> p c n"), reads=[loc], writes=[self.qiT_r])
        self.dma(self.wsc[:], self.widxd[j * T:(j + 1) * T, :].rearrange("(n p) f -> p n f", p=128), reads=[loc], writes=[self.wsc_r])
        op("dve", lambda e: e.tensor_scalar(out=self.wsc[:], in0=self.wsc[:], scalar1=IDX_SCALE, scalar2=None, op0=ALU.mult), reads=[self.wsc_r], writes=[self.wsc_r])
        op("dve", lambda e: e.tensor_scalar(out=self.wsg[:], in0=self.wsc[:], scalar1=0.0, scalar2=2.0, op0=ALU.is_ge, op1=ALU.mult), reads=[self.wsc_r], writes=[self.wsc_r])
        op("dve", lambda e: e.tensor_scalar(out=self.wsg[:], in0=self.wsg[:], scalar1=-1.0, scalar2=None, op0=ALU.add), reads=[self.wsc_r], writes=[self.wsc_r])
        op("dve", lambda e: e.tensor_tensor(out=self.wab[:], in0=self.wsc[:], in1=self.wsg[:], op=ALU.mult), reads=[self.wsc_r], writes=[self.wsc_r])
        vb_v = self.kvb[:, 0:8320].rearrange("p (k f) -> p k f", f=130)
        nkt = 2 * (j + 1)
        def agt(kt):
            return self.ag[l][kt // 2], (kt % 2) * TR, self.ag_res[l][kt // 2]
        bis = self.bis

        def _blk(b):
            qs = slice(b * 128, (b + 1) * 128)
            for kt in range(nkt):
                a_, o_, r_ = agt(kt)
                self.dma(self.kvb[:, kt * 520:kt * 520 + 512], a_[o_ + 512:o_ + 640, :], reads=[r_], writes=[self.kvb_rt[kt]])
            for h in range(4):
                op("dve", lambda e, h=h: e.tensor_scalar(out=self.dg[:, h, :], in0=self.identf[:], scalar1=self.wsg[:, b, h:h + 1], scalar2=None, op0=ALU.mult),
                   reads=[self.identf_r, self.wsc_r], writes=[self.dg_r])
            for ch in range(nch):
                cs = slice(ch * 512, (ch + 1) * 512)
                rls = []
                for h in range(4):
                    ps_ = slice((h % 2) * 64, (h % 2) * 64 + 64)
                    pb, pbr = self.bank()
                    op("pe", lambda e, pb=pb, ps_=ps_, h=h, ch=ch: e.matmul(pb, lhsT=self.qiT[ps_, h // 2, qs], rhs=self.kvb[ps_, ch * 520:ch * 520 + 512], start=True, stop=True),
                       reads=[self.qiT_r, self.kvb_rt[ch]], writes=[pbr])
                    rl, rlr = self.tmpb()
                    op("act", lambda e, pb=pb, rl=rl, h=h: e.activation(out=rl[:], in_=pb, func=AF.Relu, scale=self.wab[:, b, h:h + 1]), reads=[pbr, self.wsc_r], writes=[rlr])
                    rls.append((rl, rlr))
                ps2, ps2r = self.bank()
                for h in range(4):
                    rl, rlr = rls[h]
                    op("pe", lambda e, ps2=ps2, rl=rl, h=h: e.matmul(ps2, lhsT=self.dg[:, h, :], rhs=rl[:], start=(h == 0), stop=(h == 3)),
                       reads=[rlr, self.dg_r], writes=[ps2r])
                op("act", lambda e, ps2=ps2, cs=cs: e.copy(out=sc[:, cs], in_=ps2), reads=[ps2r], writes=[self.big_r])
            last = slice(S - 1024, S)
            for hh in range(2):
                t1, t1r = self.tmpa()
                ls = slice(S - 1024 + hh * 512, S - 1024 + (hh + 1) * 512)
                op("dve", lambda e, t1=t1, ls=ls, hh=hh: e.tensor_tensor(out=t1[:], in0=sc[:, ls], in1=self.cmask[:, b, hh * 512:(hh + 1) * 512], op=ALU.subtract),
                   reads=[self.big_r, self.cmask_r], writes=[t1r])
                op("dve", lambda e, t1=t1, hh=hh: e.tensor_reduce(out=bis[:, 40 + hh:41 + hh], in_=t1[:], axis=AX.X, op=ALU.min), reads=[t1r], writes=[self.bis_r])
            op("dve", lambda e: e.tensor_tensor(out=sc[:, last], in0=sc[:, last], in1=self.cmask[:, b, :], op=ALU.add), reads=[self.big_r, self.cmask_r], writes=[self.big_r])
            if S > 1024:
                op("dve", lambda e: e.tensor_reduce(out=bis[:, 42:43], in_=sc[:, 0:S - 1024], axis=AX.X, op=ALU.min), reads=[self.big_r], writes=[self.bis_r])
                nm = 3
            else:
                nm = 2
            op("dve", lambda e: e.tensor_reduce(out=bis[:, 0:1], in_=bis[:, 40:40 + nm], axis=AX.X, op=ALU.min), reads=[self.bis_r], writes=[self.bis_r])
            op("dve", lambda e: e.tensor_reduce(out=bis[:, 1:2], in_=sc[:, 0:S], axis=AX.X, op=ALU.max), reads=[self.big_r], writes=[self.bis_r])
            op("dve", lambda e: e.tensor_tensor(out=bis[:, 2:3], in0=bis[:, 1:2], in1=bis[:, 0:1], op=ALU.subtract), reads=[self.bis_r], writes=[self.bis_r])
            op("dve", lambda e: e.memset(bis[:, 20:20 + NITER], 0.0), writes=[self.bis_r])
            op("dve", lambda e: e.tensor_scalar(out=bis[:, 44:44 + NITER], in0=self.pw2[:, 0:NITER], scalar1=bis[:, 2:3], scalar2=None, op0=ALU.mult),
               reads=[self.bis_r, self.pw2_r], writes=[self.bis_r])
            for it in range(NITER):
                op("dve", lambda e, it=it: e.tensor_tensor(out=bis[:, 3:4], in0=bis[:, 0:1], in1=bis[:, 44 + it:45 + it], op=ALU.add),
                   reads=[self.bis_r], writes=[self.bis_r])
                op("dve", lambda e, it=it: e.tensor_scalar(out=self.sel[:, 0:S], in0=sc[:, 0:S], scalar1=bis[:, 3:4], scalar2=0.0, op0=ALU.is_ge, op1=ALU.add,
                                                          accum_out=bis[:, 20 + it:21 + it]),
                   reads=[self.big_r, self.bis_r], writes=[self.sel_r, self.bis_r])
                op("dve", lambda e, it=it: e.tensor_scalar(out=bis[:, 5:6], in0=bis[:, 20 + it:21 + it], scalar1=255.5, scalar2=bis[:, 44 + it:45 + it], op0=ALU.is_ge, op1=ALU.mult),
                   reads=[self.bis_r], writes=[self.bis_r])
                op("dve", lambda e: e.tensor_tensor(out=bis[:, 0:1], in0=bis[:, 0:1], in1=bis[:, 5:6], op=ALU.add), reads=[self.bis_r], writes=[self.bis_r])
            op("dve", lambda e: e.tensor_scalar(out=self.sel[:, 0:S], in0=sc[:, 0:S], scalar1=bis[:, 0:1], scalar2=-30000.0, op0=ALU.is_lt, op1=ALU.mult),
               reads=[self.big_r, self.bis_r], writes=[self.sel_r])
            if self.debug and j == 0:
                self.dma(self.dbg_sc[b], sc[:, 0:1024], reads=[self.big_r])
                self.dma(self.dbg_sel[b], self.sel[:, 0:1024], reads=[self.sel_r])
                self.dma(self.dbg_bis[b], bis[:], reads=[self.bis_r])
            ycp = [self.banks[6][:], self.banks[7][:]]
            ycr = [self.bank_res[6], self.bank_res[7]]
            LB, LC = 2, 4
            for c in range(4):
                for kt in range(nkt):
                    a_, o_, r_ = agt(kt)
                    self.dma(self.kbuf[:, kt * 512:(kt + 1) * 512], a_[o_ + c * 128:o_ + (c + 1) * 128, :], reads=[r_], writes=[self.kbuf_rt[kt]])
                    vv_ = a_[o_ + 640:o_ + 1160, :].rearrange("r c -> (r c)").rearrange("(t f) -> t f", f=520)
                    self.dma(vb_v[:, kt * 4:(kt + 1) * 4, :], vv_[:, c * 130:(c + 1) * 130].rearrange("(n p) f -> p n f", p=128), reads=[r_], writes=[self.kvb_rt[kt]])
                for half in range(2):
                    h = 2 * c + half
                    ps_ = slice(half * 64, half * 64 + 64)
                    for ch in range(nch):
                        cs = slice(ch * 512, (ch + 1) * 512)
                        pb, pbr = self.bank()
                        op("pe", lambda e, pb=pb, ps_=ps_, cs=cs, c=c: e.matmul(pb, lhsT=self.qT[ps_, c, qs], rhs=self.kbuf[ps_, cs], start=True, stop=True),
                           reads=[self.qT_r, self.kbuf_rt[ch]], writes=[pbr])
                        op("dve", lambda e, pb=pb, ch=ch: e.tensor_reduce(out=self.small[:, ch:ch + 1], in_=pb, axis=AX.X, op=ALU.max), reads=[pbr], writes=[self.small_r])
                    op("dve", lambda e: e.tensor_reduce(out=self.small[:, 32:33], in_=self.small[:, 0:nch], axis=AX.X, op=ALU.max), reads=[self.small_r], writes=[self.small_r])
                    nb_ = 34 + h
                    op("dve", lambda e, nb_=nb_: e.tensor_scalar(out=self.small[:, nb_:nb_ + 1], in0=self.small[:, 32:33], scalar1=-0.125, scalar2=None, op0=ALU.mult),
                       reads=[self.small_r], writes=[self.negm_r[h]])
                    yb = ycp[h // 4]
                    ybr = ycr[h // 4]
                    oc0 = (h % 4) * 65
                    pms = {}
                    pts = {}

                    def stA(ch, ps_=ps_, c=c, nb_=nb_, h=h):
                        cs = slice(ch * 512, (ch + 1) * 512)
                        pb, pbr = self.bank()
                        op("pe", lambda e: e.matmul(pb, lhsT=self.qT[ps_, c, qs], rhs=self.kbuf[ps_, cs], start=True, stop=False),
                           reads=[self.qT_r, self.kbuf_rt[ch]], writes=[pbr])
                        op("pe", lambda e: e.matmul(pb, lhsT=self.ident[:], rhs=self.sel[:, cs], start=False, stop=True),
                           reads=[self.ident_r, self.sel_r], writes=[pbr])
                        pm, pmr = self.tmpb()
                        op("act", lambda e: e.activation(out=pm[:], in_=pb, func=AF.Exp, bias=self.small[:, nb_:nb_ + 1], scale=0.125),
                           reads=[pbr, self.negm_r[h]], writes=[pmr])
                        pms[ch] = (pm, pmr)

                    def stB(ch):
                        pm, pmr = pms.pop(ch)
                        tb_, tbr = self.bank()
                        tbv = tb_.bitcast(BF16)
                        for t in range(4):
                            op("pe", lambda e, t=t: e.transpose(tbv[:, t * 128:(t + 1) * 128], pm[:, t * 128:(t + 1) * 128], self.ident[:]),
                               reads=[pmr, self.ident_r], writes=[tbr])
                        pt, ptr = self.ptbuf()
                        op("act", lambda e: e.copy(out=pt.rearrange("p a b -> p (a b)"), in_=tbv[:, 0:512]), reads=[tbr], writes=[ptr, self.rtmp_r])
                        pts[ch] = (pt, ptr)

                    def stC(ch, yb=yb, ybr=ybr, oc0=oc0, half=half):
                        pt, ptr = pts.pop(ch)
                        for t in range(4):
                            kc = ch * 4 + t
                            first = (ch == 0 and t == 0)
                            lastm = (ch == nch - 1 and t == 3)
                            op("pe", lambda e, t=t, kc=kc, first=first, lastm=lastm:
                               e.matmul(yb[:, oc0:oc0 + 65], lhsT=pt[:, t, :], rhs=self.kvb[:, kc * 130 + half * 65: kc * 130 + half * 65 + 65], start=first, stop=lastm),
                               reads=[ptr, self.kvb_rt[ch]], writes=[ybr])

                    for st_ in range(nch + LC):
                        if st_ < nch:
                            stA(st_)
                        if 0 <= st_ - LB < nch:
                            stB(st_ - LB)
                        if 0 <= st_ - LC < nch:
                            stC(st_ - LC)
            for h in range(8):
                yb = ycp[h // 4]
                ybr = ycr[h // 4]
                oc0 = (h % 4) * 65
                op("dve", lambda e, yb=yb, oc0=oc0, h=h: e.reciprocal(out=self.small[:, 44 + h:45 + h], in_=yb[:, oc0 + 64:oc0 + 65]), reads=[ybr], writes=[self.small_r])
                op("dve", lambda e, yb=yb, oc0=oc0, h=h: e.tensor_scalar(out=self.ycs[:, h * 64:(h + 1) * 64], in0=yb[:, oc0:oc0 + 64], scalar1=self.small[:, 44 + h:45 + h],
                                                                        scalar2=None, op0=ALU.mult),
                   reads=[ybr, self.small_r], writes=[self.ycs_r])
            if self.debug and j == 0:
                self.dma(self.dbg_ycs[b], self.ycs[:], reads=[self.ycs_r])
            tb_, tbr = self.bank()
            tbv = tb_.bitcast(BF16)
            for c in range(4):
                op("pe", lambda e, c=c, tbv=tbv: e.transpose(tbv[:, c * 128:(c + 1) * 128], self.ycs[:, c * 128:(c + 1) * 128], self.ident[:]),
                   reads=[self.ycs_r, self.ident_r], writes=[tbr])
            op("act", lambda e, tbv=tbv: e.copy(out=self.yT[:, 4:8, qs], in_=tbv[:, 0:512].rearrange("p (c q) -> p c q", c=4)), reads=[tbr], writes=[self.yT_r])

        for b in range(4):
            _blk(b)

    def xattn(self, l):
        op = self.op
        self.rmsnorm(self.xt, self.xt_r, l * LP + 16, self.xn, self.xn_r, 8, T)
        for blk in range(2):
            slot, sres = self.wpop()
            sv = slot[:, 0:4096].rearrange("p (k n) -> p k n", k=8)
            for cc in range(4):
                oc = blk * 4 + cc
                pb, pbr = self.bank()
                for k in range(8):
                    op("pe", lambda e, k=k, cc=cc, sv=sv, pb=pb: e.matmul(pb, lhsT=sv[:, k, cc * 128:(cc + 1) * 128], rhs=self.xn[:, k, :], start=(k == 0), stop=(k == 7)),
                       reads=[sres, self.xn_r], writes=[pbr])
                op("act", lambda e, pb=pb, oc=oc: e.copy(out=self.yT[:, oc, :], in_=pb), reads=[pbr], writes=[self.yT_r])
        o_tok, o_tok_r = self.ycs, self.ycs_r

        def _blk(b):
            qs = slice(b * 128, (b + 1) * 128)
            for half2 in range(2):
                for hh in range(2):
                    h = half2 * 2 + hh
                    pb, pbr = self.bank()
                    for k in range(2):
                        op("pe", lambda e, k=k, h=h, pb=pb: e.matmul(pb[:, 0:256], lhsT=self.yT[:, 2 * h + k, qs], rhs=self.kmT[:, 2 * h + k, :], start=(k == 0), stop=(k == 1)),
                           reads=[self.yT_r, self.kmT_r], writes=[pbr])
                    op("dve", lambda e, pb=pb: e.tensor_reduce(out=self.small[:, 50:51], in_=pb[:, 0:256], axis=AX.X, op=ALU.max), reads=[pbr], writes=[self.small_r])
                    op("dve", lambda e: e.tensor_scalar(out=self.small[:, 51:52], in0=self.small[:, 50:51], scalar1=-1.0 / 16, scalar2=None, op0=ALU.mult),
                       reads=[self.small_r], writes=[self.small_r])
                    op("dve", lambda e: e.memset(self.small[:, 52:53], 0.0), writes=[self.small_r])
                    ex, exr = self.tmpb()
                    op("act", lambda e, pb=pb, ex=ex: e.activation(out=ex[:, 0:256], in_=pb[:, 0:256], func=AF.Exp, bias=self.small[:, 51:52], scale=1.0 / 16,
                                                                  accum_out=self.small[:, 52:53]),
                       reads=[pbr, self.small_r], writes=[exr, self.small_r])
                    tb_, tbr = self.bank()
                    tbv = tb_.bitcast(BF16)
                    for t in range(2):
                        op("pe", lambda e, t=t, ex=ex, tbv=tbv: e.transpose(tbv[:, t * 128:(t + 1) * 128], ex[:, t * 128:(t + 1) * 128], self.ident[:]),
                           reads=[exr, self.ident_r], writes=[tbr])
                    pt, ptr = self.ptbuf()
                    op("act", lambda e, tbv=tbv, pt=pt: e.copy(out=pt[:, 0:2, :].rearrange("p a b -> p (a b)"), in_=tbv[:, 0:256]), reads=[tbr], writes=[ptr, self.rtmp_r])
                    po, por = self.bank()
                    for t in range(2):
                        op("pe", lambda e, t=t, h=h, po=po, pt=pt: e.matmul(po[:, 0:256], lhsT=pt[:, t, :], rhs=self.vm[:, t, h * 256:(h + 1) * 256], start=(t == 0), stop=(t == 1)),
                           reads=[ptr, self.vm_r], writes=[por])
                    op("dve", lambda e: e.reciprocal(out=self.small[:, 53:54], in_=self.small[:, 52:53]), reads=[self.small_r], writes=[self.small_r])
                    op("dve", lambda e, po=po, hh=hh: e.tensor_scalar(out=o_tok[:, hh * 256:(hh + 1) * 256], in0=po[:, 0:256], scalar1=self.small[:, 53:54], scalar2=None, op0=ALU.mult),
                       reads=[por, self.small_r], writes=[o_tok_r])
                tb_, tbr = self.bank()
                tbv = tb_.bitcast(BF16)
                for c in range(4):
                    op("pe", lambda e, c=c, tbv=tbv: e.transpose(tbv[:, c * 128:(c + 1) * 128], o_tok[:, c * 128:(c + 1) * 128], self.ident[:]),
                       reads=[o_tok_r, self.ident_r], writes=[tbr])
                op("act", lambda e, tbv=tbv, half2=half2: e.copy(out=self.xn[:, half2 * 4:half2 * 4 + 4, qs], in_=tbv[:, 0:512].rearrange("p (c q) -> p c q", c=4)),
                   reads=[tbr], writes=[self.xn_r])
        for b in range(4):
            _blk(b)
        self.linear_res(l, "wo", self.xn, self.xn_r, 8)

    def phaseB(self, l, j):
        self.push_group(l, ["wout", "wq", "wo", "gu2", "dn2"])
        if l == 0 and self.stop != "T0":
            self.push_group(1, ["gu1", "dn1", "win", "wv"])
        self.conv_taps(l, j)
        self.dsa(l, j)
        self.conv_finish(l, j)
        if self.debug and l == 0:
            self.dma(self.dbg_y[j].rearrange("c p n -> p c n"), self.yT[:], reads=[self.yT_r])
        self.linear_res(l, "wout", self.yT, self.yT_r, 8)
        if self.debug and l == 0:
            self.dma(self.dbg_x2[j].rearrange("c p n -> p c n"), self.xt[:], reads=[self.xt_r])
        self.xattn(l)
        if self.debug and l == 0:
            self.dma(self.dbg_x3[j].rearrange("c p n -> p c n"), self.xt[:], reads=[self.xt_r])
        self.ffn(l, 2)
        if self.debug and l == 0:
            self.dma(self.dbg_x4[j].rearrange("c p n -> p c n"), self.xt[:], reads=[self.xt_r])

    def final_out(self, j):
        op = self.op
        pb, pbr = self.bank()
        x = self.xt
        for c in range(8):
            sq, sqr = self.tmpa()
            op("act", lambda e, c=c, sq=sq: e.activation(out=sq[:], in_=x[:, c, :], func=AF.Square), reads=[self.xt_r], writes=[sqr])
            op("pe", lambda e, c=c, sq=sq: e.matmul(pb, lhsT=self.onesf[:], rhs=sq[:], start=(c == 0), stop=(c == 7)), reads=[sqr, self.onesf_r], writes=[pbr])
        sd, sdr = self.tmpa()
        op("act", lambda e: e.activation(out=sd[:], in_=pb, func=AF.Sqrt, bias=self.pcol(PV_EPS6), scale=1.0 / 1024), reads=[pbr, self.pvec_r], writes=[sdr])
        op("dve", lambda e: e.reciprocal(out=self.rstd[:], in_=sd[:]), reads=[sdr], writes=[self.rstd_r])
        for c in range(8):
            op("dve", lambda e, c=c: e.scalar_tensor_tensor(out=x[:, c, :], in0=x[:, c, :], scalar=self.pcol(PV_FIN + c), in1=self.rstd[:], op0=ALU.mult, op1=ALU.mult),
               reads=[self.xt_r, self.rstd_r, self.pvec_r], writes=[self.xt_r])
        n = self.dma(self.outd[j].rearrange("c p n -> p c n"), self.xt[:], reads=[self.xt_r])
        self.P.final.append(n)


def _swap(w):
    K, N = w.shape
    w = w.reshape(K, N // 64, 2, 32)
    return np.ascontiguousarray(w[:, :, ::-1, :]).reshape(K, N)


def _blk(w, kc, ncols, width):
    a = w.reshape(kc, 128, ncols).transpose(1, 0, 2).reshape(128, kc * ncols)
    if a.shape[1] < width:
        a = np.concatenate([a, np.zeros((128, width - a.shape[1]), np.float32)], 1)
    return a


def _layer_blob(p, l):
    blocks = []

    def gu(g, u):
        for i in range(11):
            w = np.concatenate([g[:, 256 * i:256 * (i + 1)], u[:, 256 * i:256 * (i + 1)]], 1)
            blocks.append(_blk(w, 8, 512, 4096))

    def dn(dw):
        for oc in range(8):
            blocks.append(_blk(dw[:, oc * 128:(oc + 1) * 128], 22, 128, 3072))

    def sq(w, nb):
        for i in range(nb):
            blocks.append(_blk(w[:, 512 * i:512 * (i + 1)], 8, 512, 4096))

    gu(p["ffn1_w_gate"][l], p["ffn1_w_up"][l])
    dn(p["ffn1_w_down"][l])
    wi = p["w_in"][l]
    a_val, a_gate, b_gate, c_gate, b_h = (wi[:, 256 * i:256 * (i + 1)] for i in range(5))
    q = wi[:, 1280:1792]
    k = wi[:, 1792:2304]
    v = wi[:, 2304:2816]
    qi = wi[:, 2816:3072]
    ki = wi[:, 3072:3136]
    wx = wi[:, 3136:3140]
    ext = np.concatenate([a_val, a_gate, b_gate, c_gate, b_h, q, _swap(q), k, _swap(k), qi, _swap(qi), ki, ki, _swap(ki), _swap(ki)], 1)
    assert ext.shape[1] == 4096
    sq(ext, 8)
    blocks.append(_blk(np.concatenate([v, wx, np.zeros((1024, 60), np.float32)], 1), 8, 576, 4608))
    sq(p["w_out"][l], 2)
    sq(p["xa_wq"][l], 2)
    sq(p["xa_wkv"][l], 4)
    sq(p["xa_wo"][l], 2)
    gu(p["ffn2_w_gate"][l], p["ffn2_w_up"][l])
    dn(p["ffn2_w_down"][l])
    flat = np.concatenate([b.reshape(-1) for b in blocks])
    assert flat.size == BLOB_ROWS * 512, (flat.size, BLOB_ROWS * 512)
    return flat.reshape(BLOB_ROWS, 512)


def _pvec(p, r):
    pv = np.zeros((128, NPV), np.float32)

    def put(c0, vec):
        n = vec.size // 128
        pv[:, c0:c0 + n] = vec.reshape(n, 128).T

    for l in range(2):
        b = l * LP
        put(b + 0, p["ffn1_norm"][l])
        put(b + 8, p["mix_norm"][l])
        put(b + 16, p["xa_norm"][l])
        put(b + 24, p["mem_norm"][l])
        put(b + 32, p["ffn2_norm"][l])
        dw = p["conf_dw"][l]
        for c in range(2):
            pv[:, b + 40 + c * 31: b + 40 + (c + 1) * 31] = dw[:, c * 128:(c + 1) * 128].T
        put(b + 102, p["conf_dw_b"][l])
        put(b + 104, p["conf_ln_g"][l])
        put(b + 106, p["conf_ln_b"][l])
        sw = p["sc_dw"][l]
        for c in range(2):
            pv[:, b + 108 + c * 3: b + 108 + (c + 1) * 3] = sw[:, c * 128:(c + 1) * 128].T
    put(PV_FIN, p["final_norm"])
    pidx = np.arange(128)
    inv_freq = (10000.0 ** (-np.arange(0, 64, 2, dtype=np.float32) / 64)).astype(np.float32)
    pv[:, PV_INVF] = inv_freq[pidx % 32]
    pv[:, PV_SGN] = np.where((pidx % 64) < 32, -1.0, 1.0)
    pv[:, PV_SEL] = 1.0 if r == 1 else 0.0
    pv[:, PV_SEL + 1] = 1.0 if r == 0 else 0.0
    pv[:, PV_EPS6] = 1e-6
    pv[:, PV_EPS5] = 1e-5
    return pv


def _cmask(r):
    i = np.arange(128)[:, None, None]
    b = np.arange(4)[None, :, None]
    c = np.arange(1024)[None, None, :]
    vis = (c - 512 * r) <= (128 * b + i)
    return np.where(vis, 0.0, NEG).astype(np.float32)


_CACHE = {}


def make_inputs(p):
    p = {k: np.asarray(v) for k, v in p.items()}
    blobs = [_layer_blob(p, l) for l in range(2)]
    ident = np.eye(128, dtype=np.float32)
    in_maps = []
    for c in range(8):
        b, r = c // 2, c % 2
        xb = p["x"][b].reshape(16, 512, 8, 128)[r::2]
        xin = np.ascontiguousarray(xb.transpose(0, 2, 3, 1))
        pos = np.ascontiguousarray(p["positions"][b].reshape(16, 512)[r::2].reshape(-1)).astype(np.int32)
        memT = np.ascontiguousarray(p["mem"][b].reshape(256, 8, 128).transpose(1, 2, 0))
        m = {"xin": xin, "pos": pos, "memT": memT, "cmask": _cmask(r), "pvec": _pvec(p, r), "ident": ident,
             "wsl0": blobs[0], "wsl1": blobs[1]}
        in_maps.append(m)
    return in_maps


def assemble(res):
    out = np.zeros((4, 16, 512, 8, 128), np.float32)
    for c in range(8):
        b, r = c // 2, c % 2
        o = res.results[c]["out"]
        out[b, r::2] = o.transpose(0, 3, 1, 2)
    return out.reshape(4, 8192, 1024)


def kernel(**inputs):
    in_maps = make_inputs(inputs)
    if "nc" not in _CACHE:
        _CACHE["nc"] = KB().build()
    res = run_bass_kernel_spmd(_CACHE["nc"], in_maps, core_ids=list(range(8)))
    return assemble(res)
```

```python
import contextlib
import numpy as np
import concourse.bass as bass
import concourse.mybir as mybir
from concourse.bass_utils import run_bass_kernel_spmd

ALU = mybir.AluOpType
AF = mybir.ActivationFunctionType
AX = mybir.AxisListType
F32 = mybir.dt.float32
BF16 = mybir.dt.bfloat16
I32 = mybir.dt.int32

D = 1024
DFF = 2816
T = 512
NT = 8
TOK = T * NT
NITER = 13
IDX_SCALE = 0.5 * 0.125
NEG = -1.0e30
NDSEM = 24
LP = 114
PV_FIN = 228
PV_INVF = 236
PV_SGN = 237
PV_SEL = 238
PV_EPS6 = 240
PV_EPS5 = 241
NPV = 242
SLOTW = 4608
NSLOT = 4
LA = 3
TWO_PI = 6.283185307179586
TR = 1192

BLK = [("gu1", 11, 4096), ("dn1", 8, 3072), ("win", 8, 4096), ("wv", 1, 4608), ("wout", 2, 4096),
       ("wq", 2, 4096), ("wkv", 4, 4096), ("wo", 2, 4096), ("gu2", 11, 4096), ("dn2", 8, 3072)]
BLK_OFF = {}
_r = 0
for _n, _c, _w in BLK:
    BLK_OFF[_n] = (_r, _w)
    _r += _c * (_w // 4)
BLOB_ROWS = _r


class Res:
    __slots__ = ("name", "lw", "rd", "rd_dma")

    def __init__(self, name=""):
        self.name = name
        self.lw = None
        self.rd = {}
        self.rd_dma = []


class Node:
    __slots__ = ("eng", "fn", "deps", "sig", "sigidx", "dma", "dsem", "dcnt", "inc")

    def __init__(self, eng, fn, dma):
        self.eng = eng
        self.fn = fn
        self.dma = dma
        self.deps = []
        self.sig = False
        self.sigidx = 0
        self.dsem = None
        self.dcnt = 0
        self.inc = 16


class Prog:
    def __init__(self, nc):
        self.nc = nc
        self.q = {k: [] for k in ("pe", "act", "dve", "pool", "sp")}
        self.dq = {k: {"n": 0, "last": [None] * NDSEM, "cnt": [0] * NDSEM} for k in ("sp", "pool", "act")}
        self.final = []

    def op(self, eng, fn, reads=(), writes=(), dma=False, inc=16):
        n = Node(eng, fn, dma)
        n.inc = inc
        deps = []
        for r in reads:
            if r.lw is not None:
                deps.append(r.lw)
        for w in writes:
            if w.lw is not None and (dma or w.lw.dma or w.lw.eng != eng or eng != "pe"):
                deps.append(w.lw)
            for e, rn in w.rd.items():
                if dma or e != eng or eng != "pe":
                    deps.append(rn)
            deps.extend(w.rd_dma)
        if dma:
            d = self.dq[eng]
            k = d["n"] % NDSEM
            d["n"] += 1
            if d["last"][k] is not None:
                deps.append(d["last"][k])
            d["cnt"][k] += inc
            n.dsem = (eng, k)
            n.dcnt = d["cnt"][k]
            d["last"][k] = n
        seen = set()
        for x in deps:
            if id(x) not in seen and x is not n:
                seen.add(id(x))
                n.deps.append(x)
                if not x.dma:
                    x.sig = True
        for r in reads:
            if dma:
                r.rd_dma.append(n)
            else:
                r.rd[eng] = n
        for w in writes:
            w.lw = n
            w.rd = {}
            w.rd_dma = []
        self.q[eng].append(n)
        return n

    def emit(self):
        nc = self.nc
        engobj = {"pe": nc.tensor, "act": nc.scalar, "dve": nc.vector, "pool": nc.gpsimd, "sp": nc.sync}
        fin = Node("sp", None, False)
        fin.deps = list(self.final)
        for x in fin.deps:
            if not x.dma:
                x.sig = True
        self.q["sp"].append(fin)
        for e in self.q:
            c = 0
            for n in self.q[e]:
                if n.dma:
                    continue
                if n.sig:
                    c += 1
                    n.sigidx = c
        with contextlib.ExitStack() as st:
            esem = {e: st.enter_context(nc.semaphore("S_" + e)) for e in self.q}
            dsem = {}
            for qn in self.dq:
                for k in range(NDSEM):
                    dsem[(qn, k)] = st.enter_context(nc.semaphore("D_%s%d" % (qn, k)))
            block = st.enter_context(nc.Block())

            def run(e):
                eo = engobj[e]
                waited = {}
                for n in self.q[e]:
                    for d in n.deps:
                        if d.dma:
                            key = ("d",) + d.dsem
                            sem = dsem[d.dsem]
                            val = d.dcnt
                        else:
                            key = ("e", d.eng)
                            sem = esem[d.eng]
                            val = d.sigidx
                        if waited.get(key, 0) < val:
                            eo.wait_ge(sem, val)
                            waited[key] = val
                    if n.fn is None:
                        continue
                    ins = n.fn(eo)
                    if n.dma:
                        ins.then_inc(dsem[n.dsem], n.inc)
                    elif n.sig:
                        ins.then_inc(esem[e], 1)

            @block.tensor
            def _(t):
                run("pe")

            @block.scalar
            def _(t):
                run("act")

            @block.vector
            def _(t):
                run("dve")

            @block.gpsimd
            def _(t):
                run("pool")

            @block.sync
            def _(t):
                run("sp")


class KB:
    def __init__(self, debug=False, stop=None):
        self.debug = debug
        self.stop = stop
        self.nc = bass.Bass("TRN2", target_bir_lowering=False)
        self.P = Prog(self.nc)
        self.st = contextlib.ExitStack()
        self.dbg_outs = []

    def dram(self, name, shape, dt, kind="Internal"):
        if self.debug and kind == "Internal" and name.startswith("dbg_"):
            kind = "ExternalOutput"
            self.dbg_outs.append(name)
        return self.nc.dram_tensor(name, shape, dt, kind=kind).ap()

    def sb(self, name, shape, dt):
        t = self.st.enter_context(self.nc.sbuf_tensor(name, shape, dt))
        return t, Res(name)

    def op(self, *a, **k):
        return self.P.op(*a, **k)

    def dma(self, out, in_, reads=(), writes=(), q="sp"):
        n = self.P.op(q, lambda e: e.dma_start(out=out, in_=in_), reads=reads, writes=writes, dma=True)
        if self.debug:
            self.P.final.append(n)
        return n

    def bank(self):
        i = self.bank_i % 6
        self.bank_i += 1
        return self.banks[i][:], self.bank_res[i]

    def wpush(self, l, name, i):
        r0, w = BLK_OFF[name]
        rows = w // 4
        a = r0 + i * rows
        ap = self.blob[l][a:a + rows, :].rearrange("(p m) c -> p (m c)", p=128)
        self.wfifo.append((ap, w, self.blk_res[(l, name, i)]))

    def wkick(self):
        while self.w_issued < min(len(self.wfifo), self.w_popped + LA):
            ap, w, bres = self.wfifo[self.w_issued]
            s = self.w_issued % NSLOT
            slot = self.wslots[s]
            self.dma(slot[:, 0:w], ap, reads=[bres], writes=[self.wslot_res[s]])
            self.w_issued += 1

    def push_group(self, l, names):
        for name in names:
            cnt = [c for n, c, w in BLK if n == name][0]
            for i in range(cnt):
                self.wpush(l, name, i)
        self.wkick()

    def wpop(self):
        while self.w_issued < min(len(self.wfifo), self.w_popped + LA):
            ap, w, bres = self.wfifo[self.w_issued]
            s = self.w_issued % NSLOT
            slot = self.wslots[s]
            self.dma(slot[:, 0:w], ap, reads=[bres], writes=[self.wslot_res[s]])
            self.w_issued += 1
        s = self.w_popped % NSLOT
        self.w_popped += 1
        return self.wslots[s], self.wslot_res[s]

    def build(self):
        nc = self.nc
        P = self.P
        self.xin = self.dram("xin", [NT, 8, 128, T], F32, kind="ExternalInput")
        self.posd = self.dram("pos", [TOK], I32, kind="ExternalInput")
        self.memT = self.dram("memT", [8, 128, 256], F32, kind="ExternalInput")
        self.cmaskd = self.dram("cmask", [128, 4, 1024], F32, kind="ExternalInput")
        self.pvecd = self.dram("pvec", [128, NPV], F32, kind="ExternalInput")
        self.identd = self.dram("ident", [128, 128], F32, kind="ExternalInput")
        self.wsl = [self.dram("wsl%d" % l, [BLOB_ROWS, 512], F32, kind="ExternalInput") for l in range(2)]
        self.outd = self.dram("out", [NT, 8, 128, T], F32, kind="ExternalOutput")
        self.blob = [self.dram("blob%d" % l, [BLOB_ROWS, 512], BF16) for l in range(2)]
        self.blk_res = {}
        self.xTd = self.dram("dbg_xT", [NT, 8, 128, T], F32)
        self.xT_res = [Res("xT%d" % j) for j in range(NT)]
        self.hAd = self.dram("dbg_hA", [2, 128, TOK], BF16)
        self.cghd = self.dram("dbg_cgh", [2, 128, TOK], BF16)
        self.bgd = self.dram("dbg_bg", [2, 128, TOK], BF16)
        self.qd = self.dram("dbg_q", [4, 128, TOK], BF16)
        self.qid = self.dram("dbg_qi", [2, 128, TOK], BF16)
        self.widxd = self.dram("dbg_widx", [TOK, 4], F32)
        self.loc_res = [Res("loc%d" % j) for j in range(NT)]
        self.send = [[self.dram("send%d_%d" % (l, j), [TR, 512], BF16) for j in range(NT)] for l in range(2)]
        self.send_res = [[Res("send") for j in range(NT)] for l in range(2)]
        self.ag = [[self.dram("agbuf%d_%d" % (l, j), [2 * TR, 512], BF16) for j in range(NT)] for l in range(2)]
        self.ag_res = [[Res("ag") for j in range(NT)] for l in range(2)]

        if self.debug:
            self.dbg_y = self.dram("dbg_y", [NT, 8, 128, T], BF16)
            self.dbg_x2 = self.dram("dbg_x2", [NT, 8, 128, T], F32)
            self.dbg_x3 = self.dram("dbg_x3", [NT, 8, 128, T], F32)
            self.dbg_x4 = self.dram("dbg_x4", [NT, 8, 128, T], F32)
            self.dbg_sc = self.dram("dbg_sc", [4, 128, 1024], F32)
            self.dbg_sel = self.dram("dbg_sel", [4, 128, 1024], BF16)
            self.dbg_bis = self.dram("dbg_bis", [4, 128, 64], F32)
            self.dbg_ycs = self.dram("dbg_ycs", [4, 128, 512], BF16)
        sb = self.sb
        self.wslots = []
        self.wslot_res = []
        for s in range(NSLOT):
            t, r = sb("wslot%d" % s, [128, SLOTW], BF16)
            self.wslots.append(t)
            self.wslot_res.append(r)
        self.wfifo = []
        self.w_issued = 0
        self.w_popped = 0
        self.xt, self.xt_r = sb("xt", [128, 8, T], F32)
        self.xn, self.xn_r = sb("xn", [128, 8, T], BF16)
        self.big, self.big_r = sb("big", [128, 8192], F32)
        self.sel, self.sel_r = sb("sel", [128, 8192], BF16)
        self.kbuf, self.kbuf_r = sb("kbuf", [128, 8192], BF16)
        self.kvb, self.kvb_r = sb("kvb", [128, 8320], BF16)
        self.cmask, self.cmask_r = sb("cmaskb", [128, 4, 1024], BF16)
        self.pvec, self.pvec_r = sb("pvecs", [128, NPV], F32)
        self.identf, self.identf_r = sb("identf", [128, 128], F32)
        self.ident, self.ident_r = sb("identb", [128, 128], BF16)
        self.onesf, self.onesf_r = sb("onesf", [128, 128], F32)
        self.cosT, self.cos_r = sb("cosT", [128, T], F32)
        self.sinT, self.sin_r = sb("sinT", [128, T], F32)
        self.tmpA = [sb("tmpA%d" % i, [128, T], F32) for i in range(4)]
        self.tmpB = [sb("tmpB%d" % i, [128, T], BF16) for i in range(4)]
        cb = self.cosT[:].bitcast(BF16)
        sbv = self.sinT[:].bitcast(BF16)
        self.tmpB += [(cb[:, 0:T], self.cos_r), (sbv[:, 0:T], self.sin_r), (cb[:, T:2 * T], self.cos_r), (sbv[:, T:2 * T], self.sin_r)]
        self.rtmp, self.rtmp_r = sb("rtmp", [128, 4, T], F32)
        rb = self.rtmp[:].rearrange("p a n -> p (a n)").bitcast(BF16)
        self.pTs = [(rb[:, i * 512:(i + 1) * 512].rearrange("p (a b) -> p a b", a=4), Res("pT%d" % i)) for i in range(8)]
        self.pT_i = 0
        self.rtmp_w = [self.rtmp_r] + [r for _, r in self.pTs]
        self.kbuf_rt = [Res("kbuf%d" % i) for i in range(16)]
        self.negm_r = [Res("negm%d" % i) for i in range(8)]
        self.kvb_rt = [Res("kvb%d" % i) for i in range(16)]
        self.yT, self.yT_r = sb("yT", [128, 8, T], BF16)
        self.qT, self.qT_r = sb("qT", [128, 4, T], BF16)
        self.qiT, self.qiT_r = sb("qiT", [128, 2, T], BF16)
        self.vt, self.vt_r = sb("vt", [128, 4, 520], BF16)
        self.wsc, self.wsc_r = sb("wsc", [128, 4, 4], F32)
        self.wab, _ = sb("wab", [128, 4, 4], F32)
        self.wsg, _ = sb("wsg", [128, 4, 4], F32)
        self.dg, self.dg_r = sb("dg", [128, 4, 128], BF16)
        self.pw2, self.pw2_r = sb("pw2", [128, 16], F32)
        self.small, self.small_r = sb("small", [128, 64], F32)
        self.bis, self.bis_r = sb("bis", [128, 64], F32)
        self.rstd, self.rstd_r = sb("rstd", [128, T], F32)
        self.ycs, self.ycs_r = sb("ycs", [128, 512], BF16)
        self.kmT, self.kmT_r = sb("kmT", [128, 8, 256], BF16)
        self.vm, self.vm_r = sb("vm", [128, 2, 1024], BF16)
        self.memx, self.memx_r = self.rtmp[:].rearrange("p a (b n) -> p (a b) n", b=2), self.rtmp_r
        self.memn, self.memn_r = self.qT[:].rearrange("p a (b n) -> p (a b) n", b=2), self.qT_r
        self.halo, self.halo_r = sb("halo", [128, 2, 2, 32], BF16)
        self.cin, self.cin_r = sb("cin", [128, 2, 32 + T], BF16)
        self.cacc, self.cacc_r = self.xn[:].rearrange("p a n -> p (a n)").bitcast(F32)[:, 0:2 * T].rearrange("p (c n) -> p c n", c=2), self.xn_r
        self.cacc2 = self.xn[:].rearrange("p a n -> p (a n)").bitcast(F32)[:, 2 * T:4 * T].rearrange("p (c n) -> p c n", c=2)
        self.cin2 = self.vt[:].rearrange("p a f -> p (a f)")[:, 0:2 * (32 + T)].rearrange("p (c n) -> p c n", c=2)
        self.ptmp, self.ptmp_r = self.tmpA[3]
        self.act = self.big[:].bitcast(BF16)
        self.banks = []
        self.bank_res = []
        for i in range(8):
            t = self.st.enter_context(nc.psum_tensor("bank%d" % i, [128, 512], F32))
            self.banks.append(t)
            self.bank_res.append(Res("bank%d" % i))
        self.bank_i = 0
        self.tmp_i = 0
        self.tmpb_i = 0

        self.dma(self.pvec[:], self.pvecd, writes=[self.pvec_r])
        self.dma(self.identf[:], self.identd, writes=[self.identf_r])
        self.op("dve", lambda e: e.tensor_copy(out=self.ident[:], in_=self.identf[:]), reads=[self.identf_r], writes=[self.ident_r])
        self.op("dve", lambda e: e.memset(self.onesf[:], 1.0), writes=[self.onesf_r])
        for i in range(16):
            self.op("dve", lambda e, i=i: e.memset(self.pw2[:, i:i + 1], 0.5 ** (i + 1)), writes=[self.pw2_r])
        self.op("dve", lambda e: e.memset(self.vt[:], 1.0), writes=[self.vt_r])
        self.dma(self.cmask[:], self.cmaskd, writes=[self.cmask_r], q="pool")
        for l in range(2):
            for name, cnt, w in BLK:
                r0, _ = BLK_OFF[name]
                rows = w // 4
                for i in range(cnt):
                    r = Res("blk")
                    self.blk_res[(l, name, i)] = r
                    a = r0 + i * rows
                    self.dma(self.blob[l][a:a + rows, :], self.wsl[l][a:a + rows, :], writes=[r], q="pool")
        if self.stop == "T0":
            self.load_x(self.xin, 0, None)
            self.phaseA(0, 0)
            self.exchange(0, 0)
            self.mem_kv(0)
            self.load_x(self.xTd, 0, self.xT_res[0])
            self.phaseB(0, 0)
            return self.finish()
        for j in range(NT):
            self.load_x(self.xin, j, None)
            self.phaseA(0, j)
            self.exchange(0, j)
        if self.stop == "A0":
            return self.finish()
        for l in range(2):
            self.mem_kv(l)
            for j in range(NT):
                self.load_x(self.xTd, j, self.xT_res[j])
                self.phaseB(l, j)
                if l == 0:
                    self.phaseA(1, j, push=False)
                    self.exchange(1, j)
                else:
                    self.final_out(j)
            if l == 0 and self.stop == "B0":
                return self.finish()
        return self.finish()

    def finish(self):
        self.P.emit()
        self.st.close()
        return self.nc

    def pcol(self, c):
        return self.pvec[:, c:c + 1]

    def load_x(self, src, j, res):
        self.dma(self.xt[:], src[j].rearrange("c p n -> p c n"), reads=[res] if res else [], writes=[self.xt_r])

    def tmpa(self):
        i = self.tmp_i % 3
        self.tmp_i += 1
        return self.tmpA[i][0], self.tmpA[i][1]

    def tmpb(self):
        i = self.tmpb_i % 8
        self.tmpb_i += 1
        return self.tmpB[i][0], self.tmpB[i][1]

    def ptbuf(self):
        i = self.pT_i % 8
        self.pT_i += 1
        return self.pTs[i]

    def rmsnorm(self, x, x_r, gc0, out, out_r, nch, N, eps_col=PV_EPS6):
        pb, pbr = self.bank()
        for c in range(nch):
            sq, sqr = self.tmpa()
            self.op("act", lambda e, c=c, sq=sq: e.activation(out=sq[:, 0:N], in_=x[:, c, :], func=AF.Square), reads=[x_r], writes=[sqr])
            self.op("pe", lambda e, c=c, sq=sq: e.matmul(pb[:, 0:N], lhsT=self.onesf[:], rhs=sq[:, 0:N], start=(c == 0), stop=(c == nch - 1)),
                    reads=[sqr, self.onesf_r], writes=[pbr])
        sd, sdr = self.tmpa()
        self.op("act", lambda e: e.activation(out=sd[:, 0:N], in_=pb[:, 0:N], func=AF.Sqrt, bias=self.pcol(eps_col), scale=1.0 / (nch * 128)),
                reads=[pbr, self.pvec_r], writes=[sdr])
        self.op("dve", lambda e: e.reciprocal(out=self.rstd[:, 0:N], in_=sd[:, 0:N]), reads=[sdr], writes=[self.rstd_r])
        for c in range(nch):
            self.op("dve", lambda e, c=c: e.scalar_tensor_tensor(out=out[:, c, :], in0=x[:, c, :], scalar=self.pcol(gc0 + c), in1=self.rstd[:, 0:N],
                                                                 op0=ALU.mult, op1=ALU.mult),
                    reads=[x_r, self.rstd_r, self.pvec_r], writes=[out_r])

    def ffn(self, l, which):
        self.rmsnorm(self.xt, self.xt_r, l * LP + (0 if which == 1 else 32), self.xn, self.xn_r, 8, T)
        act = self.act
        for i in range(11):
            slot, sres = self.wpop()
            sv = slot[:, 0:4096].rearrange("p (k n) -> p k n", k=8)
            for cc in range(2):
                pg, pgr = self.bank()
                pu, pur = self.bank()
                for k in range(8):
                    self.op("pe", lambda e, k=k, cc=cc, sv=sv, pg=pg: e.matmul(pg, lhsT=sv[:, k, cc * 128:(cc + 1) * 128], rhs=self.xn[:, k, :],
                                                                          start=(k == 0), stop=(k == 7)),
                            reads=[sres, self.xn_r], writes=[pgr])
                for k in range(8):
                    self.op("pe", lambda e, k=k, cc=cc, sv=sv, pu=pu: e.matmul(pu, lhsT=sv[:, k, 256 + cc * 128:256 + (cc + 1) * 128], rhs=self.xn[:, k, :],
                                                                          start=(k == 0), stop=(k == 7)),
                            reads=[sres, self.xn_r], writes=[pur])
                sg, sgr = self.tmpa()
                self.op("act", lambda e, sg=sg, pg=pg: e.activation(out=sg[:], in_=pg, func=AF.Silu), reads=[pgr], writes=[sgr])
                ch = 2 * i + cc
                self.op("dve", lambda e, sg=sg, pu=pu, ch=ch: e.tensor_tensor(out=act[:, ch * T:(ch + 1) * T], in0=sg[:], in1=pu, op=ALU.mult),
                        reads=[sgr, pur], writes=[self.big_r])
        for oc in range(8):
            slot, sres = self.wpop()
            sv = slot[:, 0:2816].rearrange("p (k n) -> p k n", k=22)
            po, por = self.bank()
            for k in range(22):
                self.op("pe", lambda e, k=k, sv=sv, po=po: e.matmul(po, lhsT=sv[:, k, :], rhs=act[:, k * T:(k + 1) * T], start=(k == 0), stop=(k == 21)),
                        reads=[sres, self.big_r], writes=[por])
            self.op("dve", lambda e, oc=oc, po=po: e.scalar_tensor_tensor(out=self.xt[:, oc, :], in0=po, scalar=0.5, in1=self.xt[:, oc, :],
                                                                         op0=ALU.mult, op1=ALU.add),
                    reads=[por, self.xt_r], writes=[self.xt_r])

    def rope_tables(self, j):
        e_ = self
        pi_, pi_r = self.tmpa()
        posi = pi_[:].bitcast(I32)
        self.dma(posi, self.posd[j * T:(j + 1) * T].partition_broadcast(128), writes=[pi_r])
        a, ar = self.tmpa()
        k, kr = self.tmpa()
        ki, kir = self.tmpa()
        kiv = ki[:].bitcast(I32)
        op = self.op
        op("dve", lambda e: e.tensor_copy(out=a[:], in_=posi), reads=[pi_r], writes=[ar])
        op("dve", lambda e: e.tensor_scalar(out=a[:], in0=a[:], scalar1=self.pcol(PV_INVF), scalar2=None, op0=ALU.mult), reads=[ar, self.pvec_r], writes=[ar])
        op("dve", lambda e: e.tensor_scalar(out=k[:], in0=a[:], scalar1=1.0 / TWO_PI, scalar2=None, op0=ALU.mult), reads=[ar], writes=[kr])
        op("dve", lambda e: e.tensor_copy(out=kiv, in_=k[:]), reads=[kr], writes=[kir])
        op("dve", lambda e: e.tensor_copy(out=k[:], in_=kiv), reads=[kir], writes=[kr])
        op("dve", lambda e: e.scalar_tensor_tensor(out=a[:], in0=k[:], scalar=-TWO_PI, in1=a[:], op0=ALU.mult, op1=ALU.add), reads=[kr, ar], writes=[ar])

        def fold(t, tr):
            op("dve", lambda e: e.tensor_scalar(out=k[:], in0=t[:], scalar1=np.pi, scalar2=-TWO_PI, op0=ALU.is_gt, op1=ALU.mult), reads=[tr], writes=[kr])
            op("dve", lambda e: e.tensor_tensor(out=t[:], in0=t[:], in1=k[:], op=ALU.add), reads=[tr, kr], writes=[tr])
            op("dve", lambda e: e.tensor_scalar(out=k[:], in0=t[:], scalar1=-np.pi, scalar2=TWO_PI, op0=ALU.is_lt, op1=ALU.mult), reads=[tr], writes=[kr])
            op("dve", lambda e: e.tensor_tensor(out=t[:], in0=t[:], in1=k[:], op=ALU.add), reads=[tr, kr], writes=[tr])
        fold(a, ar)
        op("act", lambda e: e.activation(out=self.sinT[:], in_=a[:], func=AF.Sin), reads=[ar], writes=[self.sin_r])
        op("dve", lambda e: e.tensor_scalar(out=self.sinT[:], in0=self.sinT[:], scalar1=self.pcol(PV_SGN), scalar2=None, op0=ALU.mult),
           reads=[self.sin_r, self.pvec_r], writes=[self.sin_r])
        op("dve", lambda e: e.tensor_scalar(out=a[:], in0=a[:], scalar1=0.5 * np.pi, scalar2=None, op0=ALU.add), reads=[ar], writes=[ar])
        fold(a, ar)
        op("act", lambda e: e.activation(out=self.cosT[:], in_=a[:], func=AF.Sin), reads=[ar], writes=[self.cos_r])

    def phaseA(self, l, j, push=True):
        op = self.op
        if push:
            self.push_group(l, ["gu1", "dn1", "win", "wv"])
        self.ffn(l, 1)
        self.dma(self.xTd[j].rearrange("c p n -> p c n"), self.xt[:], reads=[self.xt_r], writes=[self.xT_res[j]])
        self.rmsnorm(self.xt, self.xt_r, l * LP + 8, self.xn, self.xn_r, 8, T)
        self.rope_tables(j)
        hn = self.xn
        yT = self.yT
        loc = self.loc_res[j]
        cols = slice(j * T, (j + 1) * T)
        held = {}
        sendv = self.send[l][j]
        sres_ = self.send_res[l][j]
        kview = sendv[0:512, :].rearrange("(c p) n -> p c n", c=4)
        kiview = sendv[512:640, :]
        hview = [sendv[1160 + 16 * i:1160 + 16 * (i + 1), :].rearrange("(c q) (h t) -> (q h) c t", c=2, q=8, h=16, t=32) for i in range(2)]
        for blk in range(8):
            slot, sres = self.wpop()
            sv = slot[:, 0:4096].rearrange("p (k n) -> p k n", k=8)
            for cc in range(4):
                ch = blk * 4 + cc
                pb, pbr = self.bank()
                for k in range(8):
                    op("pe", lambda e, k=k, cc=cc, sv=sv, pb=pb: e.matmul(pb, lhsT=sv[:, k, cc * 128:(cc + 1) * 128], rhs=hn[:, k, :], start=(k == 0), stop=(k == 7)),
                       reads=[sres, self.xn_r], writes=[pbr])
                if ch in (0, 1, 6, 7):
                    held[ch] = (pb, pbr)
                elif ch in (2, 3):
                    c = ch - 2
                    sg, sgr = self.tmpa()
                    op("act", lambda e, sg=sg, pb=pb: e.activation(out=sg[:], in_=pb, func=AF.Sigmoid), reads=[pbr], writes=[sgr])
                    pv, pvr = held[c]
                    op("dve", lambda e, sg=sg, pv=pv, c=c: e.tensor_tensor(out=yT[:, c, :], in0=sg[:], in1=pv, op=ALU.mult), reads=[sgr, pvr], writes=[self.yT_r])
                    if c == 1:
                        self.dma(self.hAd[:, :, cols].rearrange("c p n -> p c n"), yT[:, 0:2, :], reads=[self.yT_r], writes=[loc])
                        self.dma(hview[0], yT[:, 0:2, T - 32:T], reads=[self.yT_r], writes=[sres_])
                elif ch in (4, 5):
                    c = ch - 4
                    op("act", lambda e, pb=pb, c=c: e.copy(out=yT[:, 2 + c, :], in_=pb), reads=[pbr], writes=[self.yT_r])
                    if c == 1:
                        self.dma(self.bgd[:, :, cols].rearrange("c p n -> p c n"), yT[:, 2:4, :], reads=[self.yT_r], writes=[loc])
                elif ch in (8, 9):
                    c = ch - 8
                    sg, sgr = self.tmpa()
                    pv, pvr = held[6 + c]
                    op("act", lambda e, sg=sg, pv=pv: e.copy(out=sg[:], in_=pv), reads=[pvr], writes=[sgr])
                    op("dve", lambda e, sg=sg, pb=pb, c=c: e.tensor_tensor(out=yT[:, 4 + c, :], in0=sg[:], in1=pb, op=ALU.mult), reads=[sgr, pbr], writes=[self.yT_r])
                    if c == 1:
                        self.dma(self.cghd[:, :, cols].rearrange("c p n -> p c n"), yT[:, 4:6, :], reads=[self.yT_r], writes=[loc])
                        self.dma(hview[1], yT[:, 4:6, T - 32:T], reads=[self.yT_r], writes=[sres_])
                else:
                    for (c0, n, name) in ((10, 4, "q"), (18, 4, "k"), (26, 2, "qi"), (30, 1, "ki")):
                        if c0 <= ch < c0 + n:
                            c = ch - c0
                            op("dve", lambda e, pb=pb, c=c: e.tensor_tensor(out=self.rtmp[:, c, :], in0=pb, in1=self.cosT[:], op=ALU.mult),
                               reads=[pbr, self.cos_r], writes=self.rtmp_w)
                        elif c0 + n <= ch < c0 + 2 * n:
                            c = ch - c0 - n
                            t2, t2r = self.tmpa()
                            op("dve", lambda e, pb=pb, t2=t2: e.tensor_tensor(out=t2[:], in0=pb, in1=self.sinT[:], op=ALU.mult),
                               reads=[pbr, self.sin_r], writes=[t2r])
                            op("pool", lambda e, t2=t2, c=c: e.tensor_tensor(out=yT[:, c, :], in0=t2[:], in1=self.rtmp[:, c, :], op=ALU.add),
                               reads=[t2r, self.rtmp_r], writes=[self.yT_r])
                            if c == n - 1:
                                if name == "q":
                                    self.dma(self.qd[:, :, cols].rearrange("c p n -> p c n"), yT[:, 0:4, :], reads=[self.yT_r], writes=[loc])
                                elif name == "k":
                                    self.dma(kview, yT[:, 0:4, :], reads=[self.yT_r], writes=[sres_])
                                elif name == "qi":
                                    self.dma(self.qid[:, :, cols].rearrange("c p n -> p c n"), yT[:, 0:2, :], reads=[self.yT_r], writes=[loc])
                                else:
                                    self.dma(kiview, yT[:, 0, :], reads=[self.yT_r], writes=[sres_])
        op("dve", lambda e: e.memset(self.vt[:].rearrange("p a (h f) -> p a h f", f=65)[:, :, :, 64:65], 1.0), writes=[self.vt_r])
        slot, sres = self.wpop()
        sv = slot[:, 0:4608].rearrange("p (k n) -> p k n", k=8)
        vview = sendv[640:1160, :].rearrange("r c -> (r c)").rearrange("(t f) -> t f", f=520)
        for tb in range(4):
            pv, pvr = self.bank()
            pw, pwr = self.bank()
            for k in range(8):
                op("pe", lambda e, k=k, tb=tb, sv=sv, pv=pv: e.matmul(pv, lhsT=hn[:, k, tb * 128:(tb + 1) * 128], rhs=sv[:, k, 0:512], start=(k == 0), stop=(k == 7)),
                   reads=[sres, self.xn_r], writes=[pvr])
            for k in range(8):
                op("pe", lambda e, k=k, tb=tb, sv=sv, pw=pw: e.matmul(pw[:, 0:4], lhsT=hn[:, k, tb * 128:(tb + 1) * 128], rhs=sv[:, k, 512:516], start=(k == 0), stop=(k == 7)),
                   reads=[sres, self.xn_r], writes=[pwr])
            op("act", lambda e, tb=tb, pv=pv: e.copy(out=self.vt[:, tb, :].rearrange("p (h f) -> p h f", f=65)[:, :, 0:64], in_=pv.rearrange("p (h f) -> p h f", f=64)),
               reads=[pvr], writes=[self.vt_r])
            op("dve", lambda e, tb=tb, pw=pw: e.tensor_copy(out=self.wsc[:, tb, :], in_=pw[:, 0:4]), reads=[pwr], writes=[self.wsc_r])
        self.dma(vview.rearrange("(n p) f -> p n f", p=128), self.vt[:], reads=[self.vt_r], writes=[sres_])
        self.dma(self.widxd[j * T:(j + 1) * T, :].rearrange("(n p) f -> p n f", p=128), self.wsc[:], reads=[self.wsc_r], writes=[loc])

    def exchange(self, l, j):
        self.op("pool", lambda e: e.collective_compute("AllGather", ALU.bypass, replica_groups=[[0, 1], [2, 3], [4, 5], [6, 7]],
                                                       ins=[self.send[l][j]], outs=[self.ag[l][j]]),
                reads=[self.send_res[l][j]], writes=[self.ag_res[l][j]], dma=True, inc=1)

    def mem_kv(self, l):
        op = self.op
        self.push_group(l, ["wkv"])
        self.dma(self.memx, self.memT.rearrange("c p n -> p c n"), writes=self.rtmp_w)
        self.rmsnorm(self.memx, self.memx_r, l * LP + 24, self.memn, self.memn_r, 8, 256)
        for blk in range(2):
            slot, sres = self.wpop()
            sv = slot[:, 0:4096].rearrange("p (k n) -> p k n", k=8)
            for cc in range(4):
                pb, pbr = self.bank()
                for k in range(8):
                    op("pe", lambda e, k=k, cc=cc, sv=sv, pb=pb: e.matmul(pb[:, 0:256], lhsT=sv[:, k, cc * 128:(cc + 1) * 128], rhs=self.memn[:, k, :], start=(k == 0), stop=(k == 7)),
                       reads=[sres, self.memn_r], writes=[pbr])
                op("act", lambda e, pb=pb, ch=blk * 4 + cc: e.copy(out=self.kmT[:, ch, :], in_=pb[:, 0:256]), reads=[pbr], writes=[self.kmT_r])
        for blk in range(2):
            slot, sres = self.wpop()
            sv = slot[:, 0:4096].rearrange("p (k n) -> p k n", k=8)
            for mb in range(2):
                pb, pbr = self.bank()
                for k in range(8):
                    op("pe", lambda e, k=k, mb=mb, sv=sv, pb=pb: e.matmul(pb, lhsT=self.memn[:, k, mb * 128:(mb + 1) * 128], rhs=sv[:, k, :], start=(k == 0), stop=(k == 7)),
                       reads=[sres, self.memn_r], writes=[pbr])
                op("act", lambda e, pb=pb, mb=mb, blk=blk: e.copy(out=self.vm[:, mb, blk * 512:(blk + 1) * 512], in_=pb), reads=[pbr], writes=[self.vm_r])

    def linear_res(self, l, name, inp, inp_r, nk):
        op = self.op
        for blk in range(2):
            slot, sres = self.wpop()
            sv = slot[:, 0:4096].rearrange("p (k n) -> p k n", k=8)
            for cc in range(4):
                oc = blk * 4 + cc
                pb, pbr = self.bank()
                for k in range(nk):
                    op("pe", lambda e, k=k, cc=cc, sv=sv, pb=pb: e.matmul(pb, lhsT=sv[:, k, cc * 128:(cc + 1) * 128], rhs=inp[:, k, :], start=(k == 0), stop=(k == nk - 1)),
                       reads=[sres, inp_r], writes=[pbr])
                op("dve", lambda e, oc=oc, pb=pb: e.tensor_tensor(out=self.xt[:, oc, :], in0=pb, in1=self.xt[:, oc, :], op=ALU.add),
                   reads=[pbr, self.xt_r], writes=[self.xt_r])

    def conv_taps(self, l, j):
        op = self.op
        base = l * LP
        loc = self.loc_res[j]
        cols = slice(j * T, (j + 1) * T)
        for br in range(2):
            src = self.hAd if br == 0 else self.cghd
            cin, cin_r = (self.cin, self.cin_r) if br == 0 else (self.cin2, self.vt_r)
            cacc = self.cacc if br == 0 else self.cacc2
            def hv(jj, rr, br=br):
                a = rr * TR + 1160 + 16 * br
                return self.ag[l][jj][a:a + 16, :].rearrange("(c q) (h t) -> (q h) c t", c=2, q=8, h=16, t=32)
            self.dma(self.halo[:, 0, :, :], hv(j, 0), reads=[self.ag_res[l][j]], writes=[self.halo_r])
            if j > 0:
                self.dma(self.halo[:, 1, :, :], hv(j - 1, 1), reads=[self.ag_res[l][j - 1]], writes=[self.halo_r])
            else:
                op("dve", lambda e: e.memset(self.halo[:, 1, :, :], 0.0), writes=[self.halo_r])
            self.dma(cin[:, :, 32:32 + T], src[:, :, cols].rearrange("c p n -> p c n"), reads=[loc], writes=[cin_r])
            op("dve", lambda e, cin=cin: e.tensor_scalar(out=cin[:, :, 0:32], in0=self.halo[:, 0, :, :], scalar1=self.pcol(PV_SEL), scalar2=None, op0=ALU.mult),
               reads=[self.halo_r, self.pvec_r], writes=[cin_r])
            op("dve", lambda e, cin=cin: e.scalar_tensor_tensor(out=cin[:, :, 0:32], in0=self.halo[:, 1, :, :], scalar=self.pcol(PV_SEL + 1), in1=cin[:, :, 0:32],
                                                                op0=ALU.mult, op1=ALU.add),
               reads=[self.halo_r, self.pvec_r, cin_r], writes=[cin_r])
            W = 31 if br == 0 else 3
            wc0 = base + (40 if br == 0 else 108)
            for c in range(2):
                for tap in range(W):
                    sh = 32 - (W - 1) + tap
                    colw = self.pcol(wc0 + c * W + tap)
                    if tap == 0:
                        op("pool", lambda e, c=c, sh=sh, colw=colw, cin=cin, cacc=cacc: e.tensor_scalar(out=cacc[:, c, :], in0=cin[:, c, sh:sh + T], scalar1=colw, scalar2=None, op0=ALU.mult),
                           reads=[cin_r, self.pvec_r], writes=[self.cacc_r])
                    else:
                        op("pool", lambda e, c=c, sh=sh, colw=colw, cin=cin: e.tensor_scalar(out=self.ptmp[:], in0=cin[:, c, sh:sh + T], scalar1=colw, scalar2=None, op0=ALU.mult),
                           reads=[cin_r, self.pvec_r], writes=[self.ptmp_r])
                        op("pool", lambda e, c=c, cacc=cacc: e.tensor_tensor(out=cacc[:, c, :], in0=cacc[:, c, :], in1=self.ptmp[:], op=ALU.add),
                           reads=[self.ptmp_r, self.cacc_r], writes=[self.cacc_r])

    def conv_finish(self, l, j):
        op = self.op
        base = l * LP
        loc = self.loc_res[j]
        cols = slice(j * T, (j + 1) * T)
        for c in range(2):
            op("dve", lambda e, c=c: e.tensor_scalar(out=self.cacc[:, c, :], in0=self.cacc[:, c, :], scalar1=self.pcol(base + 102 + c), scalar2=None, op0=ALU.add),
               reads=[self.cacc_r, self.pvec_r], writes=[self.cacc_r])
        pm, pmr = self.bank()
        for c in range(2):
            op("pe", lambda e, c=c, pm=pm: e.matmul(pm, lhsT=self.onesf[:], rhs=self.cacc[:, c, :], start=(c == 0), stop=(c == 1)),
               reads=[self.cacc_r, self.onesf_r], writes=[pmr])
        mean, meanr = self.tmpa()
        op("act", lambda e, pm=pm, mean=mean: e.activation(out=mean[:], in_=pm, func=AF.Copy, scale=1.0 / 256), reads=[pmr], writes=[meanr])
        for c in range(2):
            op("dve", lambda e, c=c, mean=mean: e.tensor_tensor(out=self.cacc[:, c, :], in0=self.cacc[:, c, :], in1=mean[:], op=ALU.subtract),
               reads=[self.cacc_r, meanr], writes=[self.cacc_r])
        pq, pqr = self.bank()
        for c in range(2):
            sq, sqr = self.tmpa()
            op("act", lambda e, c=c, sq=sq: e.activation(out=sq[:], in_=self.cacc[:, c, :], func=AF.Square), reads=[self.cacc_r], writes=[sqr])
            op("pe", lambda e, c=c, sq=sq, pq=pq: e.matmul(pq, lhsT=self.onesf[:], rhs=sq[:], start=(c == 0), stop=(c == 1)), reads=[sqr, self.onesf_r], writes=[pqr])
        sd, sdr = self.tmpa()
        op("act", lambda e, sd=sd, pq=pq: e.activation(out=sd[:], in_=pq, func=AF.Sqrt, bias=self.pcol(PV_EPS5), scale=1.0 / 256), reads=[pqr, self.pvec_r], writes=[sdr])
        op("dve", lambda e, sd=sd: e.reciprocal(out=self.rstd[:], in_=sd[:]), reads=[sdr], writes=[self.rstd_r])
        for c in range(2):
            op("dve", lambda e, c=c: e.scalar_tensor_tensor(out=self.cacc[:, c, :], in0=self.cacc[:, c, :], scalar=self.pcol(base + 104 + c), in1=self.rstd[:],
                                                            op0=ALU.mult, op1=ALU.mult), reads=[self.cacc_r, self.rstd_r, self.pvec_r], writes=[self.cacc_r])
            op("act", lambda e, c=c: e.activation(out=self.yT[:, c, :], in_=self.cacc[:, c, :], func=AF.Silu, bias=self.pcol(base + 106 + c), scale=1.0),
               reads=[self.cacc_r, self.pvec_r], writes=[self.yT_r])
        self.dma(self.cin[:, :, 32:32 + T], self.bgd[:, :, cols].rearrange("c p n -> p c n"), reads=[loc], writes=[self.cin_r])
        for c in range(2):
            op("dve", lambda e, c=c: e.tensor_tensor(out=self.yT[:, 2 + c, :], in0=self.cacc2[:, c, :], in1=self.cin[:, c, 32:32 + T], op=ALU.mult),
               reads=[self.cacc_r, self.cin_r], writes=[self.yT_r])

    def dsa(self, l, j):
        op = self.op
        loc = self.loc_res[j]
        cols = slice(j * T, (j + 1) * T)
        S = 1024 * (j + 1)
        nch = S // 512
        sc = self.big
        self.dma(self.qT[:], self.qd[:, :, cols].rearrange("c p n -> p c n"), reads=[loc], writes=[self.qT_r])
        self.dma(self.qiT[:], self.qid[:, :, cols].rearrange("c p n -> p c n"), reads=[loc], writes=[self.qiT_r])
        self.dma(self.wsc[:], self.widxd[j * T:(j + 1) * T, :].rearrange("(n p) f -> p n f", p=128), reads=[loc], writes=[self.wsc_r])
        op("dve", lambda e: e.tensor_scalar(out=self.wsc[:], in0=self.wsc[:], scalar1=IDX_SCALE, scalar2=None, op0=ALU.mult), reads=[self.wsc_r], writes=[self.wsc_r])
        op("dve", lambda e: e.tensor_scalar(out=self.wsg[:], in0=self.wsc[:], scalar1=0.0, scalar2=2.0, op0=ALU.is_ge, op1=ALU.mult), reads=[self.wsc_r], writes=[self.wsc_r])
        op("dve", lambda e: e.tensor_scalar(out=self.wsg[:], in0=self.wsg[:], scalar1=-1.0, scalar2=None, op0=ALU.add), reads=[self.wsc_r], writes=[self.wsc_r])
        op("dve", lambda e: e.tensor_tensor(out=self.wab[:], in0=self.wsc[:], in1=self.wsg[:], op=ALU.mult), reads=[self.wsc_r], writes=[self.wsc_r])
        vb_v = self.kvb[:, 0:8320].rearrange("p (k f) -> p k f", f=130)
        nkt = 2 * (j + 1)
        def agt(kt):
            return self.ag[l][kt // 2], (kt % 2) * TR, self.ag_res[l][kt // 2]
        bis = self.bis

        def _blk(b):
            qs = slice(b * 128, (b + 1) * 128)
            for kt in range(nkt):
                a_, o_, r_ = agt(kt)
                self.dma(self.kvb[:, kt * 520:kt * 520 + 512], a_[o_ + 512:o_ + 640, :], reads=[r_], writes=[self.kvb_rt[kt]])
            for h in range(4):
                op("dve", lambda e, h=h: e.tensor_scalar(out=self.dg[:, h, :], in0=self.identf[:], scalar1=self.wsg[:, b, h:h + 1], scalar2=None, op0=ALU.mult),
                   reads=[self.identf_r, self.wsc_r], writes=[self.dg_r])
            for ch in range(nch):
                cs = slice(ch * 512, (ch + 1) * 512)
                rls = []
                for h in range(4):
                    ps_ = slice((h % 2) * 64, (h % 2) * 64 + 64)
                    pb, pbr = self.bank()
                    op("pe", lambda e, pb=pb, ps_=ps_, h=h, ch=ch: e.matmul(pb, lhsT=self.qiT[ps_, h // 2, qs], rhs=self.kvb[ps_, ch * 520:ch * 520 + 512], start=True, stop=True),
                       reads=[self.qiT_r, self.kvb_rt[ch]], writes=[pbr])
                    rl, rlr = self.tmpb()
                    op("act", lambda e, pb=pb, rl=rl, h=h: e.activation(out=rl[:], in_=pb, func=AF.Relu, scale=self.wab[:, b, h:h + 1]), reads=[pbr, self.wsc_r], writes=[rlr])
                    rls.append((rl, rlr))
                ps2, ps2r = self.bank()
                for h in range(4):
                    rl, rlr = rls[h]
                    op("pe", lambda e, ps2=ps2, rl=rl, h=h: e.matmul(ps2, lhsT=self.dg[:, h, :], rhs=rl[:], start=(h == 0), stop=(h == 3)),
                       reads=[rlr, self.dg_r], writes=[ps2r])
                op("dve", lambda e, ps2=ps2, cs=cs: e.tensor_copy(out=sc[:, cs], in_=ps2), reads=[ps2r], writes=[self.big_r])
            last = slice(S - 1024, S)
            for hh in range(2):
                t1, t1r = self.tmpa()
                ls = slice(S - 1024 + hh * 512, S - 1024 + (hh + 1) * 512)
                op("dve", lambda e, t1=t1, ls=ls, hh=hh: e.tensor_tensor(out=t1[:], in0=sc[:, ls], in1=self.cmask[:, b, hh * 512:(hh + 1) * 512], op=ALU.subtract),
                   reads=[self.big_r, self.cmask_r], writes=[t1r])
                op("dve", lambda e, t1=t1, hh=hh: e.tensor_reduce(out=bis[:, 40 + hh:41 + hh], in_=t1[:], axis=AX.X, op=ALU.min), reads=[t1r], writes=[self.bis_r])
            op("dve", lambda e: e.tensor_tensor(out=sc[:, last], in0=sc[:, last], in1=self.cmask[:, b, :], op=ALU.add), reads=[self.big_r, self.cmask_r], writes=[self.big_r])
            if S > 1024:
                op("dve", lambda e: e.tensor_reduce(out=bis[:, 42:43], in_=sc[:, 0:S - 1024], axis=AX.X, op=ALU.min), reads=[self.big_r], writes=[self.bis_r])
                nm = 3
            else:
                nm = 2
            op("dve", lambda e: e.tensor_reduce(out=bis[:, 0:1], in_=bis[:, 40:40 + nm], axis=AX.X, op=ALU.min), reads=[self.bis_r], writes=[self.bis_r])
            op("dve", lambda e: e.tensor_reduce(out=bis[:, 1:2], in_=sc[:, 0:S], axis=AX.X, op=ALU.max), reads=[self.big_r], writes=[self.bis_r])
            op("dve", lambda e: e.tensor_tensor(out=bis[:, 2:3], in0=bis[:, 1:2], in1=bis[:, 0:1], op=ALU.subtract), reads=[self.bis_r], writes=[self.bis_r])
            op("dve", lambda e: e.memset(bis[:, 20:20 + NITER], 0.0), writes=[self.bis_r])
            op("dve", lambda e: e.tensor_scalar(out=bis[:, 44:44 + NITER], in0=self.pw2[:, 0:NITER], scalar1=bis[:, 2:3], scalar2=None, op0=ALU.mult),
               reads=[self.bis_r, self.pw2_r], writes=[self.bis_r])
            for it in range(NITER):
                op("dve", lambda e, it=it: e.tensor_tensor(out=bis[:, 3:4], in0=bis[:, 0:1], in1=bis[:, 44 + it:45 + it], op=ALU.add),
                   reads=[self.bis_r], writes=[self.bis_r])
                op("dve", lambda e, it=it: e.tensor_scalar(out=self.sel[:, 0:S], in0=sc[:, 0:S], scalar1=bis[:, 3:4], scalar2=0.0, op0=ALU.is_ge, op1=ALU.add,
                                                          accum_out=bis[:, 20 + it:21 + it]),
                   reads=[self.big_r, self.bis_r], writes=[self.sel_r, self.bis_r])
                op("dve", lambda e, it=it: e.tensor_scalar(out=bis[:, 5:6], in0=bis[:, 20 + it:21 + it], scalar1=255.5, scalar2=bis[:, 44 + it:45 + it], op0=ALU.is_ge, op1=ALU.mult),
                   reads=[self.bis_r], writes=[self.bis_r])
                op("dve", lambda e: e.tensor_tensor(out=bis[:, 0:1], in0=bis[:, 0:1], in1=bis[:, 5:6], op=ALU.add), reads=[self.bis_r], writes=[self.bis_r])
            op("dve", lambda e: e.tensor_scalar(out=self.sel[:, 0:S], in0=sc[:, 0:S], scalar1=bis[:, 0:1], scalar2=-30000.0, op0=ALU.is_lt, op1=ALU.mult),
               reads=[self.big_r, self.bis_r], writes=[self.sel_r])
            if self.debug and j == 0:
                self.dma(self.dbg_sc[b], sc[:, 0:1024], reads=[self.big_r])
                self.dma(self.dbg_sel[b], self.sel[:, 0:1024], reads=[self.sel_r])
                self.dma(self.dbg_bis[b], bis[:], reads=[self.bis_r])
            ycp = [self.banks[6][:], self.banks[7][:]]
            ycr = [self.bank_res[6], self.bank_res[7]]
            LB, LC = 3, 6
            for c in range(4):
                for kt in range(nkt):
                    a_, o_, r_ = agt(kt)
                    self.dma(self.kbuf[:, kt * 512:(kt + 1) * 512], a_[o_ + c * 128:o_ + (c + 1) * 128, :], reads=[r_], writes=[self.kbuf_rt[kt]])
                    vv_ = a_[o_ + 640:o_ + 1160, :].rearrange("r c -> (r c)").rearrange("(t f) -> t f", f=520)
                    self.dma(vb_v[:, kt * 4:(kt + 1) * 4, :], vv_[:, c * 130:(c + 1) * 130].rearrange("(n p) f -> p n f", p=128), reads=[r_], writes=[self.kvb_rt[kt]])
                for half in range(2):
                    h = 2 * c + half
                    ps_ = slice(half * 64, half * 64 + 64)
                    for ch in range(nch):
                        cs = slice(ch * 512, (ch + 1) * 512)
                        pb, pbr = self.bank()
                        op("pe", lambda e, pb=pb, ps_=ps_, cs=cs, c=c: e.matmul(pb, lhsT=self.qT[ps_, c, qs], rhs=self.kbuf[ps_, cs], start=True, stop=True),
                           reads=[self.qT_r, self.kbuf_rt[ch]], writes=[pbr])
                        op("dve", lambda e, pb=pb, ch=ch: e.tensor_reduce(out=self.small[:, ch:ch + 1], in_=pb, axis=AX.X, op=ALU.max), reads=[pbr], writes=[self.small_r])
                    op("dve", lambda e: e.tensor_reduce(out=self.small[:, 32:33], in_=self.small[:, 0:nch], axis=AX.X, op=ALU.max), reads=[self.small_r], writes=[self.small_r])
                    nb_ = 34 + h
                    op("dve", lambda e, nb_=nb_: e.tensor_scalar(out=self.small[:, nb_:nb_ + 1], in0=self.small[:, 32:33], scalar1=-0.125, scalar2=None, op0=ALU.mult),
                       reads=[self.small_r], writes=[self.negm_r[h]])
                    yb = ycp[h // 4]
                    ybr = ycr[h // 4]
                    oc0 = (h % 4) * 65
                    pms = {}
                    pts = {}

                    def stA(ch, ps_=ps_, c=c, nb_=nb_, h=h):
                        cs = slice(ch * 512, (ch + 1) * 512)
                        pb, pbr = self.bank()
                        op("pe", lambda e: e.matmul(pb, lhsT=self.qT[ps_, c, qs], rhs=self.kbuf[ps_, cs], start=True, stop=False),
                           reads=[self.qT_r, self.kbuf_rt[ch]], writes=[pbr])
                        op("pe", lambda e: e.matmul(pb, lhsT=self.ident[:], rhs=self.sel[:, cs], start=False, stop=True),
                           reads=[self.ident_r, self.sel_r], writes=[pbr])
                        pm, pmr = self.tmpb()
                        op("act", lambda e: e.activation(out=pm[:], in_=pb, func=AF.Exp, bias=self.small[:, nb_:nb_ + 1], scale=0.125),
                           reads=[pbr, self.negm_r[h]], writes=[pmr])
                        pms[ch] = (pm, pmr)

                    def stB(ch):
                        pm, pmr = pms.pop(ch)
                        tb_, tbr = self.bank()
                        tbv = tb_.bitcast(BF16)
                        for t in range(4):
                            op("pe", lambda e, t=t: e.transpose(tbv[:, t * 128:(t + 1) * 128], pm[:, t * 128:(t + 1) * 128], self.ident[:]),
                               reads=[pmr, self.ident_r], writes=[tbr])
                        pt, ptr = self.ptbuf()
                        if ch % 2 == 0:
                            op("act", lambda e: e.copy(out=pt.rearrange("p a b -> p (a b)"), in_=tbv[:, 0:512]), reads=[tbr], writes=[ptr, self.rtmp_r])
                        else:
                            op("dve", lambda e: e.tensor_copy(out=pt.rearrange("p a b -> p (a b)"), in_=tbv[:, 0:512]), reads=[tbr], writes=[ptr, self.rtmp_r])
                        pts[ch] = (pt, ptr)

                    def stC(ch, yb=yb, ybr=ybr, oc0=oc0, half=half):
                        pt, ptr = pts.pop(ch)
                        for t in range(4):
                            kc = ch * 4 + t
                            first = (ch == 0 and t == 0)
                            lastm = (ch == nch - 1 and t == 3)
                            op("pe", lambda e, t=t, kc=kc, first=first, lastm=lastm:
                               e.matmul(yb[:, oc0:oc0 + 65], lhsT=pt[:, t, :], rhs=self.kvb[:, kc * 130 + half * 65: kc * 130 + half * 65 + 65], start=first, stop=lastm),
                               reads=[ptr, self.kvb_rt[ch]], writes=[ybr])

                    for st_ in range(nch + LC):
                        if st_ < nch:
                            stA(st_)
                        if 0 <= st_ - LB < nch:
                            stB(st_ - LB)
                        if 0 <= st_ - LC < nch:
                            stC(st_ - LC)
            for h in range(8):
                yb = ycp[h // 4]
                ybr = ycr[h // 4]
                oc0 = (h % 4) * 65
                op("dve", lambda e, yb=yb, oc0=oc0, h=h: e.reciprocal(out=self.small[:, 44 + h:45 + h], in_=yb[:, oc0 + 64:oc0 + 65]), reads=[ybr], writes=[self.small_r])
                op("dve", lambda e, yb=yb, oc0=oc0, h=h: e.tensor_scalar(out=self.ycs[:, h * 64:(h + 1) * 64], in0=yb[:, oc0:oc0 + 64], scalar1=self.small[:, 44 + h:45 + h],
                                                                        scalar2=None, op0=ALU.mult),
                   reads=[ybr, self.small_r], writes=[self.ycs_r])
            if self.debug and j == 0:
                self.dma(self.dbg_ycs[b], self.ycs[:], reads=[self.ycs_r])
            tb_, tbr = self.bank()
            tbv = tb_.bitcast(BF16)
            for c in range(4):
                op("pe", lambda e, c=c, tbv=tbv: e.transpose(tbv[:, c * 128:(c + 1) * 128], self.ycs[:, c * 128:(c + 1) * 128], self.ident[:]),
                   reads=[self.ycs_r, self.ident_r], writes=[tbr])
            op("act", lambda e, tbv=tbv: e.copy(out=self.yT[:, 4:8, qs], in_=tbv[:, 0:512].rearrange("p (c q) -> p c q", c=4)), reads=[tbr], writes=[self.yT_r])

        for b in range(4):
            _blk(b)

    def xattn(self, l):
        op = self.op
        self.rmsnorm(self.xt, self.xt_r, l * LP + 16, self.xn, self.xn_r, 8, T)
        for blk in range(2):
            slot, sres = self.wpop()
            sv = slot[:, 0:4096].rearrange("p (k n) -> p k n", k=8)
            for cc in range(4):
                oc = blk * 4 + cc
                pb, pbr = self.bank()
                for k in range(8):
                    op("pe", lambda e, k=k, cc=cc, sv=sv, pb=pb: e.matmul(pb, lhsT=sv[:, k, cc * 128:(cc + 1) * 128], rhs=self.xn[:, k, :], start=(k == 0), stop=(k == 7)),
                       reads=[sres, self.xn_r], writes=[pbr])
                op("act", lambda e, pb=pb, oc=oc: e.copy(out=self.yT[:, oc, :], in_=pb), reads=[pbr], writes=[self.yT_r])
        o_tok, o_tok_r = self.ycs, self.ycs_r

        def _blk(b):
            qs = slice(b * 128, (b + 1) * 128)
            for half2 in range(2):
                for hh in range(2):
                    h = half2 * 2 + hh
                    pb, pbr = self.bank()
                    for k in range(2):
                        op("pe", lambda e, k=k, h=h, pb=pb: e.matmul(pb[:, 0:256], lhsT=self.yT[:, 2 * h + k, qs], rhs=self.kmT[:, 2 * h + k, :], start=(k == 0), stop=(k == 1)),
                           reads=[self.yT_r, self.kmT_r], writes=[pbr])
                    op("dve", lambda e, pb=pb: e.tensor_reduce(out=self.small[:, 50:51], in_=pb[:, 0:256], axis=AX.X, op=ALU.max), reads=[pbr], writes=[self.small_r])
                    op("dve", lambda e: e.tensor_scalar(out=self.small[:, 51:52], in0=self.small[:, 50:51], scalar1=-1.0 / 16, scalar2=None, op0=ALU.mult),
                       reads=[self.small_r], writes=[self.small_r])
                    op("dve", lambda e: e.memset(self.small[:, 52:53], 0.0), writes=[self.small_r])
                    ex, exr = self.tmpb()
                    op("act", lambda e, pb=pb, ex=ex: e.activation(out=ex[:, 0:256], in_=pb[:, 0:256], func=AF.Exp, bias=self.small[:, 51:52], scale=1.0 / 16,
                                                                  accum_out=self.small[:, 52:53]),
                       reads=[pbr, self.small_r], writes=[exr, self.small_r])
                    tb_, tbr = self.bank()
                    tbv = tb_.bitcast(BF16)
                    for t in range(2):
                        op("pe", lambda e, t=t, ex=ex, tbv=tbv: e.transpose(tbv[:, t * 128:(t + 1) * 128], ex[:, t * 128:(t + 1) * 128], self.ident[:]),
                           reads=[exr, self.ident_r], writes=[tbr])
                    pt, ptr = self.ptbuf()
                    op("act", lambda e, tbv=tbv, pt=pt: e.copy(out=pt[:, 0:2, :].rearrange("p a b -> p (a b)"), in_=tbv[:, 0:256]), reads=[tbr], writes=[ptr, self.rtmp_r])
                    po, por = self.bank()
                    for t in range(2):
                        op("pe", lambda e, t=t, h=h, po=po, pt=pt: e.matmul(po[:, 0:256], lhsT=pt[:, t, :], rhs=self.vm[:, t, h * 256:(h + 1) * 256], start=(t == 0), stop=(t == 1)),
                           reads=[ptr, self.vm_r], writes=[por])
                    op("dve", lambda e: e.reciprocal(out=self.small[:, 53:54], in_=self.small[:, 52:53]), reads=[self.small_r], writes=[self.small_r])
                    op("dve", lambda e, po=po, hh=hh: e.tensor_scalar(out=o_tok[:, hh * 256:(hh + 1) * 256], in0=po[:, 0:256], scalar1=self.small[:, 53:54], scalar2=None, op0=ALU.mult),
                       reads=[por, self.small_r], writes=[o_tok_r])
                tb_, tbr = self.bank()
                tbv = tb_.bitcast(BF16)
                for c in range(4):
                    op("pe", lambda e, c=c, tbv=tbv: e.transpose(tbv[:, c * 128:(c + 1) * 128], o_tok[:, c * 128:(c + 1) * 128], self.ident[:]),
                       reads=[o_tok_r, self.ident_r], writes=[tbr])
                op("act", lambda e, tbv=tbv, half2=half2: e.copy(out=self.xn[:, half2 * 4:half2 * 4 + 4, qs], in_=tbv[:, 0:512].rearrange("p (c q) -> p c q", c=4)),
                   reads=[tbr], writes=[self.xn_r])
        for b in range(4):
            _blk(b)
        self.linear_res(l, "wo", self.xn, self.xn_r, 8)

    def phaseB(self, l, j):
        self.push_group(l, ["wout", "wq", "wo", "gu2", "dn2"])
        if l == 0 and self.stop != "T0":
            self.push_group(1, ["gu1", "dn1", "win", "wv"])
        self.conv_taps(l, j)
        self.dsa(l, j)
        self.conv_finish(l, j)
        if self.debug and l == 0:
            self.dma(self.dbg_y[j].rearrange("c p n -> p c n"), self.yT[:], reads=[self.yT_r])
        self.linear_res(l, "wout", self.yT, self.yT_r, 8)
        if self.debug and l == 0:
            self.dma(self.dbg_x2[j].rearrange("c p n -> p c n"), self.xt[:], reads=[self.xt_r])
        self.xattn(l)
        if self.debug and l == 0:
            self.dma(self.dbg_x3[j].rearrange("c p n -> p c n"), self.xt[:], reads=[self.xt_r])
        self.ffn(l, 2)
        if self.debug and l == 0:
            self.dma(self.dbg_x4[j].rearrange("c p n -> p c n"), self.xt[:], reads=[self.xt_r])

    def final_out(self, j):
        op = self.op
        pb, pbr = self.bank()
        x = self.xt
        for c in range(8):
            sq, sqr = self.tmpa()
            op("act", lambda e, c=c, sq=sq: e.activation(out=sq[:], in_=x[:, c, :], func=AF.Square), reads=[self.xt_r], writes=[sqr])
            op("pe", lambda e, c=c, sq=sq: e.matmul(pb, lhsT=self.onesf[:], rhs=sq[:], start=(c == 0), stop=(c == 7)), reads=[sqr, self.onesf_r], writes=[pbr])
        sd, sdr = self.tmpa()
        op("act", lambda e: e.activation(out=sd[:], in_=pb, func=AF.Sqrt, bias=self.pcol(PV_EPS6), scale=1.0 / 1024), reads=[pbr, self.pvec_r], writes=[sdr])
        op("dve", lambda e: e.reciprocal(out=self.rstd[:], in_=sd[:]), reads=[sdr], writes=[self.rstd_r])
        for c in range(8):
            op("dve", lambda e, c=c: e.scalar_tensor_tensor(out=x[:, c, :], in0=x[:, c, :], scalar=self.pcol(PV_FIN + c), in1=self.rstd[:], op0=ALU.mult, op1=ALU.mult),
               reads=[self.xt_r, self.rstd_r, self.pvec_r], writes=[self.xt_r])
        n = self.dma(self.outd[j].rearrange("c p n -> p c n"), self.xt[:], reads=[self.xt_r])
        self.P.final.append(n)


def _swap(w):
    K, N = w.shape
    w = w.reshape(K, N // 64, 2, 32)
    return np.ascontiguousarray(w[:, :, ::-1, :]).reshape(K, N)


def _blk(w, kc, ncols, width):
    a = w.reshape(kc, 128, ncols).transpose(1, 0, 2).reshape(128, kc * ncols)
    if a.shape[1] < width:
        a = np.concatenate([a, np.zeros((128, width - a.shape[1]), np.float32)], 1)
    return a


def _layer_blob(p, l):
    blocks = []

    def gu(g, u):
        for i in range(11):
            w = np.concatenate([g[:, 256 * i:256 * (i + 1)], u[:, 256 * i:256 * (i + 1)]], 1)
            blocks.append(_blk(w, 8, 512, 4096))

    def dn(dw):
        for oc in range(8):
            blocks.append(_blk(dw[:, oc * 128:(oc + 1) * 128], 22, 128, 3072))

    def sq(w, nb):
        for i in range(nb):
            blocks.append(_blk(w[:, 512 * i:512 * (i + 1)], 8, 512, 4096))

    gu(p["ffn1_w_gate"][l], p["ffn1_w_up"][l])
    dn(p["ffn1_w_down"][l])
    wi = p["w_in"][l]
    a_val, a_gate, b_gate, c_gate, b_h = (wi[:, 256 * i:256 * (i + 1)] for i in range(5))
    q = wi[:, 1280:1792]
    k = wi[:, 1792:2304]
    v = wi[:, 2304:2816]
    qi = wi[:, 2816:3072]
    ki = wi[:, 3072:3136]
    wx = wi[:, 3136:3140]
    ext = np.concatenate([a_val, a_gate, b_gate, c_gate, b_h, q, _swap(q), k, _swap(k), qi, _swap(qi), ki, ki, _swap(ki), _swap(ki)], 1)
    assert ext.shape[1] == 4096
    sq(ext, 8)
    blocks.append(_blk(np.concatenate([v, wx, np.zeros((1024, 60), np.float32)], 1), 8, 576, 4608))
    sq(p["w_out"][l], 2)
    sq(p["xa_wq"][l], 2)
    sq(p["xa_wkv"][l], 4)
    sq(p["xa_wo"][l], 2)
    gu(p["ffn2_w_gate"][l], p["ffn2_w_up"][l])
    dn(p["ffn2_w_down"][l])
    flat = np.concatenate([b.reshape(-1) for b in blocks])
    assert flat.size == BLOB_ROWS * 512, (flat.size, BLOB_ROWS * 512)
    return flat.reshape(BLOB_ROWS, 512)


def _pvec(p, r):
    pv = np.zeros((128, NPV), np.float32)

    def put(c0, vec):
        n = vec.size // 128
        pv[:, c0:c0 + n] = vec.reshape(n, 128).T

    for l in range(2):
        b = l * LP
        put(b + 0, p["ffn1_norm"][l])
        put(b + 8, p["mix_norm"][l])
        put(b + 16, p["xa_norm"][l])
        put(b + 24, p["mem_norm"][l])
        put(b + 32, p["ffn2_norm"][l])
        dw = p["conf_dw"][l]
        for c in range(2):
            pv[:, b + 40 + c * 31: b + 40 + (c + 1) * 31] = dw[:, c * 128:(c + 1) * 128].T
        put(b + 102, p["conf_dw_b"][l])
        put(b + 104, p["conf_ln_g"][l])
        put(b + 106, p["conf_ln_b"][l])
        sw = p["sc_dw"][l]
        for c in range(2):
            pv[:, b + 108 + c * 3: b + 108 + (c + 1) * 3] = sw[:, c * 128:(c + 1) * 128].T
    put(PV_FIN, p["final_norm"])
    pidx = np.arange(128)
    inv_freq = (10000.0 ** (-np.arange(0, 64, 2, dtype=np.float32) / 64)).astype(np.float32)
    pv[:, PV_INVF] = inv_freq[pidx % 32]
    pv[:, PV_SGN] = np.where((pidx % 64) < 32, -1.0, 1.0)
    pv[:, PV_SEL] = 1.0 if r == 1 else 0.0
    pv[:, PV_SEL + 1] = 1.0 if r == 0 else 0.0
    pv[:, PV_EPS6] = 1e-6
    pv[:, PV_EPS5] = 1e-5
    return pv


def _cmask(r):
    i = np.arange(128)[:, None, None]
    b = np.arange(4)[None, :, None]
    c = np.arange(1024)[None, None, :]
    vis = (c - 512 * r) <= (128 * b + i)
    return np.where(vis, 0.0, NEG).astype(np.float32)


_CACHE = {}


def make_inputs(p):
    p = {k: np.asarray(v) for k, v in p.items()}
    blobs = [_layer_blob(p, l) for l in range(2)]
    ident = np.eye(128, dtype=np.float32)
    in_maps = []
    for c in range(8):
        b, r = c // 2, c % 2
        xb = p["x"][b].reshape(16, 512, 8, 128)[r::2]
        xin = np.ascontiguousarray(xb.transpose(0, 2, 3, 1))
        pos = np.ascontiguousarray(p["positions"][b].reshape(16, 512)[r::2].reshape(-1)).astype(np.int32)
        memT = np.ascontiguousarray(p["mem"][b].reshape(256, 8, 128).transpose(1, 2, 0))
        m = {"xin": xin, "pos": pos, "memT": memT, "cmask": _cmask(r), "pvec": _pvec(p, r), "ident": ident,
             "wsl0": blobs[0], "wsl1": blobs[1]}
        in_maps.append(m)
    return in_maps


def assemble(res):
    out = np.zeros((4, 16, 512, 8, 128), np.float32)
    for c in range(8):
        b, r = c // 2, c % 2
        o = res.results[c]["out"]
        out[b, r::2] = o.transpose(0, 3, 1, 2)
    return out.reshape(4, 8192, 1024)


def kernel(**inputs):
    in_maps = make_inputs(inputs)
    if "nc" not in _CACHE:
        _CACHE["nc"] = KB().build()
    res = run_bass_kernel_spmd(_CACHE["nc"], in_maps, core_ids=list(range(8)))
    return assemble(res)
```

```python
import contextlib
import numpy as np
import concourse.bass as bass
import concourse.mybir as mybir
from concourse.bass_utils import run_bass_kernel_spmd

ALU = mybir.AluOpType
AF = mybir.ActivationFunctionType
AX = mybir.AxisListType
F32 = mybir.dt.float32
BF16 = mybir.dt.bfloat16
I32 = mybir.dt.int32

D = 1024
DFF = 2816
T = 512
NT = 8
TOK = T * NT
NITER = 13
IDX_SCALE = 0.5 * 0.125
NEG = -1.0e30
NDSEM = 24
LP = 114
PV_FIN = 228
PV_INVF = 236
PV_SGN = 237
PV_SEL = 238
PV_EPS6 = 240
PV_EPS5 = 241
NPV = 242
SLOTW = 4608
NSLOT = 4
LA = 3
TWO_PI = 6.283185307179586
TR = 1192

BLK = [("gu1", 11, 4096), ("dn1", 8, 3072), ("win", 8, 4096), ("wv", 1, 4608), ("wout", 2, 4096),
       ("wq", 2, 4096), ("wkv", 4, 4096), ("wo", 2, 4096), ("gu2", 11, 4096), ("dn2", 8, 3072)]
BLK_OFF = {}
_r = 0
for _n, _c, _w in BLK:
    BLK_OFF[_n] = (_r, _w)
    _r += _c * (_w // 4)
BLOB_ROWS = _r


class Res:
    __slots__ = ("name", "lw", "rd", "rd_dma")

    def __init__(self, name=""):
        self.name = name
        self.lw = None
        self.rd = {}
        self.rd_dma = []


class Node:
    __slots__ = ("eng", "fn", "deps", "sig", "sigidx", "dma", "dsem", "dcnt", "inc")

    def __init__(self, eng, fn, dma):
        self.eng = eng
        self.fn = fn
        self.dma = dma
        self.deps = []
        self.sig = False
        self.sigidx = 0
        self.dsem = None
        self.dcnt = 0
        self.inc = 16


class Prog:
    def __init__(self, nc):
        self.nc = nc
        self.q = {k: [] for k in ("pe", "act", "dve", "pool", "sp")}
        self.dq = {k: {"n": 0, "last": [None] * NDSEM, "cnt": [0] * NDSEM} for k in ("sp", "pool", "act")}
        self.final = []

    def op(self, eng, fn, reads=(), writes=(), dma=False, inc=16):
        n = Node(eng, fn, dma)
        n.inc = inc
        deps = []
        for r in reads:
            if r.lw is not None:
                deps.append(r.lw)
        for w in writes:
            if w.lw is not None and (dma or w.lw.dma or w.lw.eng != eng or eng != "pe"):
                deps.append(w.lw)
            for e, rn in w.rd.items():
                if dma or e != eng or eng != "pe":
                    deps.append(rn)
            deps.extend(w.rd_dma)
        if dma:
            d = self.dq[eng]
            k = d["n"] % NDSEM
            d["n"] += 1
            if d["last"][k] is not None:
                deps.append(d["last"][k])
            d["cnt"][k] += inc
            n.dsem = (eng, k)
            n.dcnt = d["cnt"][k]
            d["last"][k] = n
        seen = set()
        for x in deps:
            if id(x) not in seen and x is not n:
                seen.add(id(x))
                n.deps.append(x)
                if not x.dma:
                    x.sig = True
        for r in reads:
            if dma:
                r.rd_dma.append(n)
            else:
                r.rd[eng] = n
        for w in writes:
            w.lw = n
            w.rd = {}
            w.rd_dma = []
        self.q[eng].append(n)
        return n

    def emit(self):
        nc = self.nc
        engobj = {"pe": nc.tensor, "act": nc.scalar, "dve": nc.vector, "pool": nc.gpsimd, "sp": nc.sync}
        fin = Node("sp", None, False)
        fin.deps = list(self.final)
        for x in fin.deps:
            if not x.dma:
                x.sig = True
        self.q["sp"].append(fin)
        for e in self.q:
            c = 0
            for n in self.q[e]:
                if n.dma:
                    continue
                if n.sig:
                    c += 1
                    n.sigidx = c
        with contextlib.ExitStack() as st:
            esem = {e: st.enter_context(nc.semaphore("S_" + e)) for e in self.q}
            dsem = {}
            for qn in self.dq:
                for k in range(NDSEM):
                    dsem[(qn, k)] = st.enter_context(nc.semaphore("D_%s%d" % (qn, k)))
            block = st.enter_context(nc.Block())

            def run(e):
                eo = engobj[e]
                waited = {}
                for n in self.q[e]:
                    for d in n.deps:
                        if d.dma:
                            key = ("d",) + d.dsem
                            sem = dsem[d.dsem]
                            val = d.dcnt
                        else:
                            key = ("e", d.eng)
                            sem = esem[d.eng]
                            val = d.sigidx
                        if waited.get(key, 0) < val:
                            eo.wait_ge(sem, val)
                            waited[key] = val
                    if n.fn is None:
                        continue
                    ins = n.fn(eo)
                    if n.dma:
                        ins.then_inc(dsem[n.dsem], n.inc)
                    elif n.sig:
                        ins.then_inc(esem[e], 1)

            @block.tensor
            def _(t):
                run("pe")

            @block.scalar
            def _(t):
                run("act")

            @block.vector
            def _(t):
                run("dve")

            @block.gpsimd
            def _(t):
                run("pool")

            @block.sync
            def _(t):
                run("sp")


class KB:
    def __init__(self, debug=False, stop=None):
        self.debug = debug
        self.stop = stop
        self.nc = bass.Bass("TRN2", target_bir_lowering=False)
        self.P = Prog(self.nc)
        self.st = contextlib.ExitStack()
        self.dbg_outs = []

    def dram(self, name, shape, dt, kind="Internal"):
        if self.debug and kind == "Internal" and name.startswith("dbg_"):
            kind = "ExternalOutput"
            self.dbg_outs.append(name)
        return self.nc.dram_tensor(name, shape, dt, kind=kind).ap()

    def sb(self, name, shape, dt):
        t = self.st.enter_context(self.nc.sbuf_tensor(name, shape, dt))
        return t, Res(name)

    def op(self, *a, **k):
        return self.P.op(*a, **k)

    def dma(self, out, in_, reads=(), writes=(), q="sp"):
        n = self.P.op(q, lambda e: e.dma_start(out=out, in_=in_), reads=reads, writes=writes, dma=True)
        if self.debug:
            self.P.final.append(n)
        return n

    def bank(self):
        i = self.bank_i % 6
        self.bank_i += 1
        return self.banks[i][:], self.bank_res[i]

    def wpush(self, l, name, i):
        r0, w = BLK_OFF[name]
        rows = w // 4
        a = r0 + i * rows
        ap = self.blob[l][a:a + rows, :].rearrange("(p m) c -> p (m c)", p=128)
        self.wfifo.append((ap, w, self.blk_res[(l, name, i)]))

    def wkick(self):
        while self.w_issued < min(len(self.wfifo), self.w_popped + LA):
            ap, w, bres = self.wfifo[self.w_issued]
            s = self.w_issued % NSLOT
            slot = self.wslots[s]
            self.dma(slot[:, 0:w], ap, reads=[bres], writes=[self.wslot_res[s]])
            self.w_issued += 1

    def push_group(self, l, names):
        for name in names:
            cnt = [c for n, c, w in BLK if n == name][0]
            for i in range(cnt):
                self.wpush(l, name, i)
        self.wkick()

    def wpop(self):
        while self.w_issued < min(len(self.wfifo), self.w_popped + LA):
            ap, w, bres = self.wfifo[self.w_issued]
            s = self.w_issued % NSLOT
            slot = self.wslots[s]
            self.dma(slot[:, 0:w], ap, reads=[bres], writes=[self.wslot_res[s]])
            self.w_issued += 1
        s = self.w_popped % NSLOT
        self.w_popped += 1
        return self.wslots[s], self.wslot_res[s]

    def build(self):
        nc = self.nc
        P = self.P
        self.xin = self.dram("xin", [NT, 8, 128, T], F32, kind="ExternalInput")
        self.posd = self.dram("pos", [TOK], I32, kind="ExternalInput")
        self.memT = self.dram("memT", [8, 128, 256], F32, kind="ExternalInput")
        self.cmaskd = self.dram("cmask", [128, 4, 1024], F32, kind="ExternalInput")
        self.pvecd = self.dram("pvec", [128, NPV], F32, kind="ExternalInput")
        self.identd = self.dram("ident", [128, 128], F32, kind="ExternalInput")
        self.wsl = [self.dram("wsl%d" % l, [BLOB_ROWS, 512], F32, kind="ExternalInput") for l in range(2)]
        self.outd = self.dram("out", [NT, 8, 128, T], F32, kind="ExternalOutput")
        self.blob = [self.dram("blob%d" % l, [BLOB_ROWS, 512], BF16) for l in range(2)]
        self.blk_res = {}
        self.xTd = self.dram("dbg_xT", [NT, 8, 128, T], F32)
        self.xT_res = [Res("xT%d" % j) for j in range(NT)]
        self.hAd = self.dram("dbg_hA", [2, 128, TOK], BF16)
        self.cghd = self.dram("dbg_cgh", [2, 128, TOK], BF16)
        self.bgd = self.dram("dbg_bg", [2, 128, TOK], BF16)
        self.qd = self.dram("dbg_q", [4, 128, TOK], BF16)
        self.qid = self.dram("dbg_qi", [2, 128, TOK], BF16)
        self.widxd = self.dram("dbg_widx", [TOK, 4], F32)
        self.loc_res = [Res("loc%d" % j) for j in range(NT)]
        self.send = [[self.dram("send%d_%d" % (l, j), [TR, 512], BF16) for j in range(NT)] for l in range(2)]
        self.send_res = [[Res("send") for j in range(NT)] for l in range(2)]
        self.ag = [[self.dram("agbuf%d_%d" % (l, j), [2 * TR, 512], BF16) for j in range(NT)] for l in range(2)]
        self.ag_res = [[Res("ag") for j in range(NT)] for l in range(2)]

        if self.debug:
            self.dbg_y = self.dram("dbg_y", [NT, 8, 128, T], BF16)
            self.dbg_x2 = self.dram("dbg_x2", [NT, 8, 128, T], F32)
            self.dbg_x3 = self.dram("dbg_x3", [NT, 8, 128, T], F32)
            self.dbg_x4 = self.dram("dbg_x4", [NT, 8, 128, T], F32)
            self.dbg_sc = self.dram("dbg_sc", [4, 128, 1024], F32)
            self.dbg_sel = self.dram("dbg_sel", [4, 128, 1024], BF16)
            self.dbg_bis = self.dram("dbg_bis", [4, 128, 64], F32)
            self.dbg_ycs = self.dram("dbg_ycs", [4, 128, 512], BF16)
        sb = self.sb
        self.wslots = []
        self.wslot_res = []
        for s in range(NSLOT):
            t, r = sb("wslot%d" % s, [128, SLOTW], BF16)
            self.wslots.append(t)
            self.wslot_res.append(r)
        self.wfifo = []
        self.w_issued = 0
        self.w_popped = 0
        self.xt, self.xt_r = sb("xt", [128, 8, T], F32)
        self.xn, self.xn_r = sb("xn", [128, 8, T], BF16)
        self.big, self.big_r = sb("big", [128, 8192], F32)
        self.sel, self.sel_r = sb("sel", [128, 8192], BF16)
        self.kbuf, self.kbuf_r = sb("kbuf", [128, 8192], BF16)
        self.kvb, self.kvb_r = sb("kvb", [128, 8320], BF16)
        self.cmask, self.cmask_r = sb("cmaskb", [128, 4, 1024], BF16)
        self.pvec, self.pvec_r = sb("pvecs", [128, NPV], F32)
        self.identf, self.identf_r = sb("identf", [128, 128], F32)
        self.ident, self.ident_r = sb("identb", [128, 128], BF16)
        self.onesf, self.onesf_r = sb("onesf", [128, 128], F32)
        self.cosT, self.cos_r = sb("cosT", [128, T], F32)
        self.sinT, self.sin_r = sb("sinT", [128, T], F32)
        self.tmpA = [sb("tmpA%d" % i, [128, T], F32) for i in range(4)]
        self.tmpB = [sb("tmpB%d" % i, [128, T], BF16) for i in range(4)]
        cb = self.cosT[:].bitcast(BF16)
        sbv = self.sinT[:].bitcast(BF16)
        self.tmpB += [(cb[:, 0:T], self.cos_r), (sbv[:, 0:T], self.sin_r), (cb[:, T:2 * T], self.cos_r), (sbv[:, T:2 * T], self.sin_r)]
        self.rtmp, self.rtmp_r = sb("rtmp", [128, 4, T], F32)
        rb = self.rtmp[:].rearrange("p a n -> p (a n)").bitcast(BF16)
        self.pTs = [(rb[:, i * 512:(i + 1) * 512].rearrange("p (a b) -> p a b", a=4), Res("pT%d" % i)) for i in range(8)]
        self.pT_i = 0
        self.rtmp_w = [self.rtmp_r] + [r for _, r in self.pTs]
        self.kbuf_rt = [Res("kbuf%d" % i) for i in range(16)]
        self.kis_r = [Res("kis%d" % i) for i in range(4)]
        self.kis_i = 0
        self.negm_r = [Res("negm%d" % i) for i in range(8)]
        self.kvb_rt = [Res("kvb%d" % i) for i in range(16)]
        self.yT, self.yT_r = sb("yT", [128, 8, T], BF16)
        self.qT, self.qT_r = sb("qT", [128, 4, T], BF16)
        self.qiT, self.qiT_r = sb("qiT", [128, 2, T], BF16)
        self.vt, self.vt_r = sb("vt", [128, 4, 520], BF16)
        self.wsc, self.wsc_r = sb("wsc", [128, 4, 4], F32)
        self.wab, _ = sb("wab", [128, 4, 4], F32)
        self.wsg, _ = sb("wsg", [128, 4, 4], F32)
        self.dg, self.dg_r = sb("dg", [128, 4, 128], BF16)
        self.pw2, self.pw2_r = sb("pw2", [128, 16], F32)
        self.small, self.small_r = sb("small", [128, 64], F32)
        self.bis, self.bis_r = sb("bis", [128, 64], F32)
        self.rstd, self.rstd_r = sb("rstd", [128, T], F32)
        self.ycs, self.ycs_r = sb("ycs", [128, 512], BF16)
        self.kmT, self.kmT_r = sb("kmT", [128, 8, 256], BF16)
        self.vm, self.vm_r = sb("vm", [128, 2, 1024], BF16)
        self.memx, self.memx_r = self.rtmp[:].rearrange("p a (b n) -> p (a b) n", b=2), self.rtmp_r
        self.memn, self.memn_r = self.qT[:].rearrange("p a (b n) -> p (a b) n", b=2), self.qT_r
        self.halo, self.halo_r = sb("halo", [128, 2, 2, 32], BF16)
        self.cin, self.cin_r = sb("cin", [128, 2, 32 + T], BF16)
        self.cacc, self.cacc_r = self.xn[:].rearrange("p a n -> p (a n)").bitcast(F32)[:, 0:2 * T].rearrange("p (c n) -> p c n", c=2), self.xn_r
        self.cacc2 = self.xn[:].rearrange("p a n -> p (a n)").bitcast(F32)[:, 2 * T:4 * T].rearrange("p (c n) -> p c n", c=2)
        self.cin2 = self.vt[:].rearrange("p a f -> p (a f)")[:, 0:2 * (32 + T)].rearrange("p (c n) -> p c n", c=2)
        self.ptmp, self.ptmp_r = self.tmpA[3]
        self.act = self.big[:].bitcast(BF16)
        self.banks = []
        self.bank_res = []
        for i in range(8):
            t = self.st.enter_context(nc.psum_tensor("bank%d" % i, [128, 512], F32))
            self.banks.append(t)
            self.bank_res.append(Res("bank%d" % i))
        self.bank_i = 0
        self.tmp_i = 0
        self.tmpb_i = 0

        self.dma(self.pvec[:], self.pvecd, writes=[self.pvec_r])
        self.dma(self.identf[:], self.identd, writes=[self.identf_r])
        self.op("dve", lambda e: e.tensor_copy(out=self.ident[:], in_=self.identf[:]), reads=[self.identf_r], writes=[self.ident_r])
        self.op("dve", lambda e: e.memset(self.onesf[:], 1.0), writes=[self.onesf_r])
        for i in range(16):
            self.op("dve", lambda e, i=i: e.memset(self.pw2[:, i:i + 1], 0.5 ** (i + 1)), writes=[self.pw2_r])
        self.op("dve", lambda e: e.memset(self.vt[:], 1.0), writes=[self.vt_r])
        self.dma(self.cmask[:], self.cmaskd, writes=[self.cmask_r], q="pool")
        for l in range(2):
            for name, cnt, w in BLK:
                r0, _ = BLK_OFF[name]
                rows = w // 4
                for i in range(cnt):
                    r = Res("blk")
                    self.blk_res[(l, name, i)] = r
                    a = r0 + i * rows
                    self.dma(self.blob[l][a:a + rows, :], self.wsl[l][a:a + rows, :], writes=[r], q="pool")
        if self.stop == "T0":
            self.load_x(self.xin, 0, None)
            self.phaseA(0, 0)
            self.exchange(0, 0)
            self.mem_kv(0)
            self.load_x(self.xTd, 0, self.xT_res[0])
            self.phaseB(0, 0)
            return self.finish()
        for j in range(NT):
            self.load_x(self.xin, j, None)
            self.phaseA(0, j)
            self.exchange(0, j)
        if self.stop == "A0":
            return self.finish()
        for l in range(2):
            self.mem_kv(l)
            for j in range(NT):
                self.load_x(self.xTd, j, self.xT_res[j])
                self.phaseB(l, j)
                if l == 0:
                    self.phaseA(1, j, push=False)
                    self.exchange(1, j)
                else:
                    self.final_out(j)
            if l == 0 and self.stop == "B0":
                return self.finish()
        return self.finish()

    def finish(self):
        self.P.emit()
        self.st.close()
        return self.nc

    def pcol(self, c):
        return self.pvec[:, c:c + 1]

    def load_x(self, src, j, res):
        self.dma(self.xt[:], src[j].rearrange("c p n -> p c n"), reads=[res] if res else [], writes=[self.xt_r])

    def tmpa(self):
        i = self.tmp_i % 3
        self.tmp_i += 1
        return self.tmpA[i][0], self.tmpA[i][1]

    def tmpb(self):
        i = self.tmpb_i % 8
        self.tmpb_i += 1
        return self.tmpB[i][0], self.tmpB[i][1]

    def ptbuf(self):
        i = self.pT_i % 8
        self.pT_i += 1
        return self.pTs[i]

    def rmsnorm(self, x, x_r, gc0, out, out_r, nch, N, eps_col=PV_EPS6):
        pb, pbr = self.bank()
        for c in range(nch):
            sq, sqr = self.tmpa()
            self.op("act", lambda e, c=c, sq=sq: e.activation(out=sq[:, 0:N], in_=x[:, c, :], func=AF.Square), reads=[x_r], writes=[sqr])
            self.op("pe", lambda e, c=c, sq=sq: e.matmul(pb[:, 0:N], lhsT=self.onesf[:], rhs=sq[:, 0:N], start=(c == 0), stop=(c == nch - 1)),
                    reads=[sqr, self.onesf_r], writes=[pbr])
        sd, sdr = self.tmpa()
        self.op("act", lambda e: e.activation(out=sd[:, 0:N], in_=pb[:, 0:N], func=AF.Sqrt, bias=self.pcol(eps_col), scale=1.0 / (nch * 128)),
                reads=[pbr, self.pvec_r], writes=[sdr])
        self.op("dve", lambda e: e.reciprocal(out=self.rstd[:, 0:N], in_=sd[:, 0:N]), reads=[sdr], writes=[self.rstd_r])
        for c in range(nch):
            self.op("dve", lambda e, c=c: e.scalar_tensor_tensor(out=out[:, c, :], in0=x[:, c, :], scalar=self.pcol(gc0 + c), in1=self.rstd[:, 0:N],
                                                                 op0=ALU.mult, op1=ALU.mult),
                    reads=[x_r, self.rstd_r, self.pvec_r], writes=[out_r])

    def ffn(self, l, which):
        self.rmsnorm(self.xt, self.xt_r, l * LP + (0 if which == 1 else 32), self.xn, self.xn_r, 8, T)
        act = self.act
        for i in range(11):
            slot, sres = self.wpop()
            sv = slot[:, 0:4096].rearrange("p (k n) -> p k n", k=8)
            for cc in range(2):
                pg, pgr = self.bank()
                pu, pur = self.bank()
                for k in range(8):
                    self.op("pe", lambda e, k=k, cc=cc, sv=sv, pg=pg: e.matmul(pg, lhsT=sv[:, k, cc * 128:(cc + 1) * 128], rhs=self.xn[:, k, :],
                                                                          start=(k == 0), stop=(k == 7)),
                            reads=[sres, self.xn_r], writes=[pgr])
                for k in range(8):
                    self.op("pe", lambda e, k=k, cc=cc, sv=sv, pu=pu: e.matmul(pu, lhsT=sv[:, k, 256 + cc * 128:256 + (cc + 1) * 128], rhs=self.xn[:, k, :],
                                                                          start=(k == 0), stop=(k == 7)),
                            reads=[sres, self.xn_r], writes=[pur])
                sg, sgr = self.tmpa()
                self.op("act", lambda e, sg=sg, pg=pg: e.activation(out=sg[:], in_=pg, func=AF.Silu), reads=[pgr], writes=[sgr])
                ch = 2 * i + cc
                self.op("dve", lambda e, sg=sg, pu=pu, ch=ch: e.tensor_tensor(out=act[:, ch * T:(ch + 1) * T], in0=sg[:], in1=pu, op=ALU.mult),
                        reads=[sgr, pur], writes=[self.big_r])
        for oc in range(8):
            slot, sres = self.wpop()
            sv = slot[:, 0:2816].rearrange("p (k n) -> p k n", k=22)
            po, por = self.bank()
            for k in range(22):
                self.op("pe", lambda e, k=k, sv=sv, po=po: e.matmul(po, lhsT=sv[:, k, :], rhs=act[:, k * T:(k + 1) * T], start=(k == 0), stop=(k == 21)),
                        reads=[sres, self.big_r], writes=[por])
            self.op("dve", lambda e, oc=oc, po=po: e.scalar_tensor_tensor(out=self.xt[:, oc, :], in0=po, scalar=0.5, in1=self.xt[:, oc, :],
                                                                         op0=ALU.mult, op1=ALU.add),
                    reads=[por, self.xt_r], writes=[self.xt_r])

    def rope_tables(self, j):
        e_ = self
        pi_, pi_r = self.tmpa()
        posi = pi_[:].bitcast(I32)
        self.dma(posi, self.posd[j * T:(j + 1) * T].partition_broadcast(128), writes=[pi_r])
        a, ar = self.tmpa()
        k, kr = self.tmpa()
        ki, kir = self.tmpa()
        kiv = ki[:].bitcast(I32)
        op = self.op
        op("dve", lambda e: e.tensor_copy(out=a[:], in_=posi), reads=[pi_r], writes=[ar])
        op("dve", lambda e: e.tensor_scalar(out=a[:], in0=a[:], scalar1=self.pcol(PV_INVF), scalar2=None, op0=ALU.mult), reads=[ar, self.pvec_r], writes=[ar])
        op("dve", lambda e: e.tensor_scalar(out=k[:], in0=a[:], scalar1=1.0 / TWO_PI, scalar2=None, op0=ALU.mult), reads=[ar], writes=[kr])
        op("dve", lambda e: e.tensor_copy(out=kiv, in_=k[:]), reads=[kr], writes=[kir])
        op("dve", lambda e: e.tensor_copy(out=k[:], in_=kiv), reads=[kir], writes=[kr])
        op("dve", lambda e: e.scalar_tensor_tensor(out=a[:], in0=k[:], scalar=-TWO_PI, in1=a[:], op0=ALU.mult, op1=ALU.add), reads=[kr, ar], writes=[ar])

        def fold(t, tr):
            op("dve", lambda e: e.tensor_scalar(out=k[:], in0=t[:], scalar1=np.pi, scalar2=-TWO_PI, op0=ALU.is_gt, op1=ALU.mult), reads=[tr], writes=[kr])
            op("dve", lambda e: e.tensor_tensor(out=t[:], in0=t[:], in1=k[:], op=ALU.add), reads=[tr, kr], writes=[tr])
            op("dve", lambda e: e.tensor_scalar(out=k[:], in0=t[:], scalar1=-np.pi, scalar2=TWO_PI, op0=ALU.is_lt, op1=ALU.mult), reads=[tr], writes=[kr])
            op("dve", lambda e: e.tensor_tensor(out=t[:], in0=t[:], in1=k[:], op=ALU.add), reads=[tr, kr], writes=[tr])
        fold(a, ar)
        op("act", lambda e: e.activation(out=self.sinT[:], in_=a[:], func=AF.Sin), reads=[ar], writes=[self.sin_r])
        op("dve", lambda e: e.tensor_scalar(out=self.sinT[:], in0=self.sinT[:], scalar1=self.pcol(PV_SGN), scalar2=None, op0=ALU.mult),
           reads=[self.sin_r, self.pvec_r], writes=[self.sin_r])
        op("dve", lambda e: e.tensor_scalar(out=a[:], in0=a[:], scalar1=0.5 * np.pi, scalar2=None, op0=ALU.add), reads=[ar], writes=[ar])
        fold(a, ar)
        op("act", lambda e: e.activation(out=self.cosT[:], in_=a[:], func=AF.Sin), reads=[ar], writes=[self.cos_r])

    def phaseA(self, l, j, push=True):
        op = self.op
        if push:
            self.push_group(l, ["gu1", "dn1", "win", "wv"])
        self.ffn(l, 1)
        self.dma(self.xTd[j].rearrange("c p n -> p c n"), self.xt[:], reads=[self.xt_r], writes=[self.xT_res[j]])
        self.rmsnorm(self.xt, self.xt_r, l * LP + 8, self.xn, self.xn_r, 8, T)
        self.rope_tables(j)
        hn = self.xn
        yT = self.yT
        loc = self.loc_res[j]
        cols = slice(j * T, (j + 1) * T)
        held = {}
        sendv = self.send[l][j]
        sres_ = self.send_res[l][j]
        kview = sendv[0:512, :].rearrange("(c p) n -> p c n", c=4)
        kiview = sendv[512:640, :]
        hview = [sendv[1160 + 16 * i:1160 + 16 * (i + 1), :].rearrange("(c q) (h t) -> (q h) c t", c=2, q=8, h=16, t=32) for i in range(2)]
        for blk in range(8):
            slot, sres = self.wpop()
            sv = slot[:, 0:4096].rearrange("p (k n) -> p k n", k=8)
            for cc in range(4):
                ch = blk * 4 + cc
                pb, pbr = self.bank()
                for k in range(8):
                    op("pe", lambda e, k=k, cc=cc, sv=sv, pb=pb: e.matmul(pb, lhsT=sv[:, k, cc * 128:(cc + 1) * 128], rhs=hn[:, k, :], start=(k == 0), stop=(k == 7)),
                       reads=[sres, self.xn_r], writes=[pbr])
                if ch in (0, 1, 6, 7):
                    held[ch] = (pb, pbr)
                elif ch in (2, 3):
                    c = ch - 2
                    sg, sgr = self.tmpa()
                    op("act", lambda e, sg=sg, pb=pb: e.activation(out=sg[:], in_=pb, func=AF.Sigmoid), reads=[pbr], writes=[sgr])
                    pv, pvr = held[c]
                    op("dve", lambda e, sg=sg, pv=pv, c=c: e.tensor_tensor(out=yT[:, c, :], in0=sg[:], in1=pv, op=ALU.mult), reads=[sgr, pvr], writes=[self.yT_r])
                    if c == 1:
                        self.dma(self.hAd[:, :, cols].rearrange("c p n -> p c n"), yT[:, 0:2, :], reads=[self.yT_r], writes=[loc])
                        self.dma(hview[0], yT[:, 0:2, T - 32:T], reads=[self.yT_r], writes=[sres_])
                elif ch in (4, 5):
                    c = ch - 4
                    op("act", lambda e, pb=pb, c=c: e.copy(out=yT[:, 2 + c, :], in_=pb), reads=[pbr], writes=[self.yT_r])
                    if c == 1:
                        self.dma(self.bgd[:, :, cols].rearrange("c p n -> p c n"), yT[:, 2:4, :], reads=[self.yT_r], writes=[loc])
                elif ch in (8, 9):
                    c = ch - 8
                    sg, sgr = self.tmpa()
                    pv, pvr = held[6 + c]
                    op("act", lambda e, sg=sg, pv=pv: e.copy(out=sg[:], in_=pv), reads=[pvr], writes=[sgr])
                    op("dve", lambda e, sg=sg, pb=pb, c=c: e.tensor_tensor(out=yT[:, 4 + c, :], in0=sg[:], in1=pb, op=ALU.mult), reads=[sgr, pbr], writes=[self.yT_r])
                    if c == 1:
                        self.dma(self.cghd[:, :, cols].rearrange("c p n -> p c n"), yT[:, 4:6, :], reads=[self.yT_r], writes=[loc])
                        self.dma(hview[1], yT[:, 4:6, T - 32:T], reads=[self.yT_r], writes=[sres_])
                else:
                    for (c0, n, name) in ((10, 4, "q"), (18, 4, "k"), (26, 2, "qi"), (30, 1, "ki")):
                        if c0 <= ch < c0 + n:
                            c = ch - c0
                            op("dve", lambda e, pb=pb, c=c: e.tensor_tensor(out=self.rtmp[:, c, :], in0=pb, in1=self.cosT[:], op=ALU.mult),
                               reads=[pbr, self.cos_r], writes=self.rtmp_w)
                        elif c0 + n <= ch < c0 + 2 * n:
                            c = ch - c0 - n
                            t2, t2r = self.tmpa()
                            op("dve", lambda e, pb=pb, t2=t2: e.tensor_tensor(out=t2[:], in0=pb, in1=self.sinT[:], op=ALU.mult),
                               reads=[pbr, self.sin_r], writes=[t2r])
                            op("pool", lambda e, t2=t2, c=c: e.tensor_tensor(out=yT[:, c, :], in0=t2[:], in1=self.rtmp[:, c, :], op=ALU.add),
                               reads=[t2r, self.rtmp_r], writes=[self.yT_r])
                            if c == n - 1:
                                if name == "q":
                                    self.dma(self.qd[:, :, cols].rearrange("c p n -> p c n"), yT[:, 0:4, :], reads=[self.yT_r], writes=[loc])
                                elif name == "k":
                                    self.dma(kview, yT[:, 0:4, :], reads=[self.yT_r], writes=[sres_])
                                elif name == "qi":
                                    self.dma(self.qid[:, :, cols].rearrange("c p n -> p c n"), yT[:, 0:2, :], reads=[self.yT_r], writes=[loc])
                                else:
                                    self.dma(kiview, yT[:, 0, :], reads=[self.yT_r], writes=[sres_])
        op("dve", lambda e: e.memset(self.vt[:].rearrange("p a (h f) -> p a h f", f=65)[:, :, :, 64:65], 1.0), writes=[self.vt_r])
        slot, sres = self.wpop()
        sv = slot[:, 0:4608].rearrange("p (k n) -> p k n", k=8)
        vview = sendv[640:1160, :].rearrange("r c -> (r c)").rearrange("(t f) -> t f", f=520)
        for tb in range(4):
            pv, pvr = self.bank()
            pw, pwr = self.bank()
            for k in range(8):
                op("pe", lambda e, k=k, tb=tb, sv=sv, pv=pv: e.matmul(pv, lhsT=hn[:, k, tb * 128:(tb + 1) * 128], rhs=sv[:, k, 0:512], start=(k == 0), stop=(k == 7)),
                   reads=[sres, self.xn_r], writes=[pvr])
            for k in range(8):
                op("pe", lambda e, k=k, tb=tb, sv=sv, pw=pw: e.matmul(pw[:, 0:4], lhsT=hn[:, k, tb * 128:(tb + 1) * 128], rhs=sv[:, k, 512:516], start=(k == 0), stop=(k == 7)),
                   reads=[sres, self.xn_r], writes=[pwr])
            op("act", lambda e, tb=tb, pv=pv: e.copy(out=self.vt[:, tb, :].rearrange("p (h f) -> p h f", f=65)[:, :, 0:64], in_=pv.rearrange("p (h f) -> p h f", f=64)),
               reads=[pvr], writes=[self.vt_r])
            op("dve", lambda e, tb=tb, pw=pw: e.tensor_copy(out=self.wsc[:, tb, :], in_=pw[:, 0:4]), reads=[pwr], writes=[self.wsc_r])
        self.dma(vview.rearrange("(n p) f -> p n f", p=128), self.vt[:], reads=[self.vt_r], writes=[sres_])
        self.dma(self.widxd[j * T:(j + 1) * T, :].rearrange("(n p) f -> p n f", p=128), self.wsc[:], reads=[self.wsc_r], writes=[loc])

    def exchange(self, l, j):
        self.op("pool", lambda e: e.collective_compute("AllGather", ALU.bypass, replica_groups=[[0, 1], [2, 3], [4, 5], [6, 7]],
                                                       ins=[self.send[l][j]], outs=[self.ag[l][j]]),
                reads=[self.send_res[l][j]], writes=[self.ag_res[l][j]], dma=True, inc=1)

    def mem_kv(self, l):
        op = self.op
        self.push_group(l, ["wkv"])
        self.dma(self.memx, self.memT.rearrange("c p n -> p c n"), writes=self.rtmp_w)
        self.rmsnorm(self.memx, self.memx_r, l * LP + 24, self.memn, self.memn_r, 8, 256)
        for blk in range(2):
            slot, sres = self.wpop()
            sv = slot[:, 0:4096].rearrange("p (k n) -> p k n", k=8)
            for cc in range(4):
                pb, pbr = self.bank()
                for k in range(8):
                    op("pe", lambda e, k=k, cc=cc, sv=sv, pb=pb: e.matmul(pb[:, 0:256], lhsT=sv[:, k, cc * 128:(cc + 1) * 128], rhs=self.memn[:, k, :], start=(k == 0), stop=(k == 7)),
                       reads=[sres, self.memn_r], writes=[pbr])
                op("act", lambda e, pb=pb, ch=blk * 4 + cc: e.copy(out=self.kmT[:, ch, :], in_=pb[:, 0:256]), reads=[pbr], writes=[self.kmT_r])
        for blk in range(2):
            slot, sres = self.wpop()
            sv = slot[:, 0:4096].rearrange("p (k n) -> p k n", k=8)
            for mb in range(2):
                pb, pbr = self.bank()
                for k in range(8):
                    op("pe", lambda e, k=k, mb=mb, sv=sv, pb=pb: e.matmul(pb, lhsT=self.memn[:, k, mb * 128:(mb + 1) * 128], rhs=sv[:, k, :], start=(k == 0), stop=(k == 7)),
                       reads=[sres, self.memn_r], writes=[pbr])
                op("act", lambda e, pb=pb, mb=mb, blk=blk: e.copy(out=self.vm[:, mb, blk * 512:(blk + 1) * 512], in_=pb), reads=[pbr], writes=[self.vm_r])

    def linear_res(self, l, name, inp, inp_r, nk):
        op = self.op
        for blk in range(2):
            slot, sres = self.wpop()
            sv = slot[:, 0:4096].rearrange("p (k n) -> p k n", k=8)
            for cc in range(4):
                oc = blk * 4 + cc
                pb, pbr = self.bank()
                for k in range(nk):
                    op("pe", lambda e, k=k, cc=cc, sv=sv, pb=pb: e.matmul(pb, lhsT=sv[:, k, cc * 128:(cc + 1) * 128], rhs=inp[:, k, :], start=(k == 0), stop=(k == nk - 1)),
                       reads=[sres, inp_r], writes=[pbr])
                op("dve", lambda e, oc=oc, pb=pb: e.tensor_tensor(out=self.xt[:, oc, :], in0=pb, in1=self.xt[:, oc, :], op=ALU.add),
                   reads=[pbr, self.xt_r], writes=[self.xt_r])

    def conv_taps(self, l, j):
        op = self.op
        base = l * LP
        loc = self.loc_res[j]
        cols = slice(j * T, (j + 1) * T)
        for br in range(2):
            src = self.hAd if br == 0 else self.cghd
            cin, cin_r = (self.cin, self.cin_r) if br == 0 else (self.cin2, self.vt_r)
            cacc = self.cacc if br == 0 else self.cacc2
            def hv(jj, rr, br=br):
                a = rr * TR + 1160 + 16 * br
                return self.ag[l][jj][a:a + 16, :].rearrange("(c q) (h t) -> (q h) c t", c=2, q=8, h=16, t=32)
            self.dma(self.halo[:, 0, :, :], hv(j, 0), reads=[self.ag_res[l][j]], writes=[self.halo_r])
            if j > 0:
                self.dma(self.halo[:, 1, :, :], hv(j - 1, 1), reads=[self.ag_res[l][j - 1]], writes=[self.halo_r])
            else:
                op("dve", lambda e: e.memset(self.halo[:, 1, :, :], 0.0), writes=[self.halo_r])
            self.dma(cin[:, :, 32:32 + T], src[:, :, cols].rearrange("c p n -> p c n"), reads=[loc], writes=[cin_r])
            op("dve", lambda e, cin=cin: e.tensor_scalar(out=cin[:, :, 0:32], in0=self.halo[:, 0, :, :], scalar1=self.pcol(PV_SEL), scalar2=None, op0=ALU.mult),
               reads=[self.halo_r, self.pvec_r], writes=[cin_r])
            op("dve", lambda e, cin=cin: e.scalar_tensor_tensor(out=cin[:, :, 0:32], in0=self.halo[:, 1, :, :], scalar=self.pcol(PV_SEL + 1), in1=cin[:, :, 0:32],
                                                                op0=ALU.mult, op1=ALU.add),
               reads=[self.halo_r, self.pvec_r, cin_r], writes=[cin_r])
            W = 31 if br == 0 else 3
            wc0 = base + (40 if br == 0 else 108)
            for c in range(2):
                for tap in range(W):
                    sh = 32 - (W - 1) + tap
                    colw = self.pcol(wc0 + c * W + tap)
                    if tap == 0:
                        op("pool", lambda e, c=c, sh=sh, colw=colw, cin=cin, cacc=cacc: e.tensor_scalar(out=cacc[:, c, :], in0=cin[:, c, sh:sh + T], scalar1=colw, scalar2=None, op0=ALU.mult),
                           reads=[cin_r, self.pvec_r], writes=[self.cacc_r])
                    else:
                        op("pool", lambda e, c=c, sh=sh, colw=colw, cin=cin: e.tensor_scalar(out=self.ptmp[:], in0=cin[:, c, sh:sh + T], scalar1=colw, scalar2=None, op0=ALU.mult),
                           reads=[cin_r, self.pvec_r], writes=[self.ptmp_r])
                        op("pool", lambda e, c=c, cacc=cacc: e.tensor_tensor(out=cacc[:, c, :], in0=cacc[:, c, :], in1=self.ptmp[:], op=ALU.add),
                           reads=[self.ptmp_r, self.cacc_r], writes=[self.cacc_r])

    def conv_finish(self, l, j):
        op = self.op
        base = l * LP
        loc = self.loc_res[j]
        cols = slice(j * T, (j + 1) * T)
        for c in range(2):
            op("dve", lambda e, c=c: e.tensor_scalar(out=self.cacc[:, c, :], in0=self.cacc[:, c, :], scalar1=self.pcol(base + 102 + c), scalar2=None, op0=ALU.add),
               reads=[self.cacc_r, self.pvec_r], writes=[self.cacc_r])
        pm, pmr = self.bank()
        for c in range(2):
            op("pe", lambda e, c=c, pm=pm: e.matmul(pm, lhsT=self.onesf[:], rhs=self.cacc[:, c, :], start=(c == 0), stop=(c == 1)),
               reads=[self.cacc_r, self.onesf_r], writes=[pmr])
        mean, meanr = self.tmpa()
        op("act", lambda e, pm=pm, mean=mean: e.activation(out=mean[:], in_=pm, func=AF.Copy, scale=1.0 / 256), reads=[pmr], writes=[meanr])
        for c in range(2):
            op("dve", lambda e, c=c, mean=mean: e.tensor_tensor(out=self.cacc[:, c, :], in0=self.cacc[:, c, :], in1=mean[:], op=ALU.subtract),
               reads=[self.cacc_r, meanr], writes=[self.cacc_r])
        pq, pqr = self.bank()
        for c in range(2):
            sq, sqr = self.tmpa()
            op("act", lambda e, c=c, sq=sq: e.activation(out=sq[:], in_=self.cacc[:, c, :], func=AF.Square), reads=[self.cacc_r], writes=[sqr])
            op("pe", lambda e, c=c, sq=sq, pq=pq: e.matmul(pq, lhsT=self.onesf[:], rhs=sq[:], start=(c == 0), stop=(c == 1)), reads=[sqr, self.onesf_r], writes=[pqr])
        sd, sdr = self.tmpa()
        op("act", lambda e, sd=sd, pq=pq: e.activation(out=sd[:], in_=pq, func=AF.Sqrt, bias=self.pcol(PV_EPS5), scale=1.0 / 256), reads=[pqr, self.pvec_r], writes=[sdr])
        op("dve", lambda e, sd=sd: e.reciprocal(out=self.rstd[:], in_=sd[:]), reads=[sdr], writes=[self.rstd_r])
        for c in range(2):
            op("dve", lambda e, c=c: e.scalar_tensor_tensor(out=self.cacc[:, c, :], in0=self.cacc[:, c, :], scalar=self.pcol(base + 104 + c), in1=self.rstd[:],
                                                            op0=ALU.mult, op1=ALU.mult), reads=[self.cacc_r, self.rstd_r, self.pvec_r], writes=[self.cacc_r])
            op("act", lambda e, c=c: e.activation(out=self.yT[:, c, :], in_=self.cacc[:, c, :], func=AF.Silu, bias=self.pcol(base + 106 + c), scale=1.0),
               reads=[self.cacc_r, self.pvec_r], writes=[self.yT_r] + self.kis_r)
        self.dma(self.cin[:, :, 32:32 + T], self.bgd[:, :, cols].rearrange("c p n -> p c n"), reads=[loc], writes=[self.cin_r])
        for c in range(2):
            op("dve", lambda e, c=c: e.tensor_tensor(out=self.yT[:, 2 + c, :], in0=self.cacc2[:, c, :], in1=self.cin[:, c, 32:32 + T], op=ALU.mult),
               reads=[self.cacc_r, self.cin_r], writes=[self.yT_r] + self.kis_r)

    def dsa(self, l, j):
        op = self.op
        loc = self.loc_res[j]
        cols = slice(j * T, (j + 1) * T)
        S = 1024 * (j + 1)
        nch = S // 512
        sc = self.big
        self.dma(self.qT[:], self.qd[:, :, cols].rearrange("c p n -> p c n"), reads=[loc], writes=[self.qT_r])
        self.dma(self.qiT[:], self.qid[:, :, cols].rearrange("c p n -> p c n"), reads=[loc], writes=[self.qiT_r])
        self.dma(self.wsc[:], self.widxd[j * T:(j + 1) * T, :].rearrange("(n p) f -> p n f", p=128), reads=[loc], writes=[self.wsc_r])
        op("dve", lambda e: e.tensor_scalar(out=self.wsc[:], in0=self.wsc[:], scalar1=IDX_SCALE, scalar2=None, op0=ALU.mult), reads=[self.wsc_r], writes=[self.wsc_r])
        op("dve", lambda e: e.tensor_scalar(out=self.wsg[:], in0=self.wsc[:], scalar1=0.0, scalar2=2.0, op0=ALU.is_ge, op1=ALU.mult), reads=[self.wsc_r], writes=[self.wsc_r])
        op("dve", lambda e: e.tensor_scalar(out=self.wsg[:], in0=self.wsg[:], scalar1=-1.0, scalar2=None, op0=ALU.add), reads=[self.wsc_r], writes=[self.wsc_r])
        op("dve", lambda e: e.tensor_tensor(out=self.wab[:], in0=self.wsc[:], in1=self.wsg[:], op=ALU.mult), reads=[self.wsc_r], writes=[self.wsc_r])
        op("dve", lambda e: e.memset(self.small[:, 60:61], 0.0), writes=[self.yT_r, self.small_r] + self.kis_r)
        vb_v = self.kvb[:, 0:8320].rearrange("p (k f) -> p k f", f=130)
        nkt = 2 * (j + 1)
        bis = self.bis
        junk = bis[:, 6:7].to_broadcast([128, S])

        def agt(kt):
            return self.ag[l][kt // 2], (kt % 2) * TR, self.ag_res[l][kt // 2]

        def pre_gen(b):
            qs = slice(b * 128, (b + 1) * 128)
            for h in range(4):
                op("dve", lambda e, h=h: e.tensor_scalar(out=self.dg[:, h, :], in0=self.identf[:], scalar1=self.wsg[:, b, h:h + 1], scalar2=None, op0=ALU.mult),
                   reads=[self.identf_r, self.wsc_r], writes=[self.dg_r])
            for ch in range(nch):
                cs = slice(ch * 512, (ch + 1) * 512)
                ks = self.kis_i % 4
                self.kis_i += 1
                kt_ = self.yT[:, ks, :]
                ktr = self.kis_r[ks]
                a_, o_, r_ = agt(ch)
                self.dma(kt_, a_[o_ + 512:o_ + 640, :], reads=[r_], writes=[ktr])
                rls = []
                for h in range(4):
                    ps_ = slice((h % 2) * 64, (h % 2) * 64 + 64)
                    pb, pbr = self.bank()
                    op("pe", lambda e, pb=pb, ps_=ps_, h=h, kt_=kt_: e.matmul(pb, lhsT=self.qiT[ps_, h // 2, qs], rhs=kt_[ps_, :], start=True, stop=True),
                       reads=[self.qiT_r, ktr], writes=[pbr])
                    rl, rlr = self.tmpb()
                    op("act", lambda e, pb=pb, rl=rl, h=h: e.activation(out=rl[:], in_=pb, func=AF.Relu, scale=self.wab[:, b, h:h + 1]), reads=[pbr, self.wsc_r], writes=[rlr])
                    rls.append((rl, rlr))
                ps2, ps2r = self.bank()
                for h in range(4):
                    rl, rlr = rls[h]
                    op("pe", lambda e, ps2=ps2, rl=rl, h=h: e.matmul(ps2, lhsT=self.dg[:, h, :], rhs=rl[:], start=(h == 0), stop=(h == 3)),
                       reads=[rlr, self.dg_r], writes=[ps2r])
                op("dve", lambda e, ps2=ps2, cs=cs: e.tensor_copy(out=sc[:, cs], in_=ps2), reads=[ps2r], writes=[self.big_r])
                yield
            last = slice(S - 1024, S)
            for hh in range(2):
                t1, t1r = self.tmpa()
                ls = slice(S - 1024 + hh * 512, S - 1024 + (hh + 1) * 512)
                op("dve", lambda e, t1=t1, ls=ls, hh=hh: e.tensor_tensor(out=t1[:], in0=sc[:, ls], in1=self.cmask[:, b, hh * 512:(hh + 1) * 512], op=ALU.subtract),
                   reads=[self.big_r, self.cmask_r], writes=[t1r])
                op("dve", lambda e, t1=t1, hh=hh: e.tensor_reduce(out=bis[:, 40 + hh:41 + hh], in_=t1[:], axis=AX.X, op=ALU.min), reads=[t1r], writes=[self.bis_r])
            op("dve", lambda e: e.tensor_tensor(out=sc[:, last], in0=sc[:, last], in1=self.cmask[:, b, :], op=ALU.add), reads=[self.big_r, self.cmask_r], writes=[self.big_r])
            if S > 1024:
                op("dve", lambda e: e.tensor_reduce(out=bis[:, 42:43], in_=sc[:, 0:S - 1024], axis=AX.X, op=ALU.min), reads=[self.big_r], writes=[self.bis_r])
                nm = 3
            else:
                nm = 2
            op("dve", lambda e: e.tensor_reduce(out=bis[:, 0:1], in_=bis[:, 40:40 + nm], axis=AX.X, op=ALU.min), reads=[self.bis_r], writes=[self.bis_r])
            op("dve", lambda e: e.tensor_reduce(out=bis[:, 1:2], in_=sc[:, 0:S], axis=AX.X, op=ALU.max), reads=[self.big_r], writes=[self.bis_r])
            op("dve", lambda e: e.tensor_tensor(out=bis[:, 2:3], in0=bis[:, 1:2], in1=bis[:, 0:1], op=ALU.subtract), reads=[self.bis_r], writes=[self.bis_r])
            op("dve", lambda e: e.memset(bis[:, 20:20 + NITER], 0.0), writes=[self.bis_r])
            op("dve", lambda e: e.tensor_scalar(out=bis[:, 44:44 + NITER], in0=self.pw2[:, 0:NITER], scalar1=bis[:, 2:3], scalar2=None, op0=ALU.mult),
               reads=[self.bis_r, self.pw2_r], writes=[self.bis_r])
            yield
            for it in range(NITER):
                op("dve", lambda e, it=it: e.tensor_tensor(out=bis[:, 3:4], in0=bis[:, 0:1], in1=bis[:, 44 + it:45 + it], op=ALU.add),
                   reads=[self.bis_r], writes=[self.bis_r])
                op("dve", lambda e, it=it: e.tensor_scalar(out=junk, in0=sc[:, 0:S], scalar1=bis[:, 3:4], scalar2=0.0, op0=ALU.is_ge, op1=ALU.add,
                                                          accum_out=bis[:, 20 + it:21 + it]),
                   reads=[self.big_r, self.bis_r], writes=[self.bis_r])
                op("dve", lambda e, it=it: e.tensor_scalar(out=bis[:, 5:6], in0=bis[:, 20 + it:21 + it], scalar1=255.5, scalar2=bis[:, 44 + it:45 + it], op0=ALU.is_ge, op1=ALU.mult),
                   reads=[self.bis_r], writes=[self.bis_r])
                op("dve", lambda e: e.tensor_tensor(out=bis[:, 0:1], in0=bis[:, 0:1], in1=bis[:, 5:6], op=ALU.add), reads=[self.bis_r], writes=[self.bis_r])
                yield

        def mask_bias(b):
            op("dve", lambda e: e.tensor_scalar(out=self.sel[:, 0:S], in0=sc[:, 0:S], scalar1=bis[:, 0:1], scalar2=-30000.0, op0=ALU.is_lt, op1=ALU.mult),
               reads=[self.big_r, self.bis_r], writes=[self.sel_r])
            if self.debug and j == 0:
                self.dma(self.dbg_sc[b], sc[:, 0:1024], reads=[self.big_r])
                self.dma(self.dbg_sel[b], self.sel[:, 0:1024], reads=[self.sel_r])
                self.dma(self.dbg_bis[b], bis[:], reads=[self.bis_r])

        def attn(b, nxt):
            qs = slice(b * 128, (b + 1) * 128)
            ycp = [self.banks[6][:], self.banks[7][:]]
            ycr = [self.bank_res[6], self.bank_res[7]]
            LB, LC = 3, 6
            gsteps = nch + 1 + NITER
            psteps = 8 * (nch + LC)
            stride = max(1, psteps // (gsteps + 1))
            cnt = [0]

            def tick():
                cnt[0] += 1
                if nxt is not None and cnt[0] % stride == 0:
                    next(nxt, None)

            for c in range(4):
                for kt in range(nkt):
                    a_, o_, r_ = agt(kt)
                    self.dma(self.kbuf[:, kt * 512:(kt + 1) * 512], a_[o_ + c * 128:o_ + (c + 1) * 128, :], reads=[r_], writes=[self.kbuf_rt[kt]])
                    vv_ = a_[o_ + 640:o_ + 1160, :].rearrange("r c -> (r c)").rearrange("(t f) -> t f", f=520)
                    self.dma(vb_v[:, kt * 4:(kt + 1) * 4, :], vv_[:, c * 130:(c + 1) * 130].rearrange("(n p) f -> p n f", p=128), reads=[r_], writes=[self.kvb_rt[kt]])
                for half in range(2):
                    h = 2 * c + half
                    ps_ = slice(half * 64, half * 64 + 64)
                    for ch in range(nch):
                        cs = slice(ch * 512, (ch + 1) * 512)
                        pb, pbr = self.bank()
                        op("pe", lambda e, pb=pb, ps_=ps_, cs=cs, c=c: e.matmul(pb, lhsT=self.qT[ps_, c, qs], rhs=self.kbuf[ps_, cs], start=True, stop=True),
                           reads=[self.qT_r, self.kbuf_rt[ch]], writes=[pbr])
                        op("dve", lambda e, pb=pb, ch=ch: e.tensor_reduce(out=self.small[:, ch:ch + 1], in_=pb, axis=AX.X, op=ALU.max), reads=[pbr], writes=[self.small_r])
                    op("dve", lambda e: e.tensor_reduce(out=self.small[:, 32:33], in_=self.small[:, 0:nch], axis=AX.X, op=ALU.max), reads=[self.small_r], writes=[self.small_r])
                    nb_ = 34 + h
                    op("dve", lambda e, nb_=nb_: e.tensor_scalar(out=self.small[:, nb_:nb_ + 1], in0=self.small[:, 32:33], scalar1=-0.125, scalar2=None, op0=ALU.mult),
                       reads=[self.small_r], writes=[self.negm_r[h]])
                    yb = ycp[h // 4]
                    ybr = ycr[h // 4]
                    oc0 = (h % 4) * 65
                    pms = {}
                    pts = {}

                    def stA(ch, ps_=ps_, c=c, nb_=nb_, h=h):
                        cs = slice(ch * 512, (ch + 1) * 512)
                        pb, pbr = self.bank()
                        op("pe", lambda e: e.matmul(pb, lhsT=self.qT[ps_, c, qs], rhs=self.kbuf[ps_, cs], start=True, stop=False),
                           reads=[self.qT_r, self.kbuf_rt[ch]], writes=[pbr])
                        op("pe", lambda e: e.matmul(pb, lhsT=self.ident[:], rhs=self.sel[:, cs], start=False, stop=True),
                           reads=[self.ident_r, self.sel_r], writes=[pbr])
                        pm, pmr = self.tmpb()
                        op("act", lambda e: e.activation(out=pm[:], in_=pb, func=AF.Exp, bias=self.small[:, nb_:nb_ + 1], scale=0.125),
                           reads=[pbr, self.negm_r[h]], writes=[pmr])
                        pms[ch] = (pm, pmr)

                    def stB(ch):
                        pm, pmr = pms.pop(ch)
                        tb_, tbr = self.bank()
                        tbv = tb_.bitcast(BF16)
                        for t in range(4):
                            op("pe", lambda e, t=t: e.transpose(tbv[:, t * 128:(t + 1) * 128], pm[:, t * 128:(t + 1) * 128], self.ident[:]),
                               reads=[pmr, self.ident_r], writes=[tbr])
                        pt, ptr = self.ptbuf()
                        if ch % 2 == 0:
                            op("act", lambda e: e.copy(out=pt.rearrange("p a b -> p (a b)"), in_=tbv[:, 0:512]), reads=[tbr], writes=[ptr, self.rtmp_r])
                        else:
                            op("dve", lambda e: e.tensor_copy(out=pt.rearrange("p a b -> p (a b)"), in_=tbv[:, 0:512]), reads=[tbr], writes=[ptr, self.rtmp_r])
                        pts[ch] = (pt, ptr)

                    def stC(ch, yb=yb, ybr=ybr, oc0=oc0, half=half):
                        pt, ptr = pts.pop(ch)
                        for t in range(4):
                            kc = ch * 4 + t
                            first = (ch == 0 and t == 0)
                            lastm = (ch == nch - 1 and t == 3)
                            op("pe", lambda e, t=t, kc=kc, first=first, lastm=lastm:
                               e.matmul(yb[:, oc0:oc0 + 65], lhsT=pt[:, t, :], rhs=self.kvb[:, kc * 130 + half * 65: kc * 130 + half * 65 + 65], start=first, stop=lastm),
                               reads=[ptr, self.kvb_rt[ch]], writes=[ybr])

                    for st_ in range(nch + LC):
                        if st_ < nch:
                            stA(st_)
                        if 0 <= st_ - LB < nch:
                            stB(st_ - LB)
                        if 0 <= st_ - LC < nch:
                            stC(st_ - LC)
                        tick()
            for h in range(8):
                yb = ycp[h // 4]
                ybr = ycr[h // 4]
                oc0 = (h % 4) * 65
                op("dve", lambda e, yb=yb, oc0=oc0, h=h: e.reciprocal(out=self.small[:, 44 + h:45 + h], in_=yb[:, oc0 + 64:oc0 + 65]), reads=[ybr], writes=[self.small_r])
                op("dve", lambda e, yb=yb, oc0=oc0, h=h: e.tensor_scalar(out=self.ycs[:, h * 64:(h + 1) * 64], in0=yb[:, oc0:oc0 + 64], scalar1=self.small[:, 44 + h:45 + h],
                                                                        scalar2=None, op0=ALU.mult),
                   reads=[ybr, self.small_r], writes=[self.ycs_r])
            if self.debug and j == 0:
                self.dma(self.dbg_ycs[b], self.ycs[:], reads=[self.ycs_r])
            tb_, tbr = self.bank()
            tbv = tb_.bitcast(BF16)
            for c in range(4):
                op("pe", lambda e, c=c, tbv=tbv: e.transpose(tbv[:, c * 128:(c + 1) * 128], self.ycs[:, c * 128:(c + 1) * 128], self.ident[:]),
                   reads=[self.ycs_r, self.ident_r], writes=[tbr])
            op("act", lambda e, tbv=tbv: e.copy(out=self.yT[:, 4:8, qs], in_=tbv[:, 0:512].rearrange("p (c q) -> p c q", c=4)), reads=[tbr], writes=[self.yT_r])

        g = pre_gen(0)
        for _ in g:
            pass
        for b in range(4):
            mask_bias(b)
            nxt = pre_gen(b + 1) if b < 3 else None
            attn(b, nxt)
            if nxt is not None:
                for _ in nxt:
                    pass

    def xattn(self, l):
        op = self.op
        self.rmsnorm(self.xt, self.xt_r, l * LP + 16, self.xn, self.xn_r, 8, T)
        for blk in range(2):
            slot, sres = self.wpop()
            sv = slot[:, 0:4096].rearrange("p (k n) -> p k n", k=8)
            for cc in range(4):
                oc = blk * 4 + cc
                pb, pbr = self.bank()
                for k in range(8):
                    op("pe", lambda e, k=k, cc=cc, sv=sv, pb=pb: e.matmul(pb, lhsT=sv[:, k, cc * 128:(cc + 1) * 128], rhs=self.xn[:, k, :], start=(k == 0), stop=(k == 7)),
                       reads=[sres, self.xn_r], writes=[pbr])
                op("act", lambda e, pb=pb, oc=oc: e.copy(out=self.yT[:, oc, :], in_=pb), reads=[pbr], writes=[self.yT_r])
        o_tok, o_tok_r = self.ycs, self.ycs_r

        def _blk(b):
            qs = slice(b * 128, (b + 1) * 128)
            for half2 in range(2):
                for hh in range(2):
                    h = half2 * 2 + hh
                    pb, pbr = self.bank()
                    for k in range(2):
                        op("pe", lambda e, k=k, h=h, pb=pb: e.matmul(pb[:, 0:256], lhsT=self.yT[:, 2 * h + k, qs], rhs=self.kmT[:, 2 * h + k, :], start=(k == 0), stop=(k == 1)),
                           reads=[self.yT_r, self.kmT_r], writes=[pbr])
                    op("dve", lambda e, pb=pb: e.tensor_reduce(out=self.small[:, 50:51], in_=pb[:, 0:256], axis=AX.X, op=ALU.max), reads=[pbr], writes=[self.small_r])
                    op("dve", lambda e: e.tensor_scalar(out=self.small[:, 51:52], in0=self.small[:, 50:51], scalar1=-1.0 / 16, scalar2=None, op0=ALU.mult),
                       reads=[self.small_r], writes=[self.small_r])
                    op("dve", lambda e: e.memset(self.small[:, 52:53], 0.0), writes=[self.small_r])
                    ex, exr = self.tmpb()
                    op("act", lambda e, pb=pb, ex=ex: e.activation(out=ex[:, 0:256], in_=pb[:, 0:256], func=AF.Exp, bias=self.small[:, 51:52], scale=1.0 / 16,
                                                                  accum_out=self.small[:, 52:53]),
                       reads=[pbr, self.small_r], writes=[exr, self.small_r])
                    tb_, tbr = self.bank()
                    tbv = tb_.bitcast(BF16)
                    for t in range(2):
                        op("pe", lambda e, t=t, ex=ex, tbv=tbv: e.transpose(tbv[:, t * 128:(t + 1) * 128], ex[:, t * 128:(t + 1) * 128], self.ident[:]),
                           reads=[exr, self.ident_r], writes=[tbr])
                    pt, ptr = self.ptbuf()
                    op("act", lambda e, tbv=tbv, pt=pt: e.copy(out=pt[:, 0:2, :].rearrange("p a b -> p (a b)"), in_=tbv[:, 0:256]), reads=[tbr], writes=[ptr, self.rtmp_r])
                    po, por = self.bank()
                    for t in range(2):
                        op("pe", lambda e, t=t, h=h, po=po, pt=pt: e.matmul(po[:, 0:256], lhsT=pt[:, t, :], rhs=self.vm[:, t, h * 256:(h + 1) * 256], start=(t == 0), stop=(t == 1)),
                           reads=[ptr, self.vm_r], writes=[por])
                    op("dve", lambda e: e.reciprocal(out=self.small[:, 53:54], in_=self.small[:, 52:53]), reads=[self.small_r], writes=[self.small_r])
                    op("dve", lambda e, po=po, hh=hh: e.tensor_scalar(out=o_tok[:, hh * 256:(hh + 1) * 256], in0=po[:, 0:256], scalar1=self.small[:, 53:54], scalar2=None, op0=ALU.mult),
                       reads=[por, self.small_r], writes=[o_tok_r])
                tb_, tbr = self.bank()
                tbv = tb_.bitcast(BF16)
                for c in range(4):
                    op("pe", lambda e, c=c, tbv=tbv: e.transpose(tbv[:, c * 128:(c + 1) * 128], o_tok[:, c * 128:(c + 1) * 128], self.ident[:]),
                       reads=[o_tok_r, self.ident_r], writes=[tbr])
                op("act", lambda e, tbv=tbv, half2=half2: e.copy(out=self.xn[:, half2 * 4:half2 * 4 + 4, qs], in_=tbv[:, 0:512].rearrange("p (c q) -> p c q", c=4)),
                   reads=[tbr], writes=[self.xn_r])
        for b in range(4):
            _blk(b)
        self.linear_res(l, "wo", self.xn, self.xn_r, 8)

    def phaseB(self, l, j):
        self.push_group(l, ["wout", "wq", "wo", "gu2", "dn2"])
        if l == 0 and self.stop != "T0":
            self.push_group(1, ["gu1", "dn1", "win", "wv"])
        self.conv_taps(l, j)
        self.dsa(l, j)
        self.conv_finish(l, j)
        if self.debug and l == 0:
            self.dma(self.dbg_y[j].rearrange("c p n -> p c n"), self.yT[:], reads=[self.yT_r])
        self.linear_res(l, "wout", self.yT, self.yT_r, 8)
        if self.debug and l == 0:
            self.dma(self.dbg_x2[j].rearrange("c p n -> p c n"), self.xt[:], reads=[self.xt_r])
        self.xattn(l)
        if self.debug and l == 0:
            self.dma(self.dbg_x3[j].rearrange("c p n -> p c n"), self.xt[:], reads=[self.xt_r])
        self.ffn(l, 2)
        if self.debug and l == 0:
            self.dma(self.dbg_x4[j].rearrange("c p n -> p c n"), self.xt[:], reads=[self.xt_r])

    def final_out(self, j):
        op = self.op
        pb, pbr = self.bank()
        x = self.xt
        for c in range(8):
            sq, sqr = self.tmpa()
            op("act", lambda e, c=c, sq=sq: e.activation(out=sq[:], in_=x[:, c, :], func=AF.Square), reads=[self.xt_r], writes=[sqr])
            op("pe", lambda e, c=c, sq=sq: e.matmul(pb, lhsT=self.onesf[:], rhs=sq[:], start=(c == 0), stop=(c == 7)), reads=[sqr, self.onesf_r], writes=[pbr])
        sd, sdr = self.tmpa()
        op("act", lambda e: e.activation(out=sd[:], in_=pb, func=AF.Sqrt, bias=self.pcol(PV_EPS6), scale=1.0 / 1024), reads=[pbr, self.pvec_r], writes=[sdr])
        op("dve", lambda e: e.reciprocal(out=self.rstd[:], in_=sd[:]), reads=[sdr], writes=[self.rstd_r])
        for c in range(8):
            op("dve", lambda e, c=c: e.scalar_tensor_tensor(out=x[:, c, :], in0=x[:, c, :], scalar=self.pcol(PV_FIN + c), in1=self.rstd[:], op0=ALU.mult, op1=ALU.mult),
               reads=[self.xt_r, self.rstd_r, self.pvec_r], writes=[self.xt_r])
        n = self.dma(self.outd[j].rearrange("c p n -> p c n"), self.xt[:], reads=[self.xt_r])
        self.P.final.append(n)


def _swap(w):
    K, N = w.shape
    w = w.reshape(K, N // 64, 2, 32)
    return np.ascontiguousarray(w[:, :, ::-1, :]).reshape(K, N)


def _blk(w, kc, ncols, width):
    a = w.reshape(kc, 128, ncols).transpose(1, 0, 2).reshape(128, kc * ncols)
    if a.shape[1] < width:
        a = np.concatenate([a, np.zeros((128, width - a.shape[1]), np.float32)], 1)
    return a


def _layer_blob(p, l):
    blocks = []

    def gu(g, u):
        for i in range(11):
            w = np.concatenate([g[:, 256 * i:256 * (i + 1)], u[:, 256 * i:256 * (i + 1)]], 1)
            blocks.append(_blk(w, 8, 512, 4096))

    def dn(dw):
        for oc in range(8):
            blocks.append(_blk(dw[:, oc * 128:(oc + 1) * 128], 22, 128, 3072))

    def sq(w, nb):
        for i in range(nb):
            blocks.append(_blk(w[:, 512 * i:512 * (i + 1)], 8, 512, 4096))

    gu(p["ffn1_w_gate"][l], p["ffn1_w_up"][l])
    dn(p["ffn1_w_down"][l])
    wi = p["w_in"][l]
    a_val, a_gate, b_gate, c_gate, b_h = (wi[:, 256 * i:256 * (i + 1)] for i in range(5))
    q = wi[:, 1280:1792]
    k = wi[:, 1792:2304]
    v = wi[:, 2304:2816]
    qi = wi[:, 2816:3072]
    ki = wi[:, 3072:3136]
    wx = wi[:, 3136:3140]
    ext = np.concatenate([a_val, a_gate, b_gate, c_gate, b_h, q, _swap(q), k, _swap(k), qi, _swap(qi), ki, ki, _swap(ki), _swap(ki)], 1)
    assert ext.shape[1] == 4096
    sq(ext, 8)
    blocks.append(_blk(np.concatenate([v, wx, np.zeros((1024, 60), np.float32)], 1), 8, 576, 4608))
    sq(p["w_out"][l], 2)
    sq(p["xa_wq"][l], 2)
    sq(p["xa_wkv"][l], 4)
    sq(p["xa_wo"][l], 2)
    gu(p["ffn2_w_gate"][l], p["ffn2_w_up"][l])
    dn(p["ffn2_w_down"][l])
    flat = np.concatenate([b.reshape(-1) for b in blocks])
    assert flat.size == BLOB_ROWS * 512, (flat.size, BLOB_ROWS * 512)
    return flat.reshape(BLOB_ROWS, 512)


def _pvec(p, r):
    pv = np.zeros((128, NPV), np.float32)

    def put(c0, vec):
        n = vec.size // 128
        pv[:, c0:c0 + n] = vec.reshape(n, 128).T

    for l in range(2):
        b = l * LP
        put(b + 0, p["ffn1_norm"][l])
        put(b + 8, p["mix_norm"][l])
        put(b + 16, p["xa_norm"][l])
        put(b + 24, p["mem_norm"][l])
        put(b + 32, p["ffn2_norm"][l])
        dw = p["conf_dw"][l]
        for c in range(2):
            pv[:, b + 40 + c * 31: b + 40 + (c + 1) * 31] = dw[:, c * 128:(c + 1) * 128].T
        put(b + 102, p["conf_dw_b"][l])
        put(b + 104, p["conf_ln_g"][l])
        put(b + 106, p["conf_ln_b"][l])
        sw = p["sc_dw"][l]
        for c in range(2):
            pv[:, b + 108 + c * 3: b + 108 + (c + 1) * 3] = sw[:, c * 128:(c + 1) * 128].T
    put(PV_FIN, p["final_norm"])
    pidx = np.arange(128)
    inv_freq = (10000.0 ** (-np.arange(0, 64, 2, dtype=np.float32) / 64)).astype(np.float32)
    pv[:, PV_INVF] = inv_freq[pidx % 32]
    pv[:, PV_SGN] = np.where((pidx % 64) < 32, -1.0, 1.0)
    pv[:, PV_SEL] = 1.0 if r == 1 else 0.0
    pv[:, PV_SEL + 1] = 1.0 if r == 0 else 0.0
    pv[:, PV_EPS6] = 1e-6
    pv[:, PV_EPS5] = 1e-5
    return pv


def _cmask(r):
    i = np.arange(128)[:, None, None]
    b = np.arange(4)[None, :, None]
    c = np.arange(1024)[None, None, :]
    vis = (c - 512 * r) <= (128 * b + i)
    return np.where(vis, 0.0, NEG).astype(np.float32)


_CACHE = {}


def make_inputs(p):
    p = {k: np.asarray(v) for k, v in p.items()}
    blobs = [_layer_blob(p, l) for l in range(2)]
    ident = np.eye(128, dtype=np.float32)
    in_maps = []
    for c in range(8):
        b, r = c // 2, c % 2
        xb = p["x"][b].reshape(16, 512, 8, 128)[r::2]
        xin = np.ascontiguousarray(xb.transpose(0, 2, 3, 1))
        pos = np.ascontiguousarray(p["positions"][b].reshape(16, 512)[r::2].reshape(-1)).astype(np.int32)
        memT = np.ascontiguousarray(p["mem"][b].reshape(256, 8, 128).transpose(1, 2, 0))
        m = {"xin": xin, "pos": pos, "memT": memT, "cmask": _cmask(r), "pvec": _pvec(p, r), "ident": ident,
             "wsl0": blobs[0], "wsl1": blobs[1]}
        in_maps.append(m)
    return in_maps


def assemble(res):
    out = np.zeros((4, 16, 512, 8, 128), np.float32)
    for c in range(8):
        b, r = c // 2, c % 2
        o = res.results[c]["out"]
        out[b, r::2] = o.transpose(0, 3, 1, 2)
    return out.reshape(4, 8192, 1024)


def kernel(**inputs):
    in_maps = make_inputs(inputs)
    if "nc" not in _CACHE:
        _CACHE["nc"] = KB().build()
    res = run_bass_kernel_spmd(_CACHE["nc"], in_maps, core_ids=list(range(8)))
    return assemble(res)
```

```python
import contextlib
import numpy as np
import concourse.bass as bass
import concourse.mybir as mybir
from concourse.bass_utils import run_bass_kernel_spmd

ALU = mybir.AluOpType
AF = mybir.ActivationFunctionType
AX = mybir.AxisListType
F32 = mybir.dt.float32
BF16 = mybir.dt.bfloat16
I32 = mybir.dt.int32

D = 1024
DFF = 2816
T = 512
NT = 8
TOK = T * NT
NITER = 13
IDX_SCALE = 0.5 * 0.125
NEG = -1.0e30
NDSEM = 24
LP = 114
PV_FIN = 228
PV_INVF = 236
PV_SGN = 237
PV_SEL = 238
PV_EPS6 = 240
PV_EPS5 = 241
NPV = 242
SLOTW = 4608
NSLOT = 4
LA = 3
TWO_PI = 6.283185307179586
TR = 1192

BLK = [("gu1", 11, 4096), ("dn1", 8, 3072), ("win", 8, 4096), ("wv", 1, 4608), ("wout", 2, 4096),
       ("wq", 2, 4096), ("wkv", 4, 4096), ("wo", 2, 4096), ("gu2", 11, 4096), ("dn2", 8, 3072)]
BLK_OFF = {}
_r = 0
for _n, _c, _w in BLK:
    BLK_OFF[_n] = (_r, _w)
    _r += _c * (_w // 4)
BLOB_ROWS = _r


class Res:
    __slots__ = ("name", "lw", "rd", "rd_dma")

    def __init__(self, name=""):
        self.name = name
        self.lw = None
        self.rd = {}
        self.rd_dma = []


class Node:
    __slots__ = ("eng", "fn", "deps", "sig", "sigidx", "dma", "dsem", "dcnt", "inc")

    def __init__(self, eng, fn, dma):
        self.eng = eng
        self.fn = fn
        self.dma = dma
        self.deps = []
        self.sig = False
        self.sigidx = 0
        self.dsem = None
        self.dcnt = 0
        self.inc = 16


class Prog:
    def __init__(self, nc):
        self.nc = nc
        self.q = {k: [] for k in ("pe", "act", "dve", "pool", "sp")}
        self.dq = {k: {"n": 0, "last": [None] * NDSEM, "cnt": [0] * NDSEM} for k in ("sp", "pool", "act", "cc")}
        self.final = []

    def op(self, eng, fn, reads=(), writes=(), dma=False, inc=16, sq=None):
        n = Node(eng, fn, dma)
        n.inc = inc
        deps = []
        for r in reads:
            if r.lw is not None:
                deps.append(r.lw)
        for w in writes:
            if w.lw is not None and (dma or w.lw.dma or w.lw.eng != eng or eng != "pe"):
                deps.append(w.lw)
            for e, rn in w.rd.items():
                if dma or e != eng or eng != "pe":
                    deps.append(rn)
            deps.extend(w.rd_dma)
        if dma:
            sq = sq or eng
            d = self.dq[sq]
            k = d["n"] % NDSEM
            d["n"] += 1
            if d["last"][k] is not None:
                deps.append(d["last"][k])
            d["cnt"][k] += inc
            n.dsem = (sq, k)
            n.dcnt = d["cnt"][k]
            d["last"][k] = n
        seen = set()
        for x in deps:
            if id(x) not in seen and x is not n:
                seen.add(id(x))
                n.deps.append(x)
                if not x.dma:
                    x.sig = True
        for r in reads:
            if dma:
                r.rd_dma.append(n)
            else:
                r.rd[eng] = n
        for w in writes:
            w.lw = n
            w.rd = {}
            w.rd_dma = []
        self.q[eng].append(n)
        return n

    def emit(self):
        nc = self.nc
        engobj = {"pe": nc.tensor, "act": nc.scalar, "dve": nc.vector, "pool": nc.gpsimd, "sp": nc.sync}
        fin = Node("sp", None, False)
        fin.deps = list(self.final)
        for x in fin.deps:
            if not x.dma:
                x.sig = True
        self.q["sp"].append(fin)
        for e in self.q:
            c = 0
            for n in self.q[e]:
                if n.dma:
                    continue
                if n.sig:
                    c += 1
                    n.sigidx = c
        with contextlib.ExitStack() as st:
            esem = {e: st.enter_context(nc.semaphore("S_" + e)) for e in self.q}
            dsem = {}
            for qn in self.dq:
                for k in range(NDSEM):
                    dsem[(qn, k)] = st.enter_context(nc.semaphore("D_%s%d" % (qn, k)))
            block = st.enter_context(nc.Block())

            def run(e):
                eo = engobj[e]
                waited = {}
                for n in self.q[e]:
                    for d in n.deps:
                        if d.dma:
                            key = ("d",) + d.dsem
                            sem = dsem[d.dsem]
                            val = d.dcnt
                        else:
                            key = ("e", d.eng)
                            sem = esem[d.eng]
                            val = d.sigidx
                        if waited.get(key, 0) < val:
                            eo.wait_ge(sem, val)
                            waited[key] = val
                    if n.fn is None:
                        continue
                    ins = n.fn(eo)
                    if n.dma:
                        ins.then_inc(dsem[n.dsem], n.inc)
                    elif n.sig:
                        ins.then_inc(esem[e], 1)

            @block.tensor
            def _(t):
                run("pe")

            @block.scalar
            def _(t):
                run("act")

            @block.vector
            def _(t):
                run("dve")

            @block.gpsimd
            def _(t):
                run("pool")

            @block.sync
            def _(t):
                run("sp")


class KB:
    def __init__(self, debug=False, stop=None):
        self.debug = debug
        self.stop = stop
        self.nc = bass.Bass("TRN2", target_bir_lowering=False)
        self.P = Prog(self.nc)
        self.st = contextlib.ExitStack()
        self.dbg_outs = []

    def dram(self, name, shape, dt, kind="Internal"):
        if self.debug and kind == "Internal" and name.startswith("dbg_"):
            kind = "ExternalOutput"
            self.dbg_outs.append(name)
        return self.nc.dram_tensor(name, shape, dt, kind=kind).ap()

    def sb(self, name, shape, dt):
        t = self.st.enter_context(self.nc.sbuf_tensor(name, shape, dt))
        return t, Res(name)

    def op(self, *a, **k):
        return self.P.op(*a, **k)

    def dma(self, out, in_, reads=(), writes=(), q="sp"):
        n = self.P.op(q, lambda e: e.dma_start(out=out, in_=in_), reads=reads, writes=writes, dma=True)
        if self.debug:
            self.P.final.append(n)
        return n

    def bank(self):
        i = self.bank_i % 6
        self.bank_i += 1
        return self.banks[i][:], self.bank_res[i]

    def wpush(self, l, name, i):
        r0, w = BLK_OFF[name]
        rows = w // 4
        a = r0 + i * rows
        ap = self.blob[l][a:a + rows, :].rearrange("(p m) c -> p (m c)", p=128)
        self.wfifo.append((ap, w, self.blk_res[(l, name, i)]))

    def wkick(self):
        while self.w_issued < min(len(self.wfifo), self.w_popped + LA):
            ap, w, bres = self.wfifo[self.w_issued]
            s = self.w_issued % NSLOT
            slot = self.wslots[s]
            self.dma(slot[:, 0:w], ap, reads=[bres], writes=[self.wslot_res[s]])
            self.w_issued += 1

    def push_group(self, l, names):
        for name in names:
            cnt = [c for n, c, w in BLK if n == name][0]
            for i in range(cnt):
                self.wpush(l, name, i)
        self.wkick()

    def wpop(self):
        while self.w_issued < min(len(self.wfifo), self.w_popped + LA):
            ap, w, bres = self.wfifo[self.w_issued]
            s = self.w_issued % NSLOT
            slot = self.wslots[s]
            self.dma(slot[:, 0:w], ap, reads=[bres], writes=[self.wslot_res[s]])
            self.w_issued += 1
        s = self.w_popped % NSLOT
        self.w_popped += 1
        return self.wslots[s], self.wslot_res[s]

    def build(self):
        nc = self.nc
        P = self.P
        self.xin = self.dram("xin", [NT, 8, 128, T], F32, kind="ExternalInput")
        self.posd = self.dram("pos", [TOK], I32, kind="ExternalInput")
        self.memT = self.dram("memT", [8, 128, 256], F32, kind="ExternalInput")
        self.cmaskd = self.dram("cmask", [128, 4, 1024], F32, kind="ExternalInput")
        self.pvecd = self.dram("pvec", [128, NPV], F32, kind="ExternalInput")
        self.identd = self.dram("ident", [128, 128], F32, kind="ExternalInput")
        self.wsl = [self.dram("wsl%d" % l, [BLOB_ROWS, 512], F32, kind="ExternalInput") for l in range(2)]
        self.outd = self.dram("out", [NT, 8, 128, T], F32, kind="ExternalOutput")
        self.blob = [self.dram("blob%d" % l, [BLOB_ROWS, 512], BF16) for l in range(2)]
        self.blk_res = {}
        self.xTd = self.dram("dbg_xT", [NT, 8, 128, T], F32)
        self.xT_res = [Res("xT%d" % j) for j in range(NT)]
        self.hAd = self.dram("dbg_hA", [2, 128, TOK], BF16)
        self.cghd = self.dram("dbg_cgh", [2, 128, TOK], BF16)
        self.bgd = self.dram("dbg_bg", [2, 128, TOK], BF16)
        self.qd = self.dram("dbg_q", [4, 128, TOK], BF16)
        self.qid = self.dram("dbg_qi", [2, 128, TOK], BF16)
        self.widxd = self.dram("dbg_widx", [TOK, 4], F32)
        self.loc_res = [Res("loc%d" % j) for j in range(NT)]
        self.send = [[self.dram("send%d_%d" % (l, j), [TR, 512], BF16) for j in range(NT)] for l in range(2)]
        self.send_res = [[Res("send") for j in range(NT)] for l in range(2)]
        self.ag = [[self.dram("agbuf%d_%d" % (l, j), [2 * TR, 512], BF16) for j in range(NT)] for l in range(2)]
        self.ag_res = [[Res("ag") for j in range(NT)] for l in range(2)]

        if self.debug:
            self.dbg_y = self.dram("dbg_y", [NT, 8, 128, T], BF16)
            self.dbg_x2 = self.dram("dbg_x2", [NT, 8, 128, T], F32)
            self.dbg_x3 = self.dram("dbg_x3", [NT, 8, 128, T], F32)
            self.dbg_x4 = self.dram("dbg_x4", [NT, 8, 128, T], F32)
            self.dbg_sc = self.dram("dbg_sc", [4, 128, 1024], F32)
            self.dbg_sel = self.dram("dbg_sel", [4, 128, 1024], BF16)
            self.dbg_bis = self.dram("dbg_bis", [4, 128, 64], F32)
            self.dbg_ycs = self.dram("dbg_ycs", [4, 128, 512], BF16)
        sb = self.sb
        self.wslots = []
        self.wslot_res = []
        for s in range(NSLOT):
            t, r = sb("wslot%d" % s, [128, SLOTW], BF16)
            self.wslots.append(t)
            self.wslot_res.append(r)
        self.wfifo = []
        self.w_issued = 0
        self.w_popped = 0
        self.xt, self.xt_r = sb("xt", [128, 8, T], F32)
        self.xn, self.xn_r = sb("xn", [128, 8, T], BF16)
        self.big, self.big_r = sb("big", [128, 8192], F32)
        self.sel, self.sel_r = sb("sel", [128, 8192], BF16)
        self.kbuf, self.kbuf_r = sb("kbuf", [128, 8192], BF16)
        self.kvb, self.kvb_r = sb("kvb", [128, 8320], BF16)
        self.cmask, self.cmask_r = sb("cmaskb", [128, 4, 1024], BF16)
        self.pvec, self.pvec_r = sb("pvecs", [128, NPV], F32)
        self.identf, self.identf_r = sb("identf", [128, 128], F32)
        self.ident, self.ident_r = sb("identb", [128, 128], BF16)
        self.onesf, self.onesf_r = sb("onesf", [128, 128], F32)
        self.cosT, self.cos_r = sb("cosT", [128, T], F32)
        self.sinT, self.sin_r = sb("sinT", [128, T], F32)
        self.tmpA = [sb("tmpA%d" % i, [128, T], F32) for i in range(4)]
        self.tmpB = [sb("tmpB%d" % i, [128, T], BF16) for i in range(4)]
        cb = self.cosT[:].bitcast(BF16)
        sbv = self.sinT[:].bitcast(BF16)
        self.tmpB += [(cb[:, 0:T], self.cos_r), (sbv[:, 0:T], self.sin_r), (cb[:, T:2 * T], self.cos_r), (sbv[:, T:2 * T], self.sin_r)]
        self.rtmp, self.rtmp_r = sb("rtmp", [128, 4, T], F32)
        rb = self.rtmp[:].rearrange("p a n -> p (a n)").bitcast(BF16)
        self.pTs = [(rb[:, i * 512:(i + 1) * 512].rearrange("p (a b) -> p a b", a=4), Res("pT%d" % i)) for i in range(8)]
        self.pT_i = 0
        self.rtmp_w = [self.rtmp_r] + [r for _, r in self.pTs]
        self.kbuf_rt = [Res("kbuf%d" % i) for i in range(16)]
        self.kis_r = [Res("kis%d" % i) for i in range(4)]
        self.xs_r = [Res("xs%d" % i) for i in range(4)]
        self.kis_i = 0
        self.negm_r = [Res("negm%d" % i) for i in range(8)]
        self.kvb_rt = [Res("kvb%d" % i) for i in range(16)]
        self.yT, self.yT_r = sb("yT", [128, 8, T], BF16)
        self.qT, self.qT_r = sb("qT", [128, 4, T], BF16)
        self.qiT, self.qiT_r = sb("qiT", [128, 2, T], BF16)
        self.vt, self.vt_r = sb("vt", [128, 4, 520], BF16)
        self.wsc, self.wsc_r = sb("wsc", [128, 4, 4], F32)
        self.wab, _ = sb("wab", [128, 4, 4], F32)
        self.wsg, _ = sb("wsg", [128, 4, 4], F32)
        self.dg, self.dg_r = sb("dg", [128, 4, 128], BF16)
        self.pw2, self.pw2_r = sb("pw2", [128, 16], F32)
        self.small, self.small_r = sb("small", [128, 64], F32)
        self.bis, self.bis_r = sb("bis", [128, 64], F32)
        self.rstd, self.rstd_r = sb("rstd", [128, T], F32)
        self.ycs, self.ycs_r = sb("ycs", [128, 512], BF16)
        self.kmT, self.kmT_r = sb("kmT", [128, 8, 256], BF16)
        self.vm, self.vm_r = sb("vm", [128, 2, 1024], BF16)
        self.memx, self.memx_r = self.rtmp[:].rearrange("p a (b n) -> p (a b) n", b=2), self.rtmp_r
        self.memn, self.memn_r = self.qT[:].rearrange("p a (b n) -> p (a b) n", b=2), self.qT_r
        self.halo, self.halo_r = sb("halo", [128, 2, 2, 32], BF16)
        self.cin, self.cin_r = sb("cin", [128, 2, 32 + T], BF16)
        self.cacc, self.cacc_r = self.xn[:].rearrange("p a n -> p (a n)").bitcast(F32)[:, 0:2 * T].rearrange("p (c n) -> p c n", c=2), self.xn_r
        self.cacc2 = self.xn[:].rearrange("p a n -> p (a n)").bitcast(F32)[:, 2 * T:4 * T].rearrange("p (c n) -> p c n", c=2)
        self.cin2 = self.vt[:].rearrange("p a f -> p (a f)")[:, 0:2 * (32 + T)].rearrange("p (c n) -> p c n", c=2)
        self.ptmp, self.ptmp_r = self.tmpA[3]
        self.act = self.big[:].bitcast(BF16)
        self.banks = []
        self.bank_res = []
        for i in range(8):
            t = self.st.enter_context(nc.psum_tensor("bank%d" % i, [128, 512], F32))
            self.banks.append(t)
            self.bank_res.append(Res("bank%d" % i))
        self.bank_i = 0
        self.tmp_i = 0
        self.tmpb_i = 0

        self.dma(self.pvec[:], self.pvecd, writes=[self.pvec_r])
        self.dma(self.identf[:], self.identd, writes=[self.identf_r])
        self.op("dve", lambda e: e.tensor_copy(out=self.ident[:], in_=self.identf[:]), reads=[self.identf_r], writes=[self.ident_r])
        self.op("dve", lambda e: e.memset(self.onesf[:], 1.0), writes=[self.onesf_r])
        for i in range(16):
            self.op("dve", lambda e, i=i: e.memset(self.pw2[:, i:i + 1], 0.5 ** (i + 1)), writes=[self.pw2_r])
        self.op("dve", lambda e: e.memset(self.vt[:], 1.0), writes=[self.vt_r])
        self.dma(self.cmask[:], self.cmaskd, writes=[self.cmask_r], q="pool")
        for l in range(2):
            for name, cnt, w in BLK:
                r0, _ = BLK_OFF[name]
                rows = w // 4
                for i in range(cnt):
                    r = Res("blk")
                    self.blk_res[(l, name, i)] = r
                    a = r0 + i * rows
                    self.dma(self.blob[l][a:a + rows, :], self.wsl[l][a:a + rows, :], writes=[r], q="pool")
        if self.stop == "T0":
            self.load_x(self.xin, 0, None)
            self.phaseA(0, 0)
            self.exchange(0, 0)
            self.mem_kv(0)
            self.load_x(self.xTd, 0, self.xT_res[0])
            self.phaseB(0, 0)
            return self.finish()
        for j in range(NT):
            self.load_x(self.xin, j, None)
            self.phaseA(0, j)
            self.exchange(0, j)
        if self.stop == "A0":
            return self.finish()
        for l in range(2):
            self.mem_kv(l)
            for j in range(NT):
                self.load_x(self.xTd, j, self.xT_res[j])
                self.phaseB(l, j)
                if l == 0:
                    self.phaseA(1, j, push=False)
                    self.exchange(1, j)
                else:
                    self.final_out(j)
            if l == 0 and self.stop == "B0":
                return self.finish()
        return self.finish()

    def finish(self):
        self.P.emit()
        self.st.close()
        return self.nc

    def pcol(self, c):
        return self.pvec[:, c:c + 1]

    def load_x(self, src, j, res):
        self.dma(self.xt[:], src[j].rearrange("c p n -> p c n"), reads=[res] if res else [], writes=[self.xt_r])

    def tmpa(self):
        i = self.tmp_i % 3
        self.tmp_i += 1
        return self.tmpA[i][0], self.tmpA[i][1]

    def tmpb(self):
        i = self.tmpb_i % 8
        self.tmpb_i += 1
        return self.tmpB[i][0], self.tmpB[i][1]

    def ptbuf(self):
        i = self.pT_i % 8
        self.pT_i += 1
        return self.pTs[i]

    def rmsnorm(self, x, x_r, gc0, out, out_r, nch, N, eps_col=PV_EPS6):
        pb, pbr = self.bank()
        for c in range(nch):
            sq, sqr = self.tmpa()
            self.op("act", lambda e, c=c, sq=sq: e.activation(out=sq[:, 0:N], in_=x[:, c, :], func=AF.Square), reads=[x_r], writes=[sqr])
            self.op("pe", lambda e, c=c, sq=sq: e.matmul(pb[:, 0:N], lhsT=self.onesf[:], rhs=sq[:, 0:N], start=(c == 0), stop=(c == nch - 1)),
                    reads=[sqr, self.onesf_r], writes=[pbr])
        sd, sdr = self.tmpa()
        self.op("act", lambda e: e.activation(out=sd[:, 0:N], in_=pb[:, 0:N], func=AF.Sqrt, bias=self.pcol(eps_col), scale=1.0 / (nch * 128)),
                reads=[pbr, self.pvec_r], writes=[sdr])
        self.op("dve", lambda e: e.reciprocal(out=self.rstd[:, 0:N], in_=sd[:, 0:N]), reads=[sdr], writes=[self.rstd_r])
        for c in range(nch):
            self.op("dve", lambda e, c=c: e.scalar_tensor_tensor(out=out[:, c, :], in0=x[:, c, :], scalar=self.pcol(gc0 + c), in1=self.rstd[:, 0:N],
                                                                 op0=ALU.mult, op1=ALU.mult),
                    reads=[x_r, self.rstd_r, self.pvec_r], writes=[out_r])

    def ffn(self, l, which):
        self.rmsnorm(self.xt, self.xt_r, l * LP + (0 if which == 1 else 32), self.xn, self.xn_r, 8, T)
        act = self.act
        for i in range(11):
            slot, sres = self.wpop()
            sv = slot[:, 0:4096].rearrange("p (k n) -> p k n", k=8)
            for cc in range(2):
                pg, pgr = self.bank()
                pu, pur = self.bank()
                for k in range(8):
                    self.op("pe", lambda e, k=k, cc=cc, sv=sv, pg=pg: e.matmul(pg, lhsT=sv[:, k, cc * 128:(cc + 1) * 128], rhs=self.xn[:, k, :],
                                                                          start=(k == 0), stop=(k == 7)),
                            reads=[sres, self.xn_r], writes=[pgr])
                for k in range(8):
                    self.op("pe", lambda e, k=k, cc=cc, sv=sv, pu=pu: e.matmul(pu, lhsT=sv[:, k, 256 + cc * 128:256 + (cc + 1) * 128], rhs=self.xn[:, k, :],
                                                                          start=(k == 0), stop=(k == 7)),
                            reads=[sres, self.xn_r], writes=[pur])
                sg, sgr = self.tmpa()
                self.op("act", lambda e, sg=sg, pg=pg: e.activation(out=sg[:], in_=pg, func=AF.Silu), reads=[pgr], writes=[sgr])
                ch = 2 * i + cc
                self.op("dve", lambda e, sg=sg, pu=pu, ch=ch: e.tensor_tensor(out=act[:, ch * T:(ch + 1) * T], in0=sg[:], in1=pu, op=ALU.mult),
                        reads=[sgr, pur], writes=[self.big_r])
        for oc in range(8):
            slot, sres = self.wpop()
            sv = slot[:, 0:2816].rearrange("p (k n) -> p k n", k=22)
            po, por = self.bank()
            for k in range(22):
                self.op("pe", lambda e, k=k, sv=sv, po=po: e.matmul(po, lhsT=sv[:, k, :], rhs=act[:, k * T:(k + 1) * T], start=(k == 0), stop=(k == 21)),
                        reads=[sres, self.big_r], writes=[por])
            self.op("dve", lambda e, oc=oc, po=po: e.scalar_tensor_tensor(out=self.xt[:, oc, :], in0=po, scalar=0.5, in1=self.xt[:, oc, :],
                                                                         op0=ALU.mult, op1=ALU.add),
                    reads=[por, self.xt_r], writes=[self.xt_r])

    def rope_tables(self, j):
        e_ = self
        pi_, pi_r = self.tmpa()
        posi = pi_[:].bitcast(I32)
        self.dma(posi, self.posd[j * T:(j + 1) * T].partition_broadcast(128), writes=[pi_r])
        a, ar = self.tmpa()
        k, kr = self.tmpa()
        ki, kir = self.tmpa()
        kiv = ki[:].bitcast(I32)
        op = self.op
        op("dve", lambda e: e.tensor_copy(out=a[:], in_=posi), reads=[pi_r], writes=[ar])
        op("dve", lambda e: e.tensor_scalar(out=a[:], in0=a[:], scalar1=self.pcol(PV_INVF), scalar2=None, op0=ALU.mult), reads=[ar, self.pvec_r], writes=[ar])
        op("dve", lambda e: e.tensor_scalar(out=k[:], in0=a[:], scalar1=1.0 / TWO_PI, scalar2=None, op0=ALU.mult), reads=[ar], writes=[kr])
        op("dve", lambda e: e.tensor_copy(out=kiv, in_=k[:]), reads=[kr], writes=[kir])
        op("dve", lambda e: e.tensor_copy(out=k[:], in_=kiv), reads=[kir], writes=[kr])
        op("dve", lambda e: e.scalar_tensor_tensor(out=a[:], in0=k[:], scalar=-TWO_PI, in1=a[:], op0=ALU.mult, op1=ALU.add), reads=[kr, ar], writes=[ar])

        def fold(t, tr):
            op("dve", lambda e: e.tensor_scalar(out=k[:], in0=t[:], scalar1=np.pi, scalar2=-TWO_PI, op0=ALU.is_gt, op1=ALU.mult), reads=[tr], writes=[kr])
            op("dve", lambda e: e.tensor_tensor(out=t[:], in0=t[:], in1=k[:], op=ALU.add), reads=[tr, kr], writes=[tr])
            op("dve", lambda e: e.tensor_scalar(out=k[:], in0=t[:], scalar1=-np.pi, scalar2=TWO_PI, op0=ALU.is_lt, op1=ALU.mult), reads=[tr], writes=[kr])
            op("dve", lambda e: e.tensor_tensor(out=t[:], in0=t[:], in1=k[:], op=ALU.add), reads=[tr, kr], writes=[tr])
        fold(a, ar)
        op("act", lambda e: e.activation(out=self.sinT[:], in_=a[:], func=AF.Sin), reads=[ar], writes=[self.sin_r])
        op("dve", lambda e: e.tensor_scalar(out=self.sinT[:], in0=self.sinT[:], scalar1=self.pcol(PV_SGN), scalar2=None, op0=ALU.mult),
           reads=[self.sin_r, self.pvec_r], writes=[self.sin_r])
        op("dve", lambda e: e.tensor_scalar(out=a[:], in0=a[:], scalar1=0.5 * np.pi, scalar2=None, op0=ALU.add), reads=[ar], writes=[ar])
        fold(a, ar)
        op("act", lambda e: e.activation(out=self.cosT[:], in_=a[:], func=AF.Sin), reads=[ar], writes=[self.cos_r])

    def phaseA(self, l, j, push=True):
        op = self.op
        if push:
            self.push_group(l, ["gu1", "dn1", "win", "wv"])
        self.ffn(l, 1)
        self.dma(self.xTd[j].rearrange("c p n -> p c n"), self.xt[:], reads=[self.xt_r], writes=[self.xT_res[j]])
        self.rmsnorm(self.xt, self.xt_r, l * LP + 8, self.xn, self.xn_r, 8, T)
        self.rope_tables(j)
        hn = self.xn
        yT = self.yT
        loc = self.loc_res[j]
        cols = slice(j * T, (j + 1) * T)
        held = {}
        sendv = self.send[l][j]
        sres_ = self.send_res[l][j]
        kview = sendv[0:512, :].rearrange("(c p) n -> p c n", c=4)
        kiview = sendv[512:640, :]
        hview = [sendv[1160 + 16 * i:1160 + 16 * (i + 1), :].rearrange("(c q) (h t) -> (q h) c t", c=2, q=8, h=16, t=32) for i in range(2)]
        for blk in range(8):
            slot, sres = self.wpop()
            sv = slot[:, 0:4096].rearrange("p (k n) -> p k n", k=8)
            for cc in range(4):
                ch = blk * 4 + cc
                pb, pbr = self.bank()
                for k in range(8):
                    op("pe", lambda e, k=k, cc=cc, sv=sv, pb=pb: e.matmul(pb, lhsT=sv[:, k, cc * 128:(cc + 1) * 128], rhs=hn[:, k, :], start=(k == 0), stop=(k == 7)),
                       reads=[sres, self.xn_r], writes=[pbr])
                if ch in (0, 1, 6, 7):
                    held[ch] = (pb, pbr)
                elif ch in (2, 3):
                    c = ch - 2
                    sg, sgr = self.tmpa()
                    op("act", lambda e, sg=sg, pb=pb: e.activation(out=sg[:], in_=pb, func=AF.Sigmoid), reads=[pbr], writes=[sgr])
                    pv, pvr = held[c]
                    op("dve", lambda e, sg=sg, pv=pv, c=c: e.tensor_tensor(out=yT[:, c, :], in0=sg[:], in1=pv, op=ALU.mult), reads=[sgr, pvr], writes=[self.yT_r])
                    if c == 1:
                        self.dma(self.hAd[:, :, cols].rearrange("c p n -> p c n"), yT[:, 0:2, :], reads=[self.yT_r], writes=[loc])
                        self.dma(hview[0], yT[:, 0:2, T - 32:T], reads=[self.yT_r], writes=[sres_])
                elif ch in (4, 5):
                    c = ch - 4
                    op("act", lambda e, pb=pb, c=c: e.copy(out=yT[:, 2 + c, :], in_=pb), reads=[pbr], writes=[self.yT_r])
                    if c == 1:
                        self.dma(self.bgd[:, :, cols].rearrange("c p n -> p c n"), yT[:, 2:4, :], reads=[self.yT_r], writes=[loc])
                elif ch in (8, 9):
                    c = ch - 8
                    sg, sgr = self.tmpa()
                    pv, pvr = held[6 + c]
                    op("act", lambda e, sg=sg, pv=pv: e.copy(out=sg[:], in_=pv), reads=[pvr], writes=[sgr])
                    op("dve", lambda e, sg=sg, pb=pb, c=c: e.tensor_tensor(out=yT[:, 4 + c, :], in0=sg[:], in1=pb, op=ALU.mult), reads=[sgr, pbr], writes=[self.yT_r])
                    if c == 1:
                        self.dma(self.cghd[:, :, cols].rearrange("c p n -> p c n"), yT[:, 4:6, :], reads=[self.yT_r], writes=[loc])
                        self.dma(hview[1], yT[:, 4:6, T - 32:T], reads=[self.yT_r], writes=[sres_])
                else:
                    for (c0, n, name) in ((10, 4, "q"), (18, 4, "k"), (26, 2, "qi"), (30, 1, "ki")):
                        if c0 <= ch < c0 + n:
                            c = ch - c0
                            op("dve", lambda e, pb=pb, c=c: e.tensor_tensor(out=self.rtmp[:, c, :], in0=pb, in1=self.cosT[:], op=ALU.mult),
                               reads=[pbr, self.cos_r], writes=self.rtmp_w)
                        elif c0 + n <= ch < c0 + 2 * n:
                            c = ch - c0 - n
                            t2, t2r = self.tmpa()
                            op("dve", lambda e, pb=pb, t2=t2: e.tensor_tensor(out=t2[:], in0=pb, in1=self.sinT[:], op=ALU.mult),
                               reads=[pbr, self.sin_r], writes=[t2r])
                            op("pool", lambda e, t2=t2, c=c: e.tensor_tensor(out=yT[:, c, :], in0=t2[:], in1=self.rtmp[:, c, :], op=ALU.add),
                               reads=[t2r, self.rtmp_r], writes=[self.yT_r])
                            if c == n - 1:
                                if name == "q":
                                    self.dma(self.qd[:, :, cols].rearrange("c p n -> p c n"), yT[:, 0:4, :], reads=[self.yT_r], writes=[loc])
                                elif name == "k":
                                    self.dma(kview, yT[:, 0:4, :], reads=[self.yT_r], writes=[sres_])
                                elif name == "qi":
                                    self.dma(self.qid[:, :, cols].rearrange("c p n -> p c n"), yT[:, 0:2, :], reads=[self.yT_r], writes=[loc])
                                else:
                                    self.dma(kiview, yT[:, 0, :], reads=[self.yT_r], writes=[sres_])
        op("dve", lambda e: e.memset(self.vt[:].rearrange("p a (h f) -> p a h f", f=65)[:, :, :, 64:65], 1.0), writes=[self.vt_r])
        slot, sres = self.wpop()
        sv = slot[:, 0:4608].rearrange("p (k n) -> p k n", k=8)
        vview = sendv[640:1160, :].rearrange("r c -> (r c)").rearrange("(t f) -> t f", f=520)
        for tb in range(4):
            pv, pvr = self.bank()
            pw, pwr = self.bank()
            for k in range(8):
                op("pe", lambda e, k=k, tb=tb, sv=sv, pv=pv: e.matmul(pv, lhsT=hn[:, k, tb * 128:(tb + 1) * 128], rhs=sv[:, k, 0:512], start=(k == 0), stop=(k == 7)),
                   reads=[sres, self.xn_r], writes=[pvr])
            for k in range(8):
                op("pe", lambda e, k=k, tb=tb, sv=sv, pw=pw: e.matmul(pw[:, 0:4], lhsT=hn[:, k, tb * 128:(tb + 1) * 128], rhs=sv[:, k, 512:516], start=(k == 0), stop=(k == 7)),
                   reads=[sres, self.xn_r], writes=[pwr])
            op("act", lambda e, tb=tb, pv=pv: e.copy(out=self.vt[:, tb, :].rearrange("p (h f) -> p h f", f=65)[:, :, 0:64], in_=pv.rearrange("p (h f) -> p h f", f=64)),
               reads=[pvr], writes=[self.vt_r])
            op("dve", lambda e, tb=tb, pw=pw: e.tensor_copy(out=self.wsc[:, tb, :], in_=pw[:, 0:4]), reads=[pwr], writes=[self.wsc_r])
        self.dma(vview.rearrange("(n p) f -> p n f", p=128), self.vt[:], reads=[self.vt_r], writes=[sres_])
        self.dma(self.widxd[j * T:(j + 1) * T, :].rearrange("(n p) f -> p n f", p=128), self.wsc[:], reads=[self.wsc_r], writes=[loc])

    def exchange(self, l, j):
        self.op("pool", lambda e: e.collective_compute("AllGather", ALU.bypass, replica_groups=[[0, 1], [2, 3], [4, 5], [6, 7]],
                                                       ins=[self.send[l][j]], outs=[self.ag[l][j]]),
                reads=[self.send_res[l][j]], writes=[self.ag_res[l][j]], dma=True, inc=1, sq="cc")

    def mem_kv(self, l):
        op = self.op
        self.push_group(l, ["wkv"])
        self.dma(self.memx, self.memT.rearrange("c p n -> p c n"), writes=self.rtmp_w)
        self.rmsnorm(self.memx, self.memx_r, l * LP + 24, self.memn, self.memn_r, 8, 256)
        for blk in range(2):
            slot, sres = self.wpop()
            sv = slot[:, 0:4096].rearrange("p (k n) -> p k n", k=8)
            for cc in range(4):
                pb, pbr = self.bank()
                for k in range(8):
                    op("pe", lambda e, k=k, cc=cc, sv=sv, pb=pb: e.matmul(pb[:, 0:256], lhsT=sv[:, k, cc * 128:(cc + 1) * 128], rhs=self.memn[:, k, :], start=(k == 0), stop=(k == 7)),
                       reads=[sres, self.memn_r], writes=[pbr])
                op("act", lambda e, pb=pb, ch=blk * 4 + cc: e.copy(out=self.kmT[:, ch, :], in_=pb[:, 0:256]), reads=[pbr], writes=[self.kmT_r])
        for blk in range(2):
            slot, sres = self.wpop()
            sv = slot[:, 0:4096].rearrange("p (k n) -> p k n", k=8)
            for mb in range(2):
                pb, pbr = self.bank()
                for k in range(8):
                    op("pe", lambda e, k=k, mb=mb, sv=sv, pb=pb: e.matmul(pb, lhsT=self.memn[:, k, mb * 128:(mb + 1) * 128], rhs=sv[:, k, :], start=(k == 0), stop=(k == 7)),
                       reads=[sres, self.memn_r], writes=[pbr])
                op("act", lambda e, pb=pb, mb=mb, blk=blk: e.copy(out=self.vm[:, mb, blk * 512:(blk + 1) * 512], in_=pb), reads=[pbr], writes=[self.vm_r])

    def linear_res(self, l, name, inp, inp_r, nk):
        op = self.op
        for blk in range(2):
            slot, sres = self.wpop()
            sv = slot[:, 0:4096].rearrange("p (k n) -> p k n", k=8)
            for cc in range(4):
                oc = blk * 4 + cc
                pb, pbr = self.bank()
                for k in range(nk):
                    op("pe", lambda e, k=k, cc=cc, sv=sv, pb=pb: e.matmul(pb, lhsT=sv[:, k, cc * 128:(cc + 1) * 128], rhs=inp[:, k, :], start=(k == 0), stop=(k == nk - 1)),
                       reads=[sres, inp_r], writes=[pbr])
                op("dve", lambda e, oc=oc, pb=pb: e.tensor_tensor(out=self.xt[:, oc, :], in0=pb, in1=self.xt[:, oc, :], op=ALU.add),
                   reads=[pbr, self.xt_r], writes=[self.xt_r])

    def conv_taps(self, l, j):
        op = self.op
        base = l * LP
        loc = self.loc_res[j]
        cols = slice(j * T, (j + 1) * T)
        for br in range(2):
            src = self.hAd if br == 0 else self.cghd
            cin, cin_r = (self.cin, self.cin_r) if br == 0 else (self.cin2, self.vt_r)
            cacc = self.cacc if br == 0 else self.cacc2
            def hv(jj, rr, br=br):
                a = rr * TR + 1160 + 16 * br
                return self.ag[l][jj][a:a + 16, :].rearrange("(c q) (h t) -> (q h) c t", c=2, q=8, h=16, t=32)
            self.dma(self.halo[:, 0, :, :], hv(j, 0), reads=[self.ag_res[l][j]], writes=[self.halo_r])
            if j > 0:
                self.dma(self.halo[:, 1, :, :], hv(j - 1, 1), reads=[self.ag_res[l][j - 1]], writes=[self.halo_r])
            else:
                op("dve", lambda e: e.memset(self.halo[:, 1, :, :], 0.0), writes=[self.halo_r])
            self.dma(cin[:, :, 32:32 + T], src[:, :, cols].rearrange("c p n -> p c n"), reads=[loc], writes=[cin_r])
            op("dve", lambda e, cin=cin: e.tensor_scalar(out=cin[:, :, 0:32], in0=self.halo[:, 0, :, :], scalar1=self.pcol(PV_SEL), scalar2=None, op0=ALU.mult),
               reads=[self.halo_r, self.pvec_r], writes=[cin_r])
            op("dve", lambda e, cin=cin: e.scalar_tensor_tensor(out=cin[:, :, 0:32], in0=self.halo[:, 1, :, :], scalar=self.pcol(PV_SEL + 1), in1=cin[:, :, 0:32],
                                                                op0=ALU.mult, op1=ALU.add),
               reads=[self.halo_r, self.pvec_r, cin_r], writes=[cin_r])
            W = 31 if br == 0 else 3
            wc0 = base + (40 if br == 0 else 108)
            for c in range(2):
                for tap in range(W):
                    sh = 32 - (W - 1) + tap
                    colw = self.pcol(wc0 + c * W + tap)
                    if tap == 0:
                        op("pool", lambda e, c=c, sh=sh, colw=colw, cin=cin, cacc=cacc: e.tensor_scalar(out=cacc[:, c, :], in0=cin[:, c, sh:sh + T], scalar1=colw, scalar2=None, op0=ALU.mult),
                           reads=[cin_r, self.pvec_r], writes=[self.cacc_r])
                    else:
                        op("pool", lambda e, c=c, sh=sh, colw=colw, cin=cin: e.tensor_scalar(out=self.ptmp[:], in0=cin[:, c, sh:sh + T], scalar1=colw, scalar2=None, op0=ALU.mult),
                           reads=[cin_r, self.pvec_r], writes=[self.ptmp_r])
                        op("pool", lambda e, c=c, cacc=cacc: e.tensor_tensor(out=cacc[:, c, :], in0=cacc[:, c, :], in1=self.ptmp[:], op=ALU.add),
                           reads=[self.ptmp_r, self.cacc_r], writes=[self.cacc_r])

    def conv_finish(self, l, j):
        op = self.op
        base = l * LP
        loc = self.loc_res[j]
        cols = slice(j * T, (j + 1) * T)
        for c in range(2):
            op("dve", lambda e, c=c: e.tensor_scalar(out=self.cacc[:, c, :], in0=self.cacc[:, c, :], scalar1=self.pcol(base + 102 + c), scalar2=None, op0=ALU.add),
               reads=[self.cacc_r, self.pvec_r], writes=[self.cacc_r])
        pm, pmr = self.bank()
        for c in range(2):
            op("pe", lambda e, c=c, pm=pm: e.matmul(pm, lhsT=self.onesf[:], rhs=self.cacc[:, c, :], start=(c == 0), stop=(c == 1)),
               reads=[self.cacc_r, self.onesf_r], writes=[pmr])
        mean, meanr = self.tmpa()
        op("act", lambda e, pm=pm, mean=mean: e.activation(out=mean[:], in_=pm, func=AF.Copy, scale=1.0 / 256), reads=[pmr], writes=[meanr])
        for c in range(2):
            op("dve", lambda e, c=c, mean=mean: e.tensor_tensor(out=self.cacc[:, c, :], in0=self.cacc[:, c, :], in1=mean[:], op=ALU.subtract),
               reads=[self.cacc_r, meanr], writes=[self.cacc_r])
        pq, pqr = self.bank()
        for c in range(2):
            sq, sqr = self.tmpa()
            op("act", lambda e, c=c, sq=sq: e.activation(out=sq[:], in_=self.cacc[:, c, :], func=AF.Square), reads=[self.cacc_r], writes=[sqr])
            op("pe", lambda e, c=c, sq=sq, pq=pq: e.matmul(pq, lhsT=self.onesf[:], rhs=sq[:], start=(c == 0), stop=(c == 1)), reads=[sqr, self.onesf_r], writes=[pqr])
        sd, sdr = self.tmpa()
        op("act", lambda e, sd=sd, pq=pq: e.activation(out=sd[:], in_=pq, func=AF.Sqrt, bias=self.pcol(PV_EPS5), scale=1.0 / 256), reads=[pqr, self.pvec_r], writes=[sdr])
        op("dve", lambda e, sd=sd: e.reciprocal(out=self.rstd[:], in_=sd[:]), reads=[sdr], writes=[self.rstd_r])
        for c in range(2):
            op("dve", lambda e, c=c: e.scalar_tensor_tensor(out=self.cacc[:, c, :], in0=self.cacc[:, c, :], scalar=self.pcol(base + 104 + c), in1=self.rstd[:],
                                                            op0=ALU.mult, op1=ALU.mult), reads=[self.cacc_r, self.rstd_r, self.pvec_r], writes=[self.cacc_r])
            op("act", lambda e, c=c: e.activation(out=self.yT[:, c, :], in_=self.cacc[:, c, :], func=AF.Silu, bias=self.pcol(base + 106 + c), scale=1.0),
               reads=[self.cacc_r, self.pvec_r], writes=[self.yT_r] + self.kis_r)
        self.dma(self.cin[:, :, 32:32 + T], self.bgd[:, :, cols].rearrange("c p n -> p c n"), reads=[loc], writes=[self.cin_r])
        for c in range(2):
            op("dve", lambda e, c=c: e.tensor_tensor(out=self.yT[:, 2 + c, :], in0=self.cacc2[:, c, :], in1=self.cin[:, c, 32:32 + T], op=ALU.mult),
               reads=[self.cacc_r, self.cin_r], writes=[self.yT_r] + self.kis_r)

    def dsa(self, l, j):
        op = self.op
        loc = self.loc_res[j]
        cols = slice(j * T, (j + 1) * T)
        S = 1024 * (j + 1)
        nch = S // 512
        sc = self.big
        self.dma(self.qT[:], self.qd[:, :, cols].rearrange("c p n -> p c n"), reads=[loc], writes=[self.qT_r])
        self.dma(self.qiT[:], self.qid[:, :, cols].rearrange("c p n -> p c n"), reads=[loc], writes=[self.qiT_r])
        self.dma(self.wsc[:], self.widxd[j * T:(j + 1) * T, :].rearrange("(n p) f -> p n f", p=128), reads=[loc], writes=[self.wsc_r])
        op("dve", lambda e: e.tensor_scalar(out=self.wsc[:], in0=self.wsc[:], scalar1=IDX_SCALE, scalar2=None, op0=ALU.mult), reads=[self.wsc_r], writes=[self.wsc_r])
        op("dve", lambda e: e.tensor_scalar(out=self.wsg[:], in0=self.wsc[:], scalar1=0.0, scalar2=2.0, op0=ALU.is_ge, op1=ALU.mult), reads=[self.wsc_r], writes=[self.wsc_r])
        op("dve", lambda e: e.tensor_scalar(out=self.wsg[:], in0=self.wsg[:], scalar1=-1.0, scalar2=None, op0=ALU.add), reads=[self.wsc_r], writes=[self.wsc_r])
        op("dve", lambda e: e.tensor_tensor(out=self.wab[:], in0=self.wsc[:], in1=self.wsg[:], op=ALU.mult), reads=[self.wsc_r], writes=[self.wsc_r])
        op("dve", lambda e: e.memset(self.small[:, 60:61], 0.0), writes=[self.yT_r, self.small_r] + self.kis_r)
        vb_v = self.kvb[:, 0:8320].rearrange("p (k f) -> p k f", f=130)
        nkt = 2 * (j + 1)
        bis = self.bis
        junk = bis[:, 6:7].to_broadcast([128, S])

        def agt(kt):
            return self.ag[l][kt // 2], (kt % 2) * TR, self.ag_res[l][kt // 2]

        def pre_gen(b):
            qs = slice(b * 128, (b + 1) * 128)
            for h in range(4):
                op("dve", lambda e, h=h: e.tensor_scalar(out=self.dg[:, h, :], in0=self.identf[:], scalar1=self.wsg[:, b, h:h + 1], scalar2=None, op0=ALU.mult),
                   reads=[self.identf_r, self.wsc_r], writes=[self.dg_r])
            for ch in range(nch):
                cs = slice(ch * 512, (ch + 1) * 512)
                ks = self.kis_i % 4
                self.kis_i += 1
                kt_ = self.yT[:, ks, :]
                ktr = self.kis_r[ks]
                a_, o_, r_ = agt(ch)
                self.dma(kt_, a_[o_ + 512:o_ + 640, :], reads=[r_], writes=[ktr])
                rls = []
                for h in range(4):
                    ps_ = slice((h % 2) * 64, (h % 2) * 64 + 64)
                    pb, pbr = self.bank()
                    op("pe", lambda e, pb=pb, ps_=ps_, h=h, kt_=kt_: e.matmul(pb, lhsT=self.qiT[ps_, h // 2, qs], rhs=kt_[ps_, :], start=True, stop=True),
                       reads=[self.qiT_r, ktr], writes=[pbr])
                    rl, rlr = self.tmpb()
                    op("act", lambda e, pb=pb, rl=rl, h=h: e.activation(out=rl[:], in_=pb, func=AF.Relu, scale=self.wab[:, b, h:h + 1]), reads=[pbr, self.wsc_r], writes=[rlr])
                    rls.append((rl, rlr))
                ps2, ps2r = self.bank()
                for h in range(4):
                    rl, rlr = rls[h]
                    op("pe", lambda e, ps2=ps2, rl=rl, h=h: e.matmul(ps2, lhsT=self.dg[:, h, :], rhs=rl[:], start=(h == 0), stop=(h == 3)),
                       reads=[rlr, self.dg_r], writes=[ps2r])
                op("dve", lambda e, ps2=ps2, cs=cs: e.tensor_copy(out=sc[:, cs], in_=ps2), reads=[ps2r], writes=[self.big_r])
                yield
            last = slice(S - 1024, S)
            for hh in range(2):
                t1, t1r = self.tmpa()
                ls = slice(S - 1024 + hh * 512, S - 1024 + (hh + 1) * 512)
                op("dve", lambda e, t1=t1, ls=ls, hh=hh: e.tensor_tensor(out=t1[:], in0=sc[:, ls], in1=self.cmask[:, b, hh * 512:(hh + 1) * 512], op=ALU.subtract),
                   reads=[self.big_r, self.cmask_r], writes=[t1r])
                op("dve", lambda e, t1=t1, hh=hh: e.tensor_reduce(out=bis[:, 40 + hh:41 + hh], in_=t1[:], axis=AX.X, op=ALU.min), reads=[t1r], writes=[self.bis_r])
            op("dve", lambda e: e.tensor_tensor(out=sc[:, last], in0=sc[:, last], in1=self.cmask[:, b, :], op=ALU.add), reads=[self.big_r, self.cmask_r], writes=[self.big_r])
            if S > 1024:
                op("dve", lambda e: e.tensor_reduce(out=bis[:, 42:43], in_=sc[:, 0:S - 1024], axis=AX.X, op=ALU.min), reads=[self.big_r], writes=[self.bis_r])
                nm = 3
            else:
                nm = 2
            op("dve", lambda e: e.tensor_reduce(out=bis[:, 0:1], in_=bis[:, 40:40 + nm], axis=AX.X, op=ALU.min), reads=[self.bis_r], writes=[self.bis_r])
            op("dve", lambda e: e.tensor_reduce(out=bis[:, 1:2], in_=sc[:, 0:S], axis=AX.X, op=ALU.max), reads=[self.big_r], writes=[self.bis_r])
            op("dve", lambda e: e.tensor_tensor(out=bis[:, 2:3], in0=bis[:, 1:2], in1=bis[:, 0:1], op=ALU.subtract), reads=[self.bis_r], writes=[self.bis_r])
            op("dve", lambda e: e.memset(bis[:, 20:20 + NITER], 0.0), writes=[self.bis_r])
            op("dve", lambda e: e.tensor_scalar(out=bis[:, 44:44 + NITER], in0=self.pw2[:, 0:NITER], scalar1=bis[:, 2:3], scalar2=None, op0=ALU.mult),
               reads=[self.bis_r, self.pw2_r], writes=[self.bis_r])
            yield
            for it in range(NITER):
                op("dve", lambda e, it=it: e.tensor_tensor(out=bis[:, 3:4], in0=bis[:, 0:1], in1=bis[:, 44 + it:45 + it], op=ALU.add),
                   reads=[self.bis_r], writes=[self.bis_r])
                op("dve", lambda e, it=it: e.tensor_scalar(out=junk, in0=sc[:, 0:S], scalar1=bis[:, 3:4], scalar2=0.0, op0=ALU.is_ge, op1=ALU.add,
                                                          accum_out=bis[:, 20 + it:21 + it]),
                   reads=[self.big_r, self.bis_r], writes=[self.bis_r])
                op("dve", lambda e, it=it: e.tensor_scalar(out=bis[:, 5:6], in0=bis[:, 20 + it:21 + it], scalar1=255.5, scalar2=bis[:, 44 + it:45 + it], op0=ALU.is_ge, op1=ALU.mult),
                   reads=[self.bis_r], writes=[self.bis_r])
                op("dve", lambda e: e.tensor_tensor(out=bis[:, 0:1], in0=bis[:, 0:1], in1=bis[:, 5:6], op=ALU.add), reads=[self.bis_r], writes=[self.bis_r])
                yield

        def mask_bias(b):
            op("dve", lambda e: e.tensor_scalar(out=self.sel[:, 0:S], in0=sc[:, 0:S], scalar1=bis[:, 0:1], scalar2=-30000.0, op0=ALU.is_lt, op1=ALU.mult),
               reads=[self.big_r, self.bis_r], writes=[self.sel_r])
            if self.debug and j == 0:
                self.dma(self.dbg_sc[b], sc[:, 0:1024], reads=[self.big_r])
                self.dma(self.dbg_sel[b], self.sel[:, 0:1024], reads=[self.sel_r])
                self.dma(self.dbg_bis[b], bis[:], reads=[self.bis_r])

        def attn(b, nxt):
            qs = slice(b * 128, (b + 1) * 128)
            ycp = [self.banks[6][:], self.banks[7][:]]
            ycr = [self.bank_res[6], self.bank_res[7]]
            LB, LC = 3, 6
            gsteps = nch + 1 + NITER
            psteps = 8 * (nch + LC)
            stride = max(1, psteps // (gsteps + 1))
            cnt = [0]

            def tick():
                cnt[0] += 1
                if nxt is not None and cnt[0] % stride == 0:
                    next(nxt, None)

            for c in range(4):
                for kt in range(nkt):
                    a_, o_, r_ = agt(kt)
                    self.dma(self.kbuf[:, kt * 512:(kt + 1) * 512], a_[o_ + c * 128:o_ + (c + 1) * 128, :], reads=[r_], writes=[self.kbuf_rt[kt]])
                    vv_ = a_[o_ + 640:o_ + 1160, :].rearrange("r c -> (r c)").rearrange("(t f) -> t f", f=520)
                    self.dma(vb_v[:, kt * 4:(kt + 1) * 4, :], vv_[:, c * 130:(c + 1) * 130].rearrange("(n p) f -> p n f", p=128), reads=[r_], writes=[self.kvb_rt[kt]])
                for half in range(2):
                    h = 2 * c + half
                    ps_ = slice(half * 64, half * 64 + 64)
                    for ch in range(nch):
                        cs = slice(ch * 512, (ch + 1) * 512)
                        pb, pbr = self.bank()
                        op("pe", lambda e, pb=pb, ps_=ps_, cs=cs, c=c: e.matmul(pb, lhsT=self.qT[ps_, c, qs], rhs=self.kbuf[ps_, cs], start=True, stop=True),
                           reads=[self.qT_r, self.kbuf_rt[ch]], writes=[pbr])
                        op("dve", lambda e, pb=pb, ch=ch: e.tensor_reduce(out=self.small[:, ch:ch + 1], in_=pb, axis=AX.X, op=ALU.max), reads=[pbr], writes=[self.small_r])
                    op("dve", lambda e: e.tensor_reduce(out=self.small[:, 32:33], in_=self.small[:, 0:nch], axis=AX.X, op=ALU.max), reads=[self.small_r], writes=[self.small_r])
                    nb_ = 34 + h
                    op("dve", lambda e, nb_=nb_: e.tensor_scalar(out=self.small[:, nb_:nb_ + 1], in0=self.small[:, 32:33], scalar1=-0.125, scalar2=None, op0=ALU.mult),
                       reads=[self.small_r], writes=[self.negm_r[h]])
                    yb = ycp[h // 4]
                    ybr = ycr[h // 4]
                    oc0 = (h % 4) * 65
                    pms = {}
                    pts = {}

                    def stA(ch, ps_=ps_, c=c, nb_=nb_, h=h):
                        cs = slice(ch * 512, (ch + 1) * 512)
                        pb, pbr = self.bank()
                        op("pe", lambda e: e.matmul(pb, lhsT=self.qT[ps_, c, qs], rhs=self.kbuf[ps_, cs], start=True, stop=False),
                           reads=[self.qT_r, self.kbuf_rt[ch]], writes=[pbr])
                        op("pe", lambda e: e.matmul(pb, lhsT=self.ident[:], rhs=self.sel[:, cs], start=False, stop=True),
                           reads=[self.ident_r, self.sel_r], writes=[pbr])
                        pm, pmr = self.tmpb()
                        op("act", lambda e: e.activation(out=pm[:], in_=pb, func=AF.Exp, bias=self.small[:, nb_:nb_ + 1], scale=0.125),
                           reads=[pbr, self.negm_r[h]], writes=[pmr])
                        pms[ch] = (pm, pmr)

                    def stB(ch):
                        pm, pmr = pms.pop(ch)
                        tb_, tbr = self.bank()
                        tbv = tb_.bitcast(BF16)
                        for t in range(4):
                            op("pe", lambda e, t=t: e.transpose(tbv[:, t * 128:(t + 1) * 128], pm[:, t * 128:(t + 1) * 128], self.ident[:]),
                               reads=[pmr, self.ident_r], writes=[tbr])
                        pt, ptr = self.ptbuf()
                        if ch % 2 == 0:
                            op("act", lambda e: e.copy(out=pt.rearrange("p a b -> p (a b)"), in_=tbv[:, 0:512]), reads=[tbr], writes=[ptr, self.rtmp_r])
                        else:
                            op("dve", lambda e: e.tensor_copy(out=pt.rearrange("p a b -> p (a b)"), in_=tbv[:, 0:512]), reads=[tbr], writes=[ptr, self.rtmp_r])
                        pts[ch] = (pt, ptr)

                    def stC(ch, yb=yb, ybr=ybr, oc0=oc0, half=half):
                        pt, ptr = pts.pop(ch)
                        for t in range(4):
                            kc = ch * 4 + t
                            first = (ch == 0 and t == 0)
                            lastm = (ch == nch - 1 and t == 3)
                            op("pe", lambda e, t=t, kc=kc, first=first, lastm=lastm:
                               e.matmul(yb[:, oc0:oc0 + 65], lhsT=pt[:, t, :], rhs=self.kvb[:, kc * 130 + half * 65: kc * 130 + half * 65 + 65], start=first, stop=lastm),
                               reads=[ptr, self.kvb_rt[ch]], writes=[ybr])

                    for st_ in range(nch + LC):
                        if st_ < nch:
                            stA(st_)
                        if 0 <= st_ - LB < nch:
                            stB(st_ - LB)
                        if 0 <= st_ - LC < nch:
                            stC(st_ - LC)
                        tick()
            for h in range(8):
                yb = ycp[h // 4]
                ybr = ycr[h // 4]
                oc0 = (h % 4) * 65
                op("dve", lambda e, yb=yb, oc0=oc0, h=h: e.reciprocal(out=self.small[:, 44 + h:45 + h], in_=yb[:, oc0 + 64:oc0 + 65]), reads=[ybr], writes=[self.small_r])
                op("dve", lambda e, yb=yb, oc0=oc0, h=h: e.tensor_scalar(out=self.ycs[:, h * 64:(h + 1) * 64], in0=yb[:, oc0:oc0 + 64], scalar1=self.small[:, 44 + h:45 + h],
                                                                        scalar2=None, op0=ALU.mult),
                   reads=[ybr, self.small_r], writes=[self.ycs_r])
            if self.debug and j == 0:
                self.dma(self.dbg_ycs[b], self.ycs[:], reads=[self.ycs_r])
            tb_, tbr = self.bank()
            tbv = tb_.bitcast(BF16)
            for c in range(4):
                op("pe", lambda e, c=c, tbv=tbv: e.transpose(tbv[:, c * 128:(c + 1) * 128], self.ycs[:, c * 128:(c + 1) * 128], self.ident[:]),
                   reads=[self.ycs_r, self.ident_r], writes=[tbr])
            op("act", lambda e, tbv=tbv: e.copy(out=self.yT[:, 4:8, qs], in_=tbv[:, 0:512].rearrange("p (c q) -> p c q", c=4)), reads=[tbr], writes=[self.yT_r])

        g = pre_gen(0)
        for _ in g:
            pass
        for b in range(4):
            mask_bias(b)
            nxt = pre_gen(b + 1) if b < 3 else None
            attn(b, nxt)
            if nxt is not None:
                for _ in nxt:
                    pass

    def xattn(self, l):
        op = self.op
        self.rmsnorm(self.xt, self.xt_r, l * LP + 16, self.xn, self.xn_r, 8, T)
        for blk in range(2):
            slot, sres = self.wpop()
            sv = slot[:, 0:4096].rearrange("p (k n) -> p k n", k=8)
            for cc in range(4):
                oc = blk * 4 + cc
                pb, pbr = self.bank()
                for k in range(8):
                    op("pe", lambda e, k=k, cc=cc, sv=sv, pb=pb: e.matmul(pb, lhsT=sv[:, k, cc * 128:(cc + 1) * 128], rhs=self.xn[:, k, :], start=(k == 0), stop=(k == 7)),
                       reads=[sres, self.xn_r], writes=[pbr])
                op("act", lambda e, pb=pb, oc=oc: e.copy(out=self.yT[:, oc, :], in_=pb), reads=[pbr], writes=[self.yT_r])
        o_tok, o_tok_r = self.ycs, self.ycs_r

        def _blk(b):
            qs = slice(b * 128, (b + 1) * 128)
            sm = self.small
            pbs, exs, pts_, pos = {}, {}, {}, {}
            o2, o2r = self.tmpb()
            otok = [(o_tok, o_tok_r), (o2, o2r)]
            for h in range(4):
                pb, pbr = self.bank()
                for k in range(2):
                    op("pe", lambda e, k=k, h=h, pb=pb: e.matmul(pb[:, 0:256], lhsT=self.yT[:, 2 * h + k, qs], rhs=self.kmT[:, 2 * h + k, :], start=(k == 0), stop=(k == 1)),
                       reads=[self.yT_r, self.kmT_r], writes=[pbr])
                xr = self.xs_r[h]
                op("dve", lambda e, pb=pb, h=h: e.tensor_reduce(out=sm[:, 16 + h:17 + h], in_=pb[:, 0:256], axis=AX.X, op=ALU.max), reads=[pbr], writes=[xr])
                op("dve", lambda e, h=h: e.tensor_scalar(out=sm[:, 20 + h:21 + h], in0=sm[:, 16 + h:17 + h], scalar1=-1.0 / 16, scalar2=None, op0=ALU.mult),
                   reads=[xr], writes=[xr])
                op("dve", lambda e, h=h: e.memset(sm[:, 24 + h:25 + h], 0.0), writes=[xr])
                pbs[h] = (pb, pbr)
            for h in range(4):
                pb, pbr = pbs[h]
                xr = self.xs_r[h]
                ex, exr = self.tmpb()
                op("act", lambda e, pb=pb, ex=ex, h=h: e.activation(out=ex[:, 0:256], in_=pb[:, 0:256], func=AF.Exp, bias=sm[:, 20 + h:21 + h], scale=1.0 / 16,
                                                                   accum_out=sm[:, 24 + h:25 + h]),
                   reads=[pbr, xr], writes=[exr, xr])
                exs[h] = (ex, exr)
            for h in range(4):
                ex, exr = exs[h]
                tb_, tbr = self.bank()
                tbv = tb_.bitcast(BF16)
                for t in range(2):
                    op("pe", lambda e, t=t, ex=ex, tbv=tbv: e.transpose(tbv[:, t * 128:(t + 1) * 128], ex[:, t * 128:(t + 1) * 128], self.ident[:]),
                       reads=[exr, self.ident_r], writes=[tbr])
                pt, ptr = self.ptbuf()
                if h % 2 == 0:
                    op("act", lambda e, tbv=tbv, pt=pt: e.copy(out=pt[:, 0:2, :].rearrange("p a b -> p (a b)"), in_=tbv[:, 0:256]), reads=[tbr], writes=[ptr, self.rtmp_r])
                else:
                    op("dve", lambda e, tbv=tbv, pt=pt: e.tensor_copy(out=pt[:, 0:2, :].rearrange("p a b -> p (a b)"), in_=tbv[:, 0:256]), reads=[tbr], writes=[ptr, self.rtmp_r])
                pts_[h] = (pt, ptr)
            for h in range(4):
                pt, ptr = pts_[h]
                xr = self.xs_r[h]
                po, por = self.bank()
                for t in range(2):
                    op("pe", lambda e, t=t, h=h, po=po, pt=pt: e.matmul(po[:, 0:256], lhsT=pt[:, t, :], rhs=self.vm[:, t, h * 256:(h + 1) * 256], start=(t == 0), stop=(t == 1)),
                       reads=[ptr, self.vm_r], writes=[por])
                op("dve", lambda e, h=h: e.reciprocal(out=sm[:, 28 + h:29 + h], in_=sm[:, 24 + h:25 + h]), reads=[xr], writes=[xr])
                ot, otr = otok[h // 2]
                op("dve", lambda e, po=po, h=h, ot=ot: e.tensor_scalar(out=ot[:, (h % 2) * 256:(h % 2 + 1) * 256], in0=po[:, 0:256], scalar1=sm[:, 28 + h:29 + h], scalar2=None, op0=ALU.mult),
                   reads=[por, xr], writes=[otr])
            for half2 in range(2):
                ot, otr = otok[half2]
                tb_, tbr = self.bank()
                tbv = tb_.bitcast(BF16)
                for c in range(4):
                    op("pe", lambda e, c=c, tbv=tbv, ot=ot: e.transpose(tbv[:, c * 128:(c + 1) * 128], ot[:, c * 128:(c + 1) * 128], self.ident[:]),
                       reads=[otr, self.ident_r], writes=[tbr])
                op("act", lambda e, tbv=tbv, half2=half2: e.copy(out=self.xn[:, half2 * 4:half2 * 4 + 4, qs], in_=tbv[:, 0:512].rearrange("p (c q) -> p c q", c=4)),
                   reads=[tbr], writes=[self.xn_r])
        for b in range(4):
            _blk(b)
        self.linear_res(l, "wo", self.xn, self.xn_r, 8)

    def phaseB(self, l, j):
        self.push_group(l, ["wout", "wq", "wo", "gu2", "dn2"])
        if l == 0 and self.stop != "T0":
            self.push_group(1, ["gu1", "dn1", "win", "wv"])
        self.conv_taps(l, j)
        self.dsa(l, j)
        self.conv_finish(l, j)
        if self.debug and l == 0:
            self.dma(self.dbg_y[j].rearrange("c p n -> p c n"), self.yT[:], reads=[self.yT_r])
        self.linear_res(l, "wout", self.yT, self.yT_r, 8)
        if self.debug and l == 0:
            self.dma(self.dbg_x2[j].rearrange("c p n -> p c n"), self.xt[:], reads=[self.xt_r])
        self.xattn(l)
        if self.debug and l == 0:
            self.dma(self.dbg_x3[j].rearrange("c p n -> p c n"), self.xt[:], reads=[self.xt_r])
        self.ffn(l, 2)
        if self.debug and l == 0:
            self.dma(self.dbg_x4[j].rearrange("c p n -> p c n"), self.xt[:], reads=[self.xt_r])

    def final_out(self, j):
        op = self.op
        pb, pbr = self.bank()
        x = self.xt
        for c in range(8):
            sq, sqr = self.tmpa()
            op("act", lambda e, c=c, sq=sq: e.activation(out=sq[:], in_=x[:, c, :], func=AF.Square), reads=[self.xt_r], writes=[sqr])
            op("pe", lambda e, c=c, sq=sq: e.matmul(pb, lhsT=self.onesf[:], rhs=sq[:], start=(c == 0), stop=(c == 7)), reads=[sqr, self.onesf_r], writes=[pbr])
        sd, sdr = self.tmpa()
        op("act", lambda e: e.activation(out=sd[:], in_=pb, func=AF.Sqrt, bias=self.pcol(PV_EPS6), scale=1.0 / 1024), reads=[pbr, self.pvec_r], writes=[sdr])
        op("dve", lambda e: e.reciprocal(out=self.rstd[:], in_=sd[:]), reads=[sdr], writes=[self.rstd_r])
        for c in range(8):
            op("dve", lambda e, c=c: e.scalar_tensor_tensor(out=x[:, c, :], in0=x[:, c, :], scalar=self.pcol(PV_FIN + c), in1=self.rstd[:], op0=ALU.mult, op1=ALU.mult),
               reads=[self.xt_r, self.rstd_r, self.pvec_r], writes=[self.xt_r])
        n = self.dma(self.outd[j].rearrange("c p n -> p c n"), self.xt[:], reads=[self.xt_r])
        self.P.final.append(n)


def _swap(w):
    K, N = w.shape
    w = w.reshape(K, N // 64, 2, 32)
    return np.ascontiguousarray(w[:, :, ::-1, :]).reshape(K, N)


def _blk(w, kc, ncols, width):
    a = w.reshape(kc, 128, ncols).transpose(1, 0, 2).reshape(128, kc * ncols)
    if a.shape[1] < width:
        a = np.concatenate([a, np.zeros((128, width - a.shape[1]), np.float32)], 1)
    return a


def _layer_blob(p, l):
    blocks = []

    def gu(g, u):
        for i in range(11):
            w = np.concatenate([g[:, 256 * i:256 * (i + 1)], u[:, 256 * i:256 * (i + 1)]], 1)
            blocks.append(_blk(w, 8, 512, 4096))

    def dn(dw):
        for oc in range(8):
            blocks.append(_blk(dw[:, oc * 128:(oc + 1) * 128], 22, 128, 3072))

    def sq(w, nb):
        for i in range(nb):
            blocks.append(_blk(w[:, 512 * i:512 * (i + 1)], 8, 512, 4096))

    gu(p["ffn1_w_gate"][l], p["ffn1_w_up"][l])
    dn(p["ffn1_w_down"][l])
    wi = p["w_in"][l]
    a_val, a_gate, b_gate, c_gate, b_h = (wi[:, 256 * i:256 * (i + 1)] for i in range(5))
    q = wi[:, 1280:1792]
    k = wi[:, 1792:2304]
    v = wi[:, 2304:2816]
    qi = wi[:, 2816:3072]
    ki = wi[:, 3072:3136]
    wx = wi[:, 3136:3140]
    ext = np.concatenate([a_val, a_gate, b_gate, c_gate, b_h, q, _swap(q), k, _swap(k), qi, _swap(qi), ki, ki, _swap(ki), _swap(ki)], 1)
    assert ext.shape[1] == 4096
    sq(ext, 8)
    blocks.append(_blk(np.concatenate([v, wx, np.zeros((1024, 60), np.float32)], 1), 8, 576, 4608))
    sq(p["w_out"][l], 2)
    sq(p["xa_wq"][l], 2)
    sq(p["xa_wkv"][l], 4)
    sq(p["xa_wo"][l], 2)
    gu(p["ffn2_w_gate"][l], p["ffn2_w_up"][l])
    dn(p["ffn2_w_down"][l])
    flat = np.concatenate([b.reshape(-1) for b in blocks])
    assert flat.size == BLOB_ROWS * 512, (flat.size, BLOB_ROWS * 512)
    return flat.reshape(BLOB_ROWS, 512)


def _pvec(p, r):
    pv = np.zeros((128, NPV), np.float32)

    def put(c0, vec):
        n = vec.size // 128
        pv[:, c0:c0 + n] = vec.reshape(n, 128).T

    for l in range(2):
        b = l * LP
        put(b + 0, p["ffn1_norm"][l])
        put(b + 8, p["mix_norm"][l])
        put(b + 16, p["xa_norm"][l])
        put(b + 24, p["mem_norm"][l])
        put(b + 32, p["ffn2_norm"][l])
        dw = p["conf_dw"][l]
        for c in range(2):
            pv[:, b + 40 + c * 31: b + 40 + (c + 1) * 31] = dw[:, c * 128:(c + 1) * 128].T
        put(b + 102, p["conf_dw_b"][l])
        put(b + 104, p["conf_ln_g"][l])
        put(b + 106, p["conf_ln_b"][l])
        sw = p["sc_dw"][l]
        for c in range(2):
            pv[:, b + 108 + c * 3: b + 108 + (c + 1) * 3] = sw[:, c * 128:(c + 1) * 128].T
    put(PV_FIN, p["final_norm"])
    pidx = np.arange(128)
    inv_freq = (10000.0 ** (-np.arange(0, 64, 2, dtype=np.float32) / 64)).astype(np.float32)
    pv[:, PV_INVF] = inv_freq[pidx % 32]
    pv[:, PV_SGN] = np.where((pidx % 64) < 32, -1.0, 1.0)
    pv[:, PV_SEL] = 1.0 if r == 1 else 0.0
    pv[:, PV_SEL + 1] = 1.0 if r == 0 else 0.0
    pv[:, PV_EPS6] = 1e-6
    pv[:, PV_EPS5] = 1e-5
    return pv


def _cmask(r):
    i = np.arange(128)[:, None, None]
    b = np.arange(4)[None, :, None]
    c = np.arange(1024)[None, None, :]
    vis = (c - 512 * r) <= (128 * b + i)
    return np.where(vis, 0.0, NEG).astype(np.float32)


_CACHE = {}


def make_inputs(p):
    p = {k: np.asarray(v) for k, v in p.items()}
    blobs = [_layer_blob(p, l) for l in range(2)]
    ident = np.eye(128, dtype=np.float32)
    in_maps = []
    for c in range(8):
        b, r = c // 2, c % 2
        xb = p["x"][b].reshape(16, 512, 8, 128)[r::2]
        xin = np.ascontiguousarray(xb.transpose(0, 2, 3, 1))
        pos = np.ascontiguousarray(p["positions"][b].reshape(16, 512)[r::2].reshape(-1)).astype(np.int32)
        memT = np.ascontiguousarray(p["mem"][b].reshape(256, 8, 128).transpose(1, 2, 0))
        m = {"xin": xin, "pos": pos, "memT": memT, "cmask": _cmask(r), "pvec": _pvec(p, r), "ident": ident,
             "wsl0": blobs[0], "wsl1": blobs[1]}
        in_maps.append(m)
    return in_maps


def assemble(res):
    out = np.zeros((4, 16, 512, 8, 128), np.float32)
    for c in range(8):
        b, r = c // 2, c % 2
        o = res.results[c]["out"]
        out[b, r::2] = o.transpose(0, 3, 1, 2)
    return out.reshape(4, 8192, 1024)


def kernel(**inputs):
    in_maps = make_inputs(inputs)
    if "nc" not in _CACHE:
        _CACHE["nc"] = KB().build()
    res = run_bass_kernel_spmd(_CACHE["nc"], in_maps, core_ids=list(range(8)))
    return assemble(res)
```
